# Optimizing a Trainium2 kernel written in Bass

```python
import jax, jax.numpy as jnp
from jax import lax
import numpy as np

D_MODEL = 1024
BATCH = 2
SEQ = 8192
DEPTH = 1

RET_HEADS = 4
RET_V_DIM = D_MODEL // 2 // RET_HEADS
RET_QK_DIM = RET_V_DIM // 2
RET_CHUNK = 128
ATT_HEAD_DIM = 64
ATT_HEADS = D_MODEL // 2 // ATT_HEAD_DIM
DILATED_CONFIGS = ((128, 1), (512, 4), (2048, 16))
N_EXPERTS = 16
EC_CAPACITY_FACTOR = 2
D_FF_EXPERT = 2048

RET_QK_W = RET_HEADS * RET_QK_DIM
RET_V_W = RET_HEADS * RET_V_DIM
ATT_W = ATT_HEADS * ATT_HEAD_DIM
MIX_WIDTH = RET_V_W + ATT_W
IN_PROJ_WIDTH = 2 * RET_QK_W + 2 * RET_V_W + 3 * ATT_W
RMS_EPS = 1e-6
GN_EPS = 1e-5
MASK_VALUE = -1e30

kernel_name = "hybrid_retention_dilated_attn_ec_moe"


def rmsnorm(x, g):
    xf = x.astype(jnp.float32)
    y = xf * lax.rsqrt(jnp.mean(xf * xf, axis=-1, keepdims=True) + RMS_EPS)
    return (y * g.astype(jnp.float32)).astype(x.dtype)


def alibi_slopes(n):
    return 2.0 ** (-8.0 * (jnp.arange(n, dtype=jnp.float32) + 1.0) / n)


def retention_scan(q, k, v, log_gamma, inclusive):
    B, H, S, dk = q.shape
    dv = v.shape[-1]
    C = RET_CHUNK
    n = S // C
    qc = q.reshape(B, H, n, C, dk)
    kc = k.reshape(B, H, n, C, dk)
    vc = v.reshape(B, H, n, C, dv)
    pos = jnp.arange(C, dtype=jnp.float32)
    rel = pos[:, None] - pos[None, :]
    mask = (rel >= 0) if inclusive else (rel > 0)
    lg = log_gamma[:, None, None]
    intra_decay = jnp.where(mask[None], jnp.exp(lg * jnp.where(mask, rel, 0.0)[None]), 0.0)
    scores = jnp.einsum('bhnid,bhnjd->bhnij', qc, kc) * intra_decay[None, :, None]
    o = jnp.einsum('bhnij,bhnjv->bhniv', scores, vc)
    k_tail = kc * jnp.exp(log_gamma[:, None] * (C - 1.0 - pos))[None, :, None, :, None]
    kv = jnp.einsum('bhnjd,bhnjv->nbhdv', k_tail, vc)
    chunk_decay = jnp.exp(log_gamma * C)[None, :, None, None]

    def step(state, kv_n):
        return chunk_decay * state + kv_n, state

    _, prev = lax.scan(step, jnp.zeros((B, H, dk, dv), jnp.float32), kv)
    q_head = qc * jnp.exp(log_gamma[:, None] * (pos + 1.0))[None, :, None, :, None]
    o = o + jnp.einsum('bhnid,nbhdv->bhniv', q_head, prev)
    return o.reshape(B, H, S, dv)


def bidirectional_retention(q, k, v, logit_fwd, logit_bwd):
    qf, kf, vf = q.astype(jnp.float32), k.astype(jnp.float32), v.astype(jnp.float32)
    lg_f = jax.nn.log_sigmoid(logit_fwd.astype(jnp.float32))
    lg_b = jax.nn.log_sigmoid(logit_bwd.astype(jnp.float32))
    y_f = retention_scan(qf, kf, vf, lg_f, True)
    y_b = jnp.flip(retention_scan(jnp.flip(qf, 2), jnp.flip(kf, 2), jnp.flip(vf, 2), lg_b, False), 2)
    y = y_f + y_b
    mu = jnp.mean(y, axis=-1, keepdims=True)
    var = jnp.mean(jnp.square(y - mu), axis=-1, keepdims=True)
    return (y - mu) * lax.rsqrt(var + GN_EPS)


def banded_attention(q, k, v, half, slopes):
    N, H, L, dh = q.shape
    W = half
    nb = -(-L // W)
    pad = nb * W - L
    qb = jnp.pad(q, ((0, 0), (0, 0), (0, pad), (0, 0))).reshape(N, H, nb, W, dh)

    def windows(t):
        tb = jnp.pad(t, ((0, 0), (0, 0), (W, pad + W), (0, 0))).reshape(N, H, nb + 2, W, dh)
        return jnp.concatenate([tb[:, :, :-2], tb[:, :, 1:-1], tb[:, :, 2:]], axis=3)

    kw, vw = windows(k), windows(v)
    qpos = jnp.arange(nb * W).reshape(nb, W)
    kpos = jnp.arange(nb)[:, None] * W - W + jnp.arange(3 * W)[None, :]
    dist = jnp.abs(kpos[:, None, :] - qpos[:, :, None])
    valid = (dist <= W) & (kpos[:, None, :] >= 0) & (kpos[:, None, :] < L)
    s = jnp.einsum('nhbqd,nhbkd->nhbqk', qb, kw).astype(jnp.float32) * (dh ** -0.5)
    s = s - slopes[None, :, None, None, None] * dist.astype(jnp.float32)[None, None]
    s = jnp.where(valid[None, None], s, MASK_VALUE)
    lse = jax.nn.logsumexp(s, axis=-1)
    p = jnp.exp(s - lse[..., None])
    o = jnp.einsum('nhbqk,nhbkd->nhbqd', p, vw.astype(jnp.float32))
    return o.reshape(N, H, nb * W, dh)[:, :, :L], lse.reshape(N, H, nb * W)[:, :, :L]


def dilated_attention(q, k, v, window, dilation, slopes):
    B, H, S, dh = q.shape
    L = S // dilation

    def to_sub(t):
        return t.reshape(B, H, L, dilation, dh).transpose(0, 3, 1, 2, 4).reshape(B * dilation, H, L, dh)

    o, lse = banded_attention(to_sub(q), to_sub(k), to_sub(v), window // (2 * dilation), slopes * dilation)
    o = o.reshape(B, dilation, H, L, dh).transpose(0, 2, 3, 1, 4).reshape(B, H, S, dh)
    lse = lse.reshape(B, dilation, H, L).transpose(0, 2, 3, 1).reshape(B, H, S)
    return o, lse


def expert_choice_moe(h, w_router, w_gate, w_up, w_down):
    B, S, D = h.shape
    E = w_router.shape[-1]
    cap = EC_CAPACITY_FACTOR * S // E
    affinity = jax.nn.softmax((h @ w_router).astype(jnp.float32), axis=-1)
    gates, idx = lax.top_k(jnp.swapaxes(affinity, 1, 2), cap)
    flat_idx = idx.reshape(B, E * cap)
    b_idx = jnp.arange(B)[:, None]
    xe = h[b_idx, flat_idx].reshape(B, E, cap, D)
    a = jnp.einsum('becd,edf->becf', xe, w_gate)
    u = jnp.einsum('becd,edf->becf', xe, w_up)
    ye = jnp.einsum('becf,efd->becd', jax.nn.silu(a) * u, w_down)
    ye = ye * gates[..., None].astype(ye.dtype)
    out = jnp.zeros_like(h).at[b_idx, flat_idx].add(ye.reshape(B, E * cap, D).astype(h.dtype))
    return out


def setup_inputs(seed: int = 0) -> dict:
    key = jax.random.key(seed)
    ks = jax.random.split(key, 20)
    f32 = jnp.float32
    D, F, E = D_MODEL, D_FF_EXPERT, N_EXPERTS
    nrm = lambda k, shape, s: jax.random.normal(k, shape, f32) * s
    base_logit = jnp.log(2.0 ** (5.0 + jnp.arange(RET_HEADS, dtype=f32)) - 1.0)
    return {
        "x": nrm(ks[0], (BATCH, SEQ, D), 1.0),
        "c": nrm(ks[1], (BATCH, D), 1.0),
        "w_ada": nrm(ks[2], (DEPTH, D, 6 * D), 0.5 * D ** -0.5),
        "b_ada": nrm(ks[3], (DEPTH, 6 * D), 0.01),
        "g_pre_mix": 1.0 + nrm(ks[4], (DEPTH, D), 0.01),
        "g_post_mix": 1.0 + nrm(ks[5], (DEPTH, D), 0.01),
        "g_pre_ffn": 1.0 + nrm(ks[6], (DEPTH, D), 0.01),
        "g_post_ffn": 1.0 + nrm(ks[7], (DEPTH, D), 0.01),
        "w_in": nrm(ks[8], (DEPTH, D, IN_PROJ_WIDTH), D ** -0.5),
        "ret_decay_fwd": base_logit[None] + nrm(ks[9], (DEPTH, RET_HEADS), 0.1),
        "ret_decay_bwd": base_logit[None] + nrm(ks[10], (DEPTH, RET_HEADS), 0.1),
        "w_out": nrm(ks[11], (DEPTH, MIX_WIDTH, D), MIX_WIDTH ** -0.5),
        "w_router": nrm(ks[12], (DEPTH, D, E), D ** -0.5),
        "w_gate_e": nrm(ks[13], (DEPTH, E, D, F), D ** -0.5),
        "w_up_e": nrm(ks[14], (DEPTH, E, D, F), D ** -0.5),
        "w_down_e": nrm(ks[15], (DEPTH, E, F, D), F ** -0.5),
    }


def reference(x, c, w_ada, b_ada, g_pre_mix, g_post_mix, g_pre_ffn, g_post_ffn, w_in,
              ret_decay_fwd, ret_decay_bwd, w_out, w_router, w_gate_e, w_up_e, w_down_e):
    B, S, D = x.shape
    slopes = alibi_slopes(ATT_HEADS)
    split_at = np.cumsum([RET_QK_W, RET_QK_W, RET_V_W, RET_V_W, ATT_W, ATT_W]).tolist()

    def heads(t, n_heads):
        return t.reshape(B, S, n_heads, -1).transpose(0, 2, 1, 3)

    for l in range(DEPTH):
        mod = jax.nn.silu(c) @ w_ada[l] + b_ada[l]
        shift_m, scale_m, gate_m, shift_f, scale_f, gate_f = [m[:, None, :] for m in jnp.split(mod, 6, axis=-1)]

        h = rmsnorm(x, g_pre_mix[l]) * (1.0 + scale_m) + shift_m
        proj = h @ w_in[l]
        rq, rk, rv, rg, aq, ak, av = jnp.split(proj, split_at, axis=-1)

        y_ret = bidirectional_retention(heads(rq, RET_HEADS), heads(rk, RET_HEADS) * (RET_QK_DIM ** -0.5),
                                        heads(rv, RET_HEADS), ret_decay_fwd[l], ret_decay_bwd[l])
        y_ret = y_ret.transpose(0, 2, 1, 3).reshape(B, S, RET_V_W).astype(x.dtype)
        y_ret = jax.nn.silu(rg) * y_ret

        qh, kh, vh = heads(aq, ATT_HEADS), heads(ak, ATT_HEADS), heads(av, ATT_HEADS)
        results = [dilated_attention(qh, kh, vh, w, d, slopes) for (w, d) in DILATED_CONFIGS]
        outs = jnp.stack([r[0] for r in results])
        lses = jnp.stack([r[1] for r in results])
        mix_w = jax.nn.softmax(lses, axis=0)
        y_att = jnp.sum(mix_w[..., None] * outs, axis=0)
        y_att = y_att.transpose(0, 2, 1, 3).reshape(B, S, ATT_W).astype(x.dtype)

        mixed = jnp.concatenate([y_ret, y_att], axis=-1) @ w_out[l]
        x = x + gate_m * rmsnorm(mixed, g_post_mix[l])

        h = rmsnorm(x, g_pre_ffn[l]) * (1.0 + scale_f) + shift_f
        moe = expert_choice_moe(h, w_router[l], w_gate_e[l], w_up_e[l], w_down_e[l])
        x = x + gate_f * rmsnorm(moe, g_post_ffn[l])
    return x
```

```python
import numpy as np
from contextlib import ExitStack
import concourse.bass as bass
import concourse.mybir as mybir
from concourse.bass_utils import run_bass_kernel_spmd

F32 = mybir.dt.float32
BF16 = mybir.dt.bfloat16
I32 = mybir.dt.int32
ALU = mybir.AluOpType
AF = mybir.ActivationFunctionType
AX = mybir.AxisListType
ENGS = ["sync", "scalar", "vector", "gpsimd", "tensor"]

S = 8192
D = 1024
NT = S // 128
OWN = 2048
CAP = 1024
LN8 = -2.0794415416798357
SLOPES = [2.0 ** (-(h + 1)) for h in range(8)]
CONFIGS = (1, 4, 16)


class Reg:
    __slots__ = ("w", "r", "name", "psum")

    def __init__(self, name="", psum=False):
        self.w = None
        self.r = {}
        self.name = name
        self.psum = psum


class Buf:
    __slots__ = ("t", "r")

    def __init__(self, t, name):
        self.t = t
        self.r = Reg(name)


class Prog:
    def __init__(self, nc):
        self.nc = nc
        self.stack = ExitStack()
        self.ops = {e: [] for e in ENGS}
        self.cnt = {e: 0 for e in ENGS}
        self.sem = {e: self.stack.enter_context(nc.semaphore(f"c_{e}")) for e in ENGS}
        self.known = {e: {} for e in ENGS}
        self.dsem = {}
        self.dcnt = {}

    def _need(self, eng, tok, waits):
        if tok is None:
            return
        sem, val, src = tok
        if src == eng and eng == "tensor":
            return
        if src == eng and val <= self.cnt[eng] - 3:
            return
        k = self.known[eng]
        if k.get(id(sem), 0) >= val:
            return
        k[id(sem)] = val
        waits.append((sem, val))

    def _deps(self, eng, reads, writes):
        waits = []
        for r in reads:
            self._need(eng, r.w, waits)
            if r.psum:
                for t in r.r.values():
                    if t[2] != eng:
                        self._need(eng, t, waits)
        for w in writes:
            self._need(eng, w.w, waits)
            for t in w.r.values():
                self._need(eng, t, waits)
        best = {}
        for sem, val in waits:
            if id(sem) not in best or best[id(sem)][1] < val:
                best[id(sem)] = (sem, val)
        return list(best.values())

    def _commit(self, tok, reads, writes):
        for r in reads:
            r.r[id(tok[0])] = tok
        for w in writes:
            w.w = tok
            w.r = {}

    def op(self, eng, fn, reads=(), writes=()):
        reads = [x.r if isinstance(x, Buf) else x for x in reads]
        writes = [x.r if isinstance(x, Buf) else x for x in writes]
        waits = self._deps(eng, reads, writes)
        self.cnt[eng] += 1
        tok = (self.sem[eng], self.cnt[eng], eng)
        self.ops[eng].append((waits, fn, (self.sem[eng], 1)))
        self._commit(tok, reads, writes)
        return tok

    def dma(self, q, fn, key, reads=(), writes=(), inc=16):
        reads = [x.r if isinstance(x, Buf) else x for x in reads]
        writes = [x.r if isinstance(x, Buf) else x for x in writes]
        if key not in self.dsem:
            self.dsem[key] = self.stack.enter_context(self.nc.semaphore(f"d_{key}"))
            self.dcnt[key] = 0
        waits = self._deps(q, reads, writes)
        self.dcnt[key] += inc
        tok = (self.dsem[key], self.dcnt[key], "dma")
        self.ops[q].append((waits, fn, (self.dsem[key], inc)))
        self._commit(tok, reads, writes)
        return tok

    def barrier(self):
        for eng in ENGS:
            waits = []
            for e in ENGS:
                if self.cnt[e] > 0 and e != eng:
                    self._need(eng, (self.sem[e], self.cnt[e], e), waits)
            for key, sem in self.dsem.items():
                self._need(eng, (sem, self.dcnt[key], "dma"), waits)
            self.ops[eng].append((waits, None, None))

    def emit(self):
        return

    def finish(self):
        nc = self.nc
        ops = self.ops
        self.ops = {e: [] for e in ENGS}

        def replay(name, e):
            for waits, fn, inc in ops[name]:
                for sem, val in waits:
                    e.wait_ge(sem, val)
                if fn is not None:
                    fn(e).then_inc(inc[0], inc[1])

        with nc.Block() as block:
            @block.sync
            def _(e):
                replay("sync", e)

            @block.scalar
            def _(e):
                replay("scalar", e)

            @block.vector
            def _(e):
                replay("vector", e)

            @block.gpsimd
            def _(e):
                replay("gpsimd", e)

            @block.tensor
            def _(e):
                replay("tensor", e)


def build(stop_after=99, debug=False):
    import os
    NGRP = int(os.environ.get('NGRP', '16'))
    FLAGS = os.environ.get('KFLAGS', '').split(',')
    nc = bass.Bass("TRN2", target_bir_lowering=False)

    def din(name, shape, dt=F32):
        return nc.dram_tensor(name, list(shape), dt, kind="ExternalInput").ap()

    def dsc(name, shape, dt=F32):
        return nc.dram_tensor(name, list(shape), dt).ap()

    xb = din("xb", [S, D])
    xo = din("xo", [OWN, D])
    ccol = din("ccol", [128, 8])
    wada = din("wada", [D, 6 * D])
    vcols = din("vcols", [128, 80])
    win = din("win", [D, 832])
    decin = din("dec", [128, 2])
    wout = din("wout", [256, D])
    wrin = din("wr", [D, 16])
    ohin = din("oh", [128, 4])
    if stop_after >= 7:
        wg = din("wg", [4, D, 2048])
        wu = din("wu", [4, D, 2048])
        wd = din("wd", [4, 2048, D])
    identin = din("ident", [128, 128])
    masksin = din("masks", [128, 18 * 256])
    rcin = din("rc", [128, 514])
    slotin = din("slotid", [128, 8])
    triin = din("tri", [128, 128])
    out = nc.dram_tensor("out", [OWN, D], F32, kind="ExternalOutput").ap()
    dbg = {}
    if debug:
        if stop_after in (2, 3):
            dbg["yT"] = nc.dram_tensor("dbg_yT", [256, S], F32, kind="ExternalOutput").ap()
        if stop_after in (5, 6):
            dbg["x1"] = nc.dram_tensor("dbg_x1", [OWN, D], F32, kind="ExternalOutput").ap()
            dbg["aff"] = nc.dram_tensor("dbg_aff", [OWN, 16], F32, kind="ExternalOutput").ap()
            dbg["tok"] = nc.dram_tensor("dbg_tok", [128, 32], F32, kind="ExternalOutput").ap()
        if stop_after == 7:
            dbg["moe"] = nc.dram_tensor("dbg_moe", [OWN, D], F32, kind="ExternalOutput").ap()

    aq_d = dsc("aq_d", [128, S], BF16)
    ak_d = dsc("ak_d", [128, S], BF16)
    av_d = dsc("av_d", [128, S], BF16)
    part_d = dsc("part_d", [S, D])
    mixed_d = dsc("mixed_d", [OWN, D])
    h2_in = dsc("h2_in", [OWN, D], BF16)
    h2_all = [dsc(f"h2_all{c4}", [2048, D], BF16) for c4 in range(4)]
    h2_tab = dsc("h2_tab", [S, D], BF16)
    aff_in = dsc("aff_in", [OWN, 16])
    aff_all = dsc("aff_all", [S, 16])
    aff_tab = dsc("aff_tab", [S, 16])
    cdr = dsc("cdr", [4, S])
    contrib = dsc("contrib", [S, D])
    moe_d = dsc("moe_d", [OWN, D])
    R_aq, R_ak, R_av = Reg("aq_d"), Reg("ak_d"), Reg("av_d")
    R_part, R_mixed = Reg("part_d"), Reg("mixed_d")
    R_h2in, R_h2all, R_h2tab = Reg("h2in"), Reg("h2all"), Reg("h2tab")
    R_affin, R_affall, R_afftab = Reg("affin"), Reg("affall"), Reg("afftab")
    R_cdr, R_contrib, R_moe, R_out = Reg("cdr"), Reg("contrib"), Reg("moe"), Reg("out")
    RG = [[0, 1, 2, 3], [4, 5, 6, 7]] if 'half' not in FLAGS else [[0, 1, 2, 3]]

    p = Prog(nc)
    GS = p.stack

    def mk(stack, name, shape, dt, psum=False):
        if psum:
            t = stack.enter_context(nc.psum_tensor(name, [128, 512 if dt == F32 else 1024], dt))
        else:
            t = stack.enter_context(nc.sbuf_tensor(name, list(shape), dt))
        b = Buf(t, name)
        b.r.psum = psum
        return b

    V = lambda fn, r=(), w=(): p.op("vector", fn, r, w)
    A = lambda fn, r=(), w=(): p.op("scalar", fn, r, w)
    PL = lambda fn, r=(), w=(): p.op("gpsimd", fn, r, w)
    T = lambda fn, r=(), w=(): p.op("tensor", fn, r, w)

    def ld(q, dst, dst_ap, src_ap, reads=()):
        return p.dma(q, lambda e: e.dma_start(out=dst_ap, in_=src_ap), dst.r.name, reads=reads, writes=[dst])

    identf = mk(GS, "identf", [128, 128], F32)
    identb = mk(GS, "identb", [128, 128], BF16)
    onesf = mk(GS, "onesf", [128, 128], F32)
    vc = mk(GS, "vc", [128, 80], F32)
    mod = mk(GS, "mod", [128, 48], F32)
    der = mk(GS, "der", [128, 32], F32)
    ohs = mk(GS, "ohs", [128, 4], F32)
    ld("sync", identf, identf.t[:], identin)
    ld("gpsimd", identb, identb.t[:], identin)
    ld("sync", vc, vc.t[:], vcols)
    ld("sync", ohs, ohs.t[:], ohin)
    V(lambda e: e.memset(onesf.t[:], 1.0), w=[onesf])
    cst = mk(GS, "cst", [128, 4], F32)
    V(lambda e: e.memset(cst.t[:, 0:1], LN8), w=[cst])
    V(lambda e: e.memset(cst.t[:, 1:2], 1e-6), w=[cst])
    V(lambda e: e.memset(cst.t[:, 2:3], 1e-5), w=[cst])
    V(lambda e: e.memset(cst.t[:, 3:4], 0.0), w=[cst])
    ymix = ExitStack()
    yT = mk(ymix, "yT", [128, S], BF16)
    yA = mk(ymix, "yA", [64, S], BF16)
    yB = mk(ymix, "yB", [64, S], BF16)

    with ExitStack() as ph:
        cc = mk(ph, "cc", [128, 8], F32)
        scb = mk(ph, "scb", [128, 8], BF16)
        wa = [mk(ph, f"wa{i}", [128, 8, D], BF16) for i in range(2)]
        psm = mk(ph, "psm", [128, 48], F32, psum=True)
        ld("sync", cc, cc.t[:], ccol)
        A(lambda e: e.activation(out=scb.t[:], in_=cc.t[:], func=AF.Silu), r=[cc], w=[scb])
        wada_v = wada.rearrange("(k p) n -> p k n", p=128)
        for g in range(6):
            w_ = wa[g % 2]
            ld("gpsimd", w_, w_.t[:], wada_v[:, :, g * D:(g + 1) * D])
            for j in range(8):
                for k in range(8):
                    T(lambda e, w_=w_, g=g, j=j, k=k: e.matmul(
                        psm.t[:, g * 8 + j:g * 8 + j + 1], lhsT=w_.t[:, k, j * 128:(j + 1) * 128],
                        rhs=scb.t[:, k:k + 1], start=(k == 0), stop=(k == 7)), r=[w_, scb], w=[psm])
        V(lambda e: e.tensor_tensor(out=mod.t[:], in0=psm.t[:, 0:48], in1=vc.t[:, 0:48], op=ALU.add), r=[psm, vc], w=[mod])
        V(lambda e: e.scalar_tensor_tensor(out=der.t[:, 0:8], in0=mod.t[:, 8:16], scalar=1.0, in1=vc.t[:, 48:56],
                                           op0=ALU.add, op1=ALU.mult), r=[mod, vc], w=[der])
        V(lambda e: e.tensor_tensor(out=der.t[:, 8:16], in0=mod.t[:, 16:24], in1=vc.t[:, 56:64], op=ALU.mult), r=[mod, vc], w=[der])
        V(lambda e: e.scalar_tensor_tensor(out=der.t[:, 16:24], in0=mod.t[:, 32:40], scalar=1.0, in1=vc.t[:, 64:72],
                                           op0=ALU.add, op1=ALU.mult), r=[mod, vc], w=[der])
        V(lambda e: e.tensor_tensor(out=der.t[:, 24:32], in0=mod.t[:, 40:48], in1=vc.t[:, 72:80], op=ALU.mult), r=[mod, vc], w=[der])
        p.barrier()
        p.emit()

    if stop_after <= 0:
        p.finish()
        return nc, p, dbg
    with ExitStack() as ph:
        rqT = mk(ph, "rqT", [64, S], BF16)
        rkT = mk(ph, "rkT", [64, S], BF16)
        ktf = mk(ph, "ktf", [128, NT, 64], BF16)
        ktb = mk(ph, "ktb", [128, NT, 64], BF16)
        rv = mk(ph, "rv", [128, NT, 128], BF16)
        sg = mk(ph, "sg", [128, NT, 128], BF16)
        rcs = mk(ph, "rcs", [128, 514], F32)
        dcs = mk(ph, "dcs", [128, 2], F32)
        lg = mk(ph, "lg", [128, 2], F32)
        tfb = mk(ph, "tfb", [128, 4], F32)
        DT = mk(ph, "DT", [128, 128], F32)
        QF = mk(ph, "QF", [128, 128], BF16)
        QB = mk(ph, "QB", [128, 128], BF16)
        ld("sync", rcs, rcs.t[:], rcin)
        ld("sync", dcs, dcs.t[:], decin)
        A(lambda e: e.activation(out=lg.t[:], in_=dcs.t[:], func=AF.Exp, scale=-1.0), r=[dcs], w=[lg])
        V(lambda e: e.tensor_scalar(out=lg.t[:], in0=lg.t[:], scalar1=1.0, scalar2=None, op0=ALU.add), r=[lg], w=[lg])
        A(lambda e: e.activation(out=lg.t[:], in_=lg.t[:], func=AF.Ln), r=[lg], w=[lg])
        V(lambda e: e.tensor_scalar(out=lg.t[:], in0=lg.t[:], scalar1=-1.0, scalar2=None, op0=ALU.mult), r=[lg], w=[lg])
        A(lambda e: e.activation(out=tfb.t[:, 0:1], in_=rcs.t[:, 512:513], func=AF.Exp, scale=lg.t[:, 0:1], bias=cst.t[:, 0:1]), r=[rcs, lg, cst], w=[tfb])
        A(lambda e: e.activation(out=tfb.t[:, 1:2], in_=rcs.t[:, 513:514], func=AF.Exp, scale=lg.t[:, 1:2], bias=cst.t[:, 0:1]), r=[rcs, lg, cst], w=[tfb])
        A(lambda e: e.activation(out=tfb.t[:, 2:4], in_=lg.t[:, 0:2], func=AF.Exp, scale=128.0), r=[lg], w=[tfb])
        A(lambda e: e.activation(out=QF.t[:], in_=rcs.t[:, 256:384], func=AF.Exp, scale=lg.t[:, 0:1]), r=[rcs, lg], w=[QF])
        A(lambda e: e.activation(out=QB.t[:], in_=rcs.t[:, 384:512], func=AF.Exp, scale=lg.t[:, 1:2]), r=[rcs, lg], w=[QB])
        V(lambda e: e.tensor_scalar(out=DT.t[:], in0=rcs.t[:, 0:128], scalar1=lg.t[:, 0:1], scalar2=None, op0=ALU.mult), r=[rcs, lg], w=[DT])
        V(lambda e: e.scalar_tensor_tensor(out=DT.t[:], in0=rcs.t[:, 128:256], scalar=lg.t[:, 1:2], in1=DT.t[:],
                                           op0=ALU.mult, op1=ALU.add), r=[rcs, lg, DT], w=[DT])
        A(lambda e: e.activation(out=DT.t[:], in_=DT.t[:], func=AF.Exp, bias=cst.t[:, 0:1]), r=[DT, cst], w=[DT])

        if 'pre_only' in FLAGS:
            p.barrier()
            p.finish()
            return nc, p, dbg
        with ExitStack() as ph1:
            winb = mk(ph1, "winb", [128, 8, 832], BF16)
            xs = [mk(ph1, f"xs{i}", [128, D], F32) for i in range(2)]
            sqj = mk(ph1, "sqj", [128, D], BF16)
            ssq = [mk(ph1, f"ssq{i}", [128, 2], F32) for i in range(2)]
            xn = [mk(ph1, f"xn{i}", [128, D], BF16) for i in range(2)]
            h1T = [mk(ph1, f"h1T{i}", [128, 8, 512], BF16) for i in range(2)]
            stg = [[mk(ph1, f"stg{a}{i}", [128, 512], BF16) for i in range(2)] for a in range(3)]
            psX = [mk(ph1, f"psX{i}", [128, D], BF16, psum=True) for i in range(2)]
            psF = [mk(ph1, f"psF{i}", [128, 512], F32, psum=True) for i in range(2)]
            psT = [mk(ph1, f"psT{i}", [128, 320], F32, psum=True) for i in range(2)]
            ld("gpsimd", winb, winb.t[:], win.rearrange("(k p) n -> p k n", p=128))
            xb_v = xb.rearrange("(n p) d -> p n d", p=128)
            fcount = 0
            for Gi in range(NGRP):
                h1 = h1T[Gi % 2]
                for tt in range(4):
                    n = 4 * Gi + tt
                    x_ = xs[n % 2]
                    s_ = ssq[n % 2]
                    xn_ = xn[n % 2]
                    px = psX[n % 2]
                    ld("sync", x_, x_.t[:], xb_v[:, n, :])
                    A(lambda e, x_=x_, s_=s_: e.activation(out=sqj.t[:], in_=x_.t[:], func=AF.Square, accum_out=s_.t[:, 0:1]),
                      r=[x_], w=[sqj, s_])
                    A(lambda e, s_=s_: e.activation(out=s_.t[:, 1:2], in_=s_.t[:, 0:1], func=AF.Sqrt, scale=1.0 / D, bias=cst.t[:, 1:2]),
                      r=[s_, cst], w=[s_])
                    V(lambda e, s_=s_: e.reciprocal(out=s_.t[:, 1:2], in_=s_.t[:, 1:2]), r=[s_], w=[s_])
                    V(lambda e, x_=x_, s_=s_, xn_=xn_: e.tensor_scalar(out=xn_.t[:], in0=x_.t[:], scalar1=s_.t[:, 1:2], scalar2=None,
                                                                     op0=ALU.mult), r=[x_, s_], w=[xn_])
                    for k in range(8):
                        T(lambda e, k=k, px=px, xn_=xn_: e.transpose(out=px.t[:, k * 128:(k + 1) * 128], in_=xn_.t[:, k * 128:(k + 1) * 128],
                                                                   identity=identb.t[:]), r=[xn_, identb], w=[px])
                    for k in range(8):
                        o_ = h1.t[:, k, tt * 128:(tt + 1) * 128]
                        i_ = px.t[:, k * 128:(k + 1) * 128]
                        if k % 2 == 0:
                            A(lambda e, o_=o_, i_=i_, k=k: e.activation(out=o_, in_=i_, func=AF.Identity, scale=der.t[:, k:k + 1],
                                                                       bias=mod.t[:, k:k + 1]), r=[px, der, mod], w=[h1])
                        else:
                            V(lambda e, o_=o_, i_=i_, k=k: e.tensor_scalar(out=o_, in0=i_, scalar1=der.t[:, k:k + 1], scalar2=mod.t[:, k:k + 1],
                                                                         op0=ALU.mult, op1=ALU.add), r=[px, der, mod], w=[h1])
                for fi, (c0, wdt) in enumerate([] if 'noF' in FLAGS else [(0, 64), (64, 64), (128, 128), (256, 128), (384, 128)]):
                    pf = psF[fcount % 2]
                    fcount += 1
                    for k in range(8):
                        T(lambda e, pf=pf, k=k, c0=c0, wdt=wdt, h1=h1: e.matmul(pf.t[0:wdt, :], lhsT=winb.t[:, k, c0:c0 + wdt], rhs=h1.t[:, k, :],
                                                                             start=(k == 0), stop=(k == 7)), r=[winb, h1], w=[pf])
                    sl = slice(Gi * 512, (Gi + 1) * 512)
                    if fi == 0:
                        A(lambda e, pf=pf, sl=sl: e.copy(out=rqT.t[:, sl], in_=pf.t[0:64, :]), r=[pf], w=[rqT])
                    elif fi == 1:
                        V(lambda e, pf=pf, sl=sl: e.tensor_copy(out=rkT.t[:, sl], in_=pf.t[0:64, :]), r=[pf], w=[rkT])
                    else:
                        sb_ = stg[fi - 2][Gi % 2]
                        dr, dreg = [(aq_d, R_aq), (ak_d, R_ak), (av_d, R_av)][fi - 2]
                        if fi == 3:
                            V(lambda e, pf=pf, sb_=sb_: e.tensor_copy(out=sb_.t[:], in_=pf.t[:]), r=[pf], w=[sb_])
                        else:
                            A(lambda e, pf=pf, sb_=sb_: e.copy(out=sb_.t[:], in_=pf.t[:]), r=[pf], w=[sb_])
                        if 'nospill' not in FLAGS:
                            p.dma("sync", lambda e, dr=dr, sl=sl, sb_=sb_: e.dma_start(out=dr[:, sl], in_=sb_.t[:]), dreg.name,
                                  reads=[sb_], writes=[dreg])
                for tt in range(0 if 'noT' in FLAGS else 4):
                    n = 4 * Gi + tt
                    pt = psT[n % 2]
                    for k in range(8):
                        T(lambda e, pt=pt, k=k, tt=tt, h1=h1: e.matmul(pt.t[:, 0:320], lhsT=h1.t[:, k, tt * 128:(tt + 1) * 128], rhs=winb.t[:, k, 512:832],
                                                                     start=(k == 0), stop=(k == 7)), r=[winb, h1], w=[pt])
                    if 'noT1' not in FLAGS:
                      A(lambda e, pt=pt, n=n: e.activation(out=ktf.t[:, n, :], in_=pt.t[:, 0:64], func=AF.Identity, scale=tfb.t[:, 0:1]),
                        r=[pt, tfb], w=[ktf])
                    if 'noT2' not in FLAGS:
                      V(lambda e, pt=pt, n=n: e.tensor_scalar(out=ktb.t[:, n, :], in0=pt.t[:, 0:64], scalar1=tfb.t[:, 1:2], scalar2=None,
                                                          op0=ALU.mult), r=[pt, tfb], w=[ktb])
                    if 'noT3' not in FLAGS:
                      V(lambda e, pt=pt, n=n: e.tensor_copy(out=rv.t[:, n, :], in_=pt.t[:, 64:192]), r=[pt], w=[rv])
                    if 'noT4' not in FLAGS:
                      A(lambda e, pt=pt, n=n: e.activation(out=sg.t[:, n, :], in_=pt.t[:, 192:320], func=AF.Silu), r=[pt], w=[sg])
            p.barrier()
            p.emit()
        if stop_after <= 1:
            p.finish()
            return nc, p, dbg

        with ExitStack() as ph2:
            Nbf = mk(ph2, "Nbf", [64, NT, 128], BF16)
            Nrun = [mk(ph2, f"Nrun{i}", [64, 128], F32) for i in range(2)]
            Prun = [mk(ph2, f"Prun{i}", [64, 128], F32) for i in range(2)]
            Pbf = [mk(ph2, f"Pbf{i}", [64, 128], BF16) for i in range(2)]
            SM = [mk(ph2, f"SM{i}", [128, 128], BF16) for i in range(2)]
            qf = [mk(ph2, f"qf{i}", [64, 128], BF16) for i in range(2)]
            qb = [mk(ph2, f"qb{i}", [64, 128], BF16) for i in range(2)]
            osq = mk(ph2, "osq", [128, 4, 128], F32)
            st4 = [mk(ph2, f"st4{i}", [128, 16], F32) for i in range(2)]
            yr = [mk(ph2, f"yr{i}", [128, 128], BF16) for i in range(2)]
            psK = [mk(ph2, f"psK{i}", [64, 128], F32, psum=True) for i in range(2)]
            psS = [mk(ph2, f"psS{i}", [128, 128], F32, psum=True) for i in range(2)]
            psO = [mk(ph2, f"psO{i}", [128, 4, 128], F32, psum=True) for i in range(2)]
            psY = [mk(ph2, f"psY{i}", [128, 128], BF16, psum=True) for i in range(2)]
            V(lambda e: e.memset(Nrun[1].t[:], 0.0), w=[Nrun[1]])
            V(lambda e: e.memset(Nbf.t[:, NT - 1, :], 0.0), w=[Nbf])
            for n in range(NT - 1, 0, -1):
                pk = psK[n % 2]
                cur, nxt = Nrun[n % 2], Nrun[(n + 1) % 2]
                T(lambda e, pk=pk, n=n: e.matmul(pk.t[0:64, 0:128], lhsT=ktb.t[:, n, :], rhs=rv.t[:, n, :], start=True, stop=True), r=[ktb, rv], w=[pk])
                V(lambda e, pk=pk, cur=cur, nxt=nxt: e.scalar_tensor_tensor(out=nxt.t[:], in0=cur.t[:], scalar=tfb.t[0:64, 3:4], in1=pk.t[0:64, 0:128],
                                                                          op0=ALU.mult, op1=ALU.add), r=[cur, pk, tfb], w=[nxt])
                A(lambda e, nxt=nxt, n=n: e.copy(out=Nbf.t[:, n - 1, :], in_=nxt.t[:]), r=[nxt], w=[Nbf])
            V(lambda e: e.memset(Prun[0].t[:], 0.0), w=[Prun[0]])
            V(lambda e: e.memset(Pbf[0].t[:], 0.0), w=[Pbf[0]])
            for n in range(NT):
                cs = slice(n * 128, (n + 1) * 128)
                ps_ = psS[n % 2]
                sm_ = SM[n % 2]
                po = psO[(n // 4) % 2]
                j4 = n % 4
                qf_, qb_ = qf[n % 2], qb[n % 2]
                pb_cur, pb_nxt = Pbf[n % 2], Pbf[(n + 1) % 2]
                pr_cur, pr_nxt = Prun[n % 2], Prun[(n + 1) % 2]
                T(lambda e, ps_=ps_, cs=cs: e.matmul(ps_.t[:, 0:128], lhsT=rkT.t[:, cs], rhs=rqT.t[:, cs], start=True, stop=True), r=[rkT, rqT], w=[ps_])
                V(lambda e, ps_=ps_, sm_=sm_: e.tensor_tensor(out=sm_.t[:], in0=ps_.t[:, 0:128], in1=DT.t[:], op=ALU.mult), r=[ps_, DT], w=[sm_])
                PL(lambda e, qf_=qf_, cs=cs: e.tensor_tensor(out=qf_.t[:], in0=rqT.t[:, cs], in1=QF.t[0:64, :], op=ALU.mult), r=[rqT, QF], w=[qf_])
                PL(lambda e, qb_=qb_, cs=cs: e.tensor_tensor(out=qb_.t[:], in0=rqT.t[:, cs], in1=QB.t[0:64, :], op=ALU.mult), r=[rqT, QB], w=[qb_])
                T(lambda e, po=po, j4=j4, sm_=sm_, n=n: e.matmul(po.t[:, j4 * 128:(j4 + 1) * 128], lhsT=sm_.t[:], rhs=rv.t[:, n, :], start=True, stop=False), r=[sm_, rv], w=[po])
                T(lambda e, po=po, j4=j4, qf_=qf_, pb_cur=pb_cur: e.matmul(po.t[:, j4 * 128:(j4 + 1) * 128], lhsT=qf_.t[:], rhs=pb_cur.t[:], start=False, stop=False),
                  r=[qf_, pb_cur], w=[po])
                T(lambda e, po=po, j4=j4, qb_=qb_, n=n: e.matmul(po.t[:, j4 * 128:(j4 + 1) * 128], lhsT=qb_.t[:], rhs=Nbf.t[:, n, :], start=False, stop=True),
                  r=[qb_, Nbf], w=[po])
                if n < NT - 1:
                    pk = psK[n % 2]
                    T(lambda e, pk=pk, n=n: e.matmul(pk.t[0:64, 0:128], lhsT=ktf.t[:, n, :], rhs=rv.t[:, n, :], start=True, stop=True), r=[ktf, rv], w=[pk])
                    V(lambda e, pk=pk, pr_cur=pr_cur, pr_nxt=pr_nxt: e.scalar_tensor_tensor(out=pr_nxt.t[:], in0=pr_cur.t[:], scalar=tfb.t[0:64, 2:3],
                                                                                          in1=pk.t[0:64, 0:128], op0=ALU.mult, op1=ALU.add),
                      r=[pr_cur, pk, tfb], w=[pr_nxt])
                    A(lambda e, pr_nxt=pr_nxt, pb_nxt=pb_nxt: e.copy(out=pb_nxt.t[:], in_=pr_nxt.t[:]), r=[pr_nxt], w=[pb_nxt])
                if j4 == 3:
                    s4 = st4[(n // 4) % 2]
                    V(lambda e, po=po, s4=s4: e.tensor_reduce(out=s4.t[:, 0:4], in_=po.t[:].rearrange("p (a b) -> p a b", a=4), axis=AX.X, op=ALU.add), r=[po], w=[s4])
                    A(lambda e, po=po: e.activation(out=osq.t[:].rearrange("p a b -> p (a b)"), in_=po.t[:], func=AF.Square), r=[po], w=[osq])
                    V(lambda e, s4=s4: e.tensor_reduce(out=s4.t[:, 4:8], in_=osq.t[:], axis=AX.X, op=ALU.add), r=[osq], w=[s4])
                    V(lambda e, s4=s4: e.tensor_scalar(out=s4.t[:, 8:12], in0=s4.t[:, 0:4], scalar1=1.0 / 128, scalar2=None, op0=ALU.mult), r=[s4], w=[s4])
                    V(lambda e, s4=s4: e.tensor_tensor(out=s4.t[:, 0:4], in0=s4.t[:, 8:12], in1=s4.t[:, 8:12], op=ALU.mult), r=[s4], w=[s4])
                    V(lambda e, s4=s4: e.scalar_tensor_tensor(out=s4.t[:, 12:16], in0=s4.t[:, 4:8], scalar=1.0 / 128, in1=s4.t[:, 0:4],
                                                              op0=ALU.mult, op1=ALU.subtract), r=[s4], w=[s4])
                    A(lambda e, s4=s4: e.activation(out=s4.t[:, 12:16], in_=s4.t[:, 12:16], func=AF.Sqrt, bias=cst.t[:, 2:3]), r=[s4, cst], w=[s4])
                    V(lambda e, s4=s4: e.reciprocal(out=s4.t[:, 12:16], in_=s4.t[:, 12:16]), r=[s4], w=[s4])
                    for jj in range(4):
                        m = n - 3 + jj
                        y_ = yr[m % 2]
                        py = psY[m % 2]
                        V(lambda e, po=po, jj=jj, s4=s4, y_=y_: e.tensor_scalar(out=y_.t[:], in0=po.t[:, jj * 128:(jj + 1) * 128], scalar1=s4.t[:, 8 + jj:9 + jj],
                                                                              scalar2=s4.t[:, 12 + jj:13 + jj], op0=ALU.subtract, op1=ALU.mult),
                          r=[po, s4], w=[y_])
                        PL(lambda e, y_=y_, m=m: e.tensor_tensor(out=y_.t[:], in0=y_.t[:], in1=sg.t[:, m, :], op=ALU.mult), r=[y_, sg], w=[y_])
                        T(lambda e, py=py, y_=y_: e.transpose(out=py.t[:, 0:128], in_=y_.t[:], identity=identb.t[:]), r=[y_, identb], w=[py])
                        A(lambda e, py=py, m=m: e.copy(out=yT.t[:, m * 128:(m + 1) * 128], in_=py.t[:, 0:128]), r=[py], w=[yT])
            p.barrier()
            p.emit()
    def tap(dst, src_ap, reads):
        p.dma("gpsimd", lambda e: e.dma_start(out=dst, in_=src_ap), "tap", reads=reads)
        p.barrier()

    if stop_after <= 2:
        if debug:
            tap(dbg["yT"][0:128, :], yT.t[:], [yT])
        p.finish()
        return nc, p, dbg
    PAD = 1024
    with ExitStack() as ph:
        aqT = mk(ph, "aqT", [128, S], BF16)
        akT = mk(ph, "akT", [128, S + 2 * PAD], BF16)
        avT = mk(ph, "avT", [128, S + 2 * PAD], BF16)
        mkb = mk(ph, "mkb", [128, 18 * 256], BF16)
        acc = [mk(ph, f"acc{h}", [65, S], F32) for h in range(2)]
        Vaug = [mk(ph, f"Vaug{i}", [128, 2, 65], BF16) for i in range(3)]
        Et = [mk(ph, f"Et{i}", [128, 256], BF16) for i in range(2)]
        PTt = [mk(ph, f"PTt{i}", [128, 256], BF16) for i in range(2)]
        rd = mk(ph, "rd", [65, 512], F32)
        psA = [mk(ph, f"psA{i}", None, F32, psum=True) for i in range(2)]
        psV = [mk(ph, f"psV{i}", None, BF16, psum=True) for i in range(2)]
        psB = [mk(ph, f"psB{i}", None, F32, psum=True) for i in range(2)]
        psR = mk(ph, "psR", None, F32, psum=True)
        ld("sync", aqT, aqT.t[:], aq_d, reads=[R_aq])
        ld("sync", akT, akT.t[:, PAD:PAD + S], ak_d, reads=[R_ak])
        ld("sync", avT, avT.t[:, PAD:PAD + S], av_d, reads=[R_av])
        ld("gpsimd", mkb, mkb.t[:], masksin)
        for tns in (akT, avT):
            V(lambda e, tns=tns: e.memset(tns.t[:, 0:PAD], 0.0), w=[tns])
            V(lambda e, tns=tns: e.memset(tns.t[:, PAD + S:PAD + S + PAD], 0.0), w=[tns])
        for vb in Vaug:
            V(lambda e, vb=vb: e.memset(vb.t[:, :, 64:65], 1.0), w=[vb])
        bi = 0
        vi = 0
        for c, d in enumerate(CONFIGS):
            nb = (S // d) // 128
            for r in range(d):
                def ksl(u, d=d, r=r):
                    st0 = PAD + d * (128 * u - 64) + r
                    return slice(st0, st0 + 127 * d + 1, d)
                vt = {}
                for m in range(nb):
                    for u in (m, m + 1):
                        if u in vt:
                            continue
                        vb = Vaug[vi % 3]
                        pv = psV[vi % 2]
                        vi += 1
                        T(lambda e, pv=pv, u=u, ksl=ksl: e.transpose(out=pv.t[:, 0:128], in_=avT.t[:, ksl(u)], identity=identb.t[:]),
                          r=[avT, identb], w=[pv])
                        if vi % 2 == 0:
                            A(lambda e, pv=pv, vb=vb: e.copy(out=vb.t[:, :, 0:64], in_=pv.t[:, 0:128].rearrange("p (h x) -> p h x", h=2)),
                              r=[pv], w=[vb])
                        else:
                            V(lambda e, pv=pv, vb=vb: e.tensor_copy(out=vb.t[:, :, 0:64], in_=pv.t[:, 0:128].rearrange("p (h x) -> p h x", h=2)),
                              r=[pv], w=[vb])
                        vt[u] = vb
                    q0 = d * 128 * m + r
                    qs = slice(q0, q0 + 127 * d + 1, d)
                    var = 1 if m == 0 else (2 if m == nb - 1 else 0)
                    for h in range(2):
                        rows = slice(64 * h, 64 * h + 64)
                        pa = psA[bi % 2]
                        e_ = Et[bi % 2]
                        pt_ = PTt[bi % 2]
                        po = psB[bi % 2]
                        bi += 1
                        for j in range(2):
                            T(lambda e, pa=pa, j=j, rows=rows, m=m, qs=qs, ksl=ksl: e.matmul(pa.t[:, j * 128:(j + 1) * 128], lhsT=akT.t[rows, ksl(m + j)],
                                                                                          rhs=aqT.t[rows, qs], start=True, stop=True),
                              r=[akT, aqT], w=[pa])
                        A(lambda e, pa=pa, e_=e_: e.activation(out=e_.t[:], in_=pa.t[:, 0:256], func=AF.Exp, scale=0.125), r=[pa], w=[e_])
                        mo = ((h * 3 + c) * 3 + var) * 256
                        V(lambda e, e_=e_, pt_=pt_, mo=mo: e.tensor_tensor(out=pt_.t[:], in0=e_.t[:], in1=mkb.t[:, mo:mo + 256], op=ALU.mult),
                          r=[e_, mkb], w=[pt_])
                        for j in range(2):
                            vb = vt[m + j]
                            T(lambda e, po=po, j=j, vb=vb, pt_=pt_, h=h: e.matmul(po.t[0:65, 0:128], lhsT=vb.t[:, h, :], rhs=pt_.t[:, j * 128:(j + 1) * 128],
                                                                               start=(j == 0), stop=(j == 1)), r=[vb, pt_], w=[po])
                        ac = acc[h]
                        if c == 0:
                            A(lambda e, ac=ac, qs=qs, po=po: e.copy(out=ac.t[:, qs], in_=po.t[0:65, 0:128]), r=[po], w=[ac])
                        else:
                            V(lambda e, ac=ac, qs=qs, po=po: e.tensor_tensor(out=ac.t[:, qs], in0=ac.t[:, qs], in1=po.t[0:65, 0:128], op=ALU.add),
                              r=[po, ac], w=[ac])
        for h in range(2):
            yh = (yA, yB)[h]
            ac = acc[h]
            for t in range(16):
                sl = slice(512 * t, 512 * (t + 1))
                V(lambda e, ac=ac, sl=sl: e.reciprocal(out=rd.t[64:65, :], in_=ac.t[64:65, sl]), r=[ac], w=[rd])
                T(lambda e: e.matmul(psR.t[0:64, 0:512], lhsT=onesf.t[64:65, 0:64], rhs=rd.t[64:65, :], start=True, stop=True), r=[onesf, rd], w=[psR])
                V(lambda e, yh=yh, ac=ac, sl=sl: e.tensor_tensor(out=yh.t[:, sl], in0=ac.t[0:64, sl], in1=psR.t[0:64, 0:512], op=ALU.mult),
                  r=[ac, psR], w=[yh])
        p.barrier()
    if stop_after <= 3:
        if debug:
            tap(dbg["yT"][0:128, :], yT.t[:], [yT])
            tap(dbg["yT"][128:192, :], yA.t[:], [yA])
            tap(dbg["yT"][192:256, :], yB.t[:], [yB])
        p.finish()
        return nc, p, dbg

    with ExitStack() as ph:
        wo = mk(ph, "wo", [128, D], BF16)
        woA = mk(ph, "woA", [64, D], BF16)
        woB = mk(ph, "woB", [64, D], BF16)
        pst = [mk(ph, f"pst{i}", [128, D], F32) for i in range(2)]
        psP = [mk(ph, f"psP{i}", None, F32, psum=True) for i in range(2)]
        ld("gpsimd", wo, wo.t[:], wout[0:128, :])
        ld("gpsimd", woA, woA.t[:], wout[128:192, :])
        ld("gpsimd", woB, woB.t[:], wout[192:256, :])
        for n in range(NT):
            ts = slice(128 * n, 128 * (n + 1))
            st_ = pst[n % 2]
            for half in range(2):
                pp = psP[half]
                cs = slice(512 * half, 512 * (half + 1))
                T(lambda e, pp=pp, ts=ts, cs=cs: e.matmul(pp.t[:, 0:512], lhsT=yT.t[:, ts], rhs=wo.t[:, cs], start=True, stop=False), r=[yT, wo], w=[pp])
                T(lambda e, pp=pp, ts=ts, cs=cs: e.matmul(pp.t[:, 0:512], lhsT=yA.t[:, ts], rhs=woA.t[:, cs], start=False, stop=False), r=[yA, woA], w=[pp])
                T(lambda e, pp=pp, ts=ts, cs=cs: e.matmul(pp.t[:, 0:512], lhsT=yB.t[:, ts], rhs=woB.t[:, cs], start=False, stop=True), r=[yB, woB], w=[pp])
                if half == 0:
                    A(lambda e, pp=pp, st_=st_, cs=cs: e.copy(out=st_.t[:, cs], in_=pp.t[:, 0:512]), r=[pp], w=[st_])
                else:
                    V(lambda e, pp=pp, st_=st_, cs=cs: e.tensor_copy(out=st_.t[:, cs], in_=pp.t[:, 0:512]), r=[pp], w=[st_])
            p.dma("sync", lambda e, ts=ts, st_=st_: e.dma_start(out=part_d[ts, :], in_=st_.t[:]), "part_d", reads=[st_], writes=[R_part])
        p.dma("gpsimd", lambda e: e.collective_compute("ReduceScatter", ALU.add, replica_groups=RG, ins=[part_d], outs=[mixed_d]),
              "rs1", reads=[R_part], writes=[R_mixed], inc=1)
        p.barrier()
    ymix.close()
    if stop_after <= 4:
        p.finish()
        return nc, p, dbg
    x1_d = dsc("x1_d", [OWN, D])
    R_x1 = Reg("x1_d")
    with ExitStack() as ph58:
        rows = {k: mk(ph58, f"row_{k}", [128, D], F32) for k in ("gm", "gsf", "shf", "gf")}
        toki = mk(ph58, "toki", [128, 32], I32)
        with ExitStack() as ph:
            diag = [mk(ph, f"diag{i}", [128, 128], F32) for i in range(2)]
            psD = [mk(ph, f"psD{i}", None, F32, psum=True) for i in range(2)]
            srcs = {"gm": der.t[:, 8:16], "gsf": der.t[:, 16:24], "shf": mod.t[:, 24:32], "gf": der.t[:, 24:32]}
            di = 0
            for key in ("gm", "gsf", "shf", "gf"):
                for half in range(2):
                    pd = psD[half]
                    for kk in range(4):
                        k = half * 4 + kk
                        dg = diag[di % 2]
                        di += 1
                        V(lambda e, dg=dg, key=key, k=k: e.tensor_scalar(out=dg.t[:], in0=identf.t[:], scalar1=srcs[key][:, k:k + 1], scalar2=None,
                                                                       op0=ALU.mult), r=[identf, der, mod], w=[dg])
                        T(lambda e, pd=pd, kk=kk, dg=dg: e.matmul(pd.t[:, kk * 128:(kk + 1) * 128], lhsT=onesf.t[:], rhs=dg.t[:], start=True, stop=True),
                          r=[onesf, dg], w=[pd])
                    A(lambda e, pd=pd, key=key, half=half: e.copy(out=rows[key].t[:, half * 512:(half + 1) * 512], in_=pd.t[:, 0:512]),
                      r=[pd], w=[rows[key]])
            zt = mk(ph, "zt", [128, 2048], F32)
            V(lambda e: e.memset(zt.t[:], 0.0), w=[zt])
            for zi in range(32):
                p.dma("sync", lambda e, zi=zi: e.dma_start(out=contrib[256 * zi:256 * (zi + 1), :].rearrange("(p a) d -> p (a d)", a=2), in_=zt.t[:]),
                      "contrib0", reads=[zt], writes=[R_contrib])
            wrs = mk(ph, "wrs", [128, 8, 16], F32)
            ld("sync", wrs, wrs.t[:], wrin.rearrange("(k p) e -> p k e", p=128))
            mx = [mk(ph, f"mx{i}", [128, D], F32) for i in range(2)]
            xo_t = [mk(ph, f"xo{i}", [128, D], F32) for i in range(2)]
            x1t = [mk(ph, f"x1t{i}", [128, D], F32) for i in range(2)]
            h2t = [mk(ph, f"h2t{i}", [128, D], F32) for i in range(2)]
            h2T = [mk(ph, f"h2T{i}", [128, 8, 128], F32) for i in range(2)]
            sq2 = mk(ph, "sq2", [128, D], BF16)
            ss5 = [mk(ph, f"ss5{i}", [128, 8], F32) for i in range(2)]
            afft = [mk(ph, f"afft{i}", [128, 16], F32) for i in range(2)]
            ext = [mk(ph, f"ext{i}", [128, 16], F32) for i in range(2)]
            psH = [mk(ph, f"psH{i}", None, F32, psum=True) for i in range(2)]
            psL = [mk(ph, f"psL{i}", None, F32, psum=True) for i in range(2)]
            for n in range(16):
                ts = slice(128 * n, 128 * (n + 1))
                m_, xo_, x1_, h2_, hT_, s5, af_, ex_ = mx[n % 2], xo_t[n % 2], x1t[n % 2], h2t[n % 2], h2T[n % 2], ss5[n % 2], afft[n % 2], ext[n % 2]
                ld("sync", m_, m_.t[:], mixed_d[ts, :], reads=[R_mixed])
                ld("sync", xo_, xo_.t[:], xo[ts, :])
                A(lambda e, m_=m_, s5=s5: e.activation(out=sq2.t[:], in_=m_.t[:], func=AF.Square, accum_out=s5.t[:, 0:1]), r=[m_], w=[sq2, s5])
                A(lambda e, s5=s5: e.activation(out=s5.t[:, 1:2], in_=s5.t[:, 0:1], func=AF.Sqrt, scale=1.0 / D, bias=cst.t[:, 1:2]), r=[s5, cst], w=[s5])
                V(lambda e, s5=s5: e.reciprocal(out=s5.t[:, 1:2], in_=s5.t[:, 1:2]), r=[s5], w=[s5])
                V(lambda e, m_=m_, s5=s5: e.scalar_tensor_tensor(out=m_.t[:], in0=m_.t[:], scalar=s5.t[:, 1:2], in1=rows["gm"].t[:],
                                                               op0=ALU.mult, op1=ALU.mult), r=[m_, s5, rows["gm"]], w=[m_])
                PL(lambda e, m_=m_, xo_=xo_, x1_=x1_: e.tensor_tensor(out=x1_.t[:], in0=xo_.t[:], in1=m_.t[:], op=ALU.add), r=[m_, xo_], w=[x1_])
                p.dma("sync", lambda e, ts=ts, x1_=x1_: e.dma_start(out=x1_d[ts, :], in_=x1_.t[:]), "x1_d", reads=[x1_], writes=[R_x1])
                A(lambda e, x1_=x1_, s5=s5: e.activation(out=sq2.t[:], in_=x1_.t[:], func=AF.Square, accum_out=s5.t[:, 2:3]), r=[x1_], w=[sq2, s5])
                A(lambda e, s5=s5: e.activation(out=s5.t[:, 3:4], in_=s5.t[:, 2:3], func=AF.Sqrt, scale=1.0 / D, bias=cst.t[:, 1:2]), r=[s5, cst], w=[s5])
                V(lambda e, s5=s5: e.reciprocal(out=s5.t[:, 3:4], in_=s5.t[:, 3:4]), r=[s5], w=[s5])
                V(lambda e, x1_=x1_, s5=s5, h2_=h2_: e.scalar_tensor_tensor(out=h2_.t[:], in0=x1_.t[:], scalar=s5.t[:, 3:4], in1=rows["gsf"].t[:],
                                                                          op0=ALU.mult, op1=ALU.mult), r=[x1_, s5, rows["gsf"]], w=[h2_])
                PL(lambda e, h2_=h2_: e.tensor_tensor(out=h2_.t[:], in0=h2_.t[:], in1=rows["shf"].t[:], op=ALU.add), r=[h2_, rows["shf"]], w=[h2_])
                p.dma("gpsimd", lambda e, ts=ts, h2_=h2_: e.dma_start(out=h2_in[ts, :], in_=h2_.t[:]), "h2_in", reads=[h2_], writes=[R_h2in])
                if 'norouter' in FLAGS:
                    continue
                for k in range(8):
                    ph_ = psH[k // 4]
                    T(lambda e, ph_=ph_, k=k, h2_=h2_: e.transpose(out=ph_.t[:, (k % 4) * 128:(k % 4 + 1) * 128], in_=h2_.t[:, k * 128:(k + 1) * 128],
                                                                 identity=identf.t[:]), r=[h2_, identf], w=[ph_])
                A(lambda e, hT_=hT_: e.copy(out=hT_.t[:, 0:4, :], in_=psH[0].t[:, 0:512].rearrange("p (k s) -> p k s", k=4)), r=[psH[0]], w=[hT_])
                V(lambda e, hT_=hT_: e.tensor_copy(out=hT_.t[:, 4:8, :], in_=psH[1].t[:, 0:512].rearrange("p (k s) -> p k s", k=4)), r=[psH[1]], w=[hT_])
                pl = psL[n % 2]
                for k in range(8):
                    T(lambda e, pl=pl, k=k, hT_=hT_: e.matmul(pl.t[:, 0:16], lhsT=hT_.t[:, k, :], rhs=wrs.t[:, k, :], start=(k == 0), stop=(k == 7)),
                      r=[hT_, wrs], w=[pl])
                V(lambda e, pl=pl, s5=s5: e.tensor_reduce(out=s5.t[:, 4:5], in_=pl.t[:, 0:16], axis=AX.X, op=ALU.max), r=[pl], w=[s5])
                V(lambda e, s5=s5: e.tensor_scalar(out=s5.t[:, 5:6], in0=s5.t[:, 4:5], scalar1=-1.0, scalar2=None, op0=ALU.mult), r=[s5], w=[s5])
                A(lambda e, pl=pl, s5=s5, ex_=ex_: e.activation(out=ex_.t[:], in_=pl.t[:, 0:16], func=AF.Exp, bias=s5.t[:, 5:6], accum_out=s5.t[:, 6:7]),
                  r=[pl, s5], w=[ex_, s5])
                V(lambda e, s5=s5: e.reciprocal(out=s5.t[:, 7:8], in_=s5.t[:, 6:7]), r=[s5], w=[s5])
                V(lambda e, s5=s5, ex_=ex_, af_=af_: e.tensor_scalar(out=af_.t[:], in0=ex_.t[:], scalar1=s5.t[:, 7:8], scalar2=None, op0=ALU.mult),
                  r=[ex_, s5], w=[af_])
                p.dma("sync", lambda e, ts=ts, af_=af_: e.dma_start(out=aff_in[ts, :], in_=af_.t[:]), "aff_in", reads=[af_], writes=[R_affin])
            if 'noag' in FLAGS:
                p.barrier()
                p.finish()
                return nc, p, dbg
            for c4 in range(4):
                p.dma("gpsimd", lambda e, c4=c4: e.collective_compute("AllGather", ALU.bypass, replica_groups=RG,
                                                                      ins=[h2_in[512 * c4:512 * (c4 + 1), :]], outs=[h2_all[c4]]),
                      f"ag1_{c4}", reads=[R_h2in], writes=[R_h2all], inc=1)
            p.dma("gpsimd", lambda e: e.collective_compute("AllGather", ALU.bypass, replica_groups=RG, ins=[aff_in], outs=[aff_all]),
                  "ag2", reads=[R_affin], writes=[R_affall], inc=1)
            for c4 in range(4):
                for r4 in range(4):
                    p.dma("sync", lambda e, c4=c4, r4=r4: e.dma_start(out=h2_tab[2048 * r4 + 512 * c4:2048 * r4 + 512 * (c4 + 1), :],
                                                                      in_=h2_all[c4][512 * r4:512 * (r4 + 1), :]),
                          "h2tab", reads=[R_h2all], writes=[R_h2tab])
            p.dma("sync", lambda e: e.dma_start(out=aff_tab, in_=aff_all), "afftab", reads=[R_affall], writes=[R_afftab])
            p.barrier()
        if stop_after <= 5:
            if debug:
                tap(dbg["x1"], x1_d, [R_x1])
                tap(dbg["aff"], aff_in, [R_affin])
            p.finish()
            return nc, p, dbg

        with ExitStack() as ph:
            Aall = mk(ph, "Aall", [128, 64, 16], F32)
            A4 = mk(ph, "A4", [128, 4, 64], F32)
            cmp_ = mk(ph, "cmp", [128, 4, 64], F32)
            cc0 = mk(ph, "cc0", [128, 4, 64], F32)
            cc1 = mk(ph, "cc1", [128, 4, 64], F32)
            lo = mk(ph, "lo", [128, 4], F32)
            mid = mk(ph, "mid", [128, 4], F32)
            cnt = mk(ph, "cnt", [128, 4], F32)
            ge = mk(ph, "ge", [128, 4], F32)
            offs = mk(ph, "offs", [128, 4], F32)
            tris = mk(ph, "tris", [128, 128], F32)
            slot = mk(ph, "slot", [128, 8], F32)
            tokf = mk(ph, "tokf", [128, 32], F32)
            cb = [mk(ph, f"cb{i}", [128, S], F32) for i in range(2)]
            junk = mk(ph, "junk", [128, S], BF16)
            psC = mk(ph, "psC", None, F32, psum=True)
            ld("sync", Aall, Aall.t[:], aff_tab.rearrange("(p j) e -> p j e", j=64), reads=[R_afftab])
            ld("sync", tris, tris.t[:], triin)
            ld("sync", slot, slot.t[:], slotin)
            for i in range(4):
                V(lambda e, i=i: e.tensor_scalar(out=A4.t[:, i, :], in0=Aall.t[:, :, i], scalar1=ohs.t[:, 0:1], scalar2=None, op0=ALU.mult),
                  r=[Aall, ohs], w=[A4])
                for r in range(1, 4):
                    V(lambda e, i=i, r=r: e.scalar_tensor_tensor(out=A4.t[:, i, :], in0=Aall.t[:, :, 4 * r + i], scalar=ohs.t[:, r:r + 1], in1=A4.t[:, i, :],
                                                                 op0=ALU.mult, op1=ALU.add), r=[Aall, ohs, A4], w=[A4])
            V(lambda e: e.memset(lo.t[:], 0.0), w=[lo])
            for it in range(26):
                wv = 2.0 ** (-(it + 1))
                V(lambda e, wv=wv: e.tensor_scalar(out=mid.t[:], in0=lo.t[:], scalar1=wv, scalar2=None, op0=ALU.add), r=[lo], w=[mid])
                V(lambda e: e.memset(cnt.t[:], 0.0), w=[cnt])
                for i in range(4):
                    V(lambda e, i=i: e.tensor_scalar(out=cmp_.t[:, i, :], in0=A4.t[:, i, :], scalar1=mid.t[:, i:i + 1], scalar2=0.0, op0=ALU.is_gt,
                                                    op1=ALU.add, accum_out=cnt.t[:, i:i + 1]), r=[A4, mid, cnt], w=[cmp_, cnt])
                T(lambda e: e.matmul(psC.t[:, 0:4], lhsT=onesf.t[:], rhs=cnt.t[:], start=True, stop=True), r=[onesf, cnt], w=[psC])
                V(lambda e: e.tensor_scalar(out=ge.t[:], in0=psC.t[:, 0:4], scalar1=CAP - 0.5, scalar2=None, op0=ALU.is_ge), r=[psC], w=[ge])
                V(lambda e, wv=wv: e.scalar_tensor_tensor(out=lo.t[:], in0=ge.t[:], scalar=wv, in1=lo.t[:], op0=ALU.mult, op1=ALU.add), r=[ge, lo], w=[lo])
            for i in range(4):
                V(lambda e, i=i: e.tensor_scalar(out=cc0.t[:, i, :], in0=A4.t[:, i, :], scalar1=lo.t[:, i:i + 1], scalar2=None, op0=ALU.is_gt),
                  r=[A4, lo], w=[cc0])
            ca, cbuf = cc0, cc1
            for sh in (1, 2, 4, 8, 16, 32):
                V(lambda e, ca=ca, cbuf=cbuf, sh=sh: e.tensor_tensor(out=cbuf.t[:, :, sh:64], in0=ca.t[:, :, sh:64], in1=ca.t[:, :, 0:64 - sh], op=ALU.add),
                  r=[ca], w=[cbuf])
                V(lambda e, ca=ca, cbuf=cbuf, sh=sh: e.tensor_copy(out=cbuf.t[:, :, 0:sh], in_=ca.t[:, :, 0:sh]), r=[ca], w=[cbuf])
                ca, cbuf = cbuf, ca
            V(lambda e, ca=ca: e.tensor_copy(out=cnt.t[:], in_=ca.t[:, :, 63]), r=[ca], w=[cnt])
            T(lambda e: e.matmul(psC.t[:, 0:4], lhsT=tris.t[:], rhs=cnt.t[:], start=True, stop=True), r=[tris, cnt], w=[psC])
            V(lambda e: e.tensor_copy(out=offs.t[:], in_=psC.t[:, 0:4]), r=[psC], w=[offs])
            cdr_v = cdr.rearrange("e (p j) -> e p j", j=64)
            for i in range(4):
                V(lambda e, i=i, ca=ca: e.tensor_scalar(out=ca.t[:, i, :], in0=ca.t[:, i, :], scalar1=offs.t[:, i:i + 1], scalar2=None, op0=ALU.add),
                  r=[ca, offs], w=[ca])
                p.dma("sync", lambda e, i=i, ca=ca: e.dma_start(out=cdr_v[i], in_=ca.t[:, i, :]), "cdr", reads=[ca], writes=[R_cdr])
            V(lambda e: e.memset(tokf.t[:], 0.0), w=[tokf])
            for i in range(4):
                cb_ = cb[i % 2]
                ld("sync", cb_, cb_.t[:], cdr[i:i + 1, :].partition_broadcast(128), reads=[R_cdr])
                for st in range(8):
                    col = i * 8 + st
                    V(lambda e, cb_=cb_, st=st, col=col: e.tensor_scalar(out=junk.t[:], in0=cb_.t[:], scalar1=slot.t[:, st:st + 1], scalar2=0.0,
                                                                       op0=ALU.is_le, op1=ALU.add, accum_out=tokf.t[:, col:col + 1]),
                      r=[cb_, slot, tokf], w=[junk, tokf])
            V(lambda e: e.tensor_copy(out=toki.t[:], in_=tokf.t[:]), r=[tokf], w=[toki])
            if debug and stop_after == 6:
                tap(dbg["tok"], tokf.t[:], [tokf])
            p.barrier()
        if stop_after <= 6:
            if debug:
                tap(dbg["x1"], x1_d, [R_x1])
                tap(dbg["aff"], aff_in, [R_affin])
            p.finish()
            return nc, p, dbg

        with ExitStack() as ph:
            xeT = mk(ph, "xeT", [128, 8, CAP], BF16)
            hT = mk(ph, "hT", [128, 16, CAP], BF16)
            wdb = mk(ph, "wdb", [128, 16, D], BF16)
            wgb = [mk(ph, f"wgb{i}", [128, 8, 512], BF16) for i in range(2)]
            wub = [mk(ph, f"wub{i}", [128, 8, 512], BF16) for i in range(2)]
            xet = [mk(ph, f"xet{i}", [128, D], BF16) for i in range(2)]
            gat = [mk(ph, f"gat{i}", [128, 16], F32) for i in range(2)]
            gate = [mk(ph, f"gate{i}", [128, 8], F32) for i in range(2)]
            yet = [mk(ph, f"yet{i}", [128, D], F32) for i in range(2)]
            sgt = [mk(ph, f"sgt{i}", [128, 512], BF16) for i in range(2)]
            psXT = mk(ph, "psXT", None, BF16, psum=True)
            psG = [mk(ph, f"psG{i}", None, F32, psum=True) for i in range(2)]
            psU = [mk(ph, f"psU{i}", None, F32, psum=True) for i in range(2)]
            psY = [mk(ph, f"psYd{i}", None, F32, psum=True) for i in range(2)]
            gi = 0
            for i in range(4):
                ld("gpsimd", wdb, wdb.t[:], wd[i].rearrange("(k p) d -> p k d", p=128))
                gt_ = gate[i % 2]
                for st in range(8):
                    col = i * 8 + st
                    xe_ = xet[gi % 2]
                    ga_ = gat[gi % 2]
                    gi += 1
                    p.dma("gpsimd", lambda e, xe_=xe_, col=col: e.indirect_dma_start(
                        out=xe_.t[:], out_offset=None, in_=h2_tab, in_offset=bass.IndirectOffsetOnAxis(ap=toki.t[:, col:col + 1], axis=0)),
                        xe_.r.name, reads=[R_h2tab, toki], writes=[xe_])
                    p.dma("gpsimd", lambda e, ga_=ga_, col=col: e.indirect_dma_start(
                        out=ga_.t[:], out_offset=None, in_=aff_tab, in_offset=bass.IndirectOffsetOnAxis(ap=toki.t[:, col:col + 1], axis=0)),
                        ga_.r.name, reads=[R_afftab, toki], writes=[ga_])
                    V(lambda e, ga_=ga_, gt_=gt_, st=st, i=i: e.tensor_scalar(out=gt_.t[:, st:st + 1], in0=ga_.t[:, i:i + 1], scalar1=ohs.t[:, 0:1], scalar2=None,
                                                                            op0=ALU.mult), r=[ga_, ohs], w=[gt_])
                    for r in range(1, 4):
                        V(lambda e, ga_=ga_, gt_=gt_, st=st, i=i, r=r: e.scalar_tensor_tensor(
                            out=gt_.t[:, st:st + 1], in0=ga_.t[:, 4 * r + i:4 * r + i + 1], scalar=ohs.t[:, r:r + 1], in1=gt_.t[:, st:st + 1],
                            op0=ALU.mult, op1=ALU.add), r=[ga_, ohs, gt_], w=[gt_])
                    for k in range(8):
                        T(lambda e, k=k, xe_=xe_: e.transpose(out=psXT.t[:, k * 128:(k + 1) * 128], in_=xe_.t[:, k * 128:(k + 1) * 128], identity=identb.t[:]),
                          r=[xe_, identb], w=[psXT])
                    if st % 2 == 0:
                        A(lambda e, st=st: e.copy(out=xeT.t[:, :, st * 128:(st + 1) * 128], in_=psXT.t[:].rearrange("p (k s) -> p k s", k=8)), r=[psXT], w=[xeT])
                    else:
                        V(lambda e, st=st: e.tensor_copy(out=xeT.t[:, :, st * 128:(st + 1) * 128], in_=psXT.t[:].rearrange("p (k s) -> p k s", k=8)),
                          r=[psXT], w=[xeT])
                wg_v = wg[i].rearrange("(k p) f -> p k f", p=128)
                wu_v = wu[i].rearrange("(k p) f -> p k f", p=128)
                mi = 0
                for fq in range(4):
                    wg_, wu_ = wgb[fq % 2], wub[fq % 2]
                    ld("gpsimd", wg_, wg_.t[:], wg_v[:, :, fq * 512:(fq + 1) * 512])
                    ld("gpsimd", wu_, wu_.t[:], wu_v[:, :, fq * 512:(fq + 1) * 512])
                    for fc in range(4):
                        f = fq * 4 + fc
                        for half in range(2):
                            pg, pu, sg_ = psG[mi % 2], psU[mi % 2], sgt[mi % 2]
                            mi += 1
                            hs = slice(512 * half, 512 * (half + 1))
                            for k in range(8):
                                T(lambda e, pg=pg, k=k, wg_=wg_, fc=fc, hs=hs: e.matmul(pg.t[:, 0:512], lhsT=wg_.t[:, k, fc * 128:(fc + 1) * 128], rhs=xeT.t[:, k, hs],
                                                                                    start=(k == 0), stop=(k == 7)), r=[wg_, xeT], w=[pg])
                            for k in range(8):
                                T(lambda e, pu=pu, k=k, wu_=wu_, fc=fc, hs=hs: e.matmul(pu.t[:, 0:512], lhsT=wu_.t[:, k, fc * 128:(fc + 1) * 128], rhs=xeT.t[:, k, hs],
                                                                                    start=(k == 0), stop=(k == 7)), r=[wu_, xeT], w=[pu])
                            A(lambda e, pg=pg, sg_=sg_: e.activation(out=sg_.t[:], in_=pg.t[:, 0:512], func=AF.Silu), r=[pg], w=[sg_])
                            V(lambda e, pu=pu, sg_=sg_, f=f, hs=hs: e.tensor_tensor(out=hT.t[:, f, hs], in0=sg_.t[:], in1=pu.t[:, 0:512], op=ALU.mult),
                              r=[sg_, pu], w=[hT])
                yi = 0
                for st in range(8):
                    col = i * 8 + st
                    ye_ = yet[st % 2]
                    for dh in range(2):
                        py = psY[yi % 2]
                        yi += 1
                        ds_ = slice(512 * dh, 512 * (dh + 1))
                        for f in range(16):
                            T(lambda e, py=py, f=f, st=st, ds_=ds_: e.matmul(py.t[:, 0:512], lhsT=hT.t[:, f, st * 128:(st + 1) * 128], rhs=wdb.t[:, f, ds_],
                                                                          start=(f == 0), stop=(f == 15)), r=[hT, wdb], w=[py])
                        if dh == 0:
                            A(lambda e, py=py, ye_=ye_, ds_=ds_, gt_=gt_, st=st: e.activation(out=ye_.t[:, ds_], in_=py.t[:, 0:512], func=AF.Identity,
                                                                                           scale=gt_.t[:, st:st + 1]), r=[py, gt_], w=[ye_])
                        else:
                            V(lambda e, py=py, ye_=ye_, ds_=ds_, gt_=gt_, st=st: e.tensor_scalar(out=ye_.t[:, ds_], in0=py.t[:, 0:512], scalar1=gt_.t[:, st:st + 1],
                                                                                              scalar2=None, op0=ALU.mult), r=[py, gt_], w=[ye_])
                    p.dma("gpsimd", lambda e, ye_=ye_, col=col: e.indirect_dma_start(
                        out=contrib, out_offset=bass.IndirectOffsetOnAxis(ap=toki.t[:, col:col + 1], axis=0), in_=ye_.t[:], in_offset=None,
                        compute_op=ALU.add), "scat", reads=[ye_, toki, R_contrib], writes=[R_contrib])
            p.dma("gpsimd", lambda e: e.collective_compute("ReduceScatter", ALU.add, replica_groups=RG, ins=[contrib], outs=[moe_d]),
                  "rs2", reads=[R_contrib], writes=[R_moe], inc=1)
            p.barrier()
        if debug and stop_after == 7:
            tap(dbg["moe"], moe_d, [R_moe])

        with ExitStack() as ph:
            mo = [mk(ph, f"mo{i}", [128, D], F32) for i in range(2)]
            x1r = [mk(ph, f"x1r{i}", [128, D], F32) for i in range(2)]
            sq8 = mk(ph, "sq8", [128, D], BF16)
            s8 = [mk(ph, f"s8{i}", [128, 2], F32) for i in range(2)]
            for n in range(16):
                ts = slice(128 * n, 128 * (n + 1))
                m_, x_, s_ = mo[n % 2], x1r[n % 2], s8[n % 2]
                ld("sync", m_, m_.t[:], moe_d[ts, :], reads=[R_moe])
                ld("sync", x_, x_.t[:], x1_d[ts, :], reads=[R_x1])
                A(lambda e, m_=m_, s_=s_: e.activation(out=sq8.t[:], in_=m_.t[:], func=AF.Square, accum_out=s_.t[:, 0:1]), r=[m_], w=[sq8, s_])
                A(lambda e, s_=s_: e.activation(out=s_.t[:, 1:2], in_=s_.t[:, 0:1], func=AF.Sqrt, scale=1.0 / D, bias=cst.t[:, 1:2]), r=[s_, cst], w=[s_])
                V(lambda e, s_=s_: e.reciprocal(out=s_.t[:, 1:2], in_=s_.t[:, 1:2]), r=[s_], w=[s_])
                V(lambda e, m_=m_, s_=s_: e.scalar_tensor_tensor(out=m_.t[:], in0=m_.t[:], scalar=s_.t[:, 1:2], in1=rows["gf"].t[:],
                                                               op0=ALU.mult, op1=ALU.mult), r=[m_, s_, rows["gf"]], w=[m_])
                PL(lambda e, m_=m_, x_=x_: e.tensor_tensor(out=m_.t[:], in0=m_.t[:], in1=x_.t[:], op=ALU.add), r=[m_, x_], w=[m_])
                p.dma("sync", lambda e, ts=ts, m_=m_: e.dma_start(out=out[ts, :], in_=m_.t[:]), "out", reads=[m_], writes=[R_out])
            p.barrier()
    p.finish()
    return nc, p, dbg


def _consts(q):
    j = np.arange(128, dtype=np.float32)[:, None]
    i = np.arange(128, dtype=np.float32)[None, :]
    rc = np.zeros((128, 514), np.float32)
    rc[:, 0:128] = np.maximum(i - j, 0)
    rc[:, 128:256] = np.maximum(j - i, 0)
    rc[:, 256:384] = i + 1
    rc[:, 384:512] = 128 - i
    rc[:, 512] = 127 - j[:, 0]
    rc[:, 513] = j[:, 0]
    masks = np.zeros((128, 2, 3, 3, 2, 128), np.float32)
    kk = np.arange(128)[:, None, None]
    jj = np.arange(2)[None, :, None]
    ii = np.arange(128)[None, None, :]
    delta = np.abs(kk + 128 * jj - 64 - ii).astype(np.float32)
    band = (delta <= 64).astype(np.float32)
    for h in range(2):
        slope = SLOPES[2 * q + h]
        for c, d in enumerate(CONFIGS):
            m = band * np.exp(-slope * d * delta)
            masks[:, h, c, 0] = m
            m1 = m.copy()
            m1[0:64, 0, :] = 0
            masks[:, h, c, 1] = m1
            m2 = m.copy()
            m2[64:128, 1, :] = 0
            masks[:, h, c, 2] = m2
    slotid = (128 * np.arange(8)[None, :] + np.arange(128)[:, None]).astype(np.float32)
    tri = (np.arange(128)[:, None] < np.arange(128)[None, :]).astype(np.float32)
    return rc, masks.reshape(128, 18 * 256), slotid, tri


def prep(inputs):
    f = lambda a: np.ascontiguousarray(np.asarray(a, dtype=np.float32))
    x, c = f(inputs["x"]), f(inputs["c"])
    w_in = f(inputs["w_in"])[0]
    w_out = f(inputs["w_out"])[0]
    col = lambda v: np.ascontiguousarray(v.reshape(-1, 128).T)
    vcols = np.concatenate([col(f(inputs["b_ada"])[0]), col(f(inputs["g_pre_mix"])[0]), col(f(inputs["g_post_mix"])[0]),
                            col(f(inputs["g_pre_ffn"])[0]), col(f(inputs["g_post_ffn"])[0])], axis=1)
    wada = f(inputs["w_ada"])[0]
    wr = f(inputs["w_router"])[0]
    wge, wue, wde = f(inputs["w_gate_e"])[0], f(inputs["w_up_e"])[0], f(inputs["w_down_e"])[0]
    df, db = f(inputs["ret_decay_fwd"])[0], f(inputs["ret_decay_bwd"])[0]
    ident = np.eye(128, dtype=np.float32)
    maps = []
    for i in range(8):
        b, q = i // 4, i % 4
        rq = np.arange(64 * q, 64 * q + 64)
        rk = 256 + rq
        rvc = 512 + np.arange(128 * q, 128 * q + 128)
        rgc = 1024 + np.arange(128 * q, 128 * q + 128)
        aqc = 1536 + np.arange(128 * q, 128 * q + 128)
        akc = 2048 + np.arange(128 * q, 128 * q + 128)
        avc = 2560 + np.arange(128 * q, 128 * q + 128)
        cols = np.concatenate([rq, rk, aqc, akc, avc, rk, rvc, rgc])
        rows = np.concatenate([np.arange(128 * q, 128 * q + 128), 512 + np.arange(128 * q, 128 * q + 128)])
        rc, masks, slotid, tri = _consts(q)
        oh = np.zeros((128, 4), np.float32)
        oh[:, q] = 1.0
        dec = np.zeros((128, 2), np.float32)
        dec[:, 0] = df[q]
        dec[:, 1] = db[q]
        maps.append({
            "xb": x[b], "xo": np.ascontiguousarray(x[b, OWN * q:OWN * (q + 1)]), "ccol": col(c[b]),
            "wada": wada, "vcols": vcols, "win": np.ascontiguousarray(w_in[:, cols]), "dec": dec,
            "wout": np.ascontiguousarray(w_out[rows]), "wr": wr, "oh": oh,
            "wg": np.ascontiguousarray(wge[4 * q:4 * q + 4]), "wu": np.ascontiguousarray(wue[4 * q:4 * q + 4]),
            "wd": np.ascontiguousarray(wde[4 * q:4 * q + 4]),
            "ident": ident, "masks": masks, "rc": rc, "slotid": slotid, "tri": tri,
        })
    return maps


_NC_CACHE = {}


def kernel(**inputs):
    maps = prep(inputs)
    if "nc" not in _NC_CACHE:
        _NC_CACHE["nc"] = build()[0]
    res = run_bass_kernel_spmd(_NC_CACHE["nc"], maps, core_ids=list(range(8)))
    out = np.zeros((2, S, D), np.float32)
    for i in range(8):
        b, q = i // 4, i % 4
        out[b, OWN * q:OWN * (q + 1)] = res.results[i]["out"]
    return out
```

```python
import numpy as np
from contextlib import ExitStack
import concourse.bass as bass
import concourse.mybir as mybir
from concourse.bass_utils import run_bass_kernel_spmd

F32 = mybir.dt.float32
BF16 = mybir.dt.bfloat16
I32 = mybir.dt.int32
ALU = mybir.AluOpType
AF = mybir.ActivationFunctionType
AX = mybir.AxisListType
ENGS = ["sync", "scalar", "vector", "gpsimd", "tensor"]

S = 8192
D = 1024
NT = S // 128
OWN = 2048
CAP = 1024
LN8 = -2.0794415416798357
SLOPES = [2.0 ** (-(h + 1)) for h in range(8)]
CONFIGS = (1, 4, 16)


class Reg:
    __slots__ = ("w", "r", "name", "psum", "cw")

    def __init__(self, name="", psum=False):
        self.w = None
        self.cw = {}
        self.r = {}
        self.name = name
        self.psum = psum


class Buf:
    __slots__ = ("t", "r")

    def __init__(self, t, name):
        self.t = t
        self.r = Reg(name)


class Prog:
    def __init__(self, nc):
        self.nc = nc
        self.stack = ExitStack()
        self.ops = {e: [] for e in ENGS}
        self.cnt = {e: 0 for e in ENGS}
        self.sem = {e: self.stack.enter_context(nc.semaphore(f"c_{e}")) for e in ENGS}
        self.known = {e: {} for e in ENGS}
        self.dsem = {}
        self.dcnt = {}

    def _need(self, eng, tok, waits):
        if tok is None:
            return
        sem, val, src = tok
        if src == eng and eng == "tensor":
            return
        if src == eng and val <= self.cnt[eng] - 3:
            return
        k = self.known[eng]
        if k.get(id(sem), 0) >= val:
            return
        k[id(sem)] = val
        waits.append((sem, val))

    def _deps(self, eng, reads, writes, cwrites=()):
        waits = []
        for c in cwrites:
            self._need(eng, c.w, waits)
            for t in c.r.values():
                self._need(eng, t, waits)
        for r in reads:
            self._need(eng, r.w, waits)
            for t in r.cw.values():
                self._need(eng, t, waits)
            if r.psum:
                for t in r.r.values():
                    if t[2] != eng:
                        self._need(eng, t, waits)
        for w in writes:
            self._need(eng, w.w, waits)
            for t in w.cw.values():
                self._need(eng, t, waits)
            for t in w.r.values():
                self._need(eng, t, waits)
        best = {}
        for sem, val in waits:
            if id(sem) not in best or best[id(sem)][1] < val:
                best[id(sem)] = (sem, val)
        return list(best.values())

    def _commit(self, tok, reads, writes, cwrites=()):
        for r in reads:
            r.r[id(tok[0])] = tok
        for w in writes:
            w.w = tok
            w.cw = {}
            w.r = {}
        for c in cwrites:
            c.cw[id(tok[0])] = tok

    def op(self, eng, fn, reads=(), writes=(), cwrites=()):
        reads = [x.r if isinstance(x, Buf) else x for x in reads]
        writes = [x.r if isinstance(x, Buf) else x for x in writes]
        cwrites = [x.r if isinstance(x, Buf) else x for x in cwrites]
        waits = self._deps(eng, reads, writes, cwrites)
        self.cnt[eng] += 1
        tok = (self.sem[eng], self.cnt[eng], eng)
        self.ops[eng].append((waits, fn, (self.sem[eng], 1)))
        self._commit(tok, reads, writes, cwrites)
        return tok

    def dma(self, q, fn, key, reads=(), writes=(), inc=16):
        reads = [x.r if isinstance(x, Buf) else x for x in reads]
        writes = [x.r if isinstance(x, Buf) else x for x in writes]
        if key not in self.dsem:
            self.dsem[key] = self.stack.enter_context(self.nc.semaphore(f"d_{key}"))
            self.dcnt[key] = 0
        waits = self._deps(q, reads, writes)
        self.dcnt[key] += inc
        tok = (self.dsem[key], self.dcnt[key], "dma")
        self.ops[q].append((waits, fn, (self.dsem[key], inc)))
        self._commit(tok, reads, writes)
        return tok

    def barrier(self):
        for eng in ENGS:
            waits = []
            for e in ENGS:
                if self.cnt[e] > 0 and e != eng:
                    self._need(eng, (self.sem[e], self.cnt[e], e), waits)
            for key, sem in self.dsem.items():
                self._need(eng, (sem, self.dcnt[key], "dma"), waits)
            self.ops[eng].append((waits, None, None))

    def emit(self):
        return

    def finish(self):
        nc = self.nc
        ops = self.ops
        self.ops = {e: [] for e in ENGS}

        def replay(name, e):
            for waits, fn, inc in ops[name]:
                for sem, val in waits:
                    e.wait_ge(sem, val)
                if fn is not None:
                    fn(e).then_inc(inc[0], inc[1])

        with nc.Block() as block:
            @block.sync
            def _(e):
                replay("sync", e)

            @block.scalar
            def _(e):
                replay("scalar", e)

            @block.vector
            def _(e):
                replay("vector", e)

            @block.gpsimd
            def _(e):
                replay("gpsimd", e)

            @block.tensor
            def _(e):
                replay("tensor", e)


def build(stop_after=99, debug=False):
    import os
    NGRP = int(os.environ.get('NGRP', '16'))
    FLAGS = os.environ.get('KFLAGS', '').split(',')
    nc = bass.Bass("TRN2", target_bir_lowering=False)

    def din(name, shape, dt=F32):
        return nc.dram_tensor(name, list(shape), dt, kind="ExternalInput").ap()

    def dsc(name, shape, dt=F32):
        return nc.dram_tensor(name, list(shape), dt).ap()

    xb = din("xb", [S, D])
    xo = din("xo", [OWN, D])
    ccol = din("ccol", [128, 8])
    wada = din("wada", [D, 6 * D])
    vcols = din("vcols", [128, 80])
    win = din("win", [D, 832])
    decin = din("dec", [128, 2])
    wout = din("wout", [256, D])
    wrin = din("wr", [D, 16])
    ohin = din("oh", [128, 4])
    if stop_after >= 7:
        wg = din("wg", [4, D, 2048])
        wu = din("wu", [4, D, 2048])
        wd = din("wd", [4, 2048, D])
    identin = din("ident", [128, 128])
    masksin = din("masks", [128, 18 * 256])
    rcin = din("rc", [128, 514])
    slotin = din("slotid", [128, 8])
    triin = din("tri", [128, 128])
    out = nc.dram_tensor("out", [OWN, D], F32, kind="ExternalOutput").ap()
    dbg = {}
    if debug:
        if stop_after in (2, 3):
            dbg["yT"] = nc.dram_tensor("dbg_yT", [256, S], F32, kind="ExternalOutput").ap()
        if stop_after in (5, 6):
            dbg["x1"] = nc.dram_tensor("dbg_x1", [OWN, D], F32, kind="ExternalOutput").ap()
            dbg["aff"] = nc.dram_tensor("dbg_aff", [OWN, 16], F32, kind="ExternalOutput").ap()
            dbg["tok"] = nc.dram_tensor("dbg_tok", [128, 32], F32, kind="ExternalOutput").ap()
        if stop_after == 7:
            dbg["moe"] = nc.dram_tensor("dbg_moe", [OWN, D], F32, kind="ExternalOutput").ap()

    aq_d = dsc("aq_d", [128, S], BF16)
    ak_d = dsc("ak_d", [128, S], BF16)
    av_d = dsc("av_d", [128, S], BF16)
    part_d = dsc("part_d", [S, D])
    mixed_d = dsc("mixed_d", [OWN, D])
    h2_in = dsc("h2_in", [OWN, D], BF16)
    h2_all = [dsc(f"h2_all{c4}", [2048, D], BF16) for c4 in range(4)]
    h2_tab = dsc("h2_tab", [S, D], BF16)
    aff_in = dsc("aff_in", [OWN, 16])
    aff_all = dsc("aff_all", [S, 16])
    aff_tab = dsc("aff_tab", [S, 16])
    cdr = dsc("cdr", [4, S])
    contrib = dsc("contrib", [S, D])
    moe_d = dsc("moe_d", [OWN, D])
    R_aq, R_ak, R_av = Reg("aq_d"), Reg("ak_d"), Reg("av_d")
    R_part, R_mixed = Reg("part_d"), Reg("mixed_d")
    R_h2in, R_h2all, R_h2tab = Reg("h2in"), Reg("h2all"), Reg("h2tab")
    R_affin, R_affall, R_afftab = Reg("affin"), Reg("affall"), Reg("afftab")
    R_cdr, R_contrib, R_moe, R_out = Reg("cdr"), Reg("contrib"), Reg("moe"), Reg("out")
    RG = [[0, 1, 2, 3], [4, 5, 6, 7]] if 'half' not in FLAGS else [[0, 1, 2, 3]]

    p = Prog(nc)
    GS = p.stack

    def mk(stack, name, shape, dt, psum=False):
        if psum:
            t = stack.enter_context(nc.psum_tensor(name, [128, 512 if dt == F32 else 1024], dt))
        else:
            t = stack.enter_context(nc.sbuf_tensor(name, list(shape), dt))
        b = Buf(t, name)
        b.r.psum = psum
        return b

    V = lambda fn, r=(), w=(), cw=(): p.op("vector", fn, r, w, cw)
    A = lambda fn, r=(), w=(), cw=(): p.op("scalar", fn, r, w, cw)
    PL = lambda fn, r=(), w=(), cw=(): p.op("gpsimd", fn, r, w, cw)
    T = lambda fn, r=(), w=(), cw=(): p.op("tensor", fn, r, w, cw)

    def ld(q, dst, dst_ap, src_ap, reads=()):
        return p.dma(q, lambda e: e.dma_start(out=dst_ap, in_=src_ap), dst.r.name, reads=reads, writes=[dst])

    identf = mk(GS, "identf", [128, 128], F32)
    identb = mk(GS, "identb", [128, 128], BF16)
    onesf = mk(GS, "onesf", [128, 128], F32)
    vc = mk(GS, "vc", [128, 80], F32)
    mod = mk(GS, "mod", [128, 48], F32)
    der = mk(GS, "der", [128, 32], F32)
    ohs = mk(GS, "ohs", [128, 4], F32)
    ld("sync", identf, identf.t[:], identin)
    ld("gpsimd", identb, identb.t[:], identin)
    ld("sync", vc, vc.t[:], vcols)
    ld("sync", ohs, ohs.t[:], ohin)
    V(lambda e: e.memset(onesf.t[:], 1.0), w=[onesf])
    cst = mk(GS, "cst", [128, 4], F32)
    V(lambda e: e.memset(cst.t[:, 0:1], LN8), w=[cst])
    V(lambda e: e.memset(cst.t[:, 1:2], 1e-6), w=[cst])
    V(lambda e: e.memset(cst.t[:, 2:3], 1e-5), w=[cst])
    V(lambda e: e.memset(cst.t[:, 3:4], 0.0), w=[cst])
    ymix = ExitStack()
    yT = mk(ymix, "yT", [128, S], BF16)
    yA = mk(ymix, "yA", [64, S], BF16)
    yB = mk(ymix, "yB", [64, S], BF16)

    with ExitStack() as ph:
        cc = mk(ph, "cc", [128, 8], F32)
        scb = mk(ph, "scb", [128, 8], BF16)
        wa = [mk(ph, f"wa{i}", [128, 8, D], BF16) for i in range(2)]
        psm = mk(ph, "psm", [128, 48], F32, psum=True)
        ld("sync", cc, cc.t[:], ccol)
        A(lambda e: e.activation(out=scb.t[:], in_=cc.t[:], func=AF.Silu), r=[cc], w=[scb])
        wada_v = wada.rearrange("(k p) n -> p k n", p=128)
        for g in range(6):
            w_ = wa[g % 2]
            ld("gpsimd", w_, w_.t[:], wada_v[:, :, g * D:(g + 1) * D])
            for j in range(8):
                for k in range(8):
                    T(lambda e, w_=w_, g=g, j=j, k=k: e.matmul(
                        psm.t[:, g * 8 + j:g * 8 + j + 1], lhsT=w_.t[:, k, j * 128:(j + 1) * 128],
                        rhs=scb.t[:, k:k + 1], start=(k == 0), stop=(k == 7)), r=[w_, scb], w=[psm])
        V(lambda e: e.tensor_tensor(out=mod.t[:], in0=psm.t[:, 0:48], in1=vc.t[:, 0:48], op=ALU.add), r=[psm, vc], w=[mod])
        V(lambda e: e.scalar_tensor_tensor(out=der.t[:, 0:8], in0=mod.t[:, 8:16], scalar=1.0, in1=vc.t[:, 48:56],
                                           op0=ALU.add, op1=ALU.mult), r=[mod, vc], w=[der])
        V(lambda e: e.tensor_tensor(out=der.t[:, 8:16], in0=mod.t[:, 16:24], in1=vc.t[:, 56:64], op=ALU.mult), r=[mod, vc], w=[der])
        V(lambda e: e.scalar_tensor_tensor(out=der.t[:, 16:24], in0=mod.t[:, 32:40], scalar=1.0, in1=vc.t[:, 64:72],
                                           op0=ALU.add, op1=ALU.mult), r=[mod, vc], w=[der])
        V(lambda e: e.tensor_tensor(out=der.t[:, 24:32], in0=mod.t[:, 40:48], in1=vc.t[:, 72:80], op=ALU.mult), r=[mod, vc], w=[der])
        p.barrier()
        p.emit()

    if stop_after <= 0:
        p.finish()
        return nc, p, dbg
    with ExitStack() as ph:
        rqT = mk(ph, "rqT", [64, S], BF16)
        rkT = mk(ph, "rkT", [64, S], BF16)
        ktf = mk(ph, "ktf", [128, NT, 64], BF16)
        ktb = mk(ph, "ktb", [128, NT, 64], BF16)
        rv = mk(ph, "rv", [128, NT, 128], BF16)
        sg = mk(ph, "sg", [128, NT, 128], BF16)
        rcs = mk(ph, "rcs", [128, 514], F32)
        dcs = mk(ph, "dcs", [128, 2], F32)
        lg = mk(ph, "lg", [128, 2], F32)
        tfb = mk(ph, "tfb", [128, 4], F32)
        DT = mk(ph, "DT", [128, 128], F32)
        QF = mk(ph, "QF", [128, 128], BF16)
        QB = mk(ph, "QB", [128, 128], BF16)
        ld("sync", rcs, rcs.t[:], rcin)
        ld("sync", dcs, dcs.t[:], decin)
        A(lambda e: e.activation(out=lg.t[:], in_=dcs.t[:], func=AF.Exp, scale=-1.0), r=[dcs], w=[lg])
        V(lambda e: e.tensor_scalar(out=lg.t[:], in0=lg.t[:], scalar1=1.0, scalar2=None, op0=ALU.add), r=[lg], w=[lg])
        A(lambda e: e.activation(out=lg.t[:], in_=lg.t[:], func=AF.Ln), r=[lg], w=[lg])
        V(lambda e: e.tensor_scalar(out=lg.t[:], in0=lg.t[:], scalar1=-1.0, scalar2=None, op0=ALU.mult), r=[lg], w=[lg])
        A(lambda e: e.activation(out=tfb.t[:, 0:1], in_=rcs.t[:, 512:513], func=AF.Exp, scale=lg.t[:, 0:1], bias=cst.t[:, 0:1]), r=[rcs, lg, cst], w=[tfb])
        A(lambda e: e.activation(out=tfb.t[:, 1:2], in_=rcs.t[:, 513:514], func=AF.Exp, scale=lg.t[:, 1:2], bias=cst.t[:, 0:1]), r=[rcs, lg, cst], w=[tfb])
        A(lambda e: e.activation(out=tfb.t[:, 2:4], in_=lg.t[:, 0:2], func=AF.Exp, scale=128.0), r=[lg], w=[tfb])
        A(lambda e: e.activation(out=QF.t[:], in_=rcs.t[:, 256:384], func=AF.Exp, scale=lg.t[:, 0:1]), r=[rcs, lg], w=[QF])
        A(lambda e: e.activation(out=QB.t[:], in_=rcs.t[:, 384:512], func=AF.Exp, scale=lg.t[:, 1:2]), r=[rcs, lg], w=[QB])
        V(lambda e: e.tensor_scalar(out=DT.t[:], in0=rcs.t[:, 0:128], scalar1=lg.t[:, 0:1], scalar2=None, op0=ALU.mult), r=[rcs, lg], w=[DT])
        V(lambda e: e.scalar_tensor_tensor(out=DT.t[:], in0=rcs.t[:, 128:256], scalar=lg.t[:, 1:2], in1=DT.t[:],
                                           op0=ALU.mult, op1=ALU.add), r=[rcs, lg, DT], w=[DT])
        A(lambda e: e.activation(out=DT.t[:], in_=DT.t[:], func=AF.Exp, bias=cst.t[:, 0:1]), r=[DT, cst], w=[DT])

        if 'pre_only' in FLAGS:
            p.barrier()
            p.finish()
            return nc, p, dbg
        with ExitStack() as ph1:
            winb = mk(ph1, "winb", [128, 8, 832], BF16)
            xs = [mk(ph1, f"xs{i}", [128, D], F32) for i in range(2)]
            sqj = mk(ph1, "sqj", [128, D], BF16)
            ssq = [mk(ph1, f"ssq{i}", [128, 2], F32) for i in range(2)]
            xn = [mk(ph1, f"xn{i}", [128, D], BF16) for i in range(2)]
            h1T = [mk(ph1, f"h1T{i}", [128, 8, 512], BF16) for i in range(2)]
            stg = [[mk(ph1, f"stg{a}{i}", [128, 512], BF16) for i in range(2)] for a in range(3)]
            psXa = [mk(ph1, f"psXa{i}", None, BF16, psum=True) for i in range(2)]
            psXb = [mk(ph1, f"psXb{i}", None, BF16, psum=True) for i in range(2)]
            psF = [mk(ph1, f"psF{i}", [128, 512], F32, psum=True) for i in range(2)]
            psT = [mk(ph1, f"psT{i}", [128, 320], F32, psum=True) for i in range(2)]
            ld("gpsimd", winb, winb.t[:], win.rearrange("(k p) n -> p k n", p=128))
            zt = mk(ph1, "zt", [128, D], BF16)
            PL(lambda e: e.memset(zt.t[:], 0.0), w=[zt])
            for zi in range(64):
                p.dma("gpsimd", lambda e, zi=zi: e.dma_start(out=contrib[128 * zi:128 * (zi + 1), :], in_=zt.t[:]),
                      "contrib0", reads=[zt], writes=[R_contrib])
            xb_v = xb.rearrange("(n p) d -> p n d", p=128)
            fcount = 0
            for Gi in range(NGRP):
                h1 = h1T[Gi % 2]
                for tt in range(4):
                    n = 4 * Gi + tt
                    x_ = xs[n % 2]
                    s_ = ssq[n % 2]
                    xn_ = xn[n % 2]
                    pxa, pxb = psXa[n % 2], psXb[n % 2]
                    ld("sync", x_, x_.t[:], xb_v[:, n, :])
                    A(lambda e, x_=x_, s_=s_: e.activation(out=sqj.t[:], in_=x_.t[:], func=AF.Square, accum_out=s_.t[:, 0:1]),
                      r=[x_], w=[sqj, s_])
                    A(lambda e, s_=s_: e.activation(out=s_.t[:, 1:2], in_=s_.t[:, 0:1], func=AF.Sqrt, scale=1.0 / D, bias=cst.t[:, 1:2]),
                      r=[s_, cst], w=[s_])
                    V(lambda e, s_=s_: e.reciprocal(out=s_.t[:, 1:2], in_=s_.t[:, 1:2]), r=[s_], w=[s_])
                    V(lambda e, x_=x_, s_=s_, xn_=xn_: e.tensor_scalar(out=xn_.t[:], in0=x_.t[:], scalar1=s_.t[:, 1:2], scalar2=None,
                                                                     op0=ALU.mult), r=[x_, s_], w=[xn_])
                    for k in range(8):
                        px = pxa if k % 2 == 0 else pxb
                        T(lambda e, k=k, px=px, xn_=xn_: e.transpose(out=px.t[:, (k // 2) * 128:(k // 2 + 1) * 128], in_=xn_.t[:, k * 128:(k + 1) * 128],
                                                                   identity=identb.t[:]), r=[xn_, identb], w=[px])
                    for k in range(8):
                        px = pxa if k % 2 == 0 else pxb
                        o_ = h1.t[:, k, tt * 128:(tt + 1) * 128]
                        i_ = px.t[:, (k // 2) * 128:(k // 2 + 1) * 128]
                        if k % 2 == 0:
                            A(lambda e, o_=o_, i_=i_, k=k: e.activation(out=o_, in_=i_, func=AF.Identity, scale=der.t[:, k:k + 1],
                                                                       bias=mod.t[:, k:k + 1]), r=[px, der, mod], cw=[h1])
                        else:
                            V(lambda e, o_=o_, i_=i_, k=k: e.tensor_scalar(out=o_, in0=i_, scalar1=der.t[:, k:k + 1], scalar2=mod.t[:, k:k + 1],
                                                                         op0=ALU.mult, op1=ALU.add), r=[px, der, mod], cw=[h1])
                for fi, (c0, wdt) in enumerate([] if 'noF' in FLAGS else [(0, 64), (64, 64), (128, 128), (256, 128), (384, 128)]):
                    pf = psF[fcount % 2]
                    fcount += 1
                    for k in range(8):
                        T(lambda e, pf=pf, k=k, c0=c0, wdt=wdt, h1=h1: e.matmul(pf.t[0:wdt, :], lhsT=winb.t[:, k, c0:c0 + wdt], rhs=h1.t[:, k, :],
                                                                             start=(k == 0), stop=(k == 7)), r=[winb, h1], w=[pf])
                    sl = slice(Gi * 512, (Gi + 1) * 512)
                    if fi == 0:
                        A(lambda e, pf=pf, sl=sl: e.copy(out=rqT.t[:, sl], in_=pf.t[0:64, :]), r=[pf], cw=[rqT])
                    elif fi == 1:
                        V(lambda e, pf=pf, sl=sl: e.tensor_copy(out=rkT.t[:, sl], in_=pf.t[0:64, :]), r=[pf], cw=[rkT])
                    else:
                        sb_ = stg[fi - 2][Gi % 2]
                        dr, dreg = [(aq_d, R_aq), (ak_d, R_ak), (av_d, R_av)][fi - 2]
                        if fi == 3:
                            V(lambda e, pf=pf, sb_=sb_: e.tensor_copy(out=sb_.t[:], in_=pf.t[:]), r=[pf], w=[sb_])
                        else:
                            A(lambda e, pf=pf, sb_=sb_: e.copy(out=sb_.t[:], in_=pf.t[:]), r=[pf], w=[sb_])
                        if 'nospill' not in FLAGS:
                            p.dma("sync", lambda e, dr=dr, sl=sl, sb_=sb_: e.dma_start(out=dr[:, sl], in_=sb_.t[:]), dreg.name,
                                  reads=[sb_], writes=[dreg])
                for tt in range(0 if 'noT' in FLAGS else 4):
                    n = 4 * Gi + tt
                    pt = psT[n % 2]
                    for k in range(8):
                        T(lambda e, pt=pt, k=k, tt=tt, h1=h1: e.matmul(pt.t[:, 0:320], lhsT=h1.t[:, k, tt * 128:(tt + 1) * 128], rhs=winb.t[:, k, 512:832],
                                                                     start=(k == 0), stop=(k == 7)), r=[winb, h1], w=[pt])
                    A(lambda e, pt=pt, n=n: e.activation(out=ktf.t[:, n, :], in_=pt.t[:, 0:64], func=AF.Identity, scale=tfb.t[:, 0:1]),
                      r=[pt, tfb], cw=[ktf])
                    A(lambda e, pt=pt, n=n: e.copy(out=sg.t[:, n, :], in_=pt.t[:, 192:320]), r=[pt], cw=[sg])
                    V(lambda e, pt=pt, n=n: e.tensor_scalar(out=ktb.t[:, n, :], in0=pt.t[:, 0:64], scalar1=tfb.t[:, 1:2], scalar2=None,
                                                          op0=ALU.mult), r=[pt, tfb], cw=[ktb])
                    V(lambda e, pt=pt, n=n: e.tensor_copy(out=rv.t[:, n, :], in_=pt.t[:, 64:192]), r=[pt], cw=[rv])
            p.barrier()
            p.emit()
        if stop_after <= 1:
            p.finish()
            return nc, p, dbg

        with ExitStack() as ph2:
            Nbf = mk(ph2, "Nbf", [64, NT, 128], BF16)
            Nrun = [mk(ph2, f"Nrun{i}", [64, 128], F32) for i in range(2)]
            Prun = [mk(ph2, f"Prun{i}", [64, 128], F32) for i in range(2)]
            Pbf = [mk(ph2, f"Pbf{i}", [64, 128], BF16) for i in range(2)]
            SM = [mk(ph2, f"SM{i}", [128, 128], BF16) for i in range(2)]
            qf = [mk(ph2, f"qf{i}", [64, 128], BF16) for i in range(2)]
            qb = [mk(ph2, f"qb{i}", [64, 128], BF16) for i in range(2)]
            osq = mk(ph2, "osq", [128, 4, 128], F32)
            st4 = [mk(ph2, f"st4{i}", [128, 16], F32) for i in range(2)]
            yr = [mk(ph2, f"yr{i}", [128, 128], BF16) for i in range(2)]
            psK = [mk(ph2, f"psK{i}", [64, 128], F32, psum=True) for i in range(2)]
            psS = [mk(ph2, f"psS{i}", [128, 128], F32, psum=True) for i in range(2)]
            psO = [mk(ph2, f"psO{i}", [128, 4, 128], F32, psum=True) for i in range(2)]
            psY = [mk(ph2, f"psY{i}", [128, 128], BF16, psum=True) for i in range(2)]
            for g4 in range(4):
                A(lambda e, g4=g4: e.activation(out=sg.t[:, 16 * g4:16 * (g4 + 1), :], in_=sg.t[:, 16 * g4:16 * (g4 + 1), :], func=AF.Silu), r=[sg], w=[sg])
            V(lambda e: e.memset(Nrun[1].t[:], 0.0), w=[Nrun[1]])
            V(lambda e: e.memset(Nbf.t[:, NT - 1, :], 0.0), w=[Nbf])
            for n in range(NT - 1, 0, -1):
                pk = psK[n % 2]
                cur, nxt = Nrun[n % 2], Nrun[(n + 1) % 2]
                T(lambda e, pk=pk, n=n: e.matmul(pk.t[0:64, 0:128], lhsT=ktb.t[:, n, :], rhs=rv.t[:, n, :], start=True, stop=True), r=[ktb, rv], w=[pk])
                V(lambda e, pk=pk, cur=cur, nxt=nxt: e.scalar_tensor_tensor(out=nxt.t[:], in0=cur.t[:], scalar=tfb.t[0:64, 3:4], in1=pk.t[0:64, 0:128],
                                                                          op0=ALU.mult, op1=ALU.add), r=[cur, pk, tfb], w=[nxt])
                A(lambda e, nxt=nxt, n=n: e.copy(out=Nbf.t[:, n - 1, :], in_=nxt.t[:]), r=[nxt], w=[Nbf])
            V(lambda e: e.memset(Prun[0].t[:], 0.0), w=[Prun[0]])
            V(lambda e: e.memset(Pbf[0].t[:], 0.0), w=[Pbf[0]])
            for n in range(NT):
                cs = slice(n * 128, (n + 1) * 128)
                ps_ = psS[n % 2]
                sm_ = SM[n % 2]
                po = psO[(n // 4) % 2]
                j4 = n % 4
                qf_, qb_ = qf[n % 2], qb[n % 2]
                pb_cur, pb_nxt = Pbf[n % 2], Pbf[(n + 1) % 2]
                pr_cur, pr_nxt = Prun[n % 2], Prun[(n + 1) % 2]
                T(lambda e, ps_=ps_, cs=cs: e.matmul(ps_.t[:, 0:128], lhsT=rkT.t[:, cs], rhs=rqT.t[:, cs], start=True, stop=True), r=[rkT, rqT], w=[ps_])
                V(lambda e, ps_=ps_, sm_=sm_: e.tensor_tensor(out=sm_.t[:], in0=ps_.t[:, 0:128], in1=DT.t[:], op=ALU.mult), r=[ps_, DT], w=[sm_])
                PL(lambda e, qf_=qf_, cs=cs: e.tensor_tensor(out=qf_.t[:], in0=rqT.t[:, cs], in1=QF.t[0:64, :], op=ALU.mult), r=[rqT, QF], w=[qf_])
                PL(lambda e, qb_=qb_, cs=cs: e.tensor_tensor(out=qb_.t[:], in0=rqT.t[:, cs], in1=QB.t[0:64, :], op=ALU.mult), r=[rqT, QB], w=[qb_])
                T(lambda e, po=po, j4=j4, sm_=sm_, n=n: e.matmul(po.t[:, j4 * 128:(j4 + 1) * 128], lhsT=sm_.t[:], rhs=rv.t[:, n, :], start=True, stop=False), r=[sm_, rv], w=[po])
                T(lambda e, po=po, j4=j4, qf_=qf_, pb_cur=pb_cur: e.matmul(po.t[:, j4 * 128:(j4 + 1) * 128], lhsT=qf_.t[:], rhs=pb_cur.t[:], start=False, stop=False),
                  r=[qf_, pb_cur], w=[po])
                T(lambda e, po=po, j4=j4, qb_=qb_, n=n: e.matmul(po.t[:, j4 * 128:(j4 + 1) * 128], lhsT=qb_.t[:], rhs=Nbf.t[:, n, :], start=False, stop=True),
                  r=[qb_, Nbf], w=[po])
                if n < NT - 1:
                    pk = psK[n % 2]
                    T(lambda e, pk=pk, n=n: e.matmul(pk.t[0:64, 0:128], lhsT=ktf.t[:, n, :], rhs=rv.t[:, n, :], start=True, stop=True), r=[ktf, rv], w=[pk])
                    V(lambda e, pk=pk, pr_cur=pr_cur, pr_nxt=pr_nxt: e.scalar_tensor_tensor(out=pr_nxt.t[:], in0=pr_cur.t[:], scalar=tfb.t[0:64, 2:3],
                                                                                          in1=pk.t[0:64, 0:128], op0=ALU.mult, op1=ALU.add),
                      r=[pr_cur, pk, tfb], w=[pr_nxt])
                    A(lambda e, pr_nxt=pr_nxt, pb_nxt=pb_nxt: e.copy(out=pb_nxt.t[:], in_=pr_nxt.t[:]), r=[pr_nxt], w=[pb_nxt])
                if j4 == 3:
                    s4 = st4[(n // 4) % 2]
                    V(lambda e, po=po, s4=s4: e.tensor_reduce(out=s4.t[:, 0:4], in_=po.t[:].rearrange("p (a b) -> p a b", a=4), axis=AX.X, op=ALU.add), r=[po], w=[s4])
                    A(lambda e, po=po: e.activation(out=osq.t[:].rearrange("p a b -> p (a b)"), in_=po.t[:], func=AF.Square), r=[po], w=[osq])
                    V(lambda e, s4=s4: e.tensor_reduce(out=s4.t[:, 4:8], in_=osq.t[:], axis=AX.X, op=ALU.add), r=[osq], w=[s4])
                    V(lambda e, s4=s4: e.tensor_scalar(out=s4.t[:, 8:12], in0=s4.t[:, 0:4], scalar1=1.0 / 128, scalar2=None, op0=ALU.mult), r=[s4], w=[s4])
                    V(lambda e, s4=s4: e.tensor_tensor(out=s4.t[:, 0:4], in0=s4.t[:, 8:12], in1=s4.t[:, 8:12], op=ALU.mult), r=[s4], w=[s4])
                    V(lambda e, s4=s4: e.scalar_tensor_tensor(out=s4.t[:, 12:16], in0=s4.t[:, 4:8], scalar=1.0 / 128, in1=s4.t[:, 0:4],
                                                              op0=ALU.mult, op1=ALU.subtract), r=[s4], w=[s4])
                    A(lambda e, s4=s4: e.activation(out=s4.t[:, 12:16], in_=s4.t[:, 12:16], func=AF.Sqrt, bias=cst.t[:, 2:3]), r=[s4, cst], w=[s4])
                    V(lambda e, s4=s4: e.reciprocal(out=s4.t[:, 12:16], in_=s4.t[:, 12:16]), r=[s4], w=[s4])
                    for jj in range(4):
                        m = n - 3 + jj
                        y_ = yr[m % 2]
                        py = psY[m % 2]
                        V(lambda e, po=po, jj=jj, s4=s4, y_=y_: e.tensor_scalar(out=y_.t[:], in0=po.t[:, jj * 128:(jj + 1) * 128], scalar1=s4.t[:, 8 + jj:9 + jj],
                                                                              scalar2=s4.t[:, 12 + jj:13 + jj], op0=ALU.subtract, op1=ALU.mult),
                          r=[po, s4], w=[y_])
                        PL(lambda e, y_=y_, m=m: e.tensor_tensor(out=y_.t[:], in0=y_.t[:], in1=sg.t[:, m, :], op=ALU.mult), r=[y_, sg], w=[y_])
                        T(lambda e, py=py, y_=y_: e.transpose(out=py.t[:, 0:128], in_=y_.t[:], identity=identb.t[:]), r=[y_, identb], w=[py])
                        A(lambda e, py=py, m=m: e.copy(out=yT.t[:, m * 128:(m + 1) * 128], in_=py.t[:, 0:128]), r=[py], cw=[yT])
            p.barrier()
            p.emit()
    def tap(dst, src_ap, reads):
        p.dma("gpsimd", lambda e: e.dma_start(out=dst, in_=src_ap), "tap", reads=reads)
        p.barrier()

    if stop_after <= 2:
        if debug:
            tap(dbg["yT"][0:128, :], yT.t[:], [yT])
        p.finish()
        return nc, p, dbg
    PAD = 1024
    with ExitStack() as ph:
        aqT = mk(ph, "aqT", [128, S], BF16)
        akT = mk(ph, "akT", [128, S + 2 * PAD], BF16)
        avT = mk(ph, "avT", [128, S + 2 * PAD], BF16)
        mkb = mk(ph, "mkb", [128, 18 * 256], BF16)
        acc = [mk(ph, f"acc{h}", [65, S], F32) for h in range(2)]
        Vaug = [mk(ph, f"Vaug{i}", [128, 2, 65], BF16) for i in range(3)]
        Et = [mk(ph, f"Et{i}", [128, 256], BF16) for i in range(2)]
        PTt = [mk(ph, f"PTt{i}", [128, 256], BF16) for i in range(2)]
        rd = mk(ph, "rd", [65, 512], F32)
        psA = [mk(ph, f"psA{i}", None, F32, psum=True) for i in range(2)]
        psV = [mk(ph, f"psV{i}", None, BF16, psum=True) for i in range(2)]
        psB = [mk(ph, f"psB{i}", None, F32, psum=True) for i in range(2)]
        psR = mk(ph, "psR", None, F32, psum=True)
        ld("sync", aqT, aqT.t[:], aq_d, reads=[R_aq])
        ld("sync", akT, akT.t[:, PAD:PAD + S], ak_d, reads=[R_ak])
        ld("sync", avT, avT.t[:, PAD:PAD + S], av_d, reads=[R_av])
        ld("gpsimd", mkb, mkb.t[:], masksin)
        for tns in (akT, avT):
            V(lambda e, tns=tns: e.memset(tns.t[:, 0:PAD], 0.0), w=[tns])
            V(lambda e, tns=tns: e.memset(tns.t[:, PAD + S:PAD + S + PAD], 0.0), w=[tns])
        for vb in Vaug:
            V(lambda e, vb=vb: e.memset(vb.t[:, :, 64:65], 1.0), w=[vb])
        bi = 0
        vi = 0
        for c, d in enumerate(CONFIGS):
            nb = (S // d) // 128
            for r in range(d):
                def ksl(u, d=d, r=r):
                    st0 = PAD + d * (128 * u - 64) + r
                    return slice(st0, st0 + 127 * d + 1, d)
                vt = {}
                for m in range(nb):
                    for u in (m, m + 1):
                        if u in vt:
                            continue
                        vb = Vaug[vi % 3]
                        pv = psV[vi % 2]
                        vi += 1
                        T(lambda e, pv=pv, u=u, ksl=ksl: e.transpose(out=pv.t[:, 0:128], in_=avT.t[:, ksl(u)], identity=identb.t[:]),
                          r=[avT, identb], w=[pv])
                        if vi % 2 == 0:
                            A(lambda e, pv=pv, vb=vb: e.copy(out=vb.t[:, :, 0:64], in_=pv.t[:, 0:128].rearrange("p (h x) -> p h x", h=2)),
                              r=[pv], w=[vb])
                        else:
                            V(lambda e, pv=pv, vb=vb: e.tensor_copy(out=vb.t[:, :, 0:64], in_=pv.t[:, 0:128].rearrange("p (h x) -> p h x", h=2)),
                              r=[pv], w=[vb])
                        vt[u] = vb
                    q0 = d * 128 * m + r
                    qs = slice(q0, q0 + 127 * d + 1, d)
                    var = 1 if m == 0 else (2 if m == nb - 1 else 0)
                    for h in range(2):
                        rows = slice(64 * h, 64 * h + 64)
                        pa = psA[bi % 2]
                        e_ = Et[bi % 2]
                        pt_ = PTt[bi % 2]
                        po = psB[bi % 2]
                        bi += 1
                        for j in range(2):
                            T(lambda e, pa=pa, j=j, rows=rows, m=m, qs=qs, ksl=ksl: e.matmul(pa.t[:, j * 128:(j + 1) * 128], lhsT=akT.t[rows, ksl(m + j)],
                                                                                          rhs=aqT.t[rows, qs], start=True, stop=True),
                              r=[akT, aqT], w=[pa])
                        A(lambda e, pa=pa, e_=e_: e.activation(out=e_.t[:], in_=pa.t[:, 0:256], func=AF.Exp, scale=0.125), r=[pa], w=[e_])
                        mo = ((h * 3 + c) * 3 + var) * 256
                        V(lambda e, e_=e_, pt_=pt_, mo=mo: e.tensor_tensor(out=pt_.t[:], in0=e_.t[:], in1=mkb.t[:, mo:mo + 256], op=ALU.mult),
                          r=[e_, mkb], w=[pt_])
                        for j in range(2):
                            vb = vt[m + j]
                            T(lambda e, po=po, j=j, vb=vb, pt_=pt_, h=h: e.matmul(po.t[0:65, 0:128], lhsT=vb.t[:, h, :], rhs=pt_.t[:, j * 128:(j + 1) * 128],
                                                                               start=(j == 0), stop=(j == 1)), r=[vb, pt_], w=[po])
                        ac = acc[h]
                        if c == 0:
                            A(lambda e, ac=ac, qs=qs, po=po: e.copy(out=ac.t[:, qs], in_=po.t[0:65, 0:128]), r=[po], w=[ac])
                        else:
                            V(lambda e, ac=ac, qs=qs, po=po: e.tensor_tensor(out=ac.t[:, qs], in0=ac.t[:, qs], in1=po.t[0:65, 0:128], op=ALU.add),
                              r=[po, ac], w=[ac])
        for h in range(2):
            yh = (yA, yB)[h]
            ac = acc[h]
            for t in range(16):
                sl = slice(512 * t, 512 * (t + 1))
                V(lambda e, ac=ac, sl=sl: e.reciprocal(out=rd.t[64:65, :], in_=ac.t[64:65, sl]), r=[ac], w=[rd])
                T(lambda e: e.matmul(psR.t[0:64, 0:512], lhsT=onesf.t[64:65, 0:64], rhs=rd.t[64:65, :], start=True, stop=True), r=[onesf, rd], w=[psR])
                V(lambda e, yh=yh, ac=ac, sl=sl: e.tensor_tensor(out=yh.t[:, sl], in0=ac.t[0:64, sl], in1=psR.t[0:64, 0:512], op=ALU.mult),
                  r=[ac, psR], w=[yh])
        p.barrier()
    if stop_after <= 3:
        if debug:
            tap(dbg["yT"][0:128, :], yT.t[:], [yT])
            tap(dbg["yT"][128:192, :], yA.t[:], [yA])
            tap(dbg["yT"][192:256, :], yB.t[:], [yB])
        p.finish()
        return nc, p, dbg

    with ExitStack() as ph:
        wo = mk(ph, "wo", [128, D], BF16)
        woA = mk(ph, "woA", [64, D], BF16)
        woB = mk(ph, "woB", [64, D], BF16)
        pst = [mk(ph, f"pst{i}", [128, D], F32) for i in range(2)]
        psP = [mk(ph, f"psP{i}", None, F32, psum=True) for i in range(2)]
        ld("gpsimd", wo, wo.t[:], wout[0:128, :])
        ld("gpsimd", woA, woA.t[:], wout[128:192, :])
        ld("gpsimd", woB, woB.t[:], wout[192:256, :])
        for n in range(NT):
            ts = slice(128 * n, 128 * (n + 1))
            st_ = pst[n % 2]
            for half in range(2):
                pp = psP[half]
                cs = slice(512 * half, 512 * (half + 1))
                T(lambda e, pp=pp, ts=ts, cs=cs: e.matmul(pp.t[:, 0:512], lhsT=yT.t[:, ts], rhs=wo.t[:, cs], start=True, stop=False), r=[yT, wo], w=[pp])
                T(lambda e, pp=pp, ts=ts, cs=cs: e.matmul(pp.t[:, 0:512], lhsT=yA.t[:, ts], rhs=woA.t[:, cs], start=False, stop=False), r=[yA, woA], w=[pp])
                T(lambda e, pp=pp, ts=ts, cs=cs: e.matmul(pp.t[:, 0:512], lhsT=yB.t[:, ts], rhs=woB.t[:, cs], start=False, stop=True), r=[yB, woB], w=[pp])
                if half == 0:
                    A(lambda e, pp=pp, st_=st_, cs=cs: e.copy(out=st_.t[:, cs], in_=pp.t[:, 0:512]), r=[pp], w=[st_])
                else:
                    V(lambda e, pp=pp, st_=st_, cs=cs: e.tensor_copy(out=st_.t[:, cs], in_=pp.t[:, 0:512]), r=[pp], w=[st_])
            p.dma("sync", lambda e, ts=ts, st_=st_: e.dma_start(out=part_d[ts, :], in_=st_.t[:]), "part_d", reads=[st_], writes=[R_part])
        p.dma("gpsimd", lambda e: e.collective_compute("ReduceScatter", ALU.add, replica_groups=RG, ins=[part_d], outs=[mixed_d]),
              "rs1", reads=[R_part], writes=[R_mixed], inc=1)
        p.barrier()
    ymix.close()
    if stop_after <= 4:
        p.finish()
        return nc, p, dbg
    x1_d = dsc("x1_d", [OWN, D])
    R_x1 = Reg("x1_d")
    with ExitStack() as ph58:
        rows = {k: mk(ph58, f"row_{k}", [128, D], F32) for k in ("gm", "gsf", "shf", "gf")}
        toki = mk(ph58, "toki", [128, 32], I32)
        with ExitStack() as ph:
            diag = [mk(ph, f"diag{i}", [128, 128], F32) for i in range(2)]
            psD = [mk(ph, f"psD{i}", None, F32, psum=True) for i in range(2)]
            srcs = {"gm": der.t[:, 8:16], "gsf": der.t[:, 16:24], "shf": mod.t[:, 24:32], "gf": der.t[:, 24:32]}
            di = 0
            for key in ("gm", "gsf", "shf", "gf"):
                for half in range(2):
                    pd = psD[half]
                    for kk in range(4):
                        k = half * 4 + kk
                        dg = diag[di % 2]
                        di += 1
                        V(lambda e, dg=dg, key=key, k=k: e.tensor_scalar(out=dg.t[:], in0=identf.t[:], scalar1=srcs[key][:, k:k + 1], scalar2=None,
                                                                       op0=ALU.mult), r=[identf, der, mod], w=[dg])
                        T(lambda e, pd=pd, kk=kk, dg=dg: e.matmul(pd.t[:, kk * 128:(kk + 1) * 128], lhsT=onesf.t[:], rhs=dg.t[:], start=True, stop=True),
                          r=[onesf, dg], w=[pd])
                    A(lambda e, pd=pd, key=key, half=half: e.copy(out=rows[key].t[:, half * 512:(half + 1) * 512], in_=pd.t[:, 0:512]),
                      r=[pd], w=[rows[key]])
            wrs = mk(ph, "wrs", [128, 8, 16], F32)
            ld("sync", wrs, wrs.t[:], wrin.rearrange("(k p) e -> p k e", p=128))
            mx = [mk(ph, f"mx{i}", [128, D], F32) for i in range(2)]
            xo_t = [mk(ph, f"xo{i}", [128, D], F32) for i in range(2)]
            x1t = [mk(ph, f"x1t{i}", [128, D], F32) for i in range(2)]
            h2t = [mk(ph, f"h2t{i}", [128, D], F32) for i in range(2)]
            h2T = [mk(ph, f"h2T{i}", [128, 8, 128], F32) for i in range(2)]
            sq2 = mk(ph, "sq2", [128, D], BF16)
            ss5 = [mk(ph, f"ss5{i}", [128, 8], F32) for i in range(2)]
            afft = [mk(ph, f"afft{i}", [128, 16], F32) for i in range(2)]
            ext = [mk(ph, f"ext{i}", [128, 16], F32) for i in range(2)]
            psH = [mk(ph, f"psH{i}", None, F32, psum=True) for i in range(2)]
            psL = [mk(ph, f"psL{i}", None, F32, psum=True) for i in range(2)]
            lgall = mk(ph, "lgall", [128, 16, 16], F32)
            smx = mk(ph, "smx", [128, 32], F32)
            for n in range(16):
                ts = slice(128 * n, 128 * (n + 1))
                m_, xo_, x1_, h2_, hT_, s5, af_, ex_ = mx[n % 2], xo_t[n % 2], x1t[n % 2], h2t[n % 2], h2T[n % 2], ss5[n % 2], afft[n % 2], ext[n % 2]
                ld("sync", m_, m_.t[:], mixed_d[ts, :], reads=[R_mixed])
                ld("sync", xo_, xo_.t[:], xo[ts, :])
                A(lambda e, m_=m_, s5=s5: e.activation(out=sq2.t[:], in_=m_.t[:], func=AF.Square, accum_out=s5.t[:, 0:1]), r=[m_], w=[sq2, s5])
                A(lambda e, s5=s5: e.activation(out=s5.t[:, 1:2], in_=s5.t[:, 0:1], func=AF.Sqrt, scale=1.0 / D, bias=cst.t[:, 1:2]), r=[s5, cst], w=[s5])
                V(lambda e, s5=s5: e.reciprocal(out=s5.t[:, 1:2], in_=s5.t[:, 1:2]), r=[s5], w=[s5])
                V(lambda e, m_=m_, s5=s5: e.scalar_tensor_tensor(out=m_.t[:], in0=m_.t[:], scalar=s5.t[:, 1:2], in1=rows["gm"].t[:],
                                                               op0=ALU.mult, op1=ALU.mult), r=[m_, s5, rows["gm"]], w=[m_])
                PL(lambda e, m_=m_, xo_=xo_, x1_=x1_: e.tensor_tensor(out=x1_.t[:], in0=xo_.t[:], in1=m_.t[:], op=ALU.add), r=[m_, xo_], w=[x1_])
                p.dma("sync", lambda e, ts=ts, x1_=x1_: e.dma_start(out=x1_d[ts, :], in_=x1_.t[:]), "x1_d", reads=[x1_], writes=[R_x1])
                A(lambda e, x1_=x1_, s5=s5: e.activation(out=sq2.t[:], in_=x1_.t[:], func=AF.Square, accum_out=s5.t[:, 2:3]), r=[x1_], w=[sq2, s5])
                A(lambda e, s5=s5: e.activation(out=s5.t[:, 3:4], in_=s5.t[:, 2:3], func=AF.Sqrt, scale=1.0 / D, bias=cst.t[:, 1:2]), r=[s5, cst], w=[s5])
                V(lambda e, s5=s5: e.reciprocal(out=s5.t[:, 3:4], in_=s5.t[:, 3:4]), r=[s5], w=[s5])
                V(lambda e, x1_=x1_, s5=s5, h2_=h2_: e.scalar_tensor_tensor(out=h2_.t[:], in0=x1_.t[:], scalar=s5.t[:, 3:4], in1=rows["gsf"].t[:],
                                                                          op0=ALU.mult, op1=ALU.mult), r=[x1_, s5, rows["gsf"]], w=[h2_])
                PL(lambda e, h2_=h2_: e.tensor_tensor(out=h2_.t[:], in0=h2_.t[:], in1=rows["shf"].t[:], op=ALU.add), r=[h2_, rows["shf"]], w=[h2_])
                p.dma("gpsimd", lambda e, ts=ts, h2_=h2_: e.dma_start(out=h2_in[ts, :], in_=h2_.t[:]), "h2_in", reads=[h2_], writes=[R_h2in])
                if 'norouter' in FLAGS:
                    continue
                for k in range(8):
                    ph_ = psH[k // 4]
                    T(lambda e, ph_=ph_, k=k, h2_=h2_: e.transpose(out=ph_.t[:, (k % 4) * 128:(k % 4 + 1) * 128], in_=h2_.t[:, k * 128:(k + 1) * 128],
                                                                 identity=identf.t[:]), r=[h2_, identf], w=[ph_])
                A(lambda e, hT_=hT_: e.copy(out=hT_.t[:, 0:4, :], in_=psH[0].t[:, 0:512].rearrange("p (k s) -> p k s", k=4)), r=[psH[0]], w=[hT_])
                V(lambda e, hT_=hT_: e.tensor_copy(out=hT_.t[:, 4:8, :], in_=psH[1].t[:, 0:512].rearrange("p (k s) -> p k s", k=4)), r=[psH[1]], w=[hT_])
                pl = psL[n % 2]
                for k in range(8):
                    T(lambda e, pl=pl, k=k, hT_=hT_: e.matmul(pl.t[:, 0:16], lhsT=hT_.t[:, k, :], rhs=wrs.t[:, k, :], start=(k == 0), stop=(k == 7)),
                      r=[hT_, wrs], w=[pl])
                V(lambda e, pl=pl, n=n: e.tensor_copy(out=lgall.t[:, n, :], in_=pl.t[:, 0:16]), r=[pl], cw=[lgall])
            V(lambda e: e.tensor_reduce(out=smx.t[:, 0:16], in_=lgall.t[:], axis=AX.X, op=ALU.max), r=[lgall], w=[smx])
            for n in range(16):
                V(lambda e, n=n: e.tensor_scalar(out=lgall.t[:, n, :], in0=lgall.t[:, n, :], scalar1=smx.t[:, n:n + 1], scalar2=None, op0=ALU.subtract),
                  r=[lgall, smx], w=[lgall])
            A(lambda e: e.activation(out=lgall.t[:].rearrange("p a b -> p (a b)"), in_=lgall.t[:].rearrange("p a b -> p (a b)"), func=AF.Exp),
              r=[lgall], w=[lgall])
            V(lambda e: e.tensor_reduce(out=smx.t[:, 16:32], in_=lgall.t[:], axis=AX.X, op=ALU.add), r=[lgall], w=[smx])
            V(lambda e: e.reciprocal(out=smx.t[:, 16:32], in_=smx.t[:, 16:32]), r=[smx], w=[smx])
            for n in range(16):
                V(lambda e, n=n: e.tensor_scalar(out=lgall.t[:, n, :], in0=lgall.t[:, n, :], scalar1=smx.t[:, 16 + n:17 + n], scalar2=None, op0=ALU.mult),
                  r=[lgall, smx], w=[lgall])
            p.dma("sync", lambda e: e.dma_start(out=aff_in.rearrange("(n p) e -> p n e", p=128), in_=lgall.t[:]), "aff_in", reads=[lgall], writes=[R_affin])
            for c4 in range(4):
                p.dma("gpsimd", lambda e, c4=c4: e.collective_compute("AllGather", ALU.bypass, replica_groups=RG,
                                                                      ins=[h2_in[512 * c4:512 * (c4 + 1), :]], outs=[h2_all[c4]]),
                      f"ag1_{c4}", reads=[R_h2in], writes=[R_h2all], inc=1)
            p.dma("gpsimd", lambda e: e.collective_compute("AllGather", ALU.bypass, replica_groups=RG, ins=[aff_in], outs=[aff_all]),
                  "ag2", reads=[R_affin], writes=[R_affall], inc=1)
            for c4 in range(4):
                for r4 in range(4):
                    p.dma("sync", lambda e, c4=c4, r4=r4: e.dma_start(out=h2_tab[2048 * r4 + 512 * c4:2048 * r4 + 512 * (c4 + 1), :],
                                                                      in_=h2_all[c4][512 * r4:512 * (r4 + 1), :]),
                          "h2tab", reads=[R_h2all], writes=[R_h2tab])
            p.dma("sync", lambda e: e.dma_start(out=aff_tab, in_=aff_all), "afftab", reads=[R_affall], writes=[R_afftab])
            p.barrier()
        if stop_after <= 5:
            if debug:
                tap(dbg["x1"], x1_d, [R_x1])
                tap(dbg["aff"], aff_in, [R_affin])
            p.finish()
            return nc, p, dbg

        with ExitStack() as ph:
            Aall = mk(ph, "Aall", [128, 64, 16], F32)
            A4 = mk(ph, "A4", [128, 4, 64], F32)
            cmp_ = mk(ph, "cmp", [128, 4, 64], F32)
            cc0 = mk(ph, "cc0", [128, 4, 64], F32)
            cc1 = mk(ph, "cc1", [128, 4, 64], F32)
            lo = mk(ph, "lo", [128, 4], F32)
            mid = mk(ph, "mid", [128, 4], F32)
            cnt = mk(ph, "cnt", [128, 4], F32)
            ge = mk(ph, "ge", [128, 4], F32)
            offs = mk(ph, "offs", [128, 4], F32)
            tris = mk(ph, "tris", [128, 128], F32)
            slot = mk(ph, "slot", [128, 8], F32)
            tokf = mk(ph, "tokf", [128, 32], F32)
            cb = [mk(ph, f"cb{i}", [128, S], F32) for i in range(2)]
            junk = mk(ph, "junk", [128, S], BF16)
            psC = mk(ph, "psC", None, F32, psum=True)
            ld("sync", Aall, Aall.t[:], aff_tab.rearrange("(p j) e -> p j e", j=64), reads=[R_afftab])
            ld("sync", tris, tris.t[:], triin)
            ld("sync", slot, slot.t[:], slotin)
            for i in range(4):
                V(lambda e, i=i: e.tensor_scalar(out=A4.t[:, i, :], in0=Aall.t[:, :, i], scalar1=ohs.t[:, 0:1], scalar2=None, op0=ALU.mult),
                  r=[Aall, ohs], w=[A4])
                for r in range(1, 4):
                    V(lambda e, i=i, r=r: e.scalar_tensor_tensor(out=A4.t[:, i, :], in0=Aall.t[:, :, 4 * r + i], scalar=ohs.t[:, r:r + 1], in1=A4.t[:, i, :],
                                                                 op0=ALU.mult, op1=ALU.add), r=[Aall, ohs, A4], w=[A4])
            V(lambda e: e.memset(lo.t[:], 0.0), w=[lo])
            for it in range(26):
                wv = 2.0 ** (-(it + 1))
                V(lambda e, wv=wv: e.tensor_scalar(out=mid.t[:], in0=lo.t[:], scalar1=wv, scalar2=None, op0=ALU.add), r=[lo], w=[mid])
                V(lambda e: e.memset(cnt.t[:], 0.0), w=[cnt])
                for i in range(4):
                    V(lambda e, i=i: e.tensor_scalar(out=cmp_.t[:, i, :], in0=A4.t[:, i, :], scalar1=mid.t[:, i:i + 1], scalar2=0.0, op0=ALU.is_gt,
                                                    op1=ALU.add, accum_out=cnt.t[:, i:i + 1]), r=[A4, mid, cnt], w=[cmp_, cnt])
                T(lambda e: e.matmul(psC.t[:, 0:4], lhsT=onesf.t[:], rhs=cnt.t[:], start=True, stop=True), r=[onesf, cnt], w=[psC])
                V(lambda e: e.tensor_scalar(out=ge.t[:], in0=psC.t[:, 0:4], scalar1=CAP - 0.5, scalar2=None, op0=ALU.is_ge), r=[psC], w=[ge])
                V(lambda e, wv=wv: e.scalar_tensor_tensor(out=lo.t[:], in0=ge.t[:], scalar=wv, in1=lo.t[:], op0=ALU.mult, op1=ALU.add), r=[ge, lo], w=[lo])
            for i in range(4):
                V(lambda e, i=i: e.tensor_scalar(out=cc0.t[:, i, :], in0=A4.t[:, i, :], scalar1=lo.t[:, i:i + 1], scalar2=None, op0=ALU.is_gt),
                  r=[A4, lo], w=[cc0])
            ca, cbuf = cc0, cc1
            for sh in (1, 2, 4, 8, 16, 32):
                V(lambda e, ca=ca, cbuf=cbuf, sh=sh: e.tensor_tensor(out=cbuf.t[:, :, sh:64], in0=ca.t[:, :, sh:64], in1=ca.t[:, :, 0:64 - sh], op=ALU.add),
                  r=[ca], w=[cbuf])
                V(lambda e, ca=ca, cbuf=cbuf, sh=sh: e.tensor_copy(out=cbuf.t[:, :, 0:sh], in_=ca.t[:, :, 0:sh]), r=[ca], w=[cbuf])
                ca, cbuf = cbuf, ca
            V(lambda e, ca=ca: e.tensor_copy(out=cnt.t[:], in_=ca.t[:, :, 63]), r=[ca], w=[cnt])
            T(lambda e: e.matmul(psC.t[:, 0:4], lhsT=tris.t[:], rhs=cnt.t[:], start=True, stop=True), r=[tris, cnt], w=[psC])
            V(lambda e: e.tensor_copy(out=offs.t[:], in_=psC.t[:, 0:4]), r=[psC], w=[offs])
            cdr_v = cdr.rearrange("e (p j) -> e p j", j=64)
            for i in range(4):
                V(lambda e, i=i, ca=ca: e.tensor_scalar(out=ca.t[:, i, :], in0=ca.t[:, i, :], scalar1=offs.t[:, i:i + 1], scalar2=None, op0=ALU.add),
                  r=[ca, offs], w=[ca])
                p.dma("sync", lambda e, i=i, ca=ca: e.dma_start(out=cdr_v[i], in_=ca.t[:, i, :]), "cdr", reads=[ca], writes=[R_cdr])
            V(lambda e: e.memset(tokf.t[:], 0.0), w=[tokf])
            for i in range(4):
                cb_ = cb[i % 2]
                ld("sync", cb_, cb_.t[:], cdr[i:i + 1, :].partition_broadcast(128), reads=[R_cdr])
                for st in range(8):
                    col = i * 8 + st
                    V(lambda e, cb_=cb_, st=st, col=col: e.tensor_scalar(out=junk.t[:], in0=cb_.t[:], scalar1=slot.t[:, st:st + 1], scalar2=0.0,
                                                                       op0=ALU.is_le, op1=ALU.add, accum_out=tokf.t[:, col:col + 1]),
                      r=[cb_, slot, tokf], w=[junk, tokf])
            V(lambda e: e.tensor_copy(out=toki.t[:], in_=tokf.t[:]), r=[tokf], w=[toki])
            if debug and stop_after == 6:
                tap(dbg["tok"], tokf.t[:], [tokf])
            p.barrier()
        if stop_after <= 6:
            if debug:
                tap(dbg["x1"], x1_d, [R_x1])
                tap(dbg["aff"], aff_in, [R_affin])
            p.finish()
            return nc, p, dbg

        with ExitStack() as ph:
            xeT = [mk(ph, f"xeT{i}", [128, 8, CAP], BF16) for i in range(2)]
            hT = mk(ph, "hT", [128, 16, CAP], BF16)
            wdb = [mk(ph, f"wdb{i}", [128, 16, D], BF16) for i in range(2)]
            NPC = 8
            wgb = [mk(ph, f"wgb{i}", [128, 8, 256], BF16) for i in range(3)]
            wub = [mk(ph, f"wub{i}", [128, 8, 256], BF16) for i in range(3)]
            xet = [mk(ph, f"xet{i}", [128, D], BF16) for i in range(2)]
            gat = [mk(ph, f"gat{i}", [128, 16], F32) for i in range(2)]
            gate = [mk(ph, f"gate{i}", [128, 8], F32) for i in range(2)]
            yet = [mk(ph, f"yet{i}", [128, D], F32) for i in range(2)]
            sgt = [mk(ph, f"sgt{i}", [128, 512], BF16) for i in range(2)]
            psXT = mk(ph, "psXT", None, BF16, psum=True)
            psG = [mk(ph, f"psG{i}", None, F32, psum=True) for i in range(2)]
            psU = [mk(ph, f"psU{i}", None, F32, psum=True) for i in range(2)]
            psY = [mk(ph, f"psYd{i}", None, F32, psum=True) for i in range(2)]
            cnts = {"g": 0, "pc": 0, "m": 0, "y": 0}

            def load_piece(i, pc):
                ws = (i * NPC + pc) % 3
                wg_v = wg[i].rearrange("(k p) f -> p k f", p=128)
                wu_v = wu[i].rearrange("(k p) f -> p k f", p=128)
                ld("gpsimd", wgb[ws], wgb[ws].t[:], wg_v[:, :, pc * 256:(pc + 1) * 256])
                ld("gpsimd", wub[ws], wub[ws].t[:], wu_v[:, :, pc * 256:(pc + 1) * 256])

            def gather_expert(i):
                xT = xeT[i % 2]
                gt_ = gate[i % 2]
                for st in range(8):
                    col = i * 8 + st
                    xe_ = xet[cnts["g"] % 2]
                    ga_ = gat[cnts["g"] % 2]
                    cnts["g"] += 1
                    p.dma("gpsimd", lambda e, xe_=xe_, col=col: e.indirect_dma_start(
                        out=xe_.t[:], out_offset=None, in_=h2_tab, in_offset=bass.IndirectOffsetOnAxis(ap=toki.t[:, col:col + 1], axis=0)),
                        xe_.r.name, reads=[R_h2tab, toki], writes=[xe_])
                    p.dma("gpsimd", lambda e, ga_=ga_, col=col: e.indirect_dma_start(
                        out=ga_.t[:], out_offset=None, in_=aff_tab, in_offset=bass.IndirectOffsetOnAxis(ap=toki.t[:, col:col + 1], axis=0)),
                        ga_.r.name, reads=[R_afftab, toki], writes=[ga_])
                    V(lambda e, ga_=ga_, gt_=gt_, st=st, i=i: e.tensor_scalar(out=gt_.t[:, st:st + 1], in0=ga_.t[:, i:i + 1], scalar1=ohs.t[:, 0:1], scalar2=None,
                                                                            op0=ALU.mult), r=[ga_, ohs], w=[gt_])
                    for r in range(1, 4):
                        V(lambda e, ga_=ga_, gt_=gt_, st=st, i=i, r=r: e.scalar_tensor_tensor(
                            out=gt_.t[:, st:st + 1], in0=ga_.t[:, 4 * r + i:4 * r + i + 1], scalar=ohs.t[:, r:r + 1], in1=gt_.t[:, st:st + 1],
                            op0=ALU.mult, op1=ALU.add), r=[ga_, ohs, gt_], w=[gt_])
                    for k in range(8):
                        T(lambda e, k=k, xe_=xe_: e.transpose(out=psXT.t[:, k * 128:(k + 1) * 128], in_=xe_.t[:, k * 128:(k + 1) * 128], identity=identb.t[:]),
                          r=[xe_, identb], w=[psXT])
                    if st % 2 == 0:
                        A(lambda e, st=st, xT=xT: e.copy(out=xT.t[:, :, st * 128:(st + 1) * 128], in_=psXT.t[:].rearrange("p (k s) -> p k s", k=8)),
                          r=[psXT], cw=[xT])
                    else:
                        V(lambda e, st=st, xT=xT: e.tensor_copy(out=xT.t[:, :, st * 128:(st + 1) * 128], in_=psXT.t[:].rearrange("p (k s) -> p k s", k=8)),
                          r=[psXT], cw=[xT])

            ld("gpsimd", wdb[0], wdb[0].t[:], wd[0].rearrange("(k p) d -> p k d", p=128))
            gather_expert(0)
            load_piece(0, 0)
            load_piece(0, 1)
            for i in range(4):
                xT = xeT[i % 2]
                gt_ = gate[i % 2]
                wd_ = wdb[i % 2]
                for pc in range(NPC):
                    if pc + 2 < NPC:
                        load_piece(i, pc + 2)
                    ws = (i * NPC + pc) % 3
                    wg_, wu_ = wgb[ws], wub[ws]
                    for fc in range(2):
                        f = pc * 2 + fc
                        for half in range(2):
                            pg, pu, sg_ = psG[cnts["m"] % 2], psU[cnts["m"] % 2], sgt[cnts["m"] % 2]
                            cnts["m"] += 1
                            hs = slice(512 * half, 512 * (half + 1))
                            for k in range(8):
                                T(lambda e, pg=pg, k=k, wg_=wg_, fc=fc, hs=hs, xT=xT: e.matmul(pg.t[:, 0:512], lhsT=wg_.t[:, k, fc * 128:(fc + 1) * 128], rhs=xT.t[:, k, hs],
                                                                                           start=(k == 0), stop=(k == 7)), r=[wg_, xT], w=[pg])
                            for k in range(8):
                                T(lambda e, pu=pu, k=k, wu_=wu_, fc=fc, hs=hs, xT=xT: e.matmul(pu.t[:, 0:512], lhsT=wu_.t[:, k, fc * 128:(fc + 1) * 128], rhs=xT.t[:, k, hs],
                                                                                           start=(k == 0), stop=(k == 7)), r=[wu_, xT], w=[pu])
                            A(lambda e, pg=pg, sg_=sg_: e.activation(out=sg_.t[:], in_=pg.t[:, 0:512], func=AF.Silu), r=[pg], w=[sg_])
                            V(lambda e, pu=pu, sg_=sg_, f=f, hs=hs: e.tensor_tensor(out=hT.t[:, f, hs], in0=sg_.t[:], in1=pu.t[:, 0:512], op=ALU.mult),
                              r=[sg_, pu], cw=[hT])
                if i + 1 < 4:
                    ld("gpsimd", wdb[(i + 1) % 2], wdb[(i + 1) % 2].t[:], wd[i + 1].rearrange("(k p) d -> p k d", p=128))
                    gather_expert(i + 1)
                    load_piece(i + 1, 0)
                    load_piece(i + 1, 1)
                for st in range(8):
                    col = i * 8 + st
                    ye_ = yet[st % 2]
                    for dh in range(2):
                        py = psY[cnts["y"] % 2]
                        cnts["y"] += 1
                        ds_ = slice(512 * dh, 512 * (dh + 1))
                        for f in range(16):
                            T(lambda e, py=py, f=f, st=st, ds_=ds_, wd_=wd_: e.matmul(py.t[:, 0:512], lhsT=hT.t[:, f, st * 128:(st + 1) * 128], rhs=wd_.t[:, f, ds_],
                                                                                   start=(f == 0), stop=(f == 15)), r=[hT, wd_], w=[py])
                        if dh == 0:
                            A(lambda e, py=py, ye_=ye_, ds_=ds_, gt_=gt_, st=st: e.activation(out=ye_.t[:, ds_], in_=py.t[:, 0:512], func=AF.Identity,
                                                                                           scale=gt_.t[:, st:st + 1]), r=[py, gt_], cw=[ye_])
                        else:
                            V(lambda e, py=py, ye_=ye_, ds_=ds_, gt_=gt_, st=st: e.tensor_scalar(out=ye_.t[:, ds_], in0=py.t[:, 0:512], scalar1=gt_.t[:, st:st + 1],
                                                                                              scalar2=None, op0=ALU.mult), r=[py, gt_], cw=[ye_])
                    p.dma("gpsimd", lambda e, ye_=ye_, col=col: e.indirect_dma_start(
                        out=contrib, out_offset=bass.IndirectOffsetOnAxis(ap=toki.t[:, col:col + 1], axis=0), in_=ye_.t[:], in_offset=None,
                        compute_op=ALU.add), "scat", reads=[ye_, toki, R_contrib], writes=[R_contrib])
            p.dma("gpsimd", lambda e: e.collective_compute("ReduceScatter", ALU.add, replica_groups=RG, ins=[contrib], outs=[moe_d]),
                  "rs2", reads=[R_contrib], writes=[R_moe], inc=1)
            p.barrier()
        if debug and stop_after == 7:
            tap(dbg["moe"], moe_d, [R_moe])

        with ExitStack() as ph:
            mo = [mk(ph, f"mo{i}", [128, D], F32) for i in range(2)]
            x1r = [mk(ph, f"x1r{i}", [128, D], F32) for i in range(2)]
            sq8 = mk(ph, "sq8", [128, D], BF16)
            s8 = [mk(ph, f"s8{i}", [128, 2], F32) for i in range(2)]
            for n in range(16):
                ts = slice(128 * n, 128 * (n + 1))
                m_, x_, s_ = mo[n % 2], x1r[n % 2], s8[n % 2]
                ld("sync", m_, m_.t[:], moe_d[ts, :], reads=[R_moe])
                ld("sync", x_, x_.t[:], x1_d[ts, :], reads=[R_x1])
                A(lambda e, m_=m_, s_=s_: e.activation(out=sq8.t[:], in_=m_.t[:], func=AF.Square, accum_out=s_.t[:, 0:1]), r=[m_], w=[sq8, s_])
                A(lambda e, s_=s_: e.activation(out=s_.t[:, 1:2], in_=s_.t[:, 0:1], func=AF.Sqrt, scale=1.0 / D, bias=cst.t[:, 1:2]), r=[s_, cst], w=[s_])
                V(lambda e, s_=s_: e.reciprocal(out=s_.t[:, 1:2], in_=s_.t[:, 1:2]), r=[s_], w=[s_])
                V(lambda e, m_=m_, s_=s_: e.scalar_tensor_tensor(out=m_.t[:], in0=m_.t[:], scalar=s_.t[:, 1:2], in1=rows["gf"].t[:],
                                                               op0=ALU.mult, op1=ALU.mult), r=[m_, s_, rows["gf"]], w=[m_])
                PL(lambda e, m_=m_, x_=x_: e.tensor_tensor(out=m_.t[:], in0=m_.t[:], in1=x_.t[:], op=ALU.add), r=[m_, x_], w=[m_])
                p.dma("sync", lambda e, ts=ts, m_=m_: e.dma_start(out=out[ts, :], in_=m_.t[:]), "out", reads=[m_], writes=[R_out])
            p.barrier()
    p.finish()
    return nc, p, dbg


def _consts(q):
    j = np.arange(128, dtype=np.float32)[:, None]
    i = np.arange(128, dtype=np.float32)[None, :]
    rc = np.zeros((128, 514), np.float32)
    rc[:, 0:128] = np.maximum(i - j, 0)
    rc[:, 128:256] = np.maximum(j - i, 0)
    rc[:, 256:384] = i + 1
    rc[:, 384:512] = 128 - i
    rc[:, 512] = 127 - j[:, 0]
    rc[:, 513] = j[:, 0]
    masks = np.zeros((128, 2, 3, 3, 2, 128), np.float32)
    kk = np.arange(128)[:, None, None]
    jj = np.arange(2)[None, :, None]
    ii = np.arange(128)[None, None, :]
    delta = np.abs(kk + 128 * jj - 64 - ii).astype(np.float32)
    band = (delta <= 64).astype(np.float32)
    for h in range(2):
        slope = SLOPES[2 * q + h]
        for c, d in enumerate(CONFIGS):
            m = band * np.exp(-slope * d * delta)
            masks[:, h, c, 0] = m
            m1 = m.copy()
            m1[0:64, 0, :] = 0
            masks[:, h, c, 1] = m1
            m2 = m.copy()
            m2[64:128, 1, :] = 0
            masks[:, h, c, 2] = m2
    slotid = (128 * np.arange(8)[None, :] + np.arange(128)[:, None]).astype(np.float32)
    tri = (np.arange(128)[:, None] < np.arange(128)[None, :]).astype(np.float32)
    return rc, masks.reshape(128, 18 * 256), slotid, tri


def prep(inputs):
    f = lambda a: np.ascontiguousarray(np.asarray(a, dtype=np.float32))
    x, c = f(inputs["x"]), f(inputs["c"])
    w_in = f(inputs["w_in"])[0]
    w_out = f(inputs["w_out"])[0]
    col = lambda v: np.ascontiguousarray(v.reshape(-1, 128).T)
    vcols = np.concatenate([col(f(inputs["b_ada"])[0]), col(f(inputs["g_pre_mix"])[0]), col(f(inputs["g_post_mix"])[0]),
                            col(f(inputs["g_pre_ffn"])[0]), col(f(inputs["g_post_ffn"])[0])], axis=1)
    wada = f(inputs["w_ada"])[0]
    wr = f(inputs["w_router"])[0]
    wge, wue, wde = f(inputs["w_gate_e"])[0], f(inputs["w_up_e"])[0], f(inputs["w_down_e"])[0]
    df, db = f(inputs["ret_decay_fwd"])[0], f(inputs["ret_decay_bwd"])[0]
    ident = np.eye(128, dtype=np.float32)
    maps = []
    for i in range(8):
        b, q = i // 4, i % 4
        rq = np.arange(64 * q, 64 * q + 64)
        rk = 256 + rq
        rvc = 512 + np.arange(128 * q, 128 * q + 128)
        rgc = 1024 + np.arange(128 * q, 128 * q + 128)
        aqc = 1536 + np.arange(128 * q, 128 * q + 128)
        akc = 2048 + np.arange(128 * q, 128 * q + 128)
        avc = 2560 + np.arange(128 * q, 128 * q + 128)
        cols = np.concatenate([rq, rk, aqc, akc, avc, rk, rvc, rgc])
        rows = np.concatenate([np.arange(128 * q, 128 * q + 128), 512 + np.arange(128 * q, 128 * q + 128)])
        rc, masks, slotid, tri = _consts(q)
        oh = np.zeros((128, 4), np.float32)
        oh[:, q] = 1.0
        dec = np.zeros((128, 2), np.float32)
        dec[:, 0] = df[q]
        dec[:, 1] = db[q]
        maps.append({
            "xb": x[b], "xo": np.ascontiguousarray(x[b, OWN * q:OWN * (q + 1)]), "ccol": col(c[b]),
            "wada": wada, "vcols": vcols, "win": np.ascontiguousarray(w_in[:, cols]), "dec": dec,
            "wout": np.ascontiguousarray(w_out[rows]), "wr": wr, "oh": oh,
            "wg": np.ascontiguousarray(wge[4 * q:4 * q + 4]), "wu": np.ascontiguousarray(wue[4 * q:4 * q + 4]),
            "wd": np.ascontiguousarray(wde[4 * q:4 * q + 4]),
            "ident": ident, "masks": masks, "rc": rc, "slotid": slotid, "tri": tri,
        })
    return maps


_NC_CACHE = {}


def kernel(**inputs):
    maps = prep(inputs)
    if "nc" not in _NC_CACHE:
        _NC_CACHE["nc"] = build()[0]
    res = run_bass_kernel_spmd(_NC_CACHE["nc"], maps, core_ids=list(range(8)))
    out = np.zeros((2, S, D), np.float32)
    for i in range(8):
        b, q = i // 4, i % 4
        out[b, OWN * q:OWN * (q + 1)] = res.results[i]["out"]
    return out
```

```python
import numpy as np
from contextlib import ExitStack
import concourse.bass as bass
import concourse.mybir as mybir
from concourse.bass_utils import run_bass_kernel_spmd

F32 = mybir.dt.float32
BF16 = mybir.dt.bfloat16
I32 = mybir.dt.int32
ALU = mybir.AluOpType
AF = mybir.ActivationFunctionType
AX = mybir.AxisListType
ENGS = ["sync", "scalar", "vector", "gpsimd", "tensor"]

S = 8192
D = 1024
NT = S // 128
OWN = 2048
CAP = 1024
LN8 = -2.0794415416798357
SLOPES = [2.0 ** (-(h + 1)) for h in range(8)]
CONFIGS = (1, 4, 16)


class Reg:
    __slots__ = ("w", "r", "name", "psum", "cw")

    def __init__(self, name="", psum=False):
        self.w = None
        self.cw = {}
        self.r = {}
        self.name = name
        self.psum = psum


class Buf:
    __slots__ = ("t", "r")

    def __init__(self, t, name):
        self.t = t
        self.r = Reg(name)


class Prog:
    def __init__(self, nc):
        self.nc = nc
        self.stack = ExitStack()
        self.ops = {e: [] for e in ENGS}
        self.cnt = {e: 0 for e in ENGS}
        self.sem = {e: self.stack.enter_context(nc.semaphore(f"c_{e}")) for e in ENGS}
        self.known = {e: {} for e in ENGS}
        self.dsem = {}
        self.dcnt = {}

    def _need(self, eng, tok, waits):
        if tok is None:
            return
        sem, val, src = tok
        if src == eng and eng == "tensor":
            return
        if src == eng and val <= self.cnt[eng] - 3:
            return
        k = self.known[eng]
        if k.get(id(sem), 0) >= val:
            return
        k[id(sem)] = val
        waits.append((sem, val))

    def _deps(self, eng, reads, writes, cwrites=()):
        waits = []
        for c in cwrites:
            self._need(eng, c.w, waits)
            for t in c.r.values():
                self._need(eng, t, waits)
        for r in reads:
            self._need(eng, r.w, waits)
            for t in r.cw.values():
                self._need(eng, t, waits)
            if r.psum:
                for t in r.r.values():
                    if t[2] != eng:
                        self._need(eng, t, waits)
        for w in writes:
            self._need(eng, w.w, waits)
            for t in w.cw.values():
                self._need(eng, t, waits)
            for t in w.r.values():
                self._need(eng, t, waits)
        best = {}
        for sem, val in waits:
            if id(sem) not in best or best[id(sem)][1] < val:
                best[id(sem)] = (sem, val)
        return list(best.values())

    def _commit(self, tok, reads, writes, cwrites=()):
        for r in reads:
            r.r[id(tok[0])] = tok
        for w in writes:
            w.w = tok
            w.cw = {}
            w.r = {}
        for c in cwrites:
            c.cw[id(tok[0])] = tok

    def op(self, eng, fn, reads=(), writes=(), cwrites=()):
        reads = [x.r if isinstance(x, Buf) else x for x in reads]
        writes = [x.r if isinstance(x, Buf) else x for x in writes]
        cwrites = [x.r if isinstance(x, Buf) else x for x in cwrites]
        waits = self._deps(eng, reads, writes, cwrites)
        self.cnt[eng] += 1
        tok = (self.sem[eng], self.cnt[eng], eng)
        self.ops[eng].append((waits, fn, (self.sem[eng], 1)))
        self._commit(tok, reads, writes, cwrites)
        return tok

    def dma(self, q, fn, key, reads=(), writes=(), inc=16):
        reads = [x.r if isinstance(x, Buf) else x for x in reads]
        writes = [x.r if isinstance(x, Buf) else x for x in writes]
        if key not in self.dsem:
            self.dsem[key] = self.stack.enter_context(self.nc.semaphore(f"d_{key}"))
            self.dcnt[key] = 0
        waits = self._deps(q, reads, writes)
        self.dcnt[key] += inc
        tok = (self.dsem[key], self.dcnt[key], "dma")
        self.ops[q].append((waits, fn, (self.dsem[key], inc)))
        self._commit(tok, reads, writes)
        return tok

    def barrier(self):
        for eng in ENGS:
            waits = []
            for e in ENGS:
                if self.cnt[e] > 0 and e != eng:
                    self._need(eng, (self.sem[e], self.cnt[e], e), waits)
            for key, sem in self.dsem.items():
                self._need(eng, (sem, self.dcnt[key], "dma"), waits)
            self.ops[eng].append((waits, None, None))

    def emit(self):
        return

    def finish(self):
        nc = self.nc
        ops = self.ops
        self.ops = {e: [] for e in ENGS}

        def replay(name, e):
            for waits, fn, inc in ops[name]:
                for sem, val in waits:
                    e.wait_ge(sem, val)
                if fn is not None:
                    fn(e).then_inc(inc[0], inc[1])

        with nc.Block() as block:
            @block.sync
            def _(e):
                replay("sync", e)

            @block.scalar
            def _(e):
                replay("scalar", e)

            @block.vector
            def _(e):
                replay("vector", e)

            @block.gpsimd
            def _(e):
                replay("gpsimd", e)

            @block.tensor
            def _(e):
                replay("tensor", e)


def build(stop_after=99, debug=False):
    import os
    NGRP = int(os.environ.get('NGRP', '16'))
    FLAGS = os.environ.get('KFLAGS', '').split(',')
    nc = bass.Bass("TRN2", target_bir_lowering=False)

    def din(name, shape, dt=F32):
        return nc.dram_tensor(name, list(shape), dt, kind="ExternalInput").ap()

    def dsc(name, shape, dt=F32):
        return nc.dram_tensor(name, list(shape), dt).ap()

    xb = din("xb", [S, D])
    xo = din("xo", [OWN, D])
    ccol = din("ccol", [128, 8])
    wada = din("wada", [D, 6 * D])
    vcols = din("vcols", [128, 80])
    win = din("win", [D, 832])
    decin = din("dec", [128, 2])
    wout = din("wout", [256, D])
    wrin = din("wr", [D, 16])
    ohin = din("oh", [128, 4])
    if stop_after >= 7:
        wg = din("wg", [4, D, 2048])
        wu = din("wu", [4, D, 2048])
        wd = din("wd", [4, 2048, D])
    identin = din("ident", [128, 128])
    masksin = din("masks", [128, 18 * 256])
    rcin = din("rc", [128, 514])
    slotin = din("slotid", [128, 8])
    triin = din("tri", [128, 128])
    out = nc.dram_tensor("out", [OWN, D], F32, kind="ExternalOutput").ap()
    dbg = {}
    if debug:
        if stop_after in (2, 3):
            dbg["yT"] = nc.dram_tensor("dbg_yT", [256, S], F32, kind="ExternalOutput").ap()
        if stop_after in (5, 6):
            dbg["x1"] = nc.dram_tensor("dbg_x1", [OWN, D], F32, kind="ExternalOutput").ap()
            dbg["aff"] = nc.dram_tensor("dbg_aff", [OWN, 16], F32, kind="ExternalOutput").ap()
            dbg["tok"] = nc.dram_tensor("dbg_tok", [128, 32], F32, kind="ExternalOutput").ap()
        if stop_after == 7:
            dbg["moe"] = nc.dram_tensor("dbg_moe", [OWN, D], F32, kind="ExternalOutput").ap()

    aq_d = dsc("aq_d", [128, S], BF16)
    ak_d = dsc("ak_d", [128, S], BF16)
    av_d = dsc("av_d", [128, S], BF16)
    part_d = dsc("part_d", [S, D])
    mixed_d = dsc("mixed_d", [OWN, D])
    h2_in = dsc("h2_in", [OWN, D], BF16)
    h2_all = [dsc(f"h2_all{c4}", [2048, D], BF16) for c4 in range(4)]
    h2_tab = dsc("h2_tab", [S, D], BF16)
    aff_in = dsc("aff_in", [OWN, 16])
    aff_all = dsc("aff_all", [S, 16])
    aff_tab = dsc("aff_tab", [S, 16])
    cdr = dsc("cdr", [4, S])
    contrib = dsc("contrib", [S, D])
    moe_d = dsc("moe_d", [OWN, D])
    R_aq, R_ak, R_av = Reg("aq_d"), Reg("ak_d"), Reg("av_d")
    R_part, R_mixed = Reg("part_d"), Reg("mixed_d")
    R_h2in, R_h2all, R_h2tab = Reg("h2in"), Reg("h2all"), Reg("h2tab")
    R_affin, R_affall, R_afftab = Reg("affin"), Reg("affall"), Reg("afftab")
    R_cdr, R_contrib, R_moe, R_out = Reg("cdr"), Reg("contrib"), Reg("moe"), Reg("out")
    RG = [[0, 1, 2, 3], [4, 5, 6, 7]] if 'half' not in FLAGS else [[0, 1, 2, 3]]

    p = Prog(nc)
    GS = p.stack

    def mk(stack, name, shape, dt, psum=False):
        if psum:
            t = stack.enter_context(nc.psum_tensor(name, [128, 512 if dt == F32 else 1024], dt))
        else:
            t = stack.enter_context(nc.sbuf_tensor(name, list(shape), dt))
        b = Buf(t, name)
        b.r.psum = psum
        return b

    V = lambda fn, r=(), w=(), cw=(): p.op("vector", fn, r, w, cw)
    A = lambda fn, r=(), w=(), cw=(): p.op("scalar", fn, r, w, cw)
    PL = lambda fn, r=(), w=(), cw=(): p.op("gpsimd", fn, r, w, cw)
    T = lambda fn, r=(), w=(), cw=(): p.op("tensor", fn, r, w, cw)

    def ld(q, dst, dst_ap, src_ap, reads=()):
        return p.dma(q, lambda e: e.dma_start(out=dst_ap, in_=src_ap), dst.r.name, reads=reads, writes=[dst])

    identf = mk(GS, "identf", [128, 128], F32)
    identb = mk(GS, "identb", [128, 128], BF16)
    onesf = mk(GS, "onesf", [128, 128], F32)
    vc = mk(GS, "vc", [128, 80], F32)
    mod = mk(GS, "mod", [128, 48], F32)
    der = mk(GS, "der", [128, 32], F32)
    ohs = mk(GS, "ohs", [128, 4], F32)
    ld("sync", identf, identf.t[:], identin)
    ld("gpsimd", identb, identb.t[:], identin)
    ld("sync", vc, vc.t[:], vcols)
    ld("sync", ohs, ohs.t[:], ohin)
    V(lambda e: e.memset(onesf.t[:], 1.0), w=[onesf])
    cst = mk(GS, "cst", [128, 4], F32)
    V(lambda e: e.memset(cst.t[:, 0:1], LN8), w=[cst])
    V(lambda e: e.memset(cst.t[:, 1:2], 1e-6), w=[cst])
    V(lambda e: e.memset(cst.t[:, 2:3], 1e-5), w=[cst])
    V(lambda e: e.memset(cst.t[:, 3:4], 0.0), w=[cst])
    ymix = ExitStack()
    yT = mk(ymix, "yT", [128, S], BF16)
    yA = mk(ymix, "yA", [64, S], BF16)
    yB = mk(ymix, "yB", [64, S], BF16)

    with ExitStack() as ph:
        cc = mk(ph, "cc", [128, 8], F32)
        scb = mk(ph, "scb", [128, 8], BF16)
        wa = [mk(ph, f"wa{i}", [128, 8, D], BF16) for i in range(2)]
        psm = mk(ph, "psm", [128, 48], F32, psum=True)
        ld("sync", cc, cc.t[:], ccol)
        A(lambda e: e.activation(out=scb.t[:], in_=cc.t[:], func=AF.Silu), r=[cc], w=[scb])
        wada_v = wada.rearrange("(k p) n -> p k n", p=128)
        for g in range(6):
            w_ = wa[g % 2]
            ld("gpsimd", w_, w_.t[:], wada_v[:, :, g * D:(g + 1) * D])
            for j in range(8):
                for k in range(8):
                    T(lambda e, w_=w_, g=g, j=j, k=k: e.matmul(
                        psm.t[:, g * 8 + j:g * 8 + j + 1], lhsT=w_.t[:, k, j * 128:(j + 1) * 128],
                        rhs=scb.t[:, k:k + 1], start=(k == 0), stop=(k == 7)), r=[w_, scb], w=[psm])
        V(lambda e: e.tensor_tensor(out=mod.t[:], in0=psm.t[:, 0:48], in1=vc.t[:, 0:48], op=ALU.add), r=[psm, vc], w=[mod])
        V(lambda e: e.scalar_tensor_tensor(out=der.t[:, 0:8], in0=mod.t[:, 8:16], scalar=1.0, in1=vc.t[:, 48:56],
                                           op0=ALU.add, op1=ALU.mult), r=[mod, vc], w=[der])
        V(lambda e: e.tensor_tensor(out=der.t[:, 8:16], in0=mod.t[:, 16:24], in1=vc.t[:, 56:64], op=ALU.mult), r=[mod, vc], w=[der])
        V(lambda e: e.scalar_tensor_tensor(out=der.t[:, 16:24], in0=mod.t[:, 32:40], scalar=1.0, in1=vc.t[:, 64:72],
                                           op0=ALU.add, op1=ALU.mult), r=[mod, vc], w=[der])
        V(lambda e: e.tensor_tensor(out=der.t[:, 24:32], in0=mod.t[:, 40:48], in1=vc.t[:, 72:80], op=ALU.mult), r=[mod, vc], w=[der])
        p.barrier()
        p.emit()

    if stop_after <= 0:
        p.finish()
        return nc, p, dbg
    with ExitStack() as ph:
        rqT = mk(ph, "rqT", [64, S], BF16)
        rkT = mk(ph, "rkT", [64, S], BF16)
        ktf = mk(ph, "ktf", [128, NT, 64], BF16)
        ktb = mk(ph, "ktb", [128, NT, 64], BF16)
        rv = mk(ph, "rv", [128, NT, 128], BF16)
        sg = mk(ph, "sg", [128, NT, 128], BF16)
        rcs = mk(ph, "rcs", [128, 514], F32)
        dcs = mk(ph, "dcs", [128, 2], F32)
        lg = mk(ph, "lg", [128, 2], F32)
        tfb = mk(ph, "tfb", [128, 4], F32)
        DT = mk(ph, "DT", [128, 128], F32)
        QF = mk(ph, "QF", [128, 128], BF16)
        QB = mk(ph, "QB", [128, 128], BF16)
        ld("sync", rcs, rcs.t[:], rcin)
        ld("sync", dcs, dcs.t[:], decin)
        A(lambda e: e.activation(out=lg.t[:], in_=dcs.t[:], func=AF.Exp, scale=-1.0), r=[dcs], w=[lg])
        V(lambda e: e.tensor_scalar(out=lg.t[:], in0=lg.t[:], scalar1=1.0, scalar2=None, op0=ALU.add), r=[lg], w=[lg])
        A(lambda e: e.activation(out=lg.t[:], in_=lg.t[:], func=AF.Ln), r=[lg], w=[lg])
        V(lambda e: e.tensor_scalar(out=lg.t[:], in0=lg.t[:], scalar1=-1.0, scalar2=None, op0=ALU.mult), r=[lg], w=[lg])
        A(lambda e: e.activation(out=tfb.t[:, 0:1], in_=rcs.t[:, 512:513], func=AF.Exp, scale=lg.t[:, 0:1], bias=cst.t[:, 0:1]), r=[rcs, lg, cst], w=[tfb])
        A(lambda e: e.activation(out=tfb.t[:, 1:2], in_=rcs.t[:, 513:514], func=AF.Exp, scale=lg.t[:, 1:2], bias=cst.t[:, 0:1]), r=[rcs, lg, cst], w=[tfb])
        A(lambda e: e.activation(out=tfb.t[:, 2:4], in_=lg.t[:, 0:2], func=AF.Exp, scale=128.0), r=[lg], w=[tfb])
        A(lambda e: e.activation(out=QF.t[:], in_=rcs.t[:, 256:384], func=AF.Exp, scale=lg.t[:, 0:1]), r=[rcs, lg], w=[QF])
        A(lambda e: e.activation(out=QB.t[:], in_=rcs.t[:, 384:512], func=AF.Exp, scale=lg.t[:, 1:2]), r=[rcs, lg], w=[QB])
        V(lambda e: e.tensor_scalar(out=DT.t[:], in0=rcs.t[:, 0:128], scalar1=lg.t[:, 0:1], scalar2=None, op0=ALU.mult), r=[rcs, lg], w=[DT])
        V(lambda e: e.scalar_tensor_tensor(out=DT.t[:], in0=rcs.t[:, 128:256], scalar=lg.t[:, 1:2], in1=DT.t[:],
                                           op0=ALU.mult, op1=ALU.add), r=[rcs, lg, DT], w=[DT])
        A(lambda e: e.activation(out=DT.t[:], in_=DT.t[:], func=AF.Exp, bias=cst.t[:, 0:1]), r=[DT, cst], w=[DT])

        if 'pre_only' in FLAGS:
            p.barrier()
            p.finish()
            return nc, p, dbg
        with ExitStack() as ph1:
            winb = mk(ph1, "winb", [128, 8, 832], BF16)
            xs = [mk(ph1, f"xs{i}", [128, D], F32) for i in range(2)]
            sqj = mk(ph1, "sqj", [128, D], BF16)
            ssq = [mk(ph1, f"ssq{i}", [128, 2], F32) for i in range(2)]
            xn = [mk(ph1, f"xn{i}", [128, D], BF16) for i in range(2)]
            h1T = [mk(ph1, f"h1T{i}", [128, 8, 512], BF16) for i in range(2)]
            stg = [[mk(ph1, f"stg{a}{i}", [128, 512], BF16) for i in range(2)] for a in range(3)]
            psXa = [mk(ph1, f"psXa{i}", None, BF16, psum=True) for i in range(2)]
            psXb = [mk(ph1, f"psXb{i}", None, BF16, psum=True) for i in range(2)]
            psF = [mk(ph1, f"psF{i}", [128, 512], F32, psum=True) for i in range(2)]
            psT = [mk(ph1, f"psT{i}", [128, 320], F32, psum=True) for i in range(2)]
            ld("gpsimd", winb, winb.t[:], win.rearrange("(k p) n -> p k n", p=128))
            zt = mk(ph1, "zt", [128, D], BF16)
            PL(lambda e: e.memset(zt.t[:], 0.0), w=[zt])
            for zi in range(64):
                p.dma("gpsimd", lambda e, zi=zi: e.dma_start(out=contrib[128 * zi:128 * (zi + 1), :], in_=zt.t[:]),
                      "contrib0", reads=[zt], writes=[R_contrib])
            xb_v = xb.rearrange("(n p) d -> p n d", p=128)
            fstate = {"f": 0}

            def prep_tile(Gi, tt, part):
                h1 = h1T[Gi % 2]
                n = 4 * Gi + tt
                x_ = xs[n % 2]
                s_ = ssq[n % 2]
                xn_ = xn[n % 2]
                pxa, pxb = psXa[n % 2], psXb[n % 2]
                if part == "a":
                  ld("sync", x_, x_.t[:], xb_v[:, n, :])
                  A(lambda e, x_=x_, s_=s_: e.activation(out=sqj.t[:], in_=x_.t[:], func=AF.Square, accum_out=s_.t[:, 0:1]),
                    r=[x_], w=[sqj, s_])
                  A(lambda e, s_=s_: e.activation(out=s_.t[:, 1:2], in_=s_.t[:, 0:1], func=AF.Sqrt, scale=1.0 / D, bias=cst.t[:, 1:2]),
                    r=[s_, cst], w=[s_])
                  V(lambda e, s_=s_: e.reciprocal(out=s_.t[:, 1:2], in_=s_.t[:, 1:2]), r=[s_], w=[s_])
                  V(lambda e, x_=x_, s_=s_, xn_=xn_: e.tensor_scalar(out=xn_.t[:], in0=x_.t[:], scalar1=s_.t[:, 1:2], scalar2=None,
                                                                   op0=ALU.mult), r=[x_, s_], w=[xn_])
                  return
                for k in range(8 if part == "t" else 0):
                    px = pxa if k % 2 == 0 else pxb
                    T(lambda e, k=k, px=px, xn_=xn_: e.transpose(out=px.t[:, (k // 2) * 128:(k // 2 + 1) * 128], in_=xn_.t[:, k * 128:(k + 1) * 128],
                                                               identity=identb.t[:]), r=[xn_, identb], w=[px])
                for k in range(8 if part == "e" else 0):
                    px = pxa if k % 2 == 0 else pxb
                    o_ = h1.t[:, k, tt * 128:(tt + 1) * 128]
                    i_ = px.t[:, (k // 2) * 128:(k // 2 + 1) * 128]
                    if k % 2 == 0:
                        A(lambda e, o_=o_, i_=i_, k=k: e.activation(out=o_, in_=i_, func=AF.Identity, scale=der.t[:, k:k + 1],
                                                                   bias=mod.t[:, k:k + 1]), r=[px, der, mod], cw=[h1])
                    else:
                        V(lambda e, o_=o_, i_=i_, k=k: e.tensor_scalar(out=o_, in0=i_, scalar1=der.t[:, k:k + 1], scalar2=mod.t[:, k:k + 1],
                                                                     op0=ALU.mult, op1=ALU.add), r=[px, der, mod], cw=[h1])

            FG = [(0, 64), (64, 64), (128, 128), (256, 128), (384, 128)]

            def mm_fgroup(Gi, fi, part):
                h1 = h1T[Gi % 2]
                c0, wdt = FG[fi]
                if part == "m":
                    fstate[(Gi, fi)] = psF[fstate["f"] % 2]
                    fstate["f"] += 1
                pf = fstate[(Gi, fi)]
                for k in range(8 if part == "m" else 0):
                    T(lambda e, pf=pf, k=k, c0=c0, wdt=wdt, h1=h1: e.matmul(pf.t[0:wdt, :], lhsT=winb.t[:, k, c0:c0 + wdt], rhs=h1.t[:, k, :],
                                                                         start=(k == 0), stop=(k == 7)), r=[winb, h1], w=[pf])
                if part == "m":
                    return
                sl = slice(Gi * 512, (Gi + 1) * 512)
                if fi == 0:
                    A(lambda e, pf=pf, sl=sl: e.copy(out=rqT.t[:, sl], in_=pf.t[0:64, :]), r=[pf], cw=[rqT])
                elif fi == 1:
                    V(lambda e, pf=pf, sl=sl: e.tensor_copy(out=rkT.t[:, sl], in_=pf.t[0:64, :]), r=[pf], cw=[rkT])
                else:
                    sb_ = stg[fi - 2][Gi % 2]
                    dr, dreg = [(aq_d, R_aq), (ak_d, R_ak), (av_d, R_av)][fi - 2]
                    if fi == 3:
                        V(lambda e, pf=pf, sb_=sb_: e.tensor_copy(out=sb_.t[:], in_=pf.t[:]), r=[pf], w=[sb_])
                    else:
                        A(lambda e, pf=pf, sb_=sb_: e.copy(out=sb_.t[:], in_=pf.t[:]), r=[pf], w=[sb_])
                    p.dma("sync", lambda e, dr=dr, sl=sl, sb_=sb_: e.dma_start(out=dr[:, sl], in_=sb_.t[:]), dreg.name,
                          reads=[sb_], writes=[dreg])

            def mm_ttile(Gi, tt, part):
                h1 = h1T[Gi % 2]
                n = 4 * Gi + tt
                pt = psT[n % 2]
                for k in range(8 if part == "m" else 0):
                    T(lambda e, pt=pt, k=k, tt=tt, h1=h1: e.matmul(pt.t[:, 0:320], lhsT=h1.t[:, k, tt * 128:(tt + 1) * 128], rhs=winb.t[:, k, 512:832],
                                                                 start=(k == 0), stop=(k == 7)), r=[winb, h1], w=[pt])
                if part == "m":
                    return
                A(lambda e, pt=pt, n=n: e.activation(out=ktf.t[:, n, :], in_=pt.t[:, 0:64], func=AF.Identity, scale=tfb.t[:, 0:1]),
                  r=[pt, tfb], cw=[ktf])
                A(lambda e, pt=pt, n=n: e.copy(out=sg.t[:, n, :], in_=pt.t[:, 192:320]), r=[pt], cw=[sg])
                V(lambda e, pt=pt, n=n: e.tensor_scalar(out=ktb.t[:, n, :], in0=pt.t[:, 0:64], scalar1=tfb.t[:, 1:2], scalar2=None,
                                                      op0=ALU.mult), r=[pt, tfb], cw=[ktb])
                V(lambda e, pt=pt, n=n: e.tensor_copy(out=rv.t[:, n, :], in_=pt.t[:, 64:192]), r=[pt], cw=[rv])

            for tt in range(4):
                prep_tile(0, tt, "a")
                prep_tile(0, tt, "t")
                prep_tile(0, tt, "e")
            for Gi in range(NGRP):
                for tt in range(4):
                    nxt = Gi + 1 < NGRP
                    if nxt:
                        prep_tile(Gi + 1, tt, "a")
                    fis = ([0, 1], [2], [3], [4])[tt]
                    for fi in fis:
                        mm_fgroup(Gi, fi, "m")
                    mm_ttile(Gi, tt, "m")
                    if nxt:
                        prep_tile(Gi + 1, tt, "t")
                    for fi in fis:
                        mm_fgroup(Gi, fi, "e")
                    mm_ttile(Gi, tt, "e")
                    if nxt:
                        prep_tile(Gi + 1, tt, "e")
            p.barrier()
            p.emit()
        if stop_after <= 1:
            p.finish()
            return nc, p, dbg

        with ExitStack() as ph2:
            Nbf = mk(ph2, "Nbf", [64, NT, 128], BF16)
            Nrun = [mk(ph2, f"Nrun{i}", [64, 128], F32) for i in range(2)]
            Prun = [mk(ph2, f"Prun{i}", [64, 128], F32) for i in range(2)]
            Pbf = [mk(ph2, f"Pbf{i}", [64, 128], BF16) for i in range(2)]
            SM = [mk(ph2, f"SM{i}", [128, 128], BF16) for i in range(2)]
            qf = [mk(ph2, f"qf{i}", [64, 128], BF16) for i in range(2)]
            qb = [mk(ph2, f"qb{i}", [64, 128], BF16) for i in range(2)]
            osq = mk(ph2, "osq", [128, 4, 128], F32)
            st4 = [mk(ph2, f"st4{i}", [128, 16], F32) for i in range(2)]
            yr = [mk(ph2, f"yr{i}", [128, 128], BF16) for i in range(2)]
            psK = [mk(ph2, f"psK{i}", [64, 128], F32, psum=True) for i in range(2)]
            psS = [mk(ph2, f"psS{i}", [128, 128], F32, psum=True) for i in range(2)]
            psO = [mk(ph2, f"psO{i}", [128, 4, 128], F32, psum=True) for i in range(2)]
            psY = [mk(ph2, f"psY{i}", [128, 128], BF16, psum=True) for i in range(2)]
            for g4 in range(4):
                A(lambda e, g4=g4: e.activation(out=sg.t[:, 16 * g4:16 * (g4 + 1), :], in_=sg.t[:, 16 * g4:16 * (g4 + 1), :], func=AF.Silu), r=[sg], w=[sg])
            V(lambda e: e.memset(Nrun[1].t[:], 0.0), w=[Nrun[1]])
            V(lambda e: e.memset(Nbf.t[:, NT - 1, :], 0.0), w=[Nbf])
            for n in range(NT - 1, 0, -1):
                pk = psK[n % 2]
                cur, nxt = Nrun[n % 2], Nrun[(n + 1) % 2]
                T(lambda e, pk=pk, n=n: e.matmul(pk.t[0:64, 0:128], lhsT=ktb.t[:, n, :], rhs=rv.t[:, n, :], start=True, stop=True), r=[ktb, rv], w=[pk])
                V(lambda e, pk=pk, cur=cur, nxt=nxt: e.scalar_tensor_tensor(out=nxt.t[:], in0=cur.t[:], scalar=tfb.t[0:64, 3:4], in1=pk.t[0:64, 0:128],
                                                                          op0=ALU.mult, op1=ALU.add), r=[cur, pk, tfb], w=[nxt])
                A(lambda e, nxt=nxt, n=n: e.copy(out=Nbf.t[:, n - 1, :], in_=nxt.t[:]), r=[nxt], w=[Nbf])
            V(lambda e: e.memset(Prun[0].t[:], 0.0), w=[Prun[0]])
            V(lambda e: e.memset(Pbf[0].t[:], 0.0), w=[Pbf[0]])
            for n in range(NT):
                cs = slice(n * 128, (n + 1) * 128)
                ps_ = psS[n % 2]
                sm_ = SM[n % 2]
                po = psO[(n // 4) % 2]
                j4 = n % 4
                qf_, qb_ = qf[n % 2], qb[n % 2]
                pb_cur, pb_nxt = Pbf[n % 2], Pbf[(n + 1) % 2]
                pr_cur, pr_nxt = Prun[n % 2], Prun[(n + 1) % 2]
                T(lambda e, ps_=ps_, cs=cs: e.matmul(ps_.t[:, 0:128], lhsT=rkT.t[:, cs], rhs=rqT.t[:, cs], start=True, stop=True), r=[rkT, rqT], w=[ps_])
                V(lambda e, ps_=ps_, sm_=sm_: e.tensor_tensor(out=sm_.t[:], in0=ps_.t[:, 0:128], in1=DT.t[:], op=ALU.mult), r=[ps_, DT], w=[sm_])
                PL(lambda e, qf_=qf_, cs=cs: e.tensor_tensor(out=qf_.t[:], in0=rqT.t[:, cs], in1=QF.t[0:64, :], op=ALU.mult), r=[rqT, QF], w=[qf_])
                PL(lambda e, qb_=qb_, cs=cs: e.tensor_tensor(out=qb_.t[:], in0=rqT.t[:, cs], in1=QB.t[0:64, :], op=ALU.mult), r=[rqT, QB], w=[qb_])
                T(lambda e, po=po, j4=j4, sm_=sm_, n=n: e.matmul(po.t[:, j4 * 128:(j4 + 1) * 128], lhsT=sm_.t[:], rhs=rv.t[:, n, :], start=True, stop=False), r=[sm_, rv], w=[po])
                T(lambda e, po=po, j4=j4, qf_=qf_, pb_cur=pb_cur: e.matmul(po.t[:, j4 * 128:(j4 + 1) * 128], lhsT=qf_.t[:], rhs=pb_cur.t[:], start=False, stop=False),
                  r=[qf_, pb_cur], w=[po])
                T(lambda e, po=po, j4=j4, qb_=qb_, n=n: e.matmul(po.t[:, j4 * 128:(j4 + 1) * 128], lhsT=qb_.t[:], rhs=Nbf.t[:, n, :], start=False, stop=True),
                  r=[qb_, Nbf], w=[po])
                if n < NT - 1:
                    pk = psK[n % 2]
                    T(lambda e, pk=pk, n=n: e.matmul(pk.t[0:64, 0:128], lhsT=ktf.t[:, n, :], rhs=rv.t[:, n, :], start=True, stop=True), r=[ktf, rv], w=[pk])
                    V(lambda e, pk=pk, pr_cur=pr_cur, pr_nxt=pr_nxt: e.scalar_tensor_tensor(out=pr_nxt.t[:], in0=pr_cur.t[:], scalar=tfb.t[0:64, 2:3],
                                                                                          in1=pk.t[0:64, 0:128], op0=ALU.mult, op1=ALU.add),
                      r=[pr_cur, pk, tfb], w=[pr_nxt])
                    A(lambda e, pr_nxt=pr_nxt, pb_nxt=pb_nxt: e.copy(out=pb_nxt.t[:], in_=pr_nxt.t[:]), r=[pr_nxt], w=[pb_nxt])
                if j4 == 3:
                    s4 = st4[(n // 4) % 2]
                    V(lambda e, po=po, s4=s4: e.tensor_reduce(out=s4.t[:, 0:4], in_=po.t[:].rearrange("p (a b) -> p a b", a=4), axis=AX.X, op=ALU.add), r=[po], w=[s4])
                    A(lambda e, po=po: e.activation(out=osq.t[:].rearrange("p a b -> p (a b)"), in_=po.t[:], func=AF.Square), r=[po], w=[osq])
                    V(lambda e, s4=s4: e.tensor_reduce(out=s4.t[:, 4:8], in_=osq.t[:], axis=AX.X, op=ALU.add), r=[osq], w=[s4])
                    V(lambda e, s4=s4: e.tensor_scalar(out=s4.t[:, 8:12], in0=s4.t[:, 0:4], scalar1=1.0 / 128, scalar2=None, op0=ALU.mult), r=[s4], w=[s4])
                    V(lambda e, s4=s4: e.tensor_tensor(out=s4.t[:, 0:4], in0=s4.t[:, 8:12], in1=s4.t[:, 8:12], op=ALU.mult), r=[s4], w=[s4])
                    V(lambda e, s4=s4: e.scalar_tensor_tensor(out=s4.t[:, 12:16], in0=s4.t[:, 4:8], scalar=1.0 / 128, in1=s4.t[:, 0:4],
                                                              op0=ALU.mult, op1=ALU.subtract), r=[s4], w=[s4])
                    A(lambda e, s4=s4: e.activation(out=s4.t[:, 12:16], in_=s4.t[:, 12:16], func=AF.Sqrt, bias=cst.t[:, 2:3]), r=[s4, cst], w=[s4])
                    V(lambda e, s4=s4: e.reciprocal(out=s4.t[:, 12:16], in_=s4.t[:, 12:16]), r=[s4], w=[s4])
                    for jj in range(4):
                        m = n - 3 + jj
                        y_ = yr[m % 2]
                        py = psY[m % 2]
                        V(lambda e, po=po, jj=jj, s4=s4, y_=y_: e.tensor_scalar(out=y_.t[:], in0=po.t[:, jj * 128:(jj + 1) * 128], scalar1=s4.t[:, 8 + jj:9 + jj],
                                                                              scalar2=s4.t[:, 12 + jj:13 + jj], op0=ALU.subtract, op1=ALU.mult),
                          r=[po, s4], w=[y_])
                        PL(lambda e, y_=y_, m=m: e.tensor_tensor(out=y_.t[:], in0=y_.t[:], in1=sg.t[:, m, :], op=ALU.mult), r=[y_, sg], w=[y_])
                        T(lambda e, py=py, y_=y_: e.transpose(out=py.t[:, 0:128], in_=y_.t[:], identity=identb.t[:]), r=[y_, identb], w=[py])
                        A(lambda e, py=py, m=m: e.copy(out=yT.t[:, m * 128:(m + 1) * 128], in_=py.t[:, 0:128]), r=[py], cw=[yT])
            p.barrier()
            p.emit()
    def tap(dst, src_ap, reads):
        p.dma("gpsimd", lambda e: e.dma_start(out=dst, in_=src_ap), "tap", reads=reads)
        p.barrier()

    if stop_after <= 2:
        if debug:
            tap(dbg["yT"][0:128, :], yT.t[:], [yT])
        p.finish()
        return nc, p, dbg
    PAD = 1024
    with ExitStack() as ph:
        aqT = mk(ph, "aqT", [128, S], BF16)
        akT = mk(ph, "akT", [128, S + 2 * PAD], BF16)
        avT = mk(ph, "avT", [128, S + 2 * PAD], BF16)
        mkb = mk(ph, "mkb", [128, 18 * 256], BF16)
        acc = [mk(ph, f"acc{h}", [65, S], F32) for h in range(2)]
        Vaug = [mk(ph, f"Vaug{i}", [128, 2, 65], BF16) for i in range(8)]
        Et = [mk(ph, f"Et{i}", [128, 256], BF16) for i in range(4)]
        NPT = 6
        PTt = [mk(ph, f"PTt{i}", [128, 256], BF16) for i in range(NPT)]
        rd = mk(ph, "rd", [65, 512], F32)
        psA = [mk(ph, f"psA{i}", None, F32, psum=True) for i in range(3)]
        psV = [mk(ph, f"psV{i}", None, BF16, psum=True) for i in range(1)]
        psB = [mk(ph, f"psB{i}", None, F32, psum=True) for i in range(3)]
        psR = mk(ph, "psR", None, F32, psum=True)
        ld("sync", aqT, aqT.t[:], aq_d, reads=[R_aq])
        ld("sync", akT, akT.t[:, PAD:PAD + S], ak_d, reads=[R_ak])
        ld("sync", avT, avT.t[:, PAD:PAD + S], av_d, reads=[R_av])
        ld("gpsimd", mkb, mkb.t[:], masksin)
        for tns in (akT, avT):
            V(lambda e, tns=tns: e.memset(tns.t[:, 0:PAD], 0.0), w=[tns])
            V(lambda e, tns=tns: e.memset(tns.t[:, PAD + S:PAD + S + PAD], 0.0), w=[tns])
        for vb in Vaug:
            V(lambda e, vb=vb: e.memset(vb.t[:, :, 64:65], 1.0), w=[vb])
        items = []
        for c, d in enumerate(CONFIGS):
            nb = (S // d) // 128
            for r in range(d):
                for m in range(nb):
                    for h in range(2):
                        items.append((c, d, r, m, h, nb))
        LAG = 3
        NV = 8
        vt = {}
        vstate = {"vi": 0}

        def ksl_(d, r, u):
            st0 = PAD + d * (128 * u - 64) + r
            return slice(st0, st0 + 127 * d + 1, d)

        def stage1(idx):
            c, d, r, m, h, nb = items[idx]
            for u in (m, m + 1):
                if (c, r, u) in vt:
                    continue
                vi = vstate["vi"]
                vstate["vi"] += 1
                vb = Vaug[vi % NV]
                pv = psV[0]
                ks = ksl_(d, r, u)
                T(lambda e, pv=pv, ks=ks: e.transpose(out=pv.t[:, 0:128], in_=avT.t[:, ks], identity=identb.t[:]), r=[avT, identb], w=[pv])
                if vi % 2 == 0:
                    A(lambda e, pv=pv, vb=vb: e.copy(out=vb.t[:, :, 0:64], in_=pv.t[:, 0:128].rearrange("p (h x) -> p h x", h=2)), r=[pv], w=[vb])
                else:
                    V(lambda e, pv=pv, vb=vb: e.tensor_copy(out=vb.t[:, :, 0:64], in_=pv.t[:, 0:128].rearrange("p (h x) -> p h x", h=2)), r=[pv], w=[vb])
                vt[(c, r, u)] = vb
            q0 = d * 128 * m + r
            qs = slice(q0, q0 + 127 * d + 1, d)
            var = 1 if m == 0 else (2 if m == nb - 1 else 0)
            rows_ = slice(64 * h, 64 * h + 64)
            pa = psA[idx % 3]
            e_ = Et[idx % 4]
            pt_ = PTt[idx % NPT]
            for j in range(2):
                ks = ksl_(d, r, m + j)
                T(lambda e, pa=pa, j=j, rows_=rows_, qs=qs, ks=ks: e.matmul(pa.t[:, j * 128:(j + 1) * 128], lhsT=akT.t[rows_, ks], rhs=aqT.t[rows_, qs],
                                                                      start=True, stop=True), r=[akT, aqT], w=[pa])
            A(lambda e, pa=pa, e_=e_: e.activation(out=e_.t[:], in_=pa.t[:, 0:256], func=AF.Exp, scale=0.125), r=[pa], w=[e_])
            mo = ((h * 3 + c) * 3 + var) * 256
            V(lambda e, e_=e_, pt_=pt_, mo=mo: e.tensor_tensor(out=pt_.t[:], in0=e_.t[:], in1=mkb.t[:, mo:mo + 256], op=ALU.mult), r=[e_, mkb], w=[pt_])

        def stage2(idx):
            c, d, r, m, h, nb = items[idx]
            q0 = d * 128 * m + r
            qs = slice(q0, q0 + 127 * d + 1, d)
            pt_ = PTt[idx % NPT]
            po = psB[idx % 3]
            for j in range(2):
                vb = vt[(c, r, m + j)]
                T(lambda e, po=po, j=j, vb=vb, pt_=pt_, h=h: e.matmul(po.t[0:65, 0:128], lhsT=vb.t[:, h, :], rhs=pt_.t[:, j * 128:(j + 1) * 128],
                                                                   start=(j == 0), stop=(j == 1)), r=[vb, pt_], w=[po])
            ac = acc[h]
            if c == 0:
                A(lambda e, ac=ac, qs=qs, po=po: e.copy(out=ac.t[:, qs], in_=po.t[0:65, 0:128]), r=[po], w=[ac])
            else:
                V(lambda e, ac=ac, qs=qs, po=po: e.tensor_tensor(out=ac.t[:, qs], in0=ac.t[:, qs], in1=po.t[0:65, 0:128], op=ALU.add), r=[po, ac], w=[ac])

        for idx in range(len(items) + LAG):
            if idx < len(items):
                stage1(idx)
            if idx - LAG >= 0:
                stage2(idx - LAG)
        for h in range(2):
            yh = (yA, yB)[h]
            ac = acc[h]
            for t in range(16):
                sl = slice(512 * t, 512 * (t + 1))
                V(lambda e, ac=ac, sl=sl: e.reciprocal(out=rd.t[64:65, :], in_=ac.t[64:65, sl]), r=[ac], w=[rd])
                T(lambda e: e.matmul(psR.t[0:64, 0:512], lhsT=onesf.t[64:65, 0:64], rhs=rd.t[64:65, :], start=True, stop=True), r=[onesf, rd], w=[psR])
                V(lambda e, yh=yh, ac=ac, sl=sl: e.tensor_tensor(out=yh.t[:, sl], in0=ac.t[0:64, sl], in1=psR.t[0:64, 0:512], op=ALU.mult),
                  r=[ac, psR], w=[yh])
        p.barrier()
    if stop_after <= 3:
        if debug:
            tap(dbg["yT"][0:128, :], yT.t[:], [yT])
            tap(dbg["yT"][128:192, :], yA.t[:], [yA])
            tap(dbg["yT"][192:256, :], yB.t[:], [yB])
        p.finish()
        return nc, p, dbg

    with ExitStack() as ph:
        wo = mk(ph, "wo", [128, D], BF16)
        woA = mk(ph, "woA", [64, D], BF16)
        woB = mk(ph, "woB", [64, D], BF16)
        pst = [mk(ph, f"pst{i}", [128, D], F32) for i in range(2)]
        psP = [mk(ph, f"psP{i}", None, F32, psum=True) for i in range(2)]
        ld("gpsimd", wo, wo.t[:], wout[0:128, :])
        ld("gpsimd", woA, woA.t[:], wout[128:192, :])
        ld("gpsimd", woB, woB.t[:], wout[192:256, :])
        for n in range(NT):
            ts = slice(128 * n, 128 * (n + 1))
            st_ = pst[n % 2]
            for half in range(2):
                pp = psP[half]
                cs = slice(512 * half, 512 * (half + 1))
                T(lambda e, pp=pp, ts=ts, cs=cs: e.matmul(pp.t[:, 0:512], lhsT=yT.t[:, ts], rhs=wo.t[:, cs], start=True, stop=False), r=[yT, wo], w=[pp])
                T(lambda e, pp=pp, ts=ts, cs=cs: e.matmul(pp.t[:, 0:512], lhsT=yA.t[:, ts], rhs=woA.t[:, cs], start=False, stop=False), r=[yA, woA], w=[pp])
                T(lambda e, pp=pp, ts=ts, cs=cs: e.matmul(pp.t[:, 0:512], lhsT=yB.t[:, ts], rhs=woB.t[:, cs], start=False, stop=True), r=[yB, woB], w=[pp])
                if half == 0:
                    A(lambda e, pp=pp, st_=st_, cs=cs: e.copy(out=st_.t[:, cs], in_=pp.t[:, 0:512]), r=[pp], w=[st_])
                else:
                    V(lambda e, pp=pp, st_=st_, cs=cs: e.tensor_copy(out=st_.t[:, cs], in_=pp.t[:, 0:512]), r=[pp], w=[st_])
            p.dma("sync", lambda e, ts=ts, st_=st_: e.dma_start(out=part_d[ts, :], in_=st_.t[:]), "part_d", reads=[st_], writes=[R_part])
        p.dma("gpsimd", lambda e: e.collective_compute("ReduceScatter", ALU.add, replica_groups=RG, ins=[part_d], outs=[mixed_d]),
              "rs1", reads=[R_part], writes=[R_mixed], inc=1)
        p.barrier()
    ymix.close()
    if stop_after <= 4:
        p.finish()
        return nc, p, dbg
    x1_d = dsc("x1_d", [OWN, D])
    R_x1 = Reg("x1_d")
    with ExitStack() as ph58:
        rows = {k: mk(ph58, f"row_{k}", [128, D], F32) for k in ("gm", "gsf", "shf", "gf")}
        toki = mk(ph58, "toki", [128, 32], I32)
        with ExitStack() as ph:
            diag = [mk(ph, f"diag{i}", [128, 128], F32) for i in range(2)]
            psD = [mk(ph, f"psD{i}", None, F32, psum=True) for i in range(2)]
            srcs = {"gm": der.t[:, 8:16], "gsf": der.t[:, 16:24], "shf": mod.t[:, 24:32], "gf": der.t[:, 24:32]}
            di = 0
            for key in ("gm", "gsf", "shf", "gf"):
                for half in range(2):
                    pd = psD[half]
                    for kk in range(4):
                        k = half * 4 + kk
                        dg = diag[di % 2]
                        di += 1
                        V(lambda e, dg=dg, key=key, k=k: e.tensor_scalar(out=dg.t[:], in0=identf.t[:], scalar1=srcs[key][:, k:k + 1], scalar2=None,
                                                                       op0=ALU.mult), r=[identf, der, mod], w=[dg])
                        T(lambda e, pd=pd, kk=kk, dg=dg: e.matmul(pd.t[:, kk * 128:(kk + 1) * 128], lhsT=onesf.t[:], rhs=dg.t[:], start=True, stop=True),
                          r=[onesf, dg], w=[pd])
                    A(lambda e, pd=pd, key=key, half=half: e.copy(out=rows[key].t[:, half * 512:(half + 1) * 512], in_=pd.t[:, 0:512]),
                      r=[pd], w=[rows[key]])
            wrs = mk(ph, "wrs", [128, 8, 16], F32)
            ld("sync", wrs, wrs.t[:], wrin.rearrange("(k p) e -> p k e", p=128))
            RD5 = 4
            mx = [mk(ph, f"mx{i}", [128, D], F32) for i in range(RD5)]
            xo_t = [mk(ph, f"xo{i}", [128, D], F32) for i in range(RD5)]
            x1t = [mk(ph, f"x1t{i}", [128, D], F32) for i in range(RD5)]
            h2t = [mk(ph, f"h2t{i}", [128, D], F32) for i in range(RD5)]
            h2T = [mk(ph, f"h2T{i}", [128, 8, 128], F32) for i in range(RD5)]
            sq2 = mk(ph, "sq2", [128, D], BF16)
            ss5 = [mk(ph, f"ss5{i}", [128, 8], F32) for i in range(RD5)]
            afft = [mk(ph, f"afft{i}", [128, 16], F32) for i in range(2)]
            ext = [mk(ph, f"ext{i}", [128, 16], F32) for i in range(2)]
            psH = [mk(ph, f"psH{i}", None, F32, psum=True) for i in range(2)]
            psL = [mk(ph, f"psL{i}", None, F32, psum=True) for i in range(2)]
            lgall = mk(ph, "lgall", [128, 16, 16], F32)
            smx = mk(ph, "smx", [128, 32], F32)
            for n in range(16):
                ts = slice(128 * n, 128 * (n + 1))
                m_, xo_, x1_, h2_, hT_, s5 = mx[n % RD5], xo_t[n % RD5], x1t[n % RD5], h2t[n % RD5], h2T[n % RD5], ss5[n % RD5]
                ld("sync", m_, m_.t[:], mixed_d[ts, :], reads=[R_mixed])
                ld("sync", xo_, xo_.t[:], xo[ts, :])
                A(lambda e, m_=m_, s5=s5: e.activation(out=sq2.t[:], in_=m_.t[:], func=AF.Square, accum_out=s5.t[:, 0:1]), r=[m_], w=[sq2, s5])
                A(lambda e, s5=s5: e.activation(out=s5.t[:, 1:2], in_=s5.t[:, 0:1], func=AF.Sqrt, scale=1.0 / D, bias=cst.t[:, 1:2]), r=[s5, cst], w=[s5])
                V(lambda e, s5=s5: e.reciprocal(out=s5.t[:, 1:2], in_=s5.t[:, 1:2]), r=[s5], w=[s5])
                V(lambda e, m_=m_, s5=s5: e.scalar_tensor_tensor(out=m_.t[:], in0=m_.t[:], scalar=s5.t[:, 1:2], in1=rows["gm"].t[:],
                                                               op0=ALU.mult, op1=ALU.mult), r=[m_, s5, rows["gm"]], w=[m_])
                PL(lambda e, m_=m_, xo_=xo_, x1_=x1_: e.tensor_tensor(out=x1_.t[:], in0=xo_.t[:], in1=m_.t[:], op=ALU.add), r=[m_, xo_], w=[x1_])
                p.dma("sync", lambda e, ts=ts, x1_=x1_: e.dma_start(out=x1_d[ts, :], in_=x1_.t[:]), "x1_d", reads=[x1_], writes=[R_x1])
                A(lambda e, x1_=x1_, s5=s5: e.activation(out=sq2.t[:], in_=x1_.t[:], func=AF.Square, accum_out=s5.t[:, 2:3]), r=[x1_], w=[sq2, s5])
                A(lambda e, s5=s5: e.activation(out=s5.t[:, 3:4], in_=s5.t[:, 2:3], func=AF.Sqrt, scale=1.0 / D, bias=cst.t[:, 1:2]), r=[s5, cst], w=[s5])
                V(lambda e, s5=s5: e.reciprocal(out=s5.t[:, 3:4], in_=s5.t[:, 3:4]), r=[s5], w=[s5])
                V(lambda e, x1_=x1_, s5=s5, h2_=h2_: e.scalar_tensor_tensor(out=h2_.t[:], in0=x1_.t[:], scalar=s5.t[:, 3:4], in1=rows["gsf"].t[:],
                                                                          op0=ALU.mult, op1=ALU.mult), r=[x1_, s5, rows["gsf"]], w=[h2_])
                PL(lambda e, h2_=h2_: e.tensor_tensor(out=h2_.t[:], in0=h2_.t[:], in1=rows["shf"].t[:], op=ALU.add), r=[h2_, rows["shf"]], w=[h2_])
                p.dma("gpsimd", lambda e, ts=ts, h2_=h2_: e.dma_start(out=h2_in[ts, :], in_=h2_.t[:]), "h2_in", reads=[h2_], writes=[R_h2in])
                if 'norouter' in FLAGS:
                    continue
                for k in range(8):
                    ph_ = psH[k // 4]
                    T(lambda e, ph_=ph_, k=k, h2_=h2_: e.transpose(out=ph_.t[:, (k % 4) * 128:(k % 4 + 1) * 128], in_=h2_.t[:, k * 128:(k + 1) * 128],
                                                                 identity=identf.t[:]), r=[h2_, identf], w=[ph_])
                A(lambda e, hT_=hT_: e.copy(out=hT_.t[:, 0:4, :], in_=psH[0].t[:, 0:512].rearrange("p (k s) -> p k s", k=4)), r=[psH[0]], w=[hT_])
                V(lambda e, hT_=hT_: e.tensor_copy(out=hT_.t[:, 4:8, :], in_=psH[1].t[:, 0:512].rearrange("p (k s) -> p k s", k=4)), r=[psH[1]], w=[hT_])
                pl = psL[n % 2]
                for k in range(8):
                    T(lambda e, pl=pl, k=k, hT_=hT_: e.matmul(pl.t[:, 0:16], lhsT=hT_.t[:, k, :], rhs=wrs.t[:, k, :], start=(k == 0), stop=(k == 7)),
                      r=[hT_, wrs], w=[pl])
                V(lambda e, pl=pl, n=n: e.tensor_copy(out=lgall.t[:, n, :], in_=pl.t[:, 0:16]), r=[pl], cw=[lgall])
            V(lambda e: e.tensor_reduce(out=smx.t[:, 0:16], in_=lgall.t[:], axis=AX.X, op=ALU.max), r=[lgall], w=[smx])
            for n in range(16):
                V(lambda e, n=n: e.tensor_scalar(out=lgall.t[:, n, :], in0=lgall.t[:, n, :], scalar1=smx.t[:, n:n + 1], scalar2=None, op0=ALU.subtract),
                  r=[lgall, smx], w=[lgall])
            A(lambda e: e.activation(out=lgall.t[:].rearrange("p a b -> p (a b)"), in_=lgall.t[:].rearrange("p a b -> p (a b)"), func=AF.Exp),
              r=[lgall], w=[lgall])
            V(lambda e: e.tensor_reduce(out=smx.t[:, 16:32], in_=lgall.t[:], axis=AX.X, op=ALU.add), r=[lgall], w=[smx])
            V(lambda e: e.reciprocal(out=smx.t[:, 16:32], in_=smx.t[:, 16:32]), r=[smx], w=[smx])
            for n in range(16):
                V(lambda e, n=n: e.tensor_scalar(out=lgall.t[:, n, :], in0=lgall.t[:, n, :], scalar1=smx.t[:, 16 + n:17 + n], scalar2=None, op0=ALU.mult),
                  r=[lgall, smx], w=[lgall])
            p.dma("sync", lambda e: e.dma_start(out=aff_in.rearrange("(n p) e -> p n e", p=128), in_=lgall.t[:]), "aff_in", reads=[lgall], writes=[R_affin])
            for c4 in range(4):
                p.dma("gpsimd", lambda e, c4=c4: e.collective_compute("AllGather", ALU.bypass, replica_groups=RG,
                                                                      ins=[h2_in[512 * c4:512 * (c4 + 1), :]], outs=[h2_all[c4]]),
                      f"ag1_{c4}", reads=[R_h2in], writes=[R_h2all], inc=1)
            p.dma("gpsimd", lambda e: e.collective_compute("AllGather", ALU.bypass, replica_groups=RG, ins=[aff_in], outs=[aff_all]),
                  "ag2", reads=[R_affin], writes=[R_affall], inc=1)
            for c4 in range(4):
                for r4 in range(4):
                    p.dma("sync", lambda e, c4=c4, r4=r4: e.dma_start(out=h2_tab[2048 * r4 + 512 * c4:2048 * r4 + 512 * (c4 + 1), :],
                                                                      in_=h2_all[c4][512 * r4:512 * (r4 + 1), :]),
                          "h2tab", reads=[R_h2all], writes=[R_h2tab])
            p.dma("sync", lambda e: e.dma_start(out=aff_tab, in_=aff_all), "afftab", reads=[R_affall], writes=[R_afftab])
            p.barrier()
        if stop_after <= 5:
            if debug:
                tap(dbg["x1"], x1_d, [R_x1])
                tap(dbg["aff"], aff_in, [R_affin])
            p.finish()
            return nc, p, dbg

        with ExitStack() as ph:
            Aall = mk(ph, "Aall", [128, 64, 16], F32)
            A4 = mk(ph, "A4", [128, 4, 64], F32)
            cmp_ = mk(ph, "cmp", [128, 4, 64], F32)
            cc0 = mk(ph, "cc0", [128, 4, 64], F32)
            cc1 = mk(ph, "cc1", [128, 4, 64], F32)
            lo = mk(ph, "lo", [128, 4], F32)
            mid = mk(ph, "mid", [128, 4], F32)
            cnt = mk(ph, "cnt", [128, 4], F32)
            ge = mk(ph, "ge", [128, 4], F32)
            offs = mk(ph, "offs", [128, 4], F32)
            tris = mk(ph, "tris", [128, 128], F32)
            slot = mk(ph, "slot", [128, 8], F32)
            tokf = mk(ph, "tokf", [128, 32], F32)
            cb = [mk(ph, f"cb{i}", [128, S], F32) for i in range(2)]
            junk = mk(ph, "junk", [128, S], BF16)
            psC = mk(ph, "psC", None, F32, psum=True)
            ld("sync", Aall, Aall.t[:], aff_tab.rearrange("(p j) e -> p j e", j=64), reads=[R_afftab])
            ld("sync", tris, tris.t[:], triin)
            ld("sync", slot, slot.t[:], slotin)
            for i in range(4):
                V(lambda e, i=i: e.tensor_scalar(out=A4.t[:, i, :], in0=Aall.t[:, :, i], scalar1=ohs.t[:, 0:1], scalar2=None, op0=ALU.mult),
                  r=[Aall, ohs], w=[A4])
                for r in range(1, 4):
                    V(lambda e, i=i, r=r: e.scalar_tensor_tensor(out=A4.t[:, i, :], in0=Aall.t[:, :, 4 * r + i], scalar=ohs.t[:, r:r + 1], in1=A4.t[:, i, :],
                                                                 op0=ALU.mult, op1=ALU.add), r=[Aall, ohs, A4], w=[A4])
            V(lambda e: e.memset(lo.t[:], 0.0), w=[lo])
            for it in range(26):
                wv = 2.0 ** (-(it + 1))
                V(lambda e, wv=wv: e.tensor_scalar(out=mid.t[:], in0=lo.t[:], scalar1=wv, scalar2=None, op0=ALU.add), r=[lo], w=[mid])
                V(lambda e: e.memset(cnt.t[:], 0.0), w=[cnt])
                for i in range(4):
                    V(lambda e, i=i: e.tensor_scalar(out=cmp_.t[:, i, :], in0=A4.t[:, i, :], scalar1=mid.t[:, i:i + 1], scalar2=0.0, op0=ALU.is_gt,
                                                    op1=ALU.add, accum_out=cnt.t[:, i:i + 1]), r=[A4, mid, cnt], w=[cmp_, cnt])
                T(lambda e: e.matmul(psC.t[:, 0:4], lhsT=onesf.t[:], rhs=cnt.t[:], start=True, stop=True), r=[onesf, cnt], w=[psC])
                V(lambda e: e.tensor_scalar(out=ge.t[:], in0=psC.t[:, 0:4], scalar1=CAP - 0.5, scalar2=None, op0=ALU.is_ge), r=[psC], w=[ge])
                V(lambda e, wv=wv: e.scalar_tensor_tensor(out=lo.t[:], in0=ge.t[:], scalar=wv, in1=lo.t[:], op0=ALU.mult, op1=ALU.add), r=[ge, lo], w=[lo])
            for i in range(4):
                V(lambda e, i=i: e.tensor_scalar(out=cc0.t[:, i, :], in0=A4.t[:, i, :], scalar1=lo.t[:, i:i + 1], scalar2=None, op0=ALU.is_gt),
                  r=[A4, lo], w=[cc0])
            ca, cbuf = cc0, cc1
            for sh in (1, 2, 4, 8, 16, 32):
                V(lambda e, ca=ca, cbuf=cbuf, sh=sh: e.tensor_tensor(out=cbuf.t[:, :, sh:64], in0=ca.t[:, :, sh:64], in1=ca.t[:, :, 0:64 - sh], op=ALU.add),
                  r=[ca], w=[cbuf])
                V(lambda e, ca=ca, cbuf=cbuf, sh=sh: e.tensor_copy(out=cbuf.t[:, :, 0:sh], in_=ca.t[:, :, 0:sh]), r=[ca], w=[cbuf])
                ca, cbuf = cbuf, ca
            V(lambda e, ca=ca: e.tensor_copy(out=cnt.t[:], in_=ca.t[:, :, 63]), r=[ca], w=[cnt])
            T(lambda e: e.matmul(psC.t[:, 0:4], lhsT=tris.t[:], rhs=cnt.t[:], start=True, stop=True), r=[tris, cnt], w=[psC])
            V(lambda e: e.tensor_copy(out=offs.t[:], in_=psC.t[:, 0:4]), r=[psC], w=[offs])
            cdr_v = cdr.rearrange("e (p j) -> e p j", j=64)
            for i in range(4):
                V(lambda e, i=i, ca=ca: e.tensor_scalar(out=ca.t[:, i, :], in0=ca.t[:, i, :], scalar1=offs.t[:, i:i + 1], scalar2=None, op0=ALU.add),
                  r=[ca, offs], w=[ca])
                p.dma("sync", lambda e, i=i, ca=ca: e.dma_start(out=cdr_v[i], in_=ca.t[:, i, :]), "cdr", reads=[ca], writes=[R_cdr])
            V(lambda e: e.memset(tokf.t[:], 0.0), w=[tokf])
            for i in range(4):
                cb_ = cb[i % 2]
                ld("sync", cb_, cb_.t[:], cdr[i:i + 1, :].partition_broadcast(128), reads=[R_cdr])
                for st in range(8):
                    col = i * 8 + st
                    V(lambda e, cb_=cb_, st=st, col=col: e.tensor_scalar(out=junk.t[:], in0=cb_.t[:], scalar1=slot.t[:, st:st + 1], scalar2=0.0,
                                                                       op0=ALU.is_le, op1=ALU.add, accum_out=tokf.t[:, col:col + 1]),
                      r=[cb_, slot, tokf], w=[junk, tokf])
            V(lambda e: e.tensor_copy(out=toki.t[:], in_=tokf.t[:]), r=[tokf], w=[toki])
            if debug and stop_after == 6:
                tap(dbg["tok"], tokf.t[:], [tokf])
            p.barrier()
        if stop_after <= 6:
            if debug:
                tap(dbg["x1"], x1_d, [R_x1])
                tap(dbg["aff"], aff_in, [R_affin])
            p.finish()
            return nc, p, dbg

        with ExitStack() as ph:
            xeT = [mk(ph, f"xeT{i}", [128, 8, CAP], BF16) for i in range(2)]
            hT = mk(ph, "hT", [128, 16, CAP], BF16)
            wdb = [mk(ph, f"wdb{i}", [128, 16, D], BF16) for i in range(2)]
            NPC = 8
            wgb = [mk(ph, f"wgb{i}", [128, 8, 256], BF16) for i in range(3)]
            wub = [mk(ph, f"wub{i}", [128, 8, 256], BF16) for i in range(3)]
            xet = [mk(ph, f"xet{i}", [128, D], BF16) for i in range(2)]
            gat = [mk(ph, f"gat{i}", [128, 16], F32) for i in range(2)]
            gate = [mk(ph, f"gate{i}", [128, 8], F32) for i in range(2)]
            yet = [mk(ph, f"yet{i}", [128, D], F32) for i in range(2)]
            sgt = [mk(ph, f"sgt{i}", [128, 512], BF16) for i in range(2)]
            psXT = mk(ph, "psXT", None, BF16, psum=True)
            psG = [mk(ph, f"psG{i}", None, F32, psum=True) for i in range(2)]
            psU = [mk(ph, f"psU{i}", None, F32, psum=True) for i in range(2)]
            psY = [mk(ph, f"psYd{i}", None, F32, psum=True) for i in range(2)]
            cnts = {"g": 0, "pc": 0, "m": 0, "y": 0}

            def load_piece(i, pc):
                ws = (i * NPC + pc) % 3
                wg_v = wg[i].rearrange("(k p) f -> p k f", p=128)
                wu_v = wu[i].rearrange("(k p) f -> p k f", p=128)
                ld("gpsimd", wgb[ws], wgb[ws].t[:], wg_v[:, :, pc * 256:(pc + 1) * 256])
                ld("gpsimd", wub[ws], wub[ws].t[:], wu_v[:, :, pc * 256:(pc + 1) * 256])

            def gather_expert(i):
                xT = xeT[i % 2]
                gt_ = gate[i % 2]
                for st in range(8):
                    col = i * 8 + st
                    xe_ = xet[cnts["g"] % 2]
                    ga_ = gat[cnts["g"] % 2]
                    cnts["g"] += 1
                    p.dma("gpsimd", lambda e, xe_=xe_, col=col: e.indirect_dma_start(
                        out=xe_.t[:], out_offset=None, in_=h2_tab, in_offset=bass.IndirectOffsetOnAxis(ap=toki.t[:, col:col + 1], axis=0)),
                        xe_.r.name, reads=[R_h2tab, toki], writes=[xe_])
                    p.dma("gpsimd", lambda e, ga_=ga_, col=col: e.indirect_dma_start(
                        out=ga_.t[:], out_offset=None, in_=aff_tab, in_offset=bass.IndirectOffsetOnAxis(ap=toki.t[:, col:col + 1], axis=0)),
                        ga_.r.name, reads=[R_afftab, toki], writes=[ga_])
                    V(lambda e, ga_=ga_, gt_=gt_, st=st, i=i: e.tensor_scalar(out=gt_.t[:, st:st + 1], in0=ga_.t[:, i:i + 1], scalar1=ohs.t[:, 0:1], scalar2=None,
                                                                            op0=ALU.mult), r=[ga_, ohs], w=[gt_])
                    for r in range(1, 4):
                        V(lambda e, ga_=ga_, gt_=gt_, st=st, i=i, r=r: e.scalar_tensor_tensor(
                            out=gt_.t[:, st:st + 1], in0=ga_.t[:, 4 * r + i:4 * r + i + 1], scalar=ohs.t[:, r:r + 1], in1=gt_.t[:, st:st + 1],
                            op0=ALU.mult, op1=ALU.add), r=[ga_, ohs, gt_], w=[gt_])
                    for k in range(8):
                        T(lambda e, k=k, xe_=xe_: e.transpose(out=psXT.t[:, k * 128:(k + 1) * 128], in_=xe_.t[:, k * 128:(k + 1) * 128], identity=identb.t[:]),
                          r=[xe_, identb], w=[psXT])
                    if st % 2 == 0:
                        A(lambda e, st=st, xT=xT: e.copy(out=xT.t[:, :, st * 128:(st + 1) * 128], in_=psXT.t[:].rearrange("p (k s) -> p k s", k=8)),
                          r=[psXT], cw=[xT])
                    else:
                        V(lambda e, st=st, xT=xT: e.tensor_copy(out=xT.t[:, :, st * 128:(st + 1) * 128], in_=psXT.t[:].rearrange("p (k s) -> p k s", k=8)),
                          r=[psXT], cw=[xT])

            ld("gpsimd", wdb[0], wdb[0].t[:], wd[0].rearrange("(k p) d -> p k d", p=128))
            gather_expert(0)
            load_piece(0, 0)
            load_piece(0, 1)
            for i in range(4):
                xT = xeT[i % 2]
                gt_ = gate[i % 2]
                wd_ = wdb[i % 2]
                for pc in range(NPC):
                    if pc + 2 < NPC:
                        load_piece(i, pc + 2)
                    ws = (i * NPC + pc) % 3
                    wg_, wu_ = wgb[ws], wub[ws]
                    for fc in range(2):
                        f = pc * 2 + fc
                        for half in range(2):
                            pg, pu, sg_ = psG[cnts["m"] % 2], psU[cnts["m"] % 2], sgt[cnts["m"] % 2]
                            cnts["m"] += 1
                            hs = slice(512 * half, 512 * (half + 1))
                            for k in range(8):
                                T(lambda e, pg=pg, k=k, wg_=wg_, fc=fc, hs=hs, xT=xT: e.matmul(pg.t[:, 0:512], lhsT=wg_.t[:, k, fc * 128:(fc + 1) * 128], rhs=xT.t[:, k, hs],
                                                                                           start=(k == 0), stop=(k == 7)), r=[wg_, xT], w=[pg])
                            for k in range(8):
                                T(lambda e, pu=pu, k=k, wu_=wu_, fc=fc, hs=hs, xT=xT: e.matmul(pu.t[:, 0:512], lhsT=wu_.t[:, k, fc * 128:(fc + 1) * 128], rhs=xT.t[:, k, hs],
                                                                                           start=(k == 0), stop=(k == 7)), r=[wu_, xT], w=[pu])
                            A(lambda e, pg=pg, sg_=sg_: e.activation(out=sg_.t[:], in_=pg.t[:, 0:512], func=AF.Silu), r=[pg], w=[sg_])
                            V(lambda e, pu=pu, sg_=sg_, f=f, hs=hs: e.tensor_tensor(out=hT.t[:, f, hs], in0=sg_.t[:], in1=pu.t[:, 0:512], op=ALU.mult),
                              r=[sg_, pu], cw=[hT])
                if i + 1 < 4:
                    ld("gpsimd", wdb[(i + 1) % 2], wdb[(i + 1) % 2].t[:], wd[i + 1].rearrange("(k p) d -> p k d", p=128))
                    gather_expert(i + 1)
                    load_piece(i + 1, 0)
                    load_piece(i + 1, 1)
                for st in range(8):
                    col = i * 8 + st
                    ye_ = yet[st % 2]
                    for dh in range(2):
                        py = psY[cnts["y"] % 2]
                        cnts["y"] += 1
                        ds_ = slice(512 * dh, 512 * (dh + 1))
                        for f in range(16):
                            T(lambda e, py=py, f=f, st=st, ds_=ds_, wd_=wd_: e.matmul(py.t[:, 0:512], lhsT=hT.t[:, f, st * 128:(st + 1) * 128], rhs=wd_.t[:, f, ds_],
                                                                                   start=(f == 0), stop=(f == 15)), r=[hT, wd_], w=[py])
                        if dh == 0:
                            A(lambda e, py=py, ye_=ye_, ds_=ds_, gt_=gt_, st=st: e.activation(out=ye_.t[:, ds_], in_=py.t[:, 0:512], func=AF.Identity,
                                                                                           scale=gt_.t[:, st:st + 1]), r=[py, gt_], cw=[ye_])
                        else:
                            V(lambda e, py=py, ye_=ye_, ds_=ds_, gt_=gt_, st=st: e.tensor_scalar(out=ye_.t[:, ds_], in0=py.t[:, 0:512], scalar1=gt_.t[:, st:st + 1],
                                                                                              scalar2=None, op0=ALU.mult), r=[py, gt_], cw=[ye_])
                    p.dma("gpsimd", lambda e, ye_=ye_, col=col: e.indirect_dma_start(
                        out=contrib, out_offset=bass.IndirectOffsetOnAxis(ap=toki.t[:, col:col + 1], axis=0), in_=ye_.t[:], in_offset=None,
                        compute_op=ALU.add), "scat", reads=[ye_, toki, R_contrib], writes=[R_contrib])
            p.dma("gpsimd", lambda e: e.collective_compute("ReduceScatter", ALU.add, replica_groups=RG, ins=[contrib], outs=[moe_d]),
                  "rs2", reads=[R_contrib], writes=[R_moe], inc=1)
            p.barrier()
        if debug and stop_after == 7:
            tap(dbg["moe"], moe_d, [R_moe])

        with ExitStack() as ph:
            mo = [mk(ph, f"mo{i}", [128, D], F32) for i in range(2)]
            x1r = [mk(ph, f"x1r{i}", [128, D], F32) for i in range(2)]
            sq8 = mk(ph, "sq8", [128, D], BF16)
            s8 = [mk(ph, f"s8{i}", [128, 2], F32) for i in range(2)]
            for n in range(16):
                ts = slice(128 * n, 128 * (n + 1))
                m_, x_, s_ = mo[n % 2], x1r[n % 2], s8[n % 2]
                ld("sync", m_, m_.t[:], moe_d[ts, :], reads=[R_moe])
                ld("sync", x_, x_.t[:], x1_d[ts, :], reads=[R_x1])
                A(lambda e, m_=m_, s_=s_: e.activation(out=sq8.t[:], in_=m_.t[:], func=AF.Square, accum_out=s_.t[:, 0:1]), r=[m_], w=[sq8, s_])
                A(lambda e, s_=s_: e.activation(out=s_.t[:, 1:2], in_=s_.t[:, 0:1], func=AF.Sqrt, scale=1.0 / D, bias=cst.t[:, 1:2]), r=[s_, cst], w=[s_])
                V(lambda e, s_=s_: e.reciprocal(out=s_.t[:, 1:2], in_=s_.t[:, 1:2]), r=[s_], w=[s_])
                V(lambda e, m_=m_, s_=s_: e.scalar_tensor_tensor(out=m_.t[:], in0=m_.t[:], scalar=s_.t[:, 1:2], in1=rows["gf"].t[:],
                                                               op0=ALU.mult, op1=ALU.mult), r=[m_, s_, rows["gf"]], w=[m_])
                PL(lambda e, m_=m_, x_=x_: e.tensor_tensor(out=m_.t[:], in0=m_.t[:], in1=x_.t[:], op=ALU.add), r=[m_, x_], w=[m_])
                p.dma("sync", lambda e, ts=ts, m_=m_: e.dma_start(out=out[ts, :], in_=m_.t[:]), "out", reads=[m_], writes=[R_out])
            p.barrier()
    p.finish()
    return nc, p, dbg


def _consts(q):
    j = np.arange(128, dtype=np.float32)[:, None]
    i = np.arange(128, dtype=np.float32)[None, :]
    rc = np.zeros((128, 514), np.float32)
    rc[:, 0:128] = np.maximum(i - j, 0)
    rc[:, 128:256] = np.maximum(j - i, 0)
    rc[:, 256:384] = i + 1
    rc[:, 384:512] = 128 - i
    rc[:, 512] = 127 - j[:, 0]
    rc[:, 513] = j[:, 0]
    masks = np.zeros((128, 2, 3, 3, 2, 128), np.float32)
    kk = np.arange(128)[:, None, None]
    jj = np.arange(2)[None, :, None]
    ii = np.arange(128)[None, None, :]
    delta = np.abs(kk + 128 * jj - 64 - ii).astype(np.float32)
    band = (delta <= 64).astype(np.float32)
    for h in range(2):
        slope = SLOPES[2 * q + h]
        for c, d in enumerate(CONFIGS):
            m = band * np.exp(-slope * d * delta)
            masks[:, h, c, 0] = m
            m1 = m.copy()
            m1[0:64, 0, :] = 0
            masks[:, h, c, 1] = m1
            m2 = m.copy()
            m2[64:128, 1, :] = 0
            masks[:, h, c, 2] = m2
    slotid = (128 * np.arange(8)[None, :] + np.arange(128)[:, None]).astype(np.float32)
    tri = (np.arange(128)[:, None] < np.arange(128)[None, :]).astype(np.float32)
    return rc, masks.reshape(128, 18 * 256), slotid, tri


def prep(inputs):
    f = lambda a: np.ascontiguousarray(np.asarray(a, dtype=np.float32))
    x, c = f(inputs["x"]), f(inputs["c"])
    w_in = f(inputs["w_in"])[0]
    w_out = f(inputs["w_out"])[0]
    col = lambda v: np.ascontiguousarray(v.reshape(-1, 128).T)
    vcols = np.concatenate([col(f(inputs["b_ada"])[0]), col(f(inputs["g_pre_mix"])[0]), col(f(inputs["g_post_mix"])[0]),
                            col(f(inputs["g_pre_ffn"])[0]), col(f(inputs["g_post_ffn"])[0])], axis=1)
    wada = f(inputs["w_ada"])[0]
    wr = f(inputs["w_router"])[0]
    wge, wue, wde = f(inputs["w_gate_e"])[0], f(inputs["w_up_e"])[0], f(inputs["w_down_e"])[0]
    df, db = f(inputs["ret_decay_fwd"])[0], f(inputs["ret_decay_bwd"])[0]
    ident = np.eye(128, dtype=np.float32)
    maps = []
    for i in range(8):
        b, q = i // 4, i % 4
        rq = np.arange(64 * q, 64 * q + 64)
        rk = 256 + rq
        rvc = 512 + np.arange(128 * q, 128 * q + 128)
        rgc = 1024 + np.arange(128 * q, 128 * q + 128)
        aqc = 1536 + np.arange(128 * q, 128 * q + 128)
        akc = 2048 + np.arange(128 * q, 128 * q + 128)
        avc = 2560 + np.arange(128 * q, 128 * q + 128)
        cols = np.concatenate([rq, rk, aqc, akc, avc, rk, rvc, rgc])
        rows = np.concatenate([np.arange(128 * q, 128 * q + 128), 512 + np.arange(128 * q, 128 * q + 128)])
        rc, masks, slotid, tri = _consts(q)
        oh = np.zeros((128, 4), np.float32)
        oh[:, q] = 1.0
        dec = np.zeros((128, 2), np.float32)
        dec[:, 0] = df[q]
        dec[:, 1] = db[q]
        maps.append({
            "xb": x[b], "xo": np.ascontiguousarray(x[b, OWN * q:OWN * (q + 1)]), "ccol": col(c[b]),
            "wada": wada, "vcols": vcols, "win": np.ascontiguousarray(w_in[:, cols]), "dec": dec,
            "wout": np.ascontiguousarray(w_out[rows]), "wr": wr, "oh": oh,
            "wg": np.ascontiguousarray(wge[4 * q:4 * q + 4]), "wu": np.ascontiguousarray(wue[4 * q:4 * q + 4]),
            "wd": np.ascontiguousarray(wde[4 * q:4 * q + 4]),
            "ident": ident, "masks": masks, "rc": rc, "slotid": slotid, "tri": tri,
        })
    return maps


_NC_CACHE = {}


def kernel(**inputs):
    maps = prep(inputs)
    if "nc" not in _NC_CACHE:
        _NC_CACHE["nc"] = build()[0]
    res = run_bass_kernel_spmd(_NC_CACHE["nc"], maps, core_ids=list(range(8)))
    out = np.zeros((2, S, D), np.float32)
    for i in range(8):
        b, q = i // 4, i % 4
        out[b, OWN * q:OWN * (q + 1)] = res.results[i]["out"]
    return out
```

```python
import numpy as np
from contextlib import ExitStack
import concourse.bass as bass
import concourse.mybir as mybir
from concourse.bass_utils import run_bass_kernel_spmd

F32 = mybir.dt.float32
BF16 = mybir.dt.bfloat16
I32 = mybir.dt.int32
ALU = mybir.AluOpType
AF = mybir.ActivationFunctionType
AX = mybir.AxisListType
ENGS = ["sync", "scalar", "vector", "gpsimd", "tensor"]

S = 8192
D = 1024
NT = S // 128
OWN = 2048
CAP = 1024
LN8 = -2.0794415416798357
SLOPES = [2.0 ** (-(h + 1)) for h in range(8)]
CONFIGS = (1, 4, 16)


class Reg:
    __slots__ = ("w", "r", "name", "psum", "cw")

    def __init__(self, name="", psum=False):
        self.w = None
        self.cw = {}
        self.r = {}
        self.name = name
        self.psum = psum


class Buf:
    __slots__ = ("t", "r")

    def __init__(self, t, name):
        self.t = t
        self.r = Reg(name)


class Prog:
    def __init__(self, nc):
        self.nc = nc
        self.stack = ExitStack()
        self.ops = {e: [] for e in ENGS}
        self.cnt = {e: 0 for e in ENGS}
        self.sem = {e: self.stack.enter_context(nc.semaphore(f"c_{e}")) for e in ENGS}
        self.known = {e: {} for e in ENGS}
        self.dsem = {}
        self.dcnt = {}

    def _need(self, eng, tok, waits):
        if tok is None:
            return
        sem, val, src = tok
        if src == eng and eng == "tensor":
            return
        if src == eng and val <= self.cnt[eng] - 3:
            return
        k = self.known[eng]
        if k.get(id(sem), 0) >= val:
            return
        k[id(sem)] = val
        waits.append((sem, val))

    def _deps(self, eng, reads, writes, cwrites=()):
        waits = []
        for c in cwrites:
            self._need(eng, c.w, waits)
            for t in c.r.values():
                self._need(eng, t, waits)
        for r in reads:
            self._need(eng, r.w, waits)
            for t in r.cw.values():
                self._need(eng, t, waits)
            if r.psum:
                for t in r.r.values():
                    if t[2] != eng:
                        self._need(eng, t, waits)
        for w in writes:
            self._need(eng, w.w, waits)
            for t in w.cw.values():
                self._need(eng, t, waits)
            for t in w.r.values():
                self._need(eng, t, waits)
        best = {}
        for sem, val in waits:
            if id(sem) not in best or best[id(sem)][1] < val:
                best[id(sem)] = (sem, val)
        return list(best.values())

    def _commit(self, tok, reads, writes, cwrites=()):
        for r in reads:
            r.r[id(tok[0])] = tok
        for w in writes:
            w.w = tok
            w.cw = {}
            w.r = {}
        for c in cwrites:
            c.cw[id(tok[0])] = tok

    def op(self, eng, fn, reads=(), writes=(), cwrites=()):
        reads = [x.r if isinstance(x, Buf) else x for x in reads]
        writes = [x.r if isinstance(x, Buf) else x for x in writes]
        cwrites = [x.r if isinstance(x, Buf) else x for x in cwrites]
        waits = self._deps(eng, reads, writes, cwrites)
        self.cnt[eng] += 1
        tok = (self.sem[eng], self.cnt[eng], eng)
        self.ops[eng].append((waits, fn, (self.sem[eng], 1)))
        self._commit(tok, reads, writes, cwrites)
        return tok

    def dma(self, q, fn, key, reads=(), writes=(), inc=16):
        reads = [x.r if isinstance(x, Buf) else x for x in reads]
        writes = [x.r if isinstance(x, Buf) else x for x in writes]
        if key not in self.dsem:
            self.dsem[key] = self.stack.enter_context(self.nc.semaphore(f"d_{key}"))
            self.dcnt[key] = 0
        waits = self._deps(q, reads, writes)
        self.dcnt[key] += inc
        tok = (self.dsem[key], self.dcnt[key], "dma")
        self.ops[q].append((waits, fn, (self.dsem[key], inc)))
        self._commit(tok, reads, writes)
        return tok

    def barrier(self):
        for eng in ENGS:
            waits = []
            for e in ENGS:
                if self.cnt[e] > 0 and e != eng:
                    self._need(eng, (self.sem[e], self.cnt[e], e), waits)
            for key, sem in self.dsem.items():
                self._need(eng, (sem, self.dcnt[key], "dma"), waits)
            self.ops[eng].append((waits, None, None))

    def emit(self):
        return

    def finish(self):
        nc = self.nc
        ops = self.ops
        self.ops = {e: [] for e in ENGS}

        def replay(name, e):
            for waits, fn, inc in ops[name]:
                for sem, val in waits:
                    e.wait_ge(sem, val)
                if fn is not None:
                    fn(e).then_inc(inc[0], inc[1])

        with nc.Block() as block:
            @block.sync
            def _(e):
                replay("sync", e)

            @block.scalar
            def _(e):
                replay("scalar", e)

            @block.vector
            def _(e):
                replay("vector", e)

            @block.gpsimd
            def _(e):
                replay("gpsimd", e)

            @block.tensor
            def _(e):
                replay("tensor", e)


def build(stop_after=99, debug=False):
    import os
    NGRP = int(os.environ.get('NGRP', '16'))
    FLAGS = os.environ.get('KFLAGS', '').split(',')
    nc = bass.Bass("TRN2", target_bir_lowering=False)

    def din(name, shape, dt=F32):
        return nc.dram_tensor(name, list(shape), dt, kind="ExternalInput").ap()

    def dsc(name, shape, dt=F32):
        return nc.dram_tensor(name, list(shape), dt).ap()

    xb = din("xb", [S, D])
    xo = din("xo", [OWN, D])
    ccol = din("ccol", [128, 8])
    wada = din("wada", [D, 6 * D])
    vcols = din("vcols", [128, 80])
    win = din("win", [D, 832])
    decin = din("dec", [128, 2])
    wout = din("wout", [D, D])
    wrin = din("wr", [D, 16])
    ohin = din("oh", [128, 4])
    if stop_after >= 7:
        wg = din("wg", [4, D, 2048])
        wu = din("wu", [4, D, 2048])
        wd = din("wd", [4, 2048, D])
    identin = din("ident", [128, 128])
    masksin = din("masks", [128, 18 * 256])
    rcin = din("rc", [128, 514])
    slotin = din("slotid", [128, 8])
    triin = din("tri", [128, 128])
    out = nc.dram_tensor("out", [OWN, D], F32, kind="ExternalOutput").ap()
    dbg = {}
    if debug:
        if stop_after in (2, 3):
            dbg["yT"] = nc.dram_tensor("dbg_yT", [256, S], F32, kind="ExternalOutput").ap()
        if stop_after in (5, 6):
            dbg["x1"] = nc.dram_tensor("dbg_x1", [OWN, D], F32, kind="ExternalOutput").ap()
            dbg["aff"] = nc.dram_tensor("dbg_aff", [OWN, 16], F32, kind="ExternalOutput").ap()
            dbg["tok"] = nc.dram_tensor("dbg_tok", [128, 32], F32, kind="ExternalOutput").ap()
        if stop_after == 7:
            dbg["moe"] = nc.dram_tensor("dbg_moe", [OWN, D], F32, kind="ExternalOutput").ap()

    aq_d = dsc("aq_d", [128, S], BF16)
    ak_d = dsc("ak_d", [128, S], BF16)
    av_d = dsc("av_d", [128, S], BF16)

    h2_in = dsc("h2_in", [OWN, D], BF16)
    h2_all = [dsc(f"h2_all{c4}", [2048, D], BF16) for c4 in range(4)]
    h2_tab = dsc("h2_tab", [S, D], BF16)
    aff_in = dsc("aff_in", [OWN, 16])
    aff_all = dsc("aff_all", [S, 16])
    aff_tab = dsc("aff_tab", [S, 16])
    cdr = dsc("cdr", [4, S])
    contrib = dsc("contrib", [S, D])
    moe_d = dsc("moe_d", [OWN, D])
    R_aq, R_ak, R_av = Reg("aq_d"), Reg("ak_d"), Reg("av_d")

    R_h2in, R_h2tab = Reg("h2in"), Reg("h2tab")
    R_h2all = [Reg(f"h2all{c}") for c in range(4)]
    R_affin, R_affall, R_afftab = Reg("affin"), Reg("affall"), Reg("afftab")
    R_cdr, R_contrib, R_moe, R_out = Reg("cdr"), Reg("contrib"), Reg("moe"), Reg("out")
    RG = [[0, 1, 2, 3], [4, 5, 6, 7]] if 'half' not in FLAGS else [[0, 1, 2, 3]]

    p = Prog(nc)
    GS = p.stack

    def mk(stack, name, shape, dt, psum=False):
        if psum:
            t = stack.enter_context(nc.psum_tensor(name, [128, 512 if dt == F32 else 1024], dt))
        else:
            t = stack.enter_context(nc.sbuf_tensor(name, list(shape), dt))
        b = Buf(t, name)
        b.r.psum = psum
        return b

    V = lambda fn, r=(), w=(), cw=(): p.op("vector", fn, r, w, cw)
    A = lambda fn, r=(), w=(), cw=(): p.op("scalar", fn, r, w, cw)
    PL = lambda fn, r=(), w=(), cw=(): p.op("gpsimd", fn, r, w, cw)
    T = lambda fn, r=(), w=(), cw=(): p.op("tensor", fn, r, w, cw)

    def ld(q, dst, dst_ap, src_ap, reads=()):
        return p.dma(q, lambda e: e.dma_start(out=dst_ap, in_=src_ap), dst.r.name, reads=reads, writes=[dst])

    identf = mk(GS, "identf", [128, 128], F32)
    identb = mk(GS, "identb", [128, 128], BF16)
    onesf = mk(GS, "onesf", [128, 128], F32)
    vc = mk(GS, "vc", [128, 80], F32)
    mod = mk(GS, "mod", [128, 48], F32)
    der = mk(GS, "der", [128, 32], F32)
    ohs = mk(GS, "ohs", [128, 4], F32)
    ld("sync", identf, identf.t[:], identin)
    ld("gpsimd", identb, identb.t[:], identin)
    ld("sync", vc, vc.t[:], vcols)
    ld("sync", ohs, ohs.t[:], ohin)
    V(lambda e: e.memset(onesf.t[:], 1.0), w=[onesf])
    cst = mk(GS, "cst", [128, 4], F32)
    V(lambda e: e.memset(cst.t[:, 0:1], LN8), w=[cst])
    V(lambda e: e.memset(cst.t[:, 1:2], 1e-6), w=[cst])
    V(lambda e: e.memset(cst.t[:, 2:3], 1e-5), w=[cst])
    V(lambda e: e.memset(cst.t[:, 3:4], 0.0), w=[cst])
    ymix = ExitStack()
    yT = mk(ymix, "yT", [128, S], BF16)
    yA = mk(ymix, "yA", [64, S], BF16)
    yB = mk(ymix, "yB", [64, S], BF16)

    with ExitStack() as ph:
        cc = mk(ph, "cc", [128, 8], F32)
        scb = mk(ph, "scb", [128, 8], BF16)
        wa = [mk(ph, f"wa{i}", [128, 8, D], BF16) for i in range(2)]
        psm = mk(ph, "psm", [128, 48], F32, psum=True)
        ld("sync", cc, cc.t[:], ccol)
        A(lambda e: e.activation(out=scb.t[:], in_=cc.t[:], func=AF.Silu), r=[cc], w=[scb])
        wada_v = wada.rearrange("(k p) n -> p k n", p=128)
        for g in range(6):
            w_ = wa[g % 2]
            ld("gpsimd", w_, w_.t[:], wada_v[:, :, g * D:(g + 1) * D])
            for j in range(8):
                for k in range(8):
                    T(lambda e, w_=w_, g=g, j=j, k=k: e.matmul(
                        psm.t[:, g * 8 + j:g * 8 + j + 1], lhsT=w_.t[:, k, j * 128:(j + 1) * 128],
                        rhs=scb.t[:, k:k + 1], start=(k == 0), stop=(k == 7)), r=[w_, scb], w=[psm])
        V(lambda e: e.tensor_tensor(out=mod.t[:], in0=psm.t[:, 0:48], in1=vc.t[:, 0:48], op=ALU.add), r=[psm, vc], w=[mod])
        V(lambda e: e.scalar_tensor_tensor(out=der.t[:, 0:8], in0=mod.t[:, 8:16], scalar=1.0, in1=vc.t[:, 48:56],
                                           op0=ALU.add, op1=ALU.mult), r=[mod, vc], w=[der])
        V(lambda e: e.tensor_tensor(out=der.t[:, 8:16], in0=mod.t[:, 16:24], in1=vc.t[:, 56:64], op=ALU.mult), r=[mod, vc], w=[der])
        V(lambda e: e.scalar_tensor_tensor(out=der.t[:, 16:24], in0=mod.t[:, 32:40], scalar=1.0, in1=vc.t[:, 64:72],
                                           op0=ALU.add, op1=ALU.mult), r=[mod, vc], w=[der])
        V(lambda e: e.tensor_tensor(out=der.t[:, 24:32], in0=mod.t[:, 40:48], in1=vc.t[:, 72:80], op=ALU.mult), r=[mod, vc], w=[der])
        p.barrier()
        p.emit()

    if stop_after <= 0:
        p.finish()
        return nc, p, dbg
    with ExitStack() as ph:
        rqT = mk(ph, "rqT", [64, S], BF16)
        rkT = mk(ph, "rkT", [64, S], BF16)
        ktf = mk(ph, "ktf", [128, NT, 64], BF16)
        ktb = mk(ph, "ktb", [128, NT, 64], BF16)
        rv = mk(ph, "rv", [128, NT, 128], BF16)
        sg = mk(ph, "sg", [128, NT, 128], BF16)
        rcs = mk(ph, "rcs", [128, 514], F32)
        dcs = mk(ph, "dcs", [128, 2], F32)
        lg = mk(ph, "lg", [128, 2], F32)
        tfb = mk(ph, "tfb", [128, 4], F32)
        DT = mk(ph, "DT", [128, 128], F32)
        QF = mk(ph, "QF", [128, 128], BF16)
        QB = mk(ph, "QB", [128, 128], BF16)
        ld("sync", rcs, rcs.t[:], rcin)
        ld("sync", dcs, dcs.t[:], decin)
        A(lambda e: e.activation(out=lg.t[:], in_=dcs.t[:], func=AF.Exp, scale=-1.0), r=[dcs], w=[lg])
        V(lambda e: e.tensor_scalar(out=lg.t[:], in0=lg.t[:], scalar1=1.0, scalar2=None, op0=ALU.add), r=[lg], w=[lg])
        A(lambda e: e.activation(out=lg.t[:], in_=lg.t[:], func=AF.Ln), r=[lg], w=[lg])
        V(lambda e: e.tensor_scalar(out=lg.t[:], in0=lg.t[:], scalar1=-1.0, scalar2=None, op0=ALU.mult), r=[lg], w=[lg])
        A(lambda e: e.activation(out=tfb.t[:, 0:1], in_=rcs.t[:, 512:513], func=AF.Exp, scale=lg.t[:, 0:1], bias=cst.t[:, 0:1]), r=[rcs, lg, cst], w=[tfb])
        A(lambda e: e.activation(out=tfb.t[:, 1:2], in_=rcs.t[:, 513:514], func=AF.Exp, scale=lg.t[:, 1:2], bias=cst.t[:, 0:1]), r=[rcs, lg, cst], w=[tfb])
        A(lambda e: e.activation(out=tfb.t[:, 2:4], in_=lg.t[:, 0:2], func=AF.Exp, scale=128.0), r=[lg], w=[tfb])
        A(lambda e: e.activation(out=QF.t[:], in_=rcs.t[:, 256:384], func=AF.Exp, scale=lg.t[:, 0:1]), r=[rcs, lg], w=[QF])
        A(lambda e: e.activation(out=QB.t[:], in_=rcs.t[:, 384:512], func=AF.Exp, scale=lg.t[:, 1:2]), r=[rcs, lg], w=[QB])
        V(lambda e: e.tensor_scalar(out=DT.t[:], in0=rcs.t[:, 0:128], scalar1=lg.t[:, 0:1], scalar2=None, op0=ALU.mult), r=[rcs, lg], w=[DT])
        V(lambda e: e.scalar_tensor_tensor(out=DT.t[:], in0=rcs.t[:, 128:256], scalar=lg.t[:, 1:2], in1=DT.t[:],
                                           op0=ALU.mult, op1=ALU.add), r=[rcs, lg, DT], w=[DT])
        A(lambda e: e.activation(out=DT.t[:], in_=DT.t[:], func=AF.Exp, bias=cst.t[:, 0:1]), r=[DT, cst], w=[DT])

        if 'pre_only' in FLAGS:
            p.barrier()
            p.finish()
            return nc, p, dbg
        with ExitStack() as ph1:
            winb = mk(ph1, "winb", [128, 8, 832], BF16)
            xs = [mk(ph1, f"xs{i}", [128, D], F32) for i in range(2)]
            sqj = mk(ph1, "sqj", [128, D], BF16)
            ssq = [mk(ph1, f"ssq{i}", [128, 2], F32) for i in range(2)]
            xn = [mk(ph1, f"xn{i}", [128, D], BF16) for i in range(2)]
            h1T = [mk(ph1, f"h1T{i}", [128, 8, 512], BF16) for i in range(2)]
            stg = [[mk(ph1, f"stg{a}{i}", [128, 512], BF16) for i in range(2)] for a in range(3)]
            psXa = [mk(ph1, f"psXa{i}", None, BF16, psum=True) for i in range(2)]
            psXb = [mk(ph1, f"psXb{i}", None, BF16, psum=True) for i in range(2)]
            psF = [mk(ph1, f"psF{i}", [128, 512], F32, psum=True) for i in range(2)]
            psT = [mk(ph1, f"psT{i}", [128, 320], F32, psum=True) for i in range(2)]
            ld("gpsimd", winb, winb.t[:], win.rearrange("(k p) n -> p k n", p=128))
            zt = mk(ph1, "zt", [128, D], BF16)
            PL(lambda e: e.memset(zt.t[:], 0.0), w=[zt])
            for zi in range(64):
                p.dma("gpsimd", lambda e, zi=zi: e.dma_start(out=contrib[128 * zi:128 * (zi + 1), :], in_=zt.t[:]),
                      "contrib0", reads=[zt], writes=[R_contrib])
            xb_v = xb.rearrange("(n p) d -> p n d", p=128)
            fstate = {"f": 0}

            def prep_tile(Gi, tt, part):
                h1 = h1T[Gi % 2]
                n = 4 * Gi + tt
                x_ = xs[n % 2]
                s_ = ssq[n % 2]
                xn_ = xn[n % 2]
                pxa, pxb = psXa[n % 2], psXb[n % 2]
                if part == "a":
                  ld("sync", x_, x_.t[:], xb_v[:, n, :])
                  A(lambda e, x_=x_, s_=s_: e.activation(out=sqj.t[:], in_=x_.t[:], func=AF.Square, accum_out=s_.t[:, 0:1]),
                    r=[x_], w=[sqj, s_])
                  A(lambda e, s_=s_: e.activation(out=s_.t[:, 1:2], in_=s_.t[:, 0:1], func=AF.Sqrt, scale=1.0 / D, bias=cst.t[:, 1:2]),
                    r=[s_, cst], w=[s_])
                  V(lambda e, s_=s_: e.reciprocal(out=s_.t[:, 1:2], in_=s_.t[:, 1:2]), r=[s_], w=[s_])
                  V(lambda e, x_=x_, s_=s_, xn_=xn_: e.tensor_scalar(out=xn_.t[:], in0=x_.t[:], scalar1=s_.t[:, 1:2], scalar2=None,
                                                                   op0=ALU.mult), r=[x_, s_], w=[xn_])
                  return
                for k in range(8 if part == "t" else 0):
                    px = pxa if k % 2 == 0 else pxb
                    T(lambda e, k=k, px=px, xn_=xn_: e.transpose(out=px.t[:, (k // 2) * 128:(k // 2 + 1) * 128], in_=xn_.t[:, k * 128:(k + 1) * 128],
                                                               identity=identb.t[:]), r=[xn_, identb], w=[px])
                for k in range(8 if part == "e" else 0):
                    px = pxa if k % 2 == 0 else pxb
                    o_ = h1.t[:, k, tt * 128:(tt + 1) * 128]
                    i_ = px.t[:, (k // 2) * 128:(k // 2 + 1) * 128]
                    if k % 2 == 0:
                        A(lambda e, o_=o_, i_=i_, k=k: e.activation(out=o_, in_=i_, func=AF.Identity, scale=der.t[:, k:k + 1],
                                                                   bias=mod.t[:, k:k + 1]), r=[px, der, mod], cw=[h1])
                    else:
                        V(lambda e, o_=o_, i_=i_, k=k: e.tensor_scalar(out=o_, in0=i_, scalar1=der.t[:, k:k + 1], scalar2=mod.t[:, k:k + 1],
                                                                     op0=ALU.mult, op1=ALU.add), r=[px, der, mod], cw=[h1])

            FG = [(0, 64), (64, 64), (128, 128), (256, 128), (384, 128)]

            def mm_fgroup(Gi, fi, part):
                h1 = h1T[Gi % 2]
                c0, wdt = FG[fi]
                if part == "m":
                    fstate[(Gi, fi)] = psF[fstate["f"] % 2]
                    fstate["f"] += 1
                pf = fstate[(Gi, fi)]
                for k in range(8 if part == "m" else 0):
                    T(lambda e, pf=pf, k=k, c0=c0, wdt=wdt, h1=h1: e.matmul(pf.t[0:wdt, :], lhsT=winb.t[:, k, c0:c0 + wdt], rhs=h1.t[:, k, :],
                                                                         start=(k == 0), stop=(k == 7)), r=[winb, h1], w=[pf])
                if part == "m":
                    return
                sl = slice(Gi * 512, (Gi + 1) * 512)
                if fi == 0:
                    A(lambda e, pf=pf, sl=sl: e.copy(out=rqT.t[:, sl], in_=pf.t[0:64, :]), r=[pf], cw=[rqT])
                elif fi == 1:
                    V(lambda e, pf=pf, sl=sl: e.tensor_copy(out=rkT.t[:, sl], in_=pf.t[0:64, :]), r=[pf], cw=[rkT])
                else:
                    sb_ = stg[fi - 2][Gi % 2]
                    dr, dreg = [(aq_d, R_aq), (ak_d, R_ak), (av_d, R_av)][fi - 2]
                    if fi == 3:
                        V(lambda e, pf=pf, sb_=sb_: e.tensor_copy(out=sb_.t[:], in_=pf.t[:]), r=[pf], w=[sb_])
                    else:
                        A(lambda e, pf=pf, sb_=sb_: e.copy(out=sb_.t[:], in_=pf.t[:]), r=[pf], w=[sb_])
                    p.dma("sync", lambda e, dr=dr, sl=sl, sb_=sb_: e.dma_start(out=dr[:, sl], in_=sb_.t[:]), dreg.name,
                          reads=[sb_], writes=[dreg])

            def mm_ttile(Gi, tt, part):
                h1 = h1T[Gi % 2]
                n = 4 * Gi + tt
                pt = psT[n % 2]
                for k in range(8 if part == "m" else 0):
                    T(lambda e, pt=pt, k=k, tt=tt, h1=h1: e.matmul(pt.t[:, 0:320], lhsT=h1.t[:, k, tt * 128:(tt + 1) * 128], rhs=winb.t[:, k, 512:832],
                                                                 start=(k == 0), stop=(k == 7)), r=[winb, h1], w=[pt])
                if part == "m":
                    return
                A(lambda e, pt=pt, n=n: e.activation(out=ktf.t[:, n, :], in_=pt.t[:, 0:64], func=AF.Identity, scale=tfb.t[:, 0:1]),
                  r=[pt, tfb], cw=[ktf])
                A(lambda e, pt=pt, n=n: e.copy(out=sg.t[:, n, :], in_=pt.t[:, 192:320]), r=[pt], cw=[sg])
                V(lambda e, pt=pt, n=n: e.tensor_scalar(out=ktb.t[:, n, :], in0=pt.t[:, 0:64], scalar1=tfb.t[:, 1:2], scalar2=None,
                                                      op0=ALU.mult), r=[pt, tfb], cw=[ktb])
                V(lambda e, pt=pt, n=n: e.tensor_copy(out=rv.t[:, n, :], in_=pt.t[:, 64:192]), r=[pt], cw=[rv])

            for tt in range(4):
                prep_tile(0, tt, "a")
                prep_tile(0, tt, "t")
                prep_tile(0, tt, "e")
            for Gi in range(NGRP):
                for tt in range(4):
                    nxt = Gi + 1 < NGRP
                    if nxt:
                        prep_tile(Gi + 1, tt, "a")
                    fis = ([0, 1], [2], [3], [4])[tt]
                    for fi in fis:
                        mm_fgroup(Gi, fi, "m")
                    mm_ttile(Gi, tt, "m")
                    if nxt:
                        prep_tile(Gi + 1, tt, "t")
                    for fi in fis:
                        mm_fgroup(Gi, fi, "e")
                    mm_ttile(Gi, tt, "e")
                    if nxt:
                        prep_tile(Gi + 1, tt, "e")
            p.barrier()
            p.emit()
        if stop_after <= 1:
            p.finish()
            return nc, p, dbg

        with ExitStack() as ph2:
            Nbf = mk(ph2, "Nbf", [64, NT, 128], BF16)
            Nrun = [mk(ph2, f"Nrun{i}", [64, 128], F32) for i in range(2)]
            Prun = [mk(ph2, f"Prun{i}", [64, 128], F32) for i in range(2)]
            Pbf = [mk(ph2, f"Pbf{i}", [64, 128], BF16) for i in range(2)]
            SM = [mk(ph2, f"SM{i}", [128, 128], BF16) for i in range(2)]
            qf = [mk(ph2, f"qf{i}", [64, 128], BF16) for i in range(2)]
            qb = [mk(ph2, f"qb{i}", [64, 128], BF16) for i in range(2)]
            osq = mk(ph2, "osq", [128, 4, 128], F32)
            st4 = [mk(ph2, f"st4{i}", [128, 16], F32) for i in range(2)]
            yr = [mk(ph2, f"yr{i}", [128, 128], BF16) for i in range(2)]
            psK = [mk(ph2, f"psK{i}", [64, 128], F32, psum=True) for i in range(2)]
            psS = [mk(ph2, f"psS{i}", [128, 128], F32, psum=True) for i in range(2)]
            psO = [mk(ph2, f"psO{i}", [128, 4, 128], F32, psum=True) for i in range(2)]
            psY = [mk(ph2, f"psY{i}", [128, 128], BF16, psum=True) for i in range(2)]
            for g4 in range(4):
                A(lambda e, g4=g4: e.activation(out=sg.t[:, 16 * g4:16 * (g4 + 1), :], in_=sg.t[:, 16 * g4:16 * (g4 + 1), :], func=AF.Silu), r=[sg], w=[sg])
            V(lambda e: e.memset(Nrun[1].t[:], 0.0), w=[Nrun[1]])
            V(lambda e: e.memset(Nbf.t[:, NT - 1, :], 0.0), w=[Nbf])
            for n in range(NT - 1, 0, -1):
                pk = psK[n % 2]
                cur, nxt = Nrun[n % 2], Nrun[(n + 1) % 2]
                T(lambda e, pk=pk, n=n: e.matmul(pk.t[0:64, 0:128], lhsT=ktb.t[:, n, :], rhs=rv.t[:, n, :], start=True, stop=True), r=[ktb, rv], w=[pk])
                V(lambda e, pk=pk, cur=cur, nxt=nxt: e.scalar_tensor_tensor(out=nxt.t[:], in0=cur.t[:], scalar=tfb.t[0:64, 3:4], in1=pk.t[0:64, 0:128],
                                                                          op0=ALU.mult, op1=ALU.add), r=[cur, pk, tfb], w=[nxt])
                A(lambda e, nxt=nxt, n=n: e.copy(out=Nbf.t[:, n - 1, :], in_=nxt.t[:]), r=[nxt], w=[Nbf])
            V(lambda e: e.memset(Prun[0].t[:], 0.0), w=[Prun[0]])
            V(lambda e: e.memset(Pbf[0].t[:], 0.0), w=[Pbf[0]])
            def ret_stage1(n):
                cs = slice(n * 128, (n + 1) * 128)
                ps_ = psS[n % 2]
                sm_ = SM[n % 2]
                qf_, qb_ = qf[n % 2], qb[n % 2]
                T(lambda e, ps_=ps_, cs=cs: e.matmul(ps_.t[:, 0:128], lhsT=rkT.t[:, cs], rhs=rqT.t[:, cs], start=True, stop=True), r=[rkT, rqT], w=[ps_])
                V(lambda e, ps_=ps_, sm_=sm_: e.tensor_tensor(out=sm_.t[:], in0=ps_.t[:, 0:128], in1=DT.t[:], op=ALU.mult), r=[ps_, DT], w=[sm_])
                PL(lambda e, qf_=qf_, cs=cs: e.tensor_tensor(out=qf_.t[:], in0=rqT.t[:, cs], in1=QF.t[0:64, :], op=ALU.mult), r=[rqT, QF], w=[qf_])
                PL(lambda e, qb_=qb_, cs=cs: e.tensor_tensor(out=qb_.t[:], in0=rqT.t[:, cs], in1=QB.t[0:64, :], op=ALU.mult), r=[rqT, QB], w=[qb_])

            ret_stage1(0)
            for n in range(NT):
                if n + 1 < NT:
                    ret_stage1(n + 1)
                sm_ = SM[n % 2]
                po = psO[(n // 4) % 2]
                j4 = n % 4
                qf_, qb_ = qf[n % 2], qb[n % 2]
                pb_cur, pb_nxt = Pbf[n % 2], Pbf[(n + 1) % 2]
                pr_cur, pr_nxt = Prun[n % 2], Prun[(n + 1) % 2]
                T(lambda e, po=po, j4=j4, sm_=sm_, n=n: e.matmul(po.t[:, j4 * 128:(j4 + 1) * 128], lhsT=sm_.t[:], rhs=rv.t[:, n, :], start=True, stop=False), r=[sm_, rv], w=[po])
                T(lambda e, po=po, j4=j4, qf_=qf_, pb_cur=pb_cur: e.matmul(po.t[:, j4 * 128:(j4 + 1) * 128], lhsT=qf_.t[:], rhs=pb_cur.t[:], start=False, stop=False),
                  r=[qf_, pb_cur], w=[po])
                T(lambda e, po=po, j4=j4, qb_=qb_, n=n: e.matmul(po.t[:, j4 * 128:(j4 + 1) * 128], lhsT=qb_.t[:], rhs=Nbf.t[:, n, :], start=False, stop=True),
                  r=[qb_, Nbf], w=[po])
                if n < NT - 1:
                    pk = psK[n % 2]
                    T(lambda e, pk=pk, n=n: e.matmul(pk.t[0:64, 0:128], lhsT=ktf.t[:, n, :], rhs=rv.t[:, n, :], start=True, stop=True), r=[ktf, rv], w=[pk])
                    V(lambda e, pk=pk, pr_cur=pr_cur, pr_nxt=pr_nxt: e.scalar_tensor_tensor(out=pr_nxt.t[:], in0=pr_cur.t[:], scalar=tfb.t[0:64, 2:3],
                                                                                          in1=pk.t[0:64, 0:128], op0=ALU.mult, op1=ALU.add),
                      r=[pr_cur, pk, tfb], w=[pr_nxt])
                    A(lambda e, pr_nxt=pr_nxt, pb_nxt=pb_nxt: e.copy(out=pb_nxt.t[:], in_=pr_nxt.t[:]), r=[pr_nxt], w=[pb_nxt])
                if j4 == 3:
                    s4 = st4[(n // 4) % 2]
                    V(lambda e, po=po, s4=s4: e.tensor_reduce(out=s4.t[:, 0:4], in_=po.t[:].rearrange("p (a b) -> p a b", a=4), axis=AX.X, op=ALU.add), r=[po], w=[s4])
                    A(lambda e, po=po: e.activation(out=osq.t[:].rearrange("p a b -> p (a b)"), in_=po.t[:], func=AF.Square), r=[po], w=[osq])
                    V(lambda e, s4=s4: e.tensor_reduce(out=s4.t[:, 4:8], in_=osq.t[:], axis=AX.X, op=ALU.add), r=[osq], w=[s4])
                    V(lambda e, s4=s4: e.tensor_scalar(out=s4.t[:, 8:12], in0=s4.t[:, 0:4], scalar1=1.0 / 128, scalar2=None, op0=ALU.mult), r=[s4], w=[s4])
                    V(lambda e, s4=s4: e.tensor_tensor(out=s4.t[:, 0:4], in0=s4.t[:, 8:12], in1=s4.t[:, 8:12], op=ALU.mult), r=[s4], w=[s4])
                    V(lambda e, s4=s4: e.scalar_tensor_tensor(out=s4.t[:, 12:16], in0=s4.t[:, 4:8], scalar=1.0 / 128, in1=s4.t[:, 0:4],
                                                              op0=ALU.mult, op1=ALU.subtract), r=[s4], w=[s4])
                    A(lambda e, s4=s4: e.activation(out=s4.t[:, 12:16], in_=s4.t[:, 12:16], func=AF.Sqrt, bias=cst.t[:, 2:3]), r=[s4, cst], w=[s4])
                    V(lambda e, s4=s4: e.reciprocal(out=s4.t[:, 12:16], in_=s4.t[:, 12:16]), r=[s4], w=[s4])
                    for jj in range(4):
                        m = n - 3 + jj
                        y_ = yr[m % 2]
                        py = psY[m % 2]
                        V(lambda e, po=po, jj=jj, s4=s4, y_=y_: e.tensor_scalar(out=y_.t[:], in0=po.t[:, jj * 128:(jj + 1) * 128], scalar1=s4.t[:, 8 + jj:9 + jj],
                                                                              scalar2=s4.t[:, 12 + jj:13 + jj], op0=ALU.subtract, op1=ALU.mult),
                          r=[po, s4], w=[y_])
                        PL(lambda e, y_=y_, m=m: e.tensor_tensor(out=y_.t[:], in0=y_.t[:], in1=sg.t[:, m, :], op=ALU.mult), r=[y_, sg], w=[y_])
                        T(lambda e, py=py, y_=y_: e.transpose(out=py.t[:, 0:128], in_=y_.t[:], identity=identb.t[:]), r=[y_, identb], w=[py])
                        A(lambda e, py=py, m=m: e.copy(out=yT.t[:, m * 128:(m + 1) * 128], in_=py.t[:, 0:128]), r=[py], cw=[yT])
            p.barrier()
            p.emit()
    def tap(dst, src_ap, reads):
        p.dma("gpsimd", lambda e: e.dma_start(out=dst, in_=src_ap), "tap", reads=reads)
        p.barrier()

    if stop_after <= 2:
        if debug:
            tap(dbg["yT"][0:128, :], yT.t[:], [yT])
        p.finish()
        return nc, p, dbg
    PAD = 1024
    with ExitStack() as ph:
        aqT = mk(ph, "aqT", [128, S], BF16)
        akT = mk(ph, "akT", [128, S + 2 * PAD], BF16)
        avT = mk(ph, "avT", [128, S + 2 * PAD], BF16)
        mkb = mk(ph, "mkb", [128, 18 * 256], BF16)
        acc = [mk(ph, f"acc{h}", [65, S], F32) for h in range(2)]
        Vaug = [mk(ph, f"Vaug{i}", [128, 2, 65], BF16) for i in range(8)]
        Et = [mk(ph, f"Et{i}", [128, 256], BF16) for i in range(4)]
        NPT = 6
        PTt = [mk(ph, f"PTt{i}", [128, 256], BF16) for i in range(NPT)]
        rd = mk(ph, "rd", [65, 512], F32)
        psA = [mk(ph, f"psA{i}", None, F32, psum=True) for i in range(3)]
        psV = [mk(ph, f"psV{i}", None, BF16, psum=True) for i in range(1)]
        psB = [mk(ph, f"psB{i}", None, F32, psum=True) for i in range(3)]
        psR = mk(ph, "psR", None, F32, psum=True)
        ld("sync", aqT, aqT.t[:], aq_d, reads=[R_aq])
        ld("sync", akT, akT.t[:, PAD:PAD + S], ak_d, reads=[R_ak])
        ld("sync", avT, avT.t[:, PAD:PAD + S], av_d, reads=[R_av])
        ld("gpsimd", mkb, mkb.t[:], masksin)
        for tns in (akT, avT):
            V(lambda e, tns=tns: e.memset(tns.t[:, 0:PAD], 0.0), w=[tns])
            V(lambda e, tns=tns: e.memset(tns.t[:, PAD + S:PAD + S + PAD], 0.0), w=[tns])
        for vb in Vaug:
            V(lambda e, vb=vb: e.memset(vb.t[:, :, 64:65], 1.0), w=[vb])
        items = []
        for c, d in enumerate(CONFIGS):
            nb = (S // d) // 128
            for r in range(d):
                for m in range(nb):
                    for h in range(2):
                        items.append((c, d, r, m, h, nb))
        LAG = 3
        NV = 8
        vt = {}
        vstate = {"vi": 0}

        def ksl_(d, r, u):
            st0 = PAD + d * (128 * u - 64) + r
            return slice(st0, st0 + 127 * d + 1, d)

        def stage1(idx):
            c, d, r, m, h, nb = items[idx]
            for u in (m, m + 1):
                if (c, r, u) in vt:
                    continue
                vi = vstate["vi"]
                vstate["vi"] += 1
                vb = Vaug[vi % NV]
                pv = psV[0]
                ks = ksl_(d, r, u)
                T(lambda e, pv=pv, ks=ks: e.transpose(out=pv.t[:, 0:128], in_=avT.t[:, ks], identity=identb.t[:]), r=[avT, identb], w=[pv])
                if vi % 2 == 0:
                    A(lambda e, pv=pv, vb=vb: e.copy(out=vb.t[:, :, 0:64], in_=pv.t[:, 0:128].rearrange("p (h x) -> p h x", h=2)), r=[pv], w=[vb])
                else:
                    V(lambda e, pv=pv, vb=vb: e.tensor_copy(out=vb.t[:, :, 0:64], in_=pv.t[:, 0:128].rearrange("p (h x) -> p h x", h=2)), r=[pv], w=[vb])
                vt[(c, r, u)] = vb
            q0 = d * 128 * m + r
            qs = slice(q0, q0 + 127 * d + 1, d)
            var = 1 if m == 0 else (2 if m == nb - 1 else 0)
            rows_ = slice(64 * h, 64 * h + 64)
            pa = psA[idx % 3]
            e_ = Et[idx % 4]
            pt_ = PTt[idx % NPT]
            for j in range(2):
                ks = ksl_(d, r, m + j)
                T(lambda e, pa=pa, j=j, rows_=rows_, qs=qs, ks=ks: e.matmul(pa.t[:, j * 128:(j + 1) * 128], lhsT=akT.t[rows_, ks], rhs=aqT.t[rows_, qs],
                                                                      start=True, stop=True), r=[akT, aqT], w=[pa])
            A(lambda e, pa=pa, e_=e_: e.activation(out=e_.t[:], in_=pa.t[:, 0:256], func=AF.Exp, scale=0.125), r=[pa], w=[e_])
            mo = ((h * 3 + c) * 3 + var) * 256
            V(lambda e, e_=e_, pt_=pt_, mo=mo: e.tensor_tensor(out=pt_.t[:], in0=e_.t[:], in1=mkb.t[:, mo:mo + 256], op=ALU.mult), r=[e_, mkb], w=[pt_])

        def stage2(idx):
            c, d, r, m, h, nb = items[idx]
            q0 = d * 128 * m + r
            qs = slice(q0, q0 + 127 * d + 1, d)
            pt_ = PTt[idx % NPT]
            po = psB[idx % 3]
            for j in range(2):
                vb = vt[(c, r, m + j)]
                T(lambda e, po=po, j=j, vb=vb, pt_=pt_, h=h: e.matmul(po.t[0:65, 0:128], lhsT=vb.t[:, h, :], rhs=pt_.t[:, j * 128:(j + 1) * 128],
                                                                   start=(j == 0), stop=(j == 1)), r=[vb, pt_], w=[po])
            ac = acc[h]
            if c == 0:
                A(lambda e, ac=ac, qs=qs, po=po: e.copy(out=ac.t[:, qs], in_=po.t[0:65, 0:128]), r=[po], w=[ac])
            else:
                V(lambda e, ac=ac, qs=qs, po=po: e.tensor_tensor(out=ac.t[:, qs], in0=ac.t[:, qs], in1=po.t[0:65, 0:128], op=ALU.add), r=[po, ac], w=[ac])

        for idx in range(len(items) + LAG):
            if idx < len(items):
                stage1(idx)
            if idx - LAG >= 0:
                stage2(idx - LAG)
        for h in range(2):
            yh = (yA, yB)[h]
            ac = acc[h]
            for t in range(16):
                sl = slice(512 * t, 512 * (t + 1))
                V(lambda e, ac=ac, sl=sl: e.reciprocal(out=rd.t[64:65, :], in_=ac.t[64:65, sl]), r=[ac], w=[rd])
                T(lambda e: e.matmul(psR.t[0:64, 0:512], lhsT=onesf.t[64:65, 0:64], rhs=rd.t[64:65, :], start=True, stop=True), r=[onesf, rd], w=[psR])
                V(lambda e, yh=yh, ac=ac, sl=sl: e.tensor_tensor(out=yh.t[:, sl], in0=ac.t[0:64, sl], in1=psR.t[0:64, 0:512], op=ALU.mult),
                  r=[ac, psR], w=[yh])
        p.barrier()
    if stop_after <= 3:
        if debug:
            tap(dbg["yT"][0:128, :], yT.t[:], [yT])
            tap(dbg["yT"][128:192, :], yA.t[:], [yA])
            tap(dbg["yT"][192:256, :], yB.t[:], [yB])
        p.finish()
        return nc, p, dbg

    y_in = [dsc(f"y_in{c}", [256, OWN], BF16) for c in range(4)]
    y_all = [dsc(f"y_all{c}", [1024, OWN], BF16) for c in range(4)]
    R_yin = [Reg(f"y_in{c}") for c in range(4)]
    R_yall = [Reg(f"y_all{c}") for c in range(4)]
    for c4 in range(4):
        sl = slice(OWN * c4, OWN * (c4 + 1))
        p.dma("sync", lambda e, c4=c4, sl=sl: e.dma_start(out=y_in[c4][0:128, :], in_=yT.t[:, sl]), f"y_in{c4}", reads=[yT], writes=[R_yin[c4]])
        p.dma("sync", lambda e, c4=c4, sl=sl: e.dma_start(out=y_in[c4][128:192, :], in_=yA.t[:, sl]), f"y_in{c4}", reads=[yA], writes=[R_yin[c4]])
        p.dma("sync", lambda e, c4=c4, sl=sl: e.dma_start(out=y_in[c4][192:256, :], in_=yB.t[:, sl]), f"y_in{c4}", reads=[yB], writes=[R_yin[c4]])
        p.dma("gpsimd", lambda e, c4=c4: e.collective_compute("AllGather", ALU.bypass, replica_groups=RG, ins=[y_in[c4]], outs=[y_all[c4]]),
              f"agy_{c4}", reads=[R_yin[c4]], writes=[R_yall[c4]], inc=1)
    p.barrier()
    ymix.close()
    if stop_after <= 4:
        p.finish()
        return nc, p, dbg
    x1_d = dsc("x1_d", [OWN, D])
    R_x1 = Reg("x1_d")
    with ExitStack() as ph58:
        rows = {k: mk(ph58, f"row_{k}", [128, D], F32) for k in ("gm", "gsf", "shf", "gf")}
        toki = mk(ph58, "toki", [128, 32], I32)
        with ExitStack() as ph:
            diag = [mk(ph, f"diag{i}", [128, 128], F32) for i in range(2)]
            psD = [mk(ph, f"psD{i}", None, F32, psum=True) for i in range(2)]
            srcs = {"gm": der.t[:, 8:16], "gsf": der.t[:, 16:24], "shf": mod.t[:, 24:32], "gf": der.t[:, 24:32]}
            di = 0
            for key in ("gm", "gsf", "shf", "gf"):
                for half in range(2):
                    pd = psD[half]
                    for kk in range(4):
                        k = half * 4 + kk
                        dg = diag[di % 2]
                        di += 1
                        V(lambda e, dg=dg, key=key, k=k: e.tensor_scalar(out=dg.t[:], in0=identf.t[:], scalar1=srcs[key][:, k:k + 1], scalar2=None,
                                                                       op0=ALU.mult), r=[identf, der, mod], w=[dg])
                        T(lambda e, pd=pd, kk=kk, dg=dg: e.matmul(pd.t[:, kk * 128:(kk + 1) * 128], lhsT=onesf.t[:], rhs=dg.t[:], start=True, stop=True),
                          r=[onesf, dg], w=[pd])
                    A(lambda e, pd=pd, key=key, half=half: e.copy(out=rows[key].t[:, half * 512:(half + 1) * 512], in_=pd.t[:, 0:512]),
                      r=[pd], w=[rows[key]])
            wrs = mk(ph, "wrs", [128, 8, 16], F32)
            ld("sync", wrs, wrs.t[:], wrin.rearrange("(k p) e -> p k e", p=128))
            wob = mk(ph, "wob", [128, 8, D], BF16)
            ld("gpsimd", wob, wob.t[:], wout.rearrange("(k p) n -> p k n", p=128))
            yown = mk(ph, "yown", [128, 8, OWN], BF16)
            ytmp = [mk(ph, f"ytmp{i}", [128, 8, OWN], BF16) for i in range(1)]
            ohb = mk(ph, "ohb", [128, 4], BF16)
            V(lambda e: e.tensor_copy(out=ohb.t[:], in_=ohs.t[:]), r=[ohs], w=[ohb])
            for c4 in range(4):
                yt_ = ytmp[0]
                ld("sync", yt_, yt_.t[:], y_all[c4].rearrange("(k p) t -> p k t", p=128), reads=[R_yall[c4]])
                for kq in range(4):
                    ks_ = slice(2 * kq, 2 * kq + 2)
                    eng = V
                    if c4 == 0:
                        eng(lambda e, yt_=yt_, ks_=ks_: e.tensor_scalar(out=yown.t[:, ks_, :], in0=yt_.t[:, ks_, :], scalar1=ohs.t[:, 0:1], scalar2=None,
                                                                        op0=ALU.mult), r=[yt_, ohs], cw=[yown])
                    else:
                        eng(lambda e, yt_=yt_, ks_=ks_, c4=c4: e.scalar_tensor_tensor(out=yown.t[:, ks_, :], in0=yt_.t[:, ks_, :], scalar=ohs.t[:, c4:c4 + 1],
                                                                                      in1=yown.t[:, ks_, :], op0=ALU.mult, op1=ALU.add),
                            r=[yt_, ohs], cw=[yown])
            psM = [mk(ph, f"psM{i}", None, F32, psum=True) for i in range(2)]
            RD5 = 3
            mx = [mk(ph, f"mx{i}", [128, D], F32) for i in range(RD5)]
            xo_t = [mk(ph, f"xo{i}", [128, D], F32) for i in range(RD5)]
            x1t = [mk(ph, f"x1t{i}", [128, D], F32) for i in range(RD5)]
            h2t = [mk(ph, f"h2t{i}", [128, D], F32) for i in range(RD5)]
            h2T = [mk(ph, f"h2T{i}", [128, 8, 128], F32) for i in range(RD5)]
            sq2 = mk(ph, "sq2", [128, D], BF16)
            ss5 = [mk(ph, f"ss5{i}", [128, 8], F32) for i in range(RD5)]
            afft = [mk(ph, f"afft{i}", [128, 16], F32) for i in range(2)]
            ext = [mk(ph, f"ext{i}", [128, 16], F32) for i in range(2)]
            psH = [mk(ph, f"psH{i}", None, F32, psum=True) for i in range(2)]
            psL = [mk(ph, f"psL{i}", None, F32, psum=True) for i in range(2)]
            lgall = mk(ph, "lgall", [128, 16, 16], F32)
            smx = mk(ph, "smx", [128, 32], F32)
            def p5_a(n):
                ts = slice(128 * n, 128 * (n + 1))
                m_, xo_, x1_, h2_, hT_, s5 = mx[n % RD5], xo_t[n % RD5], x1t[n % RD5], h2t[n % RD5], h2T[n % RD5], ss5[n % RD5]
                for half in range(2):
                    pm = psM[half]
                    for kc in range(8):
                        T(lambda e, pm=pm, kc=kc, ts=ts, half=half: e.matmul(pm.t[:, 0:512], lhsT=yown.t[:, kc, ts], rhs=wob.t[:, kc, half * 512:(half + 1) * 512],
                                                                           start=(kc == 0), stop=(kc == 7)), r=[yown, wob], w=[pm])
                    if half == 0:
                        A(lambda e, pm=pm, m_=m_: e.copy(out=m_.t[:, 0:512], in_=pm.t[:, 0:512]), r=[pm], cw=[m_])
                    else:
                        V(lambda e, pm=pm, m_=m_: e.tensor_copy(out=m_.t[:, 512:1024], in_=pm.t[:, 0:512]), r=[pm], cw=[m_])
                ld("sync", xo_, xo_.t[:], xo[ts, :])
                A(lambda e, m_=m_, s5=s5: e.activation(out=sq2.t[:], in_=m_.t[:], func=AF.Square, accum_out=s5.t[:, 0:1]), r=[m_], w=[sq2, s5])
                A(lambda e, s5=s5: e.activation(out=s5.t[:, 1:2], in_=s5.t[:, 0:1], func=AF.Sqrt, scale=1.0 / D, bias=cst.t[:, 1:2]), r=[s5, cst], w=[s5])
                V(lambda e, s5=s5: e.reciprocal(out=s5.t[:, 1:2], in_=s5.t[:, 1:2]), r=[s5], w=[s5])
                V(lambda e, m_=m_, s5=s5: e.scalar_tensor_tensor(out=m_.t[:], in0=m_.t[:], scalar=s5.t[:, 1:2], in1=rows["gm"].t[:],
                                                               op0=ALU.mult, op1=ALU.mult), r=[m_, s5, rows["gm"]], w=[m_])
                PL(lambda e, m_=m_, xo_=xo_, x1_=x1_: e.tensor_tensor(out=x1_.t[:], in0=xo_.t[:], in1=m_.t[:], op=ALU.add), r=[m_, xo_], w=[x1_])
                p.dma("sync", lambda e, ts=ts, x1_=x1_: e.dma_start(out=x1_d[ts, :], in_=x1_.t[:]), "x1_d", reads=[x1_], writes=[R_x1])

            def p5_b(n):
                ts = slice(128 * n, 128 * (n + 1))
                m_, xo_, x1_, h2_, hT_, s5 = mx[n % RD5], xo_t[n % RD5], x1t[n % RD5], h2t[n % RD5], h2T[n % RD5], ss5[n % RD5]
                A(lambda e, x1_=x1_, s5=s5: e.activation(out=sq2.t[:], in_=x1_.t[:], func=AF.Square, accum_out=s5.t[:, 2:3]), r=[x1_], w=[sq2, s5])
                A(lambda e, s5=s5: e.activation(out=s5.t[:, 3:4], in_=s5.t[:, 2:3], func=AF.Sqrt, scale=1.0 / D, bias=cst.t[:, 1:2]), r=[s5, cst], w=[s5])
                V(lambda e, s5=s5: e.reciprocal(out=s5.t[:, 3:4], in_=s5.t[:, 3:4]), r=[s5], w=[s5])
                V(lambda e, x1_=x1_, s5=s5, h2_=h2_: e.scalar_tensor_tensor(out=h2_.t[:], in0=x1_.t[:], scalar=s5.t[:, 3:4], in1=rows["gsf"].t[:],
                                                                          op0=ALU.mult, op1=ALU.mult), r=[x1_, s5, rows["gsf"]], w=[h2_])
                PL(lambda e, h2_=h2_: e.tensor_tensor(out=h2_.t[:], in0=h2_.t[:], in1=rows["shf"].t[:], op=ALU.add), r=[h2_, rows["shf"]], w=[h2_])
                p.dma("gpsimd", lambda e, ts=ts, h2_=h2_: e.dma_start(out=h2_in[ts, :], in_=h2_.t[:]), "h2_in", reads=[h2_], writes=[R_h2in])
                if n % 4 == 3:
                    c4 = n // 4
                    p.dma("gpsimd", lambda e, c4=c4: e.collective_compute("AllGather", ALU.bypass, replica_groups=RG,
                                                                          ins=[h2_in[512 * c4:512 * (c4 + 1), :]], outs=[h2_all[c4]]),
                          f"ag1_{c4}", reads=[R_h2in], writes=[R_h2all[c4]], inc=1)
                    for r4 in range(4):
                        p.dma("sync", lambda e, c4=c4, r4=r4: e.dma_start(out=h2_tab[2048 * r4 + 512 * c4:2048 * r4 + 512 * (c4 + 1), :],
                                                                          in_=h2_all[c4][512 * r4:512 * (r4 + 1), :]),
                              "h2tab", reads=[R_h2all[c4]], writes=[R_h2tab])

            def p5_c(n):
                ts = slice(128 * n, 128 * (n + 1))
                m_, xo_, x1_, h2_, hT_, s5 = mx[n % RD5], xo_t[n % RD5], x1t[n % RD5], h2t[n % RD5], h2T[n % RD5], ss5[n % RD5]
                for k in range(8):
                    ph_ = psH[k // 4]
                    T(lambda e, ph_=ph_, k=k, h2_=h2_: e.transpose(out=ph_.t[:, (k % 4) * 128:(k % 4 + 1) * 128], in_=h2_.t[:, k * 128:(k + 1) * 128],
                                                                 identity=identf.t[:]), r=[h2_, identf], w=[ph_])
                A(lambda e, hT_=hT_: e.copy(out=hT_.t[:, 0:4, :], in_=psH[0].t[:, 0:512].rearrange("p (k s) -> p k s", k=4)), r=[psH[0]], w=[hT_])
                V(lambda e, hT_=hT_: e.tensor_copy(out=hT_.t[:, 4:8, :], in_=psH[1].t[:, 0:512].rearrange("p (k s) -> p k s", k=4)), r=[psH[1]], w=[hT_])
                pl = psL[n % 2]
                for k in range(8):
                    T(lambda e, pl=pl, k=k, hT_=hT_: e.matmul(pl.t[:, 0:16], lhsT=hT_.t[:, k, :], rhs=wrs.t[:, k, :], start=(k == 0), stop=(k == 7)),
                      r=[hT_, wrs], w=[pl])
                V(lambda e, pl=pl, n=n: e.tensor_copy(out=lgall.t[:, n, :], in_=pl.t[:, 0:16]), r=[pl], cw=[lgall])

            for step in range(16 + 2):
                if step < 16:
                    p5_a(step)
                if 0 <= step - 1 < 16:
                    p5_b(step - 1)
                if 0 <= step - 2 < 16:
                    p5_c(step - 2)
            V(lambda e: e.tensor_reduce(out=smx.t[:, 0:16], in_=lgall.t[:], axis=AX.X, op=ALU.max), r=[lgall], w=[smx])
            for n in range(16):
                V(lambda e, n=n: e.tensor_scalar(out=lgall.t[:, n, :], in0=lgall.t[:, n, :], scalar1=smx.t[:, n:n + 1], scalar2=None, op0=ALU.subtract),
                  r=[lgall, smx], w=[lgall])
            A(lambda e: e.activation(out=lgall.t[:].rearrange("p a b -> p (a b)"), in_=lgall.t[:].rearrange("p a b -> p (a b)"), func=AF.Exp),
              r=[lgall], w=[lgall])
            V(lambda e: e.tensor_reduce(out=smx.t[:, 16:32], in_=lgall.t[:], axis=AX.X, op=ALU.add), r=[lgall], w=[smx])
            V(lambda e: e.reciprocal(out=smx.t[:, 16:32], in_=smx.t[:, 16:32]), r=[smx], w=[smx])
            for n in range(16):
                V(lambda e, n=n: e.tensor_scalar(out=lgall.t[:, n, :], in0=lgall.t[:, n, :], scalar1=smx.t[:, 16 + n:17 + n], scalar2=None, op0=ALU.mult),
                  r=[lgall, smx], w=[lgall])
            p.dma("sync", lambda e: e.dma_start(out=aff_in.rearrange("(n p) e -> p n e", p=128), in_=lgall.t[:]), "aff_in", reads=[lgall], writes=[R_affin])
            p.dma("gpsimd", lambda e: e.collective_compute("AllGather", ALU.bypass, replica_groups=RG, ins=[aff_in], outs=[aff_all]),
                  "ag2", reads=[R_affin], writes=[R_affall], inc=1)
            p.dma("sync", lambda e: e.dma_start(out=aff_tab, in_=aff_all), "afftab", reads=[R_affall], writes=[R_afftab])
            p.barrier()
        if stop_after <= 5:
            if debug:
                tap(dbg["x1"], x1_d, [R_x1])
                tap(dbg["aff"], aff_in, [R_affin])
            p.finish()
            return nc, p, dbg

        with ExitStack() as ph:
            Aall = mk(ph, "Aall", [128, 64, 16], F32)
            A4 = mk(ph, "A4", [128, 4, 64], F32)
            cmp_ = mk(ph, "cmp", [128, 4, 64], F32)
            cc0 = mk(ph, "cc0", [128, 4, 64], F32)
            cc1 = mk(ph, "cc1", [128, 4, 64], F32)
            lo = mk(ph, "lo", [128, 4], F32)
            mid = mk(ph, "mid", [128, 4], F32)
            cnt = mk(ph, "cnt", [128, 4], F32)
            ge = mk(ph, "ge", [128, 4], F32)
            offs = mk(ph, "offs", [128, 4], F32)
            tris = mk(ph, "tris", [128, 128], F32)
            slot = mk(ph, "slot", [128, 8], F32)
            tokf = mk(ph, "tokf", [128, 32], F32)
            cb = [mk(ph, f"cb{i}", [128, S], F32) for i in range(2)]
            junk = mk(ph, "junk", [128, S], BF16)
            psC = mk(ph, "psC", None, F32, psum=True)
            ld("sync", Aall, Aall.t[:], aff_tab.rearrange("(p j) e -> p j e", j=64), reads=[R_afftab])
            ld("sync", tris, tris.t[:], triin)
            ld("sync", slot, slot.t[:], slotin)
            for i in range(4):
                V(lambda e, i=i: e.tensor_scalar(out=A4.t[:, i, :], in0=Aall.t[:, :, i], scalar1=ohs.t[:, 0:1], scalar2=None, op0=ALU.mult),
                  r=[Aall, ohs], w=[A4])
                for r in range(1, 4):
                    V(lambda e, i=i, r=r: e.scalar_tensor_tensor(out=A4.t[:, i, :], in0=Aall.t[:, :, 4 * r + i], scalar=ohs.t[:, r:r + 1], in1=A4.t[:, i, :],
                                                                 op0=ALU.mult, op1=ALU.add), r=[Aall, ohs, A4], w=[A4])
            V(lambda e: e.memset(lo.t[:], 0.0), w=[lo])
            for it in range(26):
                wv = 2.0 ** (-(it + 1))
                V(lambda e, wv=wv: e.tensor_scalar(out=mid.t[:], in0=lo.t[:], scalar1=wv, scalar2=None, op0=ALU.add), r=[lo], w=[mid])
                V(lambda e: e.memset(cnt.t[:], 0.0), w=[cnt])
                for i in range(4):
                    V(lambda e, i=i: e.tensor_scalar(out=cmp_.t[:, i, :], in0=A4.t[:, i, :], scalar1=mid.t[:, i:i + 1], scalar2=0.0, op0=ALU.is_gt,
                                                    op1=ALU.add, accum_out=cnt.t[:, i:i + 1]), r=[A4, mid, cnt], w=[cmp_, cnt])
                T(lambda e: e.matmul(psC.t[:, 0:4], lhsT=onesf.t[:], rhs=cnt.t[:], start=True, stop=True), r=[onesf, cnt], w=[psC])
                V(lambda e: e.tensor_scalar(out=ge.t[:], in0=psC.t[:, 0:4], scalar1=CAP - 0.5, scalar2=None, op0=ALU.is_ge), r=[psC], w=[ge])
                V(lambda e, wv=wv: e.scalar_tensor_tensor(out=lo.t[:], in0=ge.t[:], scalar=wv, in1=lo.t[:], op0=ALU.mult, op1=ALU.add), r=[ge, lo], w=[lo])
            for i in range(4):
                V(lambda e, i=i: e.tensor_scalar(out=cc0.t[:, i, :], in0=A4.t[:, i, :], scalar1=lo.t[:, i:i + 1], scalar2=None, op0=ALU.is_gt),
                  r=[A4, lo], w=[cc0])
            ca, cbuf = cc0, cc1
            for sh in (1, 2, 4, 8, 16, 32):
                V(lambda e, ca=ca, cbuf=cbuf, sh=sh: e.tensor_tensor(out=cbuf.t[:, :, sh:64], in0=ca.t[:, :, sh:64], in1=ca.t[:, :, 0:64 - sh], op=ALU.add),
                  r=[ca], w=[cbuf])
                V(lambda e, ca=ca, cbuf=cbuf, sh=sh: e.tensor_copy(out=cbuf.t[:, :, 0:sh], in_=ca.t[:, :, 0:sh]), r=[ca], w=[cbuf])
                ca, cbuf = cbuf, ca
            V(lambda e, ca=ca: e.tensor_copy(out=cnt.t[:], in_=ca.t[:, :, 63]), r=[ca], w=[cnt])
            T(lambda e: e.matmul(psC.t[:, 0:4], lhsT=tris.t[:], rhs=cnt.t[:], start=True, stop=True), r=[tris, cnt], w=[psC])
            V(lambda e: e.tensor_copy(out=offs.t[:], in_=psC.t[:, 0:4]), r=[psC], w=[offs])
            cdr_v = cdr.rearrange("e (p j) -> e p j", j=64)
            for i in range(4):
                V(lambda e, i=i, ca=ca: e.tensor_scalar(out=ca.t[:, i, :], in0=ca.t[:, i, :], scalar1=offs.t[:, i:i + 1], scalar2=None, op0=ALU.add),
                  r=[ca, offs], w=[ca])
                p.dma("sync", lambda e, i=i, ca=ca: e.dma_start(out=cdr_v[i], in_=ca.t[:, i, :]), "cdr", reads=[ca], writes=[R_cdr])
            V(lambda e: e.memset(tokf.t[:], 0.0), w=[tokf])
            for i in range(4):
                cb_ = cb[i % 2]
                ld("sync", cb_, cb_.t[:], cdr[i:i + 1, :].partition_broadcast(128), reads=[R_cdr])
                for st in range(8):
                    col = i * 8 + st
                    V(lambda e, cb_=cb_, st=st, col=col: e.tensor_scalar(out=junk.t[:], in0=cb_.t[:], scalar1=slot.t[:, st:st + 1], scalar2=0.0,
                                                                       op0=ALU.is_le, op1=ALU.add, accum_out=tokf.t[:, col:col + 1]),
                      r=[cb_, slot, tokf], w=[junk, tokf])
            V(lambda e: e.tensor_copy(out=toki.t[:], in_=tokf.t[:]), r=[tokf], w=[toki])
            if debug and stop_after == 6:
                tap(dbg["tok"], tokf.t[:], [tokf])
            p.barrier()
        if stop_after <= 6:
            if debug:
                tap(dbg["x1"], x1_d, [R_x1])
                tap(dbg["aff"], aff_in, [R_affin])
            p.finish()
            return nc, p, dbg

        with ExitStack() as ph:
            xeT = [mk(ph, f"xeT{i}", [128, 8, CAP], BF16) for i in range(2)]
            hT = mk(ph, "hT", [128, 16, CAP], BF16)
            wdb = [mk(ph, f"wdb{i}", [128, 16, D], BF16) for i in range(2)]
            NPC = 8
            wgb = [mk(ph, f"wgb{i}", [128, 8, 256], BF16) for i in range(3)]
            wub = [mk(ph, f"wub{i}", [128, 8, 256], BF16) for i in range(3)]
            xet = [mk(ph, f"xet{i}", [128, D], BF16) for i in range(2)]
            gat = [mk(ph, f"gat{i}", [128, 16], F32) for i in range(2)]
            gate = [mk(ph, f"gate{i}", [128, 8], F32) for i in range(2)]
            yet = [mk(ph, f"yet{i}", [128, D], F32) for i in range(2)]
            sgt = [mk(ph, f"sgt{i}", [128, 512], BF16) for i in range(2)]
            psXT = mk(ph, "psXT", None, BF16, psum=True)
            psG = [mk(ph, f"psG{i}", None, F32, psum=True) for i in range(2)]
            psU = [mk(ph, f"psU{i}", None, F32, psum=True) for i in range(2)]
            psY = [mk(ph, f"psYd{i}", None, F32, psum=True) for i in range(2)]
            cnts = {"g": 0, "pc": 0, "m": 0, "y": 0}

            def load_piece(i, pc):
                ws = (i * NPC + pc) % 3
                wg_v = wg[i].rearrange("(k p) f -> p k f", p=128)
                wu_v = wu[i].rearrange("(k p) f -> p k f", p=128)
                ld("gpsimd", wgb[ws], wgb[ws].t[:], wg_v[:, :, pc * 256:(pc + 1) * 256])
                ld("gpsimd", wub[ws], wub[ws].t[:], wu_v[:, :, pc * 256:(pc + 1) * 256])

            def gather_expert(i):
                xT = xeT[i % 2]
                gt_ = gate[i % 2]
                for st in range(8):
                    col = i * 8 + st
                    xe_ = xet[cnts["g"] % 2]
                    ga_ = gat[cnts["g"] % 2]
                    cnts["g"] += 1
                    p.dma("gpsimd", lambda e, xe_=xe_, col=col: e.indirect_dma_start(
                        out=xe_.t[:], out_offset=None, in_=h2_tab, in_offset=bass.IndirectOffsetOnAxis(ap=toki.t[:, col:col + 1], axis=0)),
                        xe_.r.name, reads=[R_h2tab, toki], writes=[xe_])
                    p.dma("gpsimd", lambda e, ga_=ga_, col=col: e.indirect_dma_start(
                        out=ga_.t[:], out_offset=None, in_=aff_tab, in_offset=bass.IndirectOffsetOnAxis(ap=toki.t[:, col:col + 1], axis=0)),
                        ga_.r.name, reads=[R_afftab, toki], writes=[ga_])
                    V(lambda e, ga_=ga_, gt_=gt_, st=st, i=i: e.tensor_scalar(out=gt_.t[:, st:st + 1], in0=ga_.t[:, i:i + 1], scalar1=ohs.t[:, 0:1], scalar2=None,
                                                                            op0=ALU.mult), r=[ga_, ohs], w=[gt_])
                    for r in range(1, 4):
                        V(lambda e, ga_=ga_, gt_=gt_, st=st, i=i, r=r: e.scalar_tensor_tensor(
                            out=gt_.t[:, st:st + 1], in0=ga_.t[:, 4 * r + i:4 * r + i + 1], scalar=ohs.t[:, r:r + 1], in1=gt_.t[:, st:st + 1],
                            op0=ALU.mult, op1=ALU.add), r=[ga_, ohs, gt_], w=[gt_])
                    for k in range(8):
                        T(lambda e, k=k, xe_=xe_: e.transpose(out=psXT.t[:, k * 128:(k + 1) * 128], in_=xe_.t[:, k * 128:(k + 1) * 128], identity=identb.t[:]),
                          r=[xe_, identb], w=[psXT])
                    if st % 2 == 0:
                        A(lambda e, st=st, xT=xT: e.copy(out=xT.t[:, :, st * 128:(st + 1) * 128], in_=psXT.t[:].rearrange("p (k s) -> p k s", k=8)),
                          r=[psXT], cw=[xT])
                    else:
                        V(lambda e, st=st, xT=xT: e.tensor_copy(out=xT.t[:, :, st * 128:(st + 1) * 128], in_=psXT.t[:].rearrange("p (k s) -> p k s", k=8)),
                          r=[psXT], cw=[xT])

            ld("gpsimd", wdb[0], wdb[0].t[:], wd[0].rearrange("(k p) d -> p k d", p=128))
            gather_expert(0)
            load_piece(0, 0)
            load_piece(0, 1)
            for i in range(4):
                xT = xeT[i % 2]
                gt_ = gate[i % 2]
                wd_ = wdb[i % 2]
                for pc in range(NPC):
                    if pc + 2 < NPC:
                        load_piece(i, pc + 2)
                    ws = (i * NPC + pc) % 3
                    wg_, wu_ = wgb[ws], wub[ws]
                    for fc in range(2):
                        f = pc * 2 + fc
                        for half in range(2):
                            pg, pu, sg_ = psG[cnts["m"] % 2], psU[cnts["m"] % 2], sgt[cnts["m"] % 2]
                            cnts["m"] += 1
                            hs = slice(512 * half, 512 * (half + 1))
                            for k in range(8):
                                T(lambda e, pg=pg, k=k, wg_=wg_, fc=fc, hs=hs, xT=xT: e.matmul(pg.t[:, 0:512], lhsT=wg_.t[:, k, fc * 128:(fc + 1) * 128], rhs=xT.t[:, k, hs],
                                                                                           start=(k == 0), stop=(k == 7)), r=[wg_, xT], w=[pg])
                            for k in range(8):
                                T(lambda e, pu=pu, k=k, wu_=wu_, fc=fc, hs=hs, xT=xT: e.matmul(pu.t[:, 0:512], lhsT=wu_.t[:, k, fc * 128:(fc + 1) * 128], rhs=xT.t[:, k, hs],
                                                                                           start=(k == 0), stop=(k == 7)), r=[wu_, xT], w=[pu])
                            A(lambda e, pg=pg, sg_=sg_: e.activation(out=sg_.t[:], in_=pg.t[:, 0:512], func=AF.Silu), r=[pg], w=[sg_])
                            V(lambda e, pu=pu, sg_=sg_, f=f, hs=hs: e.tensor_tensor(out=hT.t[:, f, hs], in0=sg_.t[:], in1=pu.t[:, 0:512], op=ALU.mult),
                              r=[sg_, pu], cw=[hT])
                if i + 1 < 4:
                    ld("gpsimd", wdb[(i + 1) % 2], wdb[(i + 1) % 2].t[:], wd[i + 1].rearrange("(k p) d -> p k d", p=128))
                    gather_expert(i + 1)
                    load_piece(i + 1, 0)
                    load_piece(i + 1, 1)
                for st in range(8):
                    col = i * 8 + st
                    ye_ = yet[st % 2]
                    for dh in range(2):
                        py = psY[cnts["y"] % 2]
                        cnts["y"] += 1
                        ds_ = slice(512 * dh, 512 * (dh + 1))
                        for f in range(16):
                            T(lambda e, py=py, f=f, st=st, ds_=ds_, wd_=wd_: e.matmul(py.t[:, 0:512], lhsT=hT.t[:, f, st * 128:(st + 1) * 128], rhs=wd_.t[:, f, ds_],
                                                                                   start=(f == 0), stop=(f == 15)), r=[hT, wd_], w=[py])
                        if dh == 0:
                            A(lambda e, py=py, ye_=ye_, ds_=ds_, gt_=gt_, st=st: e.activation(out=ye_.t[:, ds_], in_=py.t[:, 0:512], func=AF.Identity,
                                                                                           scale=gt_.t[:, st:st + 1]), r=[py, gt_], cw=[ye_])
                        else:
                            V(lambda e, py=py, ye_=ye_, ds_=ds_, gt_=gt_, st=st: e.tensor_scalar(out=ye_.t[:, ds_], in0=py.t[:, 0:512], scalar1=gt_.t[:, st:st + 1],
                                                                                              scalar2=None, op0=ALU.mult), r=[py, gt_], cw=[ye_])
                    p.dma("gpsimd", lambda e, ye_=ye_, col=col: e.indirect_dma_start(
                        out=contrib, out_offset=bass.IndirectOffsetOnAxis(ap=toki.t[:, col:col + 1], axis=0), in_=ye_.t[:], in_offset=None,
                        compute_op=ALU.add), "scat", reads=[ye_, toki, R_contrib], writes=[R_contrib])
            p.dma("gpsimd", lambda e: e.collective_compute("ReduceScatter", ALU.add, replica_groups=RG, ins=[contrib], outs=[moe_d]),
                  "rs2", reads=[R_contrib], writes=[R_moe], inc=1)
            p.barrier()
        if debug and stop_after == 7:
            tap(dbg["moe"], moe_d, [R_moe])

        with ExitStack() as ph:
            mo = [mk(ph, f"mo{i}", [128, D], F32) for i in range(2)]
            x1r = [mk(ph, f"x1r{i}", [128, D], F32) for i in range(2)]
            sq8 = mk(ph, "sq8", [128, D], BF16)
            s8 = [mk(ph, f"s8{i}", [128, 2], F32) for i in range(2)]
            for n in range(16):
                ts = slice(128 * n, 128 * (n + 1))
                m_, x_, s_ = mo[n % 2], x1r[n % 2], s8[n % 2]
                ld("sync", m_, m_.t[:], moe_d[ts, :], reads=[R_moe])
                ld("sync", x_, x_.t[:], x1_d[ts, :], reads=[R_x1])
                A(lambda e, m_=m_, s_=s_: e.activation(out=sq8.t[:], in_=m_.t[:], func=AF.Square, accum_out=s_.t[:, 0:1]), r=[m_], w=[sq8, s_])
                A(lambda e, s_=s_: e.activation(out=s_.t[:, 1:2], in_=s_.t[:, 0:1], func=AF.Sqrt, scale=1.0 / D, bias=cst.t[:, 1:2]), r=[s_, cst], w=[s_])
                V(lambda e, s_=s_: e.reciprocal(out=s_.t[:, 1:2], in_=s_.t[:, 1:2]), r=[s_], w=[s_])
                V(lambda e, m_=m_, s_=s_: e.scalar_tensor_tensor(out=m_.t[:], in0=m_.t[:], scalar=s_.t[:, 1:2], in1=rows["gf"].t[:],
                                                               op0=ALU.mult, op1=ALU.mult), r=[m_, s_, rows["gf"]], w=[m_])
                PL(lambda e, m_=m_, x_=x_: e.tensor_tensor(out=m_.t[:], in0=m_.t[:], in1=x_.t[:], op=ALU.add), r=[m_, x_], w=[m_])
                p.dma("sync", lambda e, ts=ts, m_=m_: e.dma_start(out=out[ts, :], in_=m_.t[:]), "out", reads=[m_], writes=[R_out])
            p.barrier()
    p.finish()
    return nc, p, dbg


def _consts(q):
    j = np.arange(128, dtype=np.float32)[:, None]
    i = np.arange(128, dtype=np.float32)[None, :]
    rc = np.zeros((128, 514), np.float32)
    rc[:, 0:128] = np.maximum(i - j, 0)
    rc[:, 128:256] = np.maximum(j - i, 0)
    rc[:, 256:384] = i + 1
    rc[:, 384:512] = 128 - i
    rc[:, 512] = 127 - j[:, 0]
    rc[:, 513] = j[:, 0]
    masks = np.zeros((128, 2, 3, 3, 2, 128), np.float32)
    kk = np.arange(128)[:, None, None]
    jj = np.arange(2)[None, :, None]
    ii = np.arange(128)[None, None, :]
    delta = np.abs(kk + 128 * jj - 64 - ii).astype(np.float32)
    band = (delta <= 64).astype(np.float32)
    for h in range(2):
        slope = SLOPES[2 * q + h]
        for c, d in enumerate(CONFIGS):
            m = band * np.exp(-slope * d * delta)
            masks[:, h, c, 0] = m
            m1 = m.copy()
            m1[0:64, 0, :] = 0
            masks[:, h, c, 1] = m1
            m2 = m.copy()
            m2[64:128, 1, :] = 0
            masks[:, h, c, 2] = m2
    slotid = (128 * np.arange(8)[None, :] + np.arange(128)[:, None]).astype(np.float32)
    tri = (np.arange(128)[:, None] < np.arange(128)[None, :]).astype(np.float32)
    return rc, masks.reshape(128, 18 * 256), slotid, tri


def prep(inputs):
    f = lambda a: np.ascontiguousarray(np.asarray(a, dtype=np.float32))
    x, c = f(inputs["x"]), f(inputs["c"])
    w_in = f(inputs["w_in"])[0]
    w_out = f(inputs["w_out"])[0]
    col = lambda v: np.ascontiguousarray(v.reshape(-1, 128).T)
    vcols = np.concatenate([col(f(inputs["b_ada"])[0]), col(f(inputs["g_pre_mix"])[0]), col(f(inputs["g_post_mix"])[0]),
                            col(f(inputs["g_pre_ffn"])[0]), col(f(inputs["g_post_ffn"])[0])], axis=1)
    wada = f(inputs["w_ada"])[0]
    wr = f(inputs["w_router"])[0]
    wge, wue, wde = f(inputs["w_gate_e"])[0], f(inputs["w_up_e"])[0], f(inputs["w_down_e"])[0]
    df, db = f(inputs["ret_decay_fwd"])[0], f(inputs["ret_decay_bwd"])[0]
    ident = np.eye(128, dtype=np.float32)
    maps = []
    for i in range(8):
        b, q = i // 4, i % 4
        rq = np.arange(64 * q, 64 * q + 64)
        rk = 256 + rq
        rvc = 512 + np.arange(128 * q, 128 * q + 128)
        rgc = 1024 + np.arange(128 * q, 128 * q + 128)
        aqc = 1536 + np.arange(128 * q, 128 * q + 128)
        akc = 2048 + np.arange(128 * q, 128 * q + 128)
        avc = 2560 + np.arange(128 * q, 128 * q + 128)
        cols = np.concatenate([rq, rk, aqc, akc, avc, rk, rvc, rgc])
        rows = np.concatenate([np.concatenate([np.arange(128 * r_, 128 * r_ + 128), 512 + np.arange(128 * r_, 128 * r_ + 128)]) for r_ in range(4)])
        rc, masks, slotid, tri = _consts(q)
        oh = np.zeros((128, 4), np.float32)
        oh[:, q] = 1.0
        dec = np.zeros((128, 2), np.float32)
        dec[:, 0] = df[q]
        dec[:, 1] = db[q]
        maps.append({
            "xb": x[b], "xo": np.ascontiguousarray(x[b, OWN * q:OWN * (q + 1)]), "ccol": col(c[b]),
            "wada": wada, "vcols": vcols, "win": np.ascontiguousarray(w_in[:, cols]), "dec": dec,
            "wout": np.ascontiguousarray(w_out[rows]), "wr": wr, "oh": oh,
            "wg": np.ascontiguousarray(wge[4 * q:4 * q + 4]), "wu": np.ascontiguousarray(wue[4 * q:4 * q + 4]),
            "wd": np.ascontiguousarray(wde[4 * q:4 * q + 4]),
            "ident": ident, "masks": masks, "rc": rc, "slotid": slotid, "tri": tri,
        })
    return maps


_NC_CACHE = {}


def kernel(**inputs):
    maps = prep(inputs)
    if "nc" not in _NC_CACHE:
        _NC_CACHE["nc"] = build()[0]
    res = run_bass_kernel_spmd(_NC_CACHE["nc"], maps, core_ids=list(range(8)))
    out = np.zeros((2, S, D), np.float32)
    for i in range(8):
        b, q = i // 4, i % 4
        out[b, OWN * q:OWN * (q + 1)] = res.results[i]["out"]
    return out
```

```python
import numpy as np
from contextlib import ExitStack
import concourse.bass as bass
import concourse.mybir as mybir
from concourse.bass_utils import run_bass_kernel_spmd

F32 = mybir.dt.float32
BF16 = mybir.dt.bfloat16
I32 = mybir.dt.int32
ALU = mybir.AluOpType
AF = mybir.ActivationFunctionType
AX = mybir.AxisListType
ENGS = ["sync", "scalar", "vector", "gpsimd", "tensor"]

S = 8192
D = 1024
NT = S // 128
OWN = 2048
CAP = 1024
LN8 = -2.0794415416798357
SLOPES = [2.0 ** (-(h + 1)) for h in range(8)]
CONFIGS = (1, 4, 16)


class Reg:
    __slots__ = ("w", "r", "name", "psum", "cw")

    def __init__(self, name="", psum=False):
        self.w = None
        self.cw = {}
        self.r = {}
        self.name = name
        self.psum = psum


class Buf:
    __slots__ = ("t", "r")

    def __init__(self, t, name):
        self.t = t
        self.r = Reg(name)


class Prog:
    def __init__(self, nc):
        self.nc = nc
        self.stack = ExitStack()
        self.ops = {e: [] for e in ENGS}
        self.cnt = {e: 0 for e in ENGS}
        self.sem = {e: self.stack.enter_context(nc.semaphore(f"c_{e}")) for e in ENGS}
        self.known = {e: {} for e in ENGS}
        self.dsem = {}
        self.dcnt = {}

    def _need(self, eng, tok, waits):
        if tok is None:
            return
        sem, val, src = tok
        if src == eng and eng == "tensor":
            return
        if src == eng and val <= self.cnt[eng] - 3:
            return
        k = self.known[eng]
        if k.get(id(sem), 0) >= val:
            return
        k[id(sem)] = val
        waits.append((sem, val))

    def _deps(self, eng, reads, writes, cwrites=()):
        waits = []
        for c in cwrites:
            self._need(eng, c.w, waits)
            for t in c.r.values():
                self._need(eng, t, waits)
        for r in reads:
            self._need(eng, r.w, waits)
            for t in r.cw.values():
                self._need(eng, t, waits)
            if r.psum:
                for t in r.r.values():
                    if t[2] != eng:
                        self._need(eng, t, waits)
        for w in writes:
            self._need(eng, w.w, waits)
            for t in w.cw.values():
                self._need(eng, t, waits)
            for t in w.r.values():
                self._need(eng, t, waits)
        best = {}
        for sem, val in waits:
            if id(sem) not in best or best[id(sem)][1] < val:
                best[id(sem)] = (sem, val)
        return list(best.values())

    def _commit(self, tok, reads, writes, cwrites=()):
        for r in reads:
            r.r[id(tok[0])] = tok
        for w in writes:
            w.w = tok
            w.cw = {}
            w.r = {}
        for c in cwrites:
            c.cw[id(tok[0])] = tok

    def op(self, eng, fn, reads=(), writes=(), cwrites=()):
        reads = [x.r if isinstance(x, Buf) else x for x in reads]
        writes = [x.r if isinstance(x, Buf) else x for x in writes]
        cwrites = [x.r if isinstance(x, Buf) else x for x in cwrites]
        waits = self._deps(eng, reads, writes, cwrites)
        self.cnt[eng] += 1
        tok = (self.sem[eng], self.cnt[eng], eng)
        self.ops[eng].append((waits, fn, (self.sem[eng], 1)))
        self._commit(tok, reads, writes, cwrites)
        return tok

    def dma(self, q, fn, key, reads=(), writes=(), inc=16):
        reads = [x.r if isinstance(x, Buf) else x for x in reads]
        writes = [x.r if isinstance(x, Buf) else x for x in writes]
        if key not in self.dsem:
            self.dsem[key] = self.stack.enter_context(self.nc.semaphore(f"d_{key}"))
            self.dcnt[key] = 0
        waits = self._deps(q, reads, writes)
        self.dcnt[key] += inc
        tok = (self.dsem[key], self.dcnt[key], "dma")
        self.ops[q].append((waits, fn, (self.dsem[key], inc)))
        self._commit(tok, reads, writes)
        return tok

    def barrier(self):
        for eng in ENGS:
            waits = []
            for e in ENGS:
                if self.cnt[e] > 0 and e != eng:
                    self._need(eng, (self.sem[e], self.cnt[e], e), waits)
            for key, sem in self.dsem.items():
                self._need(eng, (sem, self.dcnt[key], "dma"), waits)
            self.ops[eng].append((waits, None, None))

    def emit(self):
        return

    def finish(self):
        nc = self.nc
        ops = self.ops
        self.ops = {e: [] for e in ENGS}

        def replay(name, e):
            for waits, fn, inc in ops[name]:
                for sem, val in waits:
                    e.wait_ge(sem, val)
                if fn is not None:
                    fn(e).then_inc(inc[0], inc[1])

        with nc.Block() as block:
            @block.sync
            def _(e):
                replay("sync", e)

            @block.scalar
            def _(e):
                replay("scalar", e)

            @block.vector
            def _(e):
                replay("vector", e)

            @block.gpsimd
            def _(e):
                replay("gpsimd", e)

            @block.tensor
            def _(e):
                replay("tensor", e)


def build(stop_after=99, debug=False):
    import os
    NGRP = int(os.environ.get('NGRP', '16'))
    FLAGS = os.environ.get('KFLAGS', '').split(',')
    nc = bass.Bass("TRN2", target_bir_lowering=False)

    def din(name, shape, dt=F32):
        return nc.dram_tensor(name, list(shape), dt, kind="ExternalInput").ap()

    def dsc(name, shape, dt=F32):
        return nc.dram_tensor(name, list(shape), dt).ap()

    xb = din("xb", [S, D])
    xo = din("xo", [OWN, D])
    ccol = din("ccol", [128, 8])
    wada = din("wada", [D, 6 * D])
    vcols = din("vcols", [128, 80])
    win = din("win", [D, 832])
    decin = din("dec", [128, 2])
    wout = din("wout", [D, D])
    wrin = din("wr", [D, 16])
    ohin = din("oh", [128, 4])
    if stop_after >= 7:
        wg = din("wg", [4, D, 2048])
        wu = din("wu", [4, D, 2048])
        wd = din("wd", [4, 2048, D])
    identin = din("ident", [128, 128])
    masksin = din("masks", [128, 18 * 256])
    rcin = din("rc", [128, 514])
    slotin = din("slotid", [128, 8])
    triin = din("tri", [128, 128])
    out = nc.dram_tensor("out", [OWN, D], F32, kind="ExternalOutput").ap()
    dbg = {}
    if debug:
        if stop_after in (2, 3):
            dbg["yT"] = nc.dram_tensor("dbg_yT", [256, S], F32, kind="ExternalOutput").ap()
        if stop_after in (5, 6):
            dbg["x1"] = nc.dram_tensor("dbg_x1", [OWN, D], F32, kind="ExternalOutput").ap()
            dbg["aff"] = nc.dram_tensor("dbg_aff", [OWN, 16], F32, kind="ExternalOutput").ap()
            dbg["tok"] = nc.dram_tensor("dbg_tok", [128, 32], F32, kind="ExternalOutput").ap()
        if stop_after == 7:
            dbg["moe"] = nc.dram_tensor("dbg_moe", [OWN, D], F32, kind="ExternalOutput").ap()

    aq_d = dsc("aq_d", [128, S], BF16)
    ak_d = dsc("ak_d", [128, S], BF16)
    av_d = dsc("av_d", [128, S], BF16)

    h2_in = dsc("h2_in", [OWN, D], BF16)
    h2_all = [dsc(f"h2_all{c4}", [2048, D], BF16) for c4 in range(4)]
    h2_tab = dsc("h2_tab", [S, D], BF16)
    aff_in = dsc("aff_in", [OWN, 16])
    aff_all = dsc("aff_all", [S, 16])
    aff_tab = dsc("aff_tab", [S, 16])
    cdr = dsc("cdr", [4, S])
    contrib = dsc("contrib", [S, D])
    moe_d = dsc("moe_d", [OWN, D])
    R_aq, R_ak, R_av = Reg("aq_d"), Reg("ak_d"), Reg("av_d")

    R_h2in, R_h2tab = Reg("h2in"), Reg("h2tab")
    R_h2all = [Reg(f"h2all{c}") for c in range(4)]
    R_affin, R_affall, R_afftab = Reg("affin"), Reg("affall"), Reg("afftab")
    R_cdr, R_contrib, R_moe, R_out = Reg("cdr"), Reg("contrib"), Reg("moe"), Reg("out")
    RG = [[0, 1, 2, 3], [4, 5, 6, 7]] if 'half' not in FLAGS else [[0, 1, 2, 3]]

    p = Prog(nc)
    GS = p.stack

    def mk(stack, name, shape, dt, psum=False):
        if psum:
            t = stack.enter_context(nc.psum_tensor(name, [128, 512 if dt == F32 else 1024], dt))
        else:
            t = stack.enter_context(nc.sbuf_tensor(name, list(shape), dt))
        b = Buf(t, name)
        b.r.psum = psum
        return b

    V = lambda fn, r=(), w=(), cw=(): p.op("vector", fn, r, w, cw)
    A = lambda fn, r=(), w=(), cw=(): p.op("scalar", fn, r, w, cw)
    PL = lambda fn, r=(), w=(), cw=(): p.op("gpsimd", fn, r, w, cw)
    T = lambda fn, r=(), w=(), cw=(): p.op("tensor", fn, r, w, cw)

    def ld(q, dst, dst_ap, src_ap, reads=()):
        return p.dma(q, lambda e: e.dma_start(out=dst_ap, in_=src_ap), dst.r.name, reads=reads, writes=[dst])

    identf = mk(GS, "identf", [128, 128], F32)
    identb = mk(GS, "identb", [128, 128], BF16)
    onesf = mk(GS, "onesf", [128, 128], F32)
    vc = mk(GS, "vc", [128, 80], F32)
    mod = mk(GS, "mod", [128, 48], F32)
    der = mk(GS, "der", [128, 32], F32)
    ohs = mk(GS, "ohs", [128, 4], F32)
    ld("sync", identf, identf.t[:], identin)
    ld("gpsimd", identb, identb.t[:], identin)
    ld("sync", vc, vc.t[:], vcols)
    ld("sync", ohs, ohs.t[:], ohin)
    V(lambda e: e.memset(onesf.t[:], 1.0), w=[onesf])
    cst = mk(GS, "cst", [128, 4], F32)
    V(lambda e: e.memset(cst.t[:, 0:1], LN8), w=[cst])
    V(lambda e: e.memset(cst.t[:, 1:2], 1e-6), w=[cst])
    V(lambda e: e.memset(cst.t[:, 2:3], 1e-5), w=[cst])
    V(lambda e: e.memset(cst.t[:, 3:4], 0.0), w=[cst])
    ymix = ExitStack()
    yT = mk(ymix, "yT", [128, S], BF16)
    yA = mk(ymix, "yA", [64, S], BF16)
    yB = mk(ymix, "yB", [64, S], BF16)

    with ExitStack() as ph:
        cc = mk(ph, "cc", [128, 8], F32)
        scb = mk(ph, "scb", [128, 8], BF16)
        wa = [mk(ph, f"wa{i}", [128, 8, D], BF16) for i in range(2)]
        psm = mk(ph, "psm", [128, 48], F32, psum=True)
        ld("sync", cc, cc.t[:], ccol)
        A(lambda e: e.activation(out=scb.t[:], in_=cc.t[:], func=AF.Silu), r=[cc], w=[scb])
        wada_v = wada.rearrange("(k p) n -> p k n", p=128)
        for g in range(6):
            w_ = wa[g % 2]
            ld("gpsimd", w_, w_.t[:], wada_v[:, :, g * D:(g + 1) * D])
            for j in range(8):
                for k in range(8):
                    T(lambda e, w_=w_, g=g, j=j, k=k: e.matmul(
                        psm.t[:, g * 8 + j:g * 8 + j + 1], lhsT=w_.t[:, k, j * 128:(j + 1) * 128],
                        rhs=scb.t[:, k:k + 1], start=(k == 0), stop=(k == 7)), r=[w_, scb], w=[psm])
        V(lambda e: e.tensor_tensor(out=mod.t[:], in0=psm.t[:, 0:48], in1=vc.t[:, 0:48], op=ALU.add), r=[psm, vc], w=[mod])
        V(lambda e: e.scalar_tensor_tensor(out=der.t[:, 0:8], in0=mod.t[:, 8:16], scalar=1.0, in1=vc.t[:, 48:56],
                                           op0=ALU.add, op1=ALU.mult), r=[mod, vc], w=[der])
        V(lambda e: e.tensor_tensor(out=der.t[:, 8:16], in0=mod.t[:, 16:24], in1=vc.t[:, 56:64], op=ALU.mult), r=[mod, vc], w=[der])
        V(lambda e: e.scalar_tensor_tensor(out=der.t[:, 16:24], in0=mod.t[:, 32:40], scalar=1.0, in1=vc.t[:, 64:72],
                                           op0=ALU.add, op1=ALU.mult), r=[mod, vc], w=[der])
        V(lambda e: e.tensor_tensor(out=der.t[:, 24:32], in0=mod.t[:, 40:48], in1=vc.t[:, 72:80], op=ALU.mult), r=[mod, vc], w=[der])
        p.barrier()
        p.emit()

    if stop_after <= 0:
        p.finish()
        return nc, p, dbg
    with ExitStack() as ph:
        rqT = mk(ph, "rqT", [64, S], BF16)
        rkT = mk(ph, "rkT", [64, S], BF16)
        ktf = mk(ph, "ktf", [128, NT, 64], BF16)
        ktb = mk(ph, "ktb", [128, NT, 64], BF16)
        rv = mk(ph, "rv", [128, NT, 128], BF16)
        sg = mk(ph, "sg", [128, NT, 128], BF16)
        rcs = mk(ph, "rcs", [128, 514], F32)
        dcs = mk(ph, "dcs", [128, 2], F32)
        lg = mk(ph, "lg", [128, 2], F32)
        tfb = mk(ph, "tfb", [128, 4], F32)
        DT = mk(ph, "DT", [128, 128], F32)
        QF = mk(ph, "QF", [128, 128], BF16)
        QB = mk(ph, "QB", [128, 128], BF16)
        ld("sync", rcs, rcs.t[:], rcin)
        ld("sync", dcs, dcs.t[:], decin)
        A(lambda e: e.activation(out=lg.t[:], in_=dcs.t[:], func=AF.Exp, scale=-1.0), r=[dcs], w=[lg])
        V(lambda e: e.tensor_scalar(out=lg.t[:], in0=lg.t[:], scalar1=1.0, scalar2=None, op0=ALU.add), r=[lg], w=[lg])
        A(lambda e: e.activation(out=lg.t[:], in_=lg.t[:], func=AF.Ln), r=[lg], w=[lg])
        V(lambda e: e.tensor_scalar(out=lg.t[:], in0=lg.t[:], scalar1=-1.0, scalar2=None, op0=ALU.mult), r=[lg], w=[lg])
        A(lambda e: e.activation(out=tfb.t[:, 0:1], in_=rcs.t[:, 512:513], func=AF.Exp, scale=lg.t[:, 0:1], bias=cst.t[:, 0:1]), r=[rcs, lg, cst], w=[tfb])
        A(lambda e: e.activation(out=tfb.t[:, 1:2], in_=rcs.t[:, 513:514], func=AF.Exp, scale=lg.t[:, 1:2], bias=cst.t[:, 0:1]), r=[rcs, lg, cst], w=[tfb])
        A(lambda e: e.activation(out=tfb.t[:, 2:4], in_=lg.t[:, 0:2], func=AF.Exp, scale=128.0), r=[lg], w=[tfb])
        A(lambda e: e.activation(out=QF.t[:], in_=rcs.t[:, 256:384], func=AF.Exp, scale=lg.t[:, 0:1]), r=[rcs, lg], w=[QF])
        A(lambda e: e.activation(out=QB.t[:], in_=rcs.t[:, 384:512], func=AF.Exp, scale=lg.t[:, 1:2]), r=[rcs, lg], w=[QB])
        V(lambda e: e.tensor_scalar(out=DT.t[:], in0=rcs.t[:, 0:128], scalar1=lg.t[:, 0:1], scalar2=None, op0=ALU.mult), r=[rcs, lg], w=[DT])
        V(lambda e: e.scalar_tensor_tensor(out=DT.t[:], in0=rcs.t[:, 128:256], scalar=lg.t[:, 1:2], in1=DT.t[:],
                                           op0=ALU.mult, op1=ALU.add), r=[rcs, lg, DT], w=[DT])
        A(lambda e: e.activation(out=DT.t[:], in_=DT.t[:], func=AF.Exp, bias=cst.t[:, 0:1]), r=[DT, cst], w=[DT])

        if 'pre_only' in FLAGS:
            p.barrier()
            p.finish()
            return nc, p, dbg
        with ExitStack() as ph1:
            winb = mk(ph1, "winb", [128, 8, 832], BF16)
            xs = [mk(ph1, f"xs{i}", [128, D], F32) for i in range(2)]
            sqj = mk(ph1, "sqj", [128, D], BF16)
            ssq = [mk(ph1, f"ssq{i}", [128, 2], F32) for i in range(2)]
            xn = [mk(ph1, f"xn{i}", [128, D], BF16) for i in range(2)]
            h1T = [mk(ph1, f"h1T{i}", [128, 8, 512], BF16) for i in range(2)]
            stg = [[mk(ph1, f"stg{a}{i}", [128, 512], BF16) for i in range(2)] for a in range(3)]
            psXa = [mk(ph1, f"psXa{i}", None, BF16, psum=True) for i in range(2)]
            psXb = [mk(ph1, f"psXb{i}", None, BF16, psum=True) for i in range(2)]
            psF = [mk(ph1, f"psF{i}", [128, 512], F32, psum=True) for i in range(2)]
            psT = [mk(ph1, f"psT{i}", [128, 320], F32, psum=True) for i in range(2)]
            ld("gpsimd", winb, winb.t[:], win.rearrange("(k p) n -> p k n", p=128))
            zt = mk(ph1, "zt", [128, D], BF16)
            PL(lambda e: e.memset(zt.t[:], 0.0), w=[zt])
            for zi in range(64):
                p.dma("gpsimd", lambda e, zi=zi: e.dma_start(out=contrib[128 * zi:128 * (zi + 1), :], in_=zt.t[:]),
                      "contrib0", reads=[zt], writes=[R_contrib])
            xb_v = xb.rearrange("(n p) d -> p n d", p=128)
            fstate = {"f": 0}

            def prep_tile(Gi, tt, part):
                h1 = h1T[Gi % 2]
                n = 4 * Gi + tt
                x_ = xs[n % 2]
                s_ = ssq[n % 2]
                xn_ = xn[n % 2]
                pxa, pxb = psXa[n % 2], psXb[n % 2]
                if part == "a":
                  ld("sync", x_, x_.t[:], xb_v[:, n, :])
                  A(lambda e, x_=x_, s_=s_: e.activation(out=sqj.t[:], in_=x_.t[:], func=AF.Square, accum_out=s_.t[:, 0:1]),
                    r=[x_], w=[sqj, s_])
                  A(lambda e, s_=s_: e.activation(out=s_.t[:, 1:2], in_=s_.t[:, 0:1], func=AF.Sqrt, scale=1.0 / D, bias=cst.t[:, 1:2]),
                    r=[s_, cst], w=[s_])
                  V(lambda e, s_=s_: e.reciprocal(out=s_.t[:, 1:2], in_=s_.t[:, 1:2]), r=[s_], w=[s_])
                  V(lambda e, x_=x_, s_=s_, xn_=xn_: e.tensor_scalar(out=xn_.t[:], in0=x_.t[:], scalar1=s_.t[:, 1:2], scalar2=None,
                                                                   op0=ALU.mult), r=[x_, s_], w=[xn_])
                  return
                for k in range(8 if part == "t" else 0):
                    px = pxa if k % 2 == 0 else pxb
                    T(lambda e, k=k, px=px, xn_=xn_: e.transpose(out=px.t[:, (k // 2) * 128:(k // 2 + 1) * 128], in_=xn_.t[:, k * 128:(k + 1) * 128],
                                                               identity=identb.t[:]), r=[xn_, identb], w=[px])
                for k in range(8 if part == "e" else 0):
                    px = pxa if k % 2 == 0 else pxb
                    o_ = h1.t[:, k, tt * 128:(tt + 1) * 128]
                    i_ = px.t[:, (k // 2) * 128:(k // 2 + 1) * 128]
                    if k % 2 == 0:
                        A(lambda e, o_=o_, i_=i_, k=k: e.activation(out=o_, in_=i_, func=AF.Identity, scale=der.t[:, k:k + 1],
                                                                   bias=mod.t[:, k:k + 1]), r=[px, der, mod], cw=[h1])
                    else:
                        V(lambda e, o_=o_, i_=i_, k=k: e.tensor_scalar(out=o_, in0=i_, scalar1=der.t[:, k:k + 1], scalar2=mod.t[:, k:k + 1],
                                                                     op0=ALU.mult, op1=ALU.add), r=[px, der, mod], cw=[h1])

            FG = [(0, 64), (64, 64), (128, 128), (256, 128), (384, 128)]

            def mm_fgroup(Gi, fi, part):
                h1 = h1T[Gi % 2]
                c0, wdt = FG[fi]
                if part == "m":
                    fstate[(Gi, fi)] = psF[fstate["f"] % 2]
                    fstate["f"] += 1
                pf = fstate[(Gi, fi)]
                for k in range(8 if part == "m" else 0):
                    T(lambda e, pf=pf, k=k, c0=c0, wdt=wdt, h1=h1: e.matmul(pf.t[0:wdt, :], lhsT=winb.t[:, k, c0:c0 + wdt], rhs=h1.t[:, k, :],
                                                                         start=(k == 0), stop=(k == 7)), r=[winb, h1], w=[pf])
                if part == "m":
                    return
                sl = slice(Gi * 512, (Gi + 1) * 512)
                if fi == 0:
                    A(lambda e, pf=pf, sl=sl: e.copy(out=rqT.t[:, sl], in_=pf.t[0:64, :]), r=[pf], cw=[rqT])
                elif fi == 1:
                    V(lambda e, pf=pf, sl=sl: e.tensor_copy(out=rkT.t[:, sl], in_=pf.t[0:64, :]), r=[pf], cw=[rkT])
                else:
                    sb_ = stg[fi - 2][Gi % 2]
                    dr, dreg = [(aq_d, R_aq), (ak_d, R_ak), (av_d, R_av)][fi - 2]
                    if fi == 3:
                        V(lambda e, pf=pf, sb_=sb_: e.tensor_copy(out=sb_.t[:], in_=pf.t[:]), r=[pf], w=[sb_])
                    else:
                        A(lambda e, pf=pf, sb_=sb_: e.copy(out=sb_.t[:], in_=pf.t[:]), r=[pf], w=[sb_])
                    p.dma("sync", lambda e, dr=dr, sl=sl, sb_=sb_: e.dma_start(out=dr[:, sl], in_=sb_.t[:]), dreg.name,
                          reads=[sb_], writes=[dreg])

            def mm_ttile(Gi, tt, part):
                h1 = h1T[Gi % 2]
                n = 4 * Gi + tt
                pt = psT[n % 2]
                for k in range(8 if part == "m" else 0):
                    T(lambda e, pt=pt, k=k, tt=tt, h1=h1: e.matmul(pt.t[:, 0:320], lhsT=h1.t[:, k, tt * 128:(tt + 1) * 128], rhs=winb.t[:, k, 512:832],
                                                                 start=(k == 0), stop=(k == 7)), r=[winb, h1], w=[pt])
                if part == "m":
                    return
                A(lambda e, pt=pt, n=n: e.activation(out=ktf.t[:, n, :], in_=pt.t[:, 0:64], func=AF.Identity, scale=tfb.t[:, 0:1]),
                  r=[pt, tfb], cw=[ktf])
                A(lambda e, pt=pt, n=n: e.copy(out=sg.t[:, n, :], in_=pt.t[:, 192:320]), r=[pt], cw=[sg])
                V(lambda e, pt=pt, n=n: e.tensor_scalar(out=ktb.t[:, n, :], in0=pt.t[:, 0:64], scalar1=tfb.t[:, 1:2], scalar2=None,
                                                      op0=ALU.mult), r=[pt, tfb], cw=[ktb])
                V(lambda e, pt=pt, n=n: e.tensor_copy(out=rv.t[:, n, :], in_=pt.t[:, 64:192]), r=[pt], cw=[rv])

            for tt in range(4):
                prep_tile(0, tt, "a")
                prep_tile(0, tt, "t")
                prep_tile(0, tt, "e")
            for Gi in range(NGRP):
                for tt in range(4):
                    nxt = Gi + 1 < NGRP
                    if nxt:
                        prep_tile(Gi + 1, tt, "a")
                    fis = ([0, 1], [2], [3], [4])[tt]
                    for fi in fis:
                        mm_fgroup(Gi, fi, "m")
                    mm_ttile(Gi, tt, "m")
                    if nxt:
                        prep_tile(Gi + 1, tt, "t")
                    for fi in fis:
                        mm_fgroup(Gi, fi, "e")
                    mm_ttile(Gi, tt, "e")
                    if nxt:
                        prep_tile(Gi + 1, tt, "e")
            p.barrier()
            p.emit()
        if stop_after <= 1:
            p.finish()
            return nc, p, dbg

        with ExitStack() as ph2:
            Nbf = mk(ph2, "Nbf", [64, NT, 128], BF16)
            Nrun = [mk(ph2, f"Nrun{i}", [64, 128], F32) for i in range(2)]
            Prun = [mk(ph2, f"Prun{i}", [64, 128], F32) for i in range(2)]
            Pbf = [mk(ph2, f"Pbf{i}", [64, 128], BF16) for i in range(2)]
            SM = [mk(ph2, f"SM{i}", [128, 128], BF16) for i in range(2)]
            qf = [mk(ph2, f"qf{i}", [64, 128], BF16) for i in range(2)]
            qb = [mk(ph2, f"qb{i}", [64, 128], BF16) for i in range(2)]
            osq = mk(ph2, "osq", [128, 4, 128], F32)
            st4 = [mk(ph2, f"st4{i}", [128, 16], F32) for i in range(2)]
            yr = [mk(ph2, f"yr{i}", [128, 128], BF16) for i in range(2)]
            psK = [mk(ph2, f"psK{i}", [64, 128], F32, psum=True) for i in range(2)]
            psS = [mk(ph2, f"psS{i}", [128, 128], F32, psum=True) for i in range(2)]
            psO = [mk(ph2, f"psO{i}", [128, 4, 128], F32, psum=True) for i in range(2)]
            psY = [mk(ph2, f"psY{i}", [128, 128], BF16, psum=True) for i in range(2)]
            for g4 in range(4):
                A(lambda e, g4=g4: e.activation(out=sg.t[:, 16 * g4:16 * (g4 + 1), :], in_=sg.t[:, 16 * g4:16 * (g4 + 1), :], func=AF.Silu), r=[sg], w=[sg])
            V(lambda e: e.memset(Nrun[1].t[:], 0.0), w=[Nrun[1]])
            V(lambda e: e.memset(Nbf.t[:, NT - 1, :], 0.0), w=[Nbf])
            for n in range(NT - 1, 0, -1):
                pk = psK[n % 2]
                cur, nxt = Nrun[n % 2], Nrun[(n + 1) % 2]
                T(lambda e, pk=pk, n=n: e.matmul(pk.t[0:64, 0:128], lhsT=ktb.t[:, n, :], rhs=rv.t[:, n, :], start=True, stop=True), r=[ktb, rv], w=[pk])
                V(lambda e, pk=pk, cur=cur, nxt=nxt: e.scalar_tensor_tensor(out=nxt.t[:], in0=cur.t[:], scalar=tfb.t[0:64, 3:4], in1=pk.t[0:64, 0:128],
                                                                          op0=ALU.mult, op1=ALU.add), r=[cur, pk, tfb], w=[nxt])
                V(lambda e, pk=pk, cur=cur, n=n: e.scalar_tensor_tensor(out=Nbf.t[:, n - 1, :], in0=cur.t[:], scalar=tfb.t[0:64, 3:4], in1=pk.t[0:64, 0:128],
                                                                     op0=ALU.mult, op1=ALU.add), r=[cur, pk, tfb], cw=[Nbf])
            V(lambda e: e.memset(Prun[0].t[:], 0.0), w=[Prun[0]])
            V(lambda e: e.memset(Pbf[0].t[:], 0.0), w=[Pbf[0]])
            def ret_stage1(n):
                cs = slice(n * 128, (n + 1) * 128)
                ps_ = psS[n % 2]
                sm_ = SM[n % 2]
                qf_, qb_ = qf[n % 2], qb[n % 2]
                T(lambda e, ps_=ps_, cs=cs: e.matmul(ps_.t[:, 0:128], lhsT=rkT.t[:, cs], rhs=rqT.t[:, cs], start=True, stop=True), r=[rkT, rqT], w=[ps_])
                V(lambda e, ps_=ps_, sm_=sm_: e.tensor_tensor(out=sm_.t[:], in0=ps_.t[:, 0:128], in1=DT.t[:], op=ALU.mult), r=[ps_, DT], w=[sm_])
                PL(lambda e, qf_=qf_, cs=cs: e.tensor_tensor(out=qf_.t[:], in0=rqT.t[:, cs], in1=QF.t[0:64, :], op=ALU.mult), r=[rqT, QF], w=[qf_])
                PL(lambda e, qb_=qb_, cs=cs: e.tensor_tensor(out=qb_.t[:], in0=rqT.t[:, cs], in1=QB.t[0:64, :], op=ALU.mult), r=[rqT, QB], w=[qb_])

            ret_stage1(0)
            for n in range(NT):
                if n + 1 < NT:
                    ret_stage1(n + 1)
                sm_ = SM[n % 2]
                po = psO[(n // 4) % 2]
                j4 = n % 4
                qf_, qb_ = qf[n % 2], qb[n % 2]
                pb_cur, pb_nxt = Pbf[n % 2], Pbf[(n + 1) % 2]
                pr_cur, pr_nxt = Prun[n % 2], Prun[(n + 1) % 2]
                if n < NT - 1:
                    pk = psK[n % 2]
                    T(lambda e, pk=pk, n=n: e.matmul(pk.t[0:64, 0:128], lhsT=ktf.t[:, n, :], rhs=rv.t[:, n, :], start=True, stop=True), r=[ktf, rv], w=[pk])
                    V(lambda e, pk=pk, pr_cur=pr_cur, pr_nxt=pr_nxt: e.scalar_tensor_tensor(out=pr_nxt.t[:], in0=pr_cur.t[:], scalar=tfb.t[0:64, 2:3],
                                                                                          in1=pk.t[0:64, 0:128], op0=ALU.mult, op1=ALU.add),
                      r=[pr_cur, pk, tfb], w=[pr_nxt])
                    V(lambda e, pk=pk, pr_cur=pr_cur, pb_nxt=pb_nxt: e.scalar_tensor_tensor(out=pb_nxt.t[:], in0=pr_cur.t[:], scalar=tfb.t[0:64, 2:3],
                                                                                          in1=pk.t[0:64, 0:128], op0=ALU.mult, op1=ALU.add),
                      r=[pr_cur, pk, tfb], w=[pb_nxt])
                T(lambda e, po=po, j4=j4, sm_=sm_, n=n: e.matmul(po.t[:, j4 * 128:(j4 + 1) * 128], lhsT=sm_.t[:], rhs=rv.t[:, n, :], start=True, stop=False), r=[sm_, rv], w=[po])
                T(lambda e, po=po, j4=j4, qf_=qf_, pb_cur=pb_cur: e.matmul(po.t[:, j4 * 128:(j4 + 1) * 128], lhsT=qf_.t[:], rhs=pb_cur.t[:], start=False, stop=False),
                  r=[qf_, pb_cur], w=[po])
                T(lambda e, po=po, j4=j4, qb_=qb_, n=n: e.matmul(po.t[:, j4 * 128:(j4 + 1) * 128], lhsT=qb_.t[:], rhs=Nbf.t[:, n, :], start=False, stop=True),
                  r=[qb_, Nbf], w=[po])
                if j4 == 3:
                    s4 = st4[(n // 4) % 2]
                    V(lambda e, po=po, s4=s4: e.tensor_reduce(out=s4.t[:, 0:4], in_=po.t[:].rearrange("p (a b) -> p a b", a=4), axis=AX.X, op=ALU.add), r=[po], w=[s4])
                    A(lambda e, po=po: e.activation(out=osq.t[:].rearrange("p a b -> p (a b)"), in_=po.t[:], func=AF.Square), r=[po], w=[osq])
                    V(lambda e, s4=s4: e.tensor_reduce(out=s4.t[:, 4:8], in_=osq.t[:], axis=AX.X, op=ALU.add), r=[osq], w=[s4])
                    V(lambda e, s4=s4: e.tensor_scalar(out=s4.t[:, 8:12], in0=s4.t[:, 0:4], scalar1=1.0 / 128, scalar2=None, op0=ALU.mult), r=[s4], w=[s4])
                    V(lambda e, s4=s4: e.tensor_tensor(out=s4.t[:, 0:4], in0=s4.t[:, 8:12], in1=s4.t[:, 8:12], op=ALU.mult), r=[s4], w=[s4])
                    V(lambda e, s4=s4: e.scalar_tensor_tensor(out=s4.t[:, 12:16], in0=s4.t[:, 4:8], scalar=1.0 / 128, in1=s4.t[:, 0:4],
                                                              op0=ALU.mult, op1=ALU.subtract), r=[s4], w=[s4])
                    A(lambda e, s4=s4: e.activation(out=s4.t[:, 12:16], in_=s4.t[:, 12:16], func=AF.Sqrt, bias=cst.t[:, 2:3]), r=[s4, cst], w=[s4])
                    V(lambda e, s4=s4: e.reciprocal(out=s4.t[:, 12:16], in_=s4.t[:, 12:16]), r=[s4], w=[s4])
                    for jj in range(4):
                        m = n - 3 + jj
                        y_ = yr[m % 2]
                        py = psY[m % 2]
                        V(lambda e, po=po, jj=jj, s4=s4, y_=y_: e.tensor_scalar(out=y_.t[:], in0=po.t[:, jj * 128:(jj + 1) * 128], scalar1=s4.t[:, 8 + jj:9 + jj],
                                                                              scalar2=s4.t[:, 12 + jj:13 + jj], op0=ALU.subtract, op1=ALU.mult),
                          r=[po, s4], w=[y_])
                        PL(lambda e, y_=y_, m=m: e.tensor_tensor(out=y_.t[:], in0=y_.t[:], in1=sg.t[:, m, :], op=ALU.mult), r=[y_, sg], w=[y_])
                        T(lambda e, py=py, y_=y_: e.transpose(out=py.t[:, 0:128], in_=y_.t[:], identity=identb.t[:]), r=[y_, identb], w=[py])
                        A(lambda e, py=py, m=m: e.copy(out=yT.t[:, m * 128:(m + 1) * 128], in_=py.t[:, 0:128]), r=[py], cw=[yT])
            p.barrier()
            p.emit()
    def tap(dst, src_ap, reads):
        p.dma("gpsimd", lambda e: e.dma_start(out=dst, in_=src_ap), "tap", reads=reads)
        p.barrier()

    if stop_after <= 2:
        if debug:
            tap(dbg["yT"][0:128, :], yT.t[:], [yT])
        p.finish()
        return nc, p, dbg
    PAD = 1024
    with ExitStack() as ph:
        aqT = mk(ph, "aqT", [128, S], BF16)
        akT = mk(ph, "akT", [128, S + 2 * PAD], BF16)
        avT = mk(ph, "avT", [128, S + 2 * PAD], BF16)
        mkb = mk(ph, "mkb", [128, 18 * 256], BF16)
        acc = [mk(ph, f"acc{h}", [65, S], F32) for h in range(2)]
        Vaug = [mk(ph, f"Vaug{i}", [128, 2, 65], BF16) for i in range(8)]
        Et = [mk(ph, f"Et{i}", [128, 256], BF16) for i in range(4)]
        NPT = 6
        PTt = [mk(ph, f"PTt{i}", [128, 256], BF16) for i in range(NPT)]
        rd = mk(ph, "rd", [65, 512], F32)
        psA = [mk(ph, f"psA{i}", None, F32, psum=True) for i in range(3)]
        psV = [mk(ph, f"psV{i}", None, BF16, psum=True) for i in range(1)]
        psB = [mk(ph, f"psB{i}", None, F32, psum=True) for i in range(3)]
        psR = mk(ph, "psR", None, F32, psum=True)
        ld("sync", aqT, aqT.t[:], aq_d, reads=[R_aq])
        ld("sync", akT, akT.t[:, PAD:PAD + S], ak_d, reads=[R_ak])
        ld("sync", avT, avT.t[:, PAD:PAD + S], av_d, reads=[R_av])
        ld("gpsimd", mkb, mkb.t[:], masksin)
        for tns in (akT, avT):
            V(lambda e, tns=tns: e.memset(tns.t[:, 0:PAD], 0.0), w=[tns])
            V(lambda e, tns=tns: e.memset(tns.t[:, PAD + S:PAD + S + PAD], 0.0), w=[tns])
        for vb in Vaug:
            V(lambda e, vb=vb: e.memset(vb.t[:, :, 64:65], 1.0), w=[vb])
        items = []
        for c, d in enumerate(CONFIGS):
            nb = (S // d) // 128
            for r in range(d):
                for m in range(nb):
                    for h in range(2):
                        items.append((c, d, r, m, h, nb))
        LAG = 3
        NV = 8
        vt = {}
        vstate = {"vi": 0}

        def ksl_(d, r, u):
            st0 = PAD + d * (128 * u - 64) + r
            return slice(st0, st0 + 127 * d + 1, d)

        def stage1(idx):
            c, d, r, m, h, nb = items[idx]
            for u in (m, m + 1):
                if (c, r, u) in vt:
                    continue
                vi = vstate["vi"]
                vstate["vi"] += 1
                vb = Vaug[vi % NV]
                pv = psV[0]
                ks = ksl_(d, r, u)
                T(lambda e, pv=pv, ks=ks: e.transpose(out=pv.t[:, 0:128], in_=avT.t[:, ks], identity=identb.t[:]), r=[avT, identb], w=[pv])
                if vi % 2 == 0:
                    A(lambda e, pv=pv, vb=vb: e.copy(out=vb.t[:, :, 0:64], in_=pv.t[:, 0:128].rearrange("p (h x) -> p h x", h=2)), r=[pv], w=[vb])
                else:
                    V(lambda e, pv=pv, vb=vb: e.tensor_copy(out=vb.t[:, :, 0:64], in_=pv.t[:, 0:128].rearrange("p (h x) -> p h x", h=2)), r=[pv], w=[vb])
                vt[(c, r, u)] = vb
            q0 = d * 128 * m + r
            qs = slice(q0, q0 + 127 * d + 1, d)
            var = 1 if m == 0 else (2 if m == nb - 1 else 0)
            rows_ = slice(64 * h, 64 * h + 64)
            pa = psA[idx % 3]
            e_ = Et[idx % 4]
            pt_ = PTt[idx % NPT]
            for j in range(2):
                ks = ksl_(d, r, m + j)
                T(lambda e, pa=pa, j=j, rows_=rows_, qs=qs, ks=ks: e.matmul(pa.t[:, j * 128:(j + 1) * 128], lhsT=akT.t[rows_, ks], rhs=aqT.t[rows_, qs],
                                                                      start=True, stop=True), r=[akT, aqT], w=[pa])
            A(lambda e, pa=pa, e_=e_: e.activation(out=e_.t[:], in_=pa.t[:, 0:256], func=AF.Exp, scale=0.125), r=[pa], w=[e_])
            mo = ((h * 3 + c) * 3 + var) * 256
            V(lambda e, e_=e_, pt_=pt_, mo=mo: e.tensor_tensor(out=pt_.t[:], in0=e_.t[:], in1=mkb.t[:, mo:mo + 256], op=ALU.mult), r=[e_, mkb], w=[pt_])

        def stage2(idx):
            c, d, r, m, h, nb = items[idx]
            q0 = d * 128 * m + r
            qs = slice(q0, q0 + 127 * d + 1, d)
            pt_ = PTt[idx % NPT]
            po = psB[idx % 3]
            for j in range(2):
                vb = vt[(c, r, m + j)]
                T(lambda e, po=po, j=j, vb=vb, pt_=pt_, h=h: e.matmul(po.t[0:65, 0:128], lhsT=vb.t[:, h, :], rhs=pt_.t[:, j * 128:(j + 1) * 128],
                                                                   start=(j == 0), stop=(j == 1)), r=[vb, pt_], w=[po])
            ac = acc[h]
            if c == 0:
                A(lambda e, ac=ac, qs=qs, po=po: e.copy(out=ac.t[:, qs], in_=po.t[0:65, 0:128]), r=[po], w=[ac])
            else:
                V(lambda e, ac=ac, qs=qs, po=po: e.tensor_tensor(out=ac.t[:, qs], in0=ac.t[:, qs], in1=po.t[0:65, 0:128], op=ALU.add), r=[po, ac], w=[ac])

        for idx in range(len(items) + LAG):
            if idx < len(items):
                stage1(idx)
            if idx - LAG >= 0:
                stage2(idx - LAG)
        for h in range(2):
            yh = (yA, yB)[h]
            ac = acc[h]
            for t in range(16):
                sl = slice(512 * t, 512 * (t + 1))
                V(lambda e, ac=ac, sl=sl: e.reciprocal(out=rd.t[64:65, :], in_=ac.t[64:65, sl]), r=[ac], w=[rd])
                T(lambda e: e.matmul(psR.t[0:64, 0:512], lhsT=onesf.t[64:65, 0:64], rhs=rd.t[64:65, :], start=True, stop=True), r=[onesf, rd], w=[psR])
                V(lambda e, yh=yh, ac=ac, sl=sl: e.tensor_tensor(out=yh.t[:, sl], in0=ac.t[0:64, sl], in1=psR.t[0:64, 0:512], op=ALU.mult),
                  r=[ac, psR], w=[yh])
        p.barrier()
    if stop_after <= 3:
        if debug:
            tap(dbg["yT"][0:128, :], yT.t[:], [yT])
            tap(dbg["yT"][128:192, :], yA.t[:], [yA])
            tap(dbg["yT"][192:256, :], yB.t[:], [yB])
        p.finish()
        return nc, p, dbg

    y_in = [dsc(f"y_in{c}", [256, OWN], BF16) for c in range(4)]
    y_all = [dsc(f"y_all{c}", [1024, OWN], BF16) for c in range(4)]
    R_yin = [Reg(f"y_in{c}") for c in range(4)]
    R_yall = [Reg(f"y_all{c}") for c in range(4)]
    for c4 in range(4):
        sl = slice(OWN * c4, OWN * (c4 + 1))
        p.dma("sync", lambda e, c4=c4, sl=sl: e.dma_start(out=y_in[c4][0:128, :], in_=yT.t[:, sl]), f"y_in{c4}", reads=[yT], writes=[R_yin[c4]])
        p.dma("sync", lambda e, c4=c4, sl=sl: e.dma_start(out=y_in[c4][128:192, :], in_=yA.t[:, sl]), f"y_in{c4}", reads=[yA], writes=[R_yin[c4]])
        p.dma("sync", lambda e, c4=c4, sl=sl: e.dma_start(out=y_in[c4][192:256, :], in_=yB.t[:, sl]), f"y_in{c4}", reads=[yB], writes=[R_yin[c4]])
        p.dma("gpsimd", lambda e, c4=c4: e.collective_compute("AllGather", ALU.bypass, replica_groups=RG, ins=[y_in[c4]], outs=[y_all[c4]]),
              f"agy_{c4}", reads=[R_yin[c4]], writes=[R_yall[c4]], inc=1)
    p.barrier()
    ymix.close()
    if stop_after <= 4:
        p.finish()
        return nc, p, dbg
    x1_d = dsc("x1_d", [OWN, D])
    R_x1 = Reg("x1_d")
    with ExitStack() as ph58:
        rows = {k: mk(ph58, f"row_{k}", [128, D], F32) for k in ("gm", "gsf", "shf", "gf")}
        toki = mk(ph58, "toki", [128, 32], I32)
        with ExitStack() as ph:
            diag = [mk(ph, f"diag{i}", [128, 128], F32) for i in range(2)]
            psD = [mk(ph, f"psD{i}", None, F32, psum=True) for i in range(2)]
            srcs = {"gm": der.t[:, 8:16], "gsf": der.t[:, 16:24], "shf": mod.t[:, 24:32], "gf": der.t[:, 24:32]}
            di = 0
            for key in ("gm", "gsf", "shf", "gf"):
                for half in range(2):
                    pd = psD[half]
                    for kk in range(4):
                        k = half * 4 + kk
                        dg = diag[di % 2]
                        di += 1
                        V(lambda e, dg=dg, key=key, k=k: e.tensor_scalar(out=dg.t[:], in0=identf.t[:], scalar1=srcs[key][:, k:k + 1], scalar2=None,
                                                                       op0=ALU.mult), r=[identf, der, mod], w=[dg])
                        T(lambda e, pd=pd, kk=kk, dg=dg: e.matmul(pd.t[:, kk * 128:(kk + 1) * 128], lhsT=onesf.t[:], rhs=dg.t[:], start=True, stop=True),
                          r=[onesf, dg], w=[pd])
                    A(lambda e, pd=pd, key=key, half=half: e.copy(out=rows[key].t[:, half * 512:(half + 1) * 512], in_=pd.t[:, 0:512]),
                      r=[pd], w=[rows[key]])
            wrs = mk(ph, "wrs", [128, 8, 16], F32)
            ld("sync", wrs, wrs.t[:], wrin.rearrange("(k p) e -> p k e", p=128))
            wob = mk(ph, "wob", [128, 8, D], BF16)
            ld("gpsimd", wob, wob.t[:], wout.rearrange("(k p) n -> p k n", p=128))
            yown = mk(ph, "yown", [128, 8, OWN], BF16)
            ytmp = [mk(ph, f"ytmp{i}", [128, 8, OWN], BF16) for i in range(1)]
            ohb = mk(ph, "ohb", [128, 4], BF16)
            V(lambda e: e.tensor_copy(out=ohb.t[:], in_=ohs.t[:]), r=[ohs], w=[ohb])
            for c4 in range(4):
                yt_ = ytmp[0]
                ld("sync", yt_, yt_.t[:], y_all[c4].rearrange("(k p) t -> p k t", p=128), reads=[R_yall[c4]])
                for kq in range(4):
                    ks_ = slice(2 * kq, 2 * kq + 2)
                    eng = V
                    if c4 == 0:
                        eng(lambda e, yt_=yt_, ks_=ks_: e.tensor_scalar(out=yown.t[:, ks_, :], in0=yt_.t[:, ks_, :], scalar1=ohs.t[:, 0:1], scalar2=None,
                                                                        op0=ALU.mult), r=[yt_, ohs], cw=[yown])
                    else:
                        eng(lambda e, yt_=yt_, ks_=ks_, c4=c4: e.scalar_tensor_tensor(out=yown.t[:, ks_, :], in0=yt_.t[:, ks_, :], scalar=ohs.t[:, c4:c4 + 1],
                                                                                      in1=yown.t[:, ks_, :], op0=ALU.mult, op1=ALU.add),
                            r=[yt_, ohs], cw=[yown])
            psM = [mk(ph, f"psM{i}", None, F32, psum=True) for i in range(2)]
            RD5 = 3
            mx = [mk(ph, f"mx{i}", [128, D], F32) for i in range(RD5)]
            xo_t = [mk(ph, f"xo{i}", [128, D], F32) for i in range(RD5)]
            x1t = [mk(ph, f"x1t{i}", [128, D], F32) for i in range(RD5)]
            h2t = [mk(ph, f"h2t{i}", [128, D], F32) for i in range(RD5)]
            h2T = [mk(ph, f"h2T{i}", [128, 8, 128], F32) for i in range(RD5)]
            sq2 = mk(ph, "sq2", [128, D], BF16)
            ss5 = [mk(ph, f"ss5{i}", [128, 8], F32) for i in range(RD5)]
            afft = [mk(ph, f"afft{i}", [128, 16], F32) for i in range(2)]
            ext = [mk(ph, f"ext{i}", [128, 16], F32) for i in range(2)]
            psH = [mk(ph, f"psH{i}", None, F32, psum=True) for i in range(2)]
            psL = [mk(ph, f"psL{i}", None, F32, psum=True) for i in range(2)]
            lgall = mk(ph, "lgall", [128, 16, 16], F32)
            smx = mk(ph, "smx", [128, 32], F32)
            def p5_a(n):
                ts = slice(128 * n, 128 * (n + 1))
                m_, xo_, x1_, h2_, hT_, s5 = mx[n % RD5], xo_t[n % RD5], x1t[n % RD5], h2t[n % RD5], h2T[n % RD5], ss5[n % RD5]
                for half in range(2):
                    pm = psM[half]
                    for kc in range(8):
                        T(lambda e, pm=pm, kc=kc, ts=ts, half=half: e.matmul(pm.t[:, 0:512], lhsT=yown.t[:, kc, ts], rhs=wob.t[:, kc, half * 512:(half + 1) * 512],
                                                                           start=(kc == 0), stop=(kc == 7)), r=[yown, wob], w=[pm])
                    if half == 0:
                        A(lambda e, pm=pm, m_=m_: e.copy(out=m_.t[:, 0:512], in_=pm.t[:, 0:512]), r=[pm], cw=[m_])
                    else:
                        V(lambda e, pm=pm, m_=m_: e.tensor_copy(out=m_.t[:, 512:1024], in_=pm.t[:, 0:512]), r=[pm], cw=[m_])
                ld("sync", xo_, xo_.t[:], xo[ts, :])
                A(lambda e, m_=m_, s5=s5: e.activation(out=sq2.t[:], in_=m_.t[:], func=AF.Square, accum_out=s5.t[:, 0:1]), r=[m_], w=[sq2, s5])
                A(lambda e, s5=s5: e.activation(out=s5.t[:, 1:2], in_=s5.t[:, 0:1], func=AF.Sqrt, scale=1.0 / D, bias=cst.t[:, 1:2]), r=[s5, cst], w=[s5])
                V(lambda e, s5=s5: e.reciprocal(out=s5.t[:, 1:2], in_=s5.t[:, 1:2]), r=[s5], w=[s5])
                V(lambda e, m_=m_, s5=s5: e.scalar_tensor_tensor(out=m_.t[:], in0=m_.t[:], scalar=s5.t[:, 1:2], in1=rows["gm"].t[:],
                                                               op0=ALU.mult, op1=ALU.mult), r=[m_, s5, rows["gm"]], w=[m_])
                PL(lambda e, m_=m_, xo_=xo_, x1_=x1_: e.tensor_tensor(out=x1_.t[:], in0=xo_.t[:], in1=m_.t[:], op=ALU.add), r=[m_, xo_], w=[x1_])
                p.dma("sync", lambda e, ts=ts, x1_=x1_: e.dma_start(out=x1_d[ts, :], in_=x1_.t[:]), "x1_d", reads=[x1_], writes=[R_x1])

            def p5_b(n):
                ts = slice(128 * n, 128 * (n + 1))
                m_, xo_, x1_, h2_, hT_, s5 = mx[n % RD5], xo_t[n % RD5], x1t[n % RD5], h2t[n % RD5], h2T[n % RD5], ss5[n % RD5]
                A(lambda e, x1_=x1_, s5=s5: e.activation(out=sq2.t[:], in_=x1_.t[:], func=AF.Square, accum_out=s5.t[:, 2:3]), r=[x1_], w=[sq2, s5])
                A(lambda e, s5=s5: e.activation(out=s5.t[:, 3:4], in_=s5.t[:, 2:3], func=AF.Sqrt, scale=1.0 / D, bias=cst.t[:, 1:2]), r=[s5, cst], w=[s5])
                V(lambda e, s5=s5: e.reciprocal(out=s5.t[:, 3:4], in_=s5.t[:, 3:4]), r=[s5], w=[s5])
                V(lambda e, x1_=x1_, s5=s5, h2_=h2_: e.scalar_tensor_tensor(out=h2_.t[:], in0=x1_.t[:], scalar=s5.t[:, 3:4], in1=rows["gsf"].t[:],
                                                                          op0=ALU.mult, op1=ALU.mult), r=[x1_, s5, rows["gsf"]], w=[h2_])
                PL(lambda e, h2_=h2_: e.tensor_tensor(out=h2_.t[:], in0=h2_.t[:], in1=rows["shf"].t[:], op=ALU.add), r=[h2_, rows["shf"]], w=[h2_])
                p.dma("gpsimd", lambda e, ts=ts, h2_=h2_: e.dma_start(out=h2_in[ts, :], in_=h2_.t[:]), "h2_in", reads=[h2_], writes=[R_h2in])
                if n % 4 == 3:
                    c4 = n // 4
                    p.dma("gpsimd", lambda e, c4=c4: e.collective_compute("AllGather", ALU.bypass, replica_groups=RG,
                                                                          ins=[h2_in[512 * c4:512 * (c4 + 1), :]], outs=[h2_all[c4]]),
                          f"ag1_{c4}", reads=[R_h2in], writes=[R_h2all[c4]], inc=1)
                    for r4 in range(4):
                        p.dma("sync", lambda e, c4=c4, r4=r4: e.dma_start(out=h2_tab[2048 * r4 + 512 * c4:2048 * r4 + 512 * (c4 + 1), :],
                                                                          in_=h2_all[c4][512 * r4:512 * (r4 + 1), :]),
                              "h2tab", reads=[R_h2all[c4]], writes=[R_h2tab])

            def p5_c(n):
                ts = slice(128 * n, 128 * (n + 1))
                m_, xo_, x1_, h2_, hT_, s5 = mx[n % RD5], xo_t[n % RD5], x1t[n % RD5], h2t[n % RD5], h2T[n % RD5], ss5[n % RD5]
                for k in range(8):
                    ph_ = psH[k // 4]
                    T(lambda e, ph_=ph_, k=k, h2_=h2_: e.transpose(out=ph_.t[:, (k % 4) * 128:(k % 4 + 1) * 128], in_=h2_.t[:, k * 128:(k + 1) * 128],
                                                                 identity=identf.t[:]), r=[h2_, identf], w=[ph_])
                A(lambda e, hT_=hT_: e.copy(out=hT_.t[:, 0:4, :], in_=psH[0].t[:, 0:512].rearrange("p (k s) -> p k s", k=4)), r=[psH[0]], w=[hT_])
                V(lambda e, hT_=hT_: e.tensor_copy(out=hT_.t[:, 4:8, :], in_=psH[1].t[:, 0:512].rearrange("p (k s) -> p k s", k=4)), r=[psH[1]], w=[hT_])
                pl = psL[n % 2]
                for k in range(8):
                    T(lambda e, pl=pl, k=k, hT_=hT_: e.matmul(pl.t[:, 0:16], lhsT=hT_.t[:, k, :], rhs=wrs.t[:, k, :], start=(k == 0), stop=(k == 7)),
                      r=[hT_, wrs], w=[pl])
                V(lambda e, pl=pl, n=n: e.tensor_copy(out=lgall.t[:, n, :], in_=pl.t[:, 0:16]), r=[pl], cw=[lgall])

            for step in range(16 + 2):
                if step < 16:
                    p5_a(step)
                if 0 <= step - 1 < 16:
                    p5_b(step - 1)
                if 0 <= step - 2 < 16:
                    p5_c(step - 2)
            V(lambda e: e.tensor_reduce(out=smx.t[:, 0:16], in_=lgall.t[:], axis=AX.X, op=ALU.max), r=[lgall], w=[smx])
            for n in range(16):
                V(lambda e, n=n: e.tensor_scalar(out=lgall.t[:, n, :], in0=lgall.t[:, n, :], scalar1=smx.t[:, n:n + 1], scalar2=None, op0=ALU.subtract),
                  r=[lgall, smx], w=[lgall])
            A(lambda e: e.activation(out=lgall.t[:].rearrange("p a b -> p (a b)"), in_=lgall.t[:].rearrange("p a b -> p (a b)"), func=AF.Exp),
              r=[lgall], w=[lgall])
            V(lambda e: e.tensor_reduce(out=smx.t[:, 16:32], in_=lgall.t[:], axis=AX.X, op=ALU.add), r=[lgall], w=[smx])
            V(lambda e: e.reciprocal(out=smx.t[:, 16:32], in_=smx.t[:, 16:32]), r=[smx], w=[smx])
            for n in range(16):
                V(lambda e, n=n: e.tensor_scalar(out=lgall.t[:, n, :], in0=lgall.t[:, n, :], scalar1=smx.t[:, 16 + n:17 + n], scalar2=None, op0=ALU.mult),
                  r=[lgall, smx], w=[lgall])
            p.dma("sync", lambda e: e.dma_start(out=aff_in.rearrange("(n p) e -> p n e", p=128), in_=lgall.t[:]), "aff_in", reads=[lgall], writes=[R_affin])
            p.dma("gpsimd", lambda e: e.collective_compute("AllGather", ALU.bypass, replica_groups=RG, ins=[aff_in], outs=[aff_all]),
                  "ag2", reads=[R_affin], writes=[R_affall], inc=1)
            p.dma("sync", lambda e: e.dma_start(out=aff_tab, in_=aff_all), "afftab", reads=[R_affall], writes=[R_afftab])
            p.barrier()
        if stop_after <= 5:
            if debug:
                tap(dbg["x1"], x1_d, [R_x1])
                tap(dbg["aff"], aff_in, [R_affin])
            p.finish()
            return nc, p, dbg

        with ExitStack() as ph:
            Aall = mk(ph, "Aall", [128, 64, 16], F32)
            A4 = mk(ph, "A4", [128, 4, 64], F32)
            cmp_ = mk(ph, "cmp", [128, 4, 64], F32)
            cc0 = mk(ph, "cc0", [128, 4, 64], F32)
            cc1 = mk(ph, "cc1", [128, 4, 64], F32)
            lo = mk(ph, "lo", [128, 4], F32)
            mid = mk(ph, "mid", [128, 4], F32)
            cnt = mk(ph, "cnt", [128, 4], F32)
            ge = mk(ph, "ge", [128, 4], F32)
            offs = mk(ph, "offs", [128, 4], F32)
            tris = mk(ph, "tris", [128, 128], F32)
            slot = mk(ph, "slot", [128, 8], F32)
            tokf = mk(ph, "tokf", [128, 32], F32)
            cb = [mk(ph, f"cb{i}", [128, S], F32) for i in range(2)]
            junk = mk(ph, "junk", [128, S], BF16)
            psC = mk(ph, "psC", None, F32, psum=True)
            ld("sync", Aall, Aall.t[:], aff_tab.rearrange("(p j) e -> p j e", j=64), reads=[R_afftab])
            ld("sync", tris, tris.t[:], triin)
            ld("sync", slot, slot.t[:], slotin)
            for i in range(4):
                V(lambda e, i=i: e.tensor_scalar(out=A4.t[:, i, :], in0=Aall.t[:, :, i], scalar1=ohs.t[:, 0:1], scalar2=None, op0=ALU.mult),
                  r=[Aall, ohs], w=[A4])
                for r in range(1, 4):
                    V(lambda e, i=i, r=r: e.scalar_tensor_tensor(out=A4.t[:, i, :], in0=Aall.t[:, :, 4 * r + i], scalar=ohs.t[:, r:r + 1], in1=A4.t[:, i, :],
                                                                 op0=ALU.mult, op1=ALU.add), r=[Aall, ohs, A4], w=[A4])
            V(lambda e: e.memset(lo.t[:], 0.0), w=[lo])
            for it in range(26):
                wv = 2.0 ** (-(it + 1))
                V(lambda e, wv=wv: e.tensor_scalar(out=mid.t[:], in0=lo.t[:], scalar1=wv, scalar2=None, op0=ALU.add), r=[lo], w=[mid])
                V(lambda e: e.memset(cnt.t[:], 0.0), w=[cnt])
                for i in range(4):
                    V(lambda e, i=i: e.tensor_scalar(out=cmp_.t[:, i, :], in0=A4.t[:, i, :], scalar1=mid.t[:, i:i + 1], scalar2=0.0, op0=ALU.is_gt,
                                                    op1=ALU.add, accum_out=cnt.t[:, i:i + 1]), r=[A4, mid, cnt], w=[cmp_, cnt])
                T(lambda e: e.matmul(psC.t[:, 0:4], lhsT=onesf.t[:], rhs=cnt.t[:], start=True, stop=True), r=[onesf, cnt], w=[psC])
                V(lambda e: e.tensor_scalar(out=ge.t[:], in0=psC.t[:, 0:4], scalar1=CAP - 0.5, scalar2=None, op0=ALU.is_ge), r=[psC], w=[ge])
                V(lambda e, wv=wv: e.scalar_tensor_tensor(out=lo.t[:], in0=ge.t[:], scalar=wv, in1=lo.t[:], op0=ALU.mult, op1=ALU.add), r=[ge, lo], w=[lo])
            for i in range(4):
                V(lambda e, i=i: e.tensor_scalar(out=cc0.t[:, i, :], in0=A4.t[:, i, :], scalar1=lo.t[:, i:i + 1], scalar2=None, op0=ALU.is_gt),
                  r=[A4, lo], w=[cc0])
            ca, cbuf = cc0, cc1
            for sh in (1, 2, 4, 8, 16, 32):
                V(lambda e, ca=ca, cbuf=cbuf, sh=sh: e.tensor_tensor(out=cbuf.t[:, :, sh:64], in0=ca.t[:, :, sh:64], in1=ca.t[:, :, 0:64 - sh], op=ALU.add),
                  r=[ca], w=[cbuf])
                V(lambda e, ca=ca, cbuf=cbuf, sh=sh: e.tensor_copy(out=cbuf.t[:, :, 0:sh], in_=ca.t[:, :, 0:sh]), r=[ca], w=[cbuf])
                ca, cbuf = cbuf, ca
            V(lambda e, ca=ca: e.tensor_copy(out=cnt.t[:], in_=ca.t[:, :, 63]), r=[ca], w=[cnt])
            T(lambda e: e.matmul(psC.t[:, 0:4], lhsT=tris.t[:], rhs=cnt.t[:], start=True, stop=True), r=[tris, cnt], w=[psC])
            V(lambda e: e.tensor_copy(out=offs.t[:], in_=psC.t[:, 0:4]), r=[psC], w=[offs])
            cdr_v = cdr.rearrange("e (p j) -> e p j", j=64)
            for i in range(4):
                V(lambda e, i=i, ca=ca: e.tensor_scalar(out=ca.t[:, i, :], in0=ca.t[:, i, :], scalar1=offs.t[:, i:i + 1], scalar2=None, op0=ALU.add),
                  r=[ca, offs], w=[ca])
                p.dma("sync", lambda e, i=i, ca=ca: e.dma_start(out=cdr_v[i], in_=ca.t[:, i, :]), "cdr", reads=[ca], writes=[R_cdr])
            V(lambda e: e.memset(tokf.t[:], 0.0), w=[tokf])
            for i in range(4):
                cb_ = cb[i % 2]
                ld("sync", cb_, cb_.t[:], cdr[i:i + 1, :].partition_broadcast(128), reads=[R_cdr])
                for st in range(8):
                    col = i * 8 + st
                    V(lambda e, cb_=cb_, st=st, col=col: e.tensor_scalar(out=junk.t[:], in0=cb_.t[:], scalar1=slot.t[:, st:st + 1], scalar2=0.0,
                                                                       op0=ALU.is_le, op1=ALU.add, accum_out=tokf.t[:, col:col + 1]),
                      r=[cb_, slot, tokf], w=[junk, tokf])
            V(lambda e: e.tensor_copy(out=toki.t[:], in_=tokf.t[:]), r=[tokf], w=[toki])
            if debug and stop_after == 6:
                tap(dbg["tok"], tokf.t[:], [tokf])
            p.barrier()
        if stop_after <= 6:
            if debug:
                tap(dbg["x1"], x1_d, [R_x1])
                tap(dbg["aff"], aff_in, [R_affin])
            p.finish()
            return nc, p, dbg

        with ExitStack() as ph:
            xeT = [mk(ph, f"xeT{i}", [128, 8, CAP], BF16) for i in range(2)]
            hT = mk(ph, "hT", [128, 16, CAP], BF16)
            wdb = [mk(ph, f"wdb{i}", [128, 16, D], BF16) for i in range(2)]
            NPC = 8
            wgb = [mk(ph, f"wgb{i}", [128, 8, 256], BF16) for i in range(3)]
            wub = [mk(ph, f"wub{i}", [128, 8, 256], BF16) for i in range(3)]
            xet = [mk(ph, f"xet{i}", [128, D], BF16) for i in range(2)]
            gat = [mk(ph, f"gat{i}", [128, 16], F32) for i in range(2)]
            gate = [mk(ph, f"gate{i}", [128, 8], F32) for i in range(2)]
            yet = [mk(ph, f"yet{i}", [128, D], F32) for i in range(2)]
            sgt = [mk(ph, f"sgt{i}", [128, 512], BF16) for i in range(2)]
            psXT = mk(ph, "psXT", None, BF16, psum=True)
            psG = [mk(ph, f"psG{i}", None, F32, psum=True) for i in range(2)]
            psU = [mk(ph, f"psU{i}", None, F32, psum=True) for i in range(2)]
            psY = [mk(ph, f"psYd{i}", None, F32, psum=True) for i in range(2)]
            cnts = {"g": 0, "pc": 0, "m": 0, "y": 0}

            def load_piece(i, pc):
                ws = (i * NPC + pc) % 3
                wg_v = wg[i].rearrange("(k p) f -> p k f", p=128)
                wu_v = wu[i].rearrange("(k p) f -> p k f", p=128)
                ld("gpsimd", wgb[ws], wgb[ws].t[:], wg_v[:, :, pc * 256:(pc + 1) * 256])
                ld("gpsimd", wub[ws], wub[ws].t[:], wu_v[:, :, pc * 256:(pc + 1) * 256])

            def gather_expert(i):
                xT = xeT[i % 2]
                gt_ = gate[i % 2]
                for st in range(8):
                    col = i * 8 + st
                    xe_ = xet[cnts["g"] % 2]
                    ga_ = gat[cnts["g"] % 2]
                    cnts["g"] += 1
                    p.dma("gpsimd", lambda e, xe_=xe_, col=col: e.indirect_dma_start(
                        out=xe_.t[:], out_offset=None, in_=h2_tab, in_offset=bass.IndirectOffsetOnAxis(ap=toki.t[:, col:col + 1], axis=0)),
                        xe_.r.name, reads=[R_h2tab, toki], writes=[xe_])
                    p.dma("gpsimd", lambda e, ga_=ga_, col=col: e.indirect_dma_start(
                        out=ga_.t[:], out_offset=None, in_=aff_tab, in_offset=bass.IndirectOffsetOnAxis(ap=toki.t[:, col:col + 1], axis=0)),
                        ga_.r.name, reads=[R_afftab, toki], writes=[ga_])
                    V(lambda e, ga_=ga_, gt_=gt_, st=st, i=i: e.tensor_scalar(out=gt_.t[:, st:st + 1], in0=ga_.t[:, i:i + 1], scalar1=ohs.t[:, 0:1], scalar2=None,
                                                                            op0=ALU.mult), r=[ga_, ohs], w=[gt_])
                    for r in range(1, 4):
                        V(lambda e, ga_=ga_, gt_=gt_, st=st, i=i, r=r: e.scalar_tensor_tensor(
                            out=gt_.t[:, st:st + 1], in0=ga_.t[:, 4 * r + i:4 * r + i + 1], scalar=ohs.t[:, r:r + 1], in1=gt_.t[:, st:st + 1],
                            op0=ALU.mult, op1=ALU.add), r=[ga_, ohs, gt_], w=[gt_])
                    for k in range(8):
                        T(lambda e, k=k, xe_=xe_: e.transpose(out=psXT.t[:, k * 128:(k + 1) * 128], in_=xe_.t[:, k * 128:(k + 1) * 128], identity=identb.t[:]),
                          r=[xe_, identb], w=[psXT])
                    if st % 2 == 0:
                        A(lambda e, st=st, xT=xT: e.copy(out=xT.t[:, :, st * 128:(st + 1) * 128], in_=psXT.t[:].rearrange("p (k s) -> p k s", k=8)),
                          r=[psXT], cw=[xT])
                    else:
                        V(lambda e, st=st, xT=xT: e.tensor_copy(out=xT.t[:, :, st * 128:(st + 1) * 128], in_=psXT.t[:].rearrange("p (k s) -> p k s", k=8)),
                          r=[psXT], cw=[xT])

            ld("gpsimd", wdb[0], wdb[0].t[:], wd[0].rearrange("(k p) d -> p k d", p=128))
            gather_expert(0)
            load_piece(0, 0)
            load_piece(0, 1)
            for i in range(4):
                xT = xeT[i % 2]
                gt_ = gate[i % 2]
                wd_ = wdb[i % 2]
                for pc in range(NPC):
                    if pc + 2 < NPC:
                        load_piece(i, pc + 2)
                    ws = (i * NPC + pc) % 3
                    wg_, wu_ = wgb[ws], wub[ws]
                    for fc in range(2):
                        f = pc * 2 + fc
                        for half in range(2):
                            pg, pu, sg_ = psG[cnts["m"] % 2], psU[cnts["m"] % 2], sgt[cnts["m"] % 2]
                            cnts["m"] += 1
                            hs = slice(512 * half, 512 * (half + 1))
                            for k in range(8):
                                T(lambda e, pg=pg, k=k, wg_=wg_, fc=fc, hs=hs, xT=xT: e.matmul(pg.t[:, 0:512], lhsT=wg_.t[:, k, fc * 128:(fc + 1) * 128], rhs=xT.t[:, k, hs],
                                                                                           start=(k == 0), stop=(k == 7)), r=[wg_, xT], w=[pg])
                            for k in range(8):
                                T(lambda e, pu=pu, k=k, wu_=wu_, fc=fc, hs=hs, xT=xT: e.matmul(pu.t[:, 0:512], lhsT=wu_.t[:, k, fc * 128:(fc + 1) * 128], rhs=xT.t[:, k, hs],
                                                                                           start=(k == 0), stop=(k == 7)), r=[wu_, xT], w=[pu])
                            A(lambda e, pg=pg, sg_=sg_: e.activation(out=sg_.t[:], in_=pg.t[:, 0:512], func=AF.Silu), r=[pg], w=[sg_])
                            V(lambda e, pu=pu, sg_=sg_, f=f, hs=hs: e.tensor_tensor(out=hT.t[:, f, hs], in0=sg_.t[:], in1=pu.t[:, 0:512], op=ALU.mult),
                              r=[sg_, pu], cw=[hT])
                if i + 1 < 4:
                    ld("gpsimd", wdb[(i + 1) % 2], wdb[(i + 1) % 2].t[:], wd[i + 1].rearrange("(k p) d -> p k d", p=128))
                    gather_expert(i + 1)
                    load_piece(i + 1, 0)
                    load_piece(i + 1, 1)
                for st in range(8):
                    col = i * 8 + st
                    ye_ = yet[st % 2]
                    for dh in range(2):
                        py = psY[cnts["y"] % 2]
                        cnts["y"] += 1
                        ds_ = slice(512 * dh, 512 * (dh + 1))
                        for f in range(16):
                            T(lambda e, py=py, f=f, st=st, ds_=ds_, wd_=wd_: e.matmul(py.t[:, 0:512], lhsT=hT.t[:, f, st * 128:(st + 1) * 128], rhs=wd_.t[:, f, ds_],
                                                                                   start=(f == 0), stop=(f == 15)), r=[hT, wd_], w=[py])
                        if dh == 0:
                            A(lambda e, py=py, ye_=ye_, ds_=ds_, gt_=gt_, st=st: e.activation(out=ye_.t[:, ds_], in_=py.t[:, 0:512], func=AF.Identity,
                                                                                           scale=gt_.t[:, st:st + 1]), r=[py, gt_], cw=[ye_])
                        else:
                            V(lambda e, py=py, ye_=ye_, ds_=ds_, gt_=gt_, st=st: e.tensor_scalar(out=ye_.t[:, ds_], in0=py.t[:, 0:512], scalar1=gt_.t[:, st:st + 1],
                                                                                              scalar2=None, op0=ALU.mult), r=[py, gt_], cw=[ye_])
                    p.dma("gpsimd", lambda e, ye_=ye_, col=col: e.indirect_dma_start(
                        out=contrib, out_offset=bass.IndirectOffsetOnAxis(ap=toki.t[:, col:col + 1], axis=0), in_=ye_.t[:], in_offset=None,
                        compute_op=ALU.add), "scat", reads=[ye_, toki, R_contrib], writes=[R_contrib])
            p.dma("gpsimd", lambda e: e.collective_compute("ReduceScatter", ALU.add, replica_groups=RG, ins=[contrib], outs=[moe_d]),
                  "rs2", reads=[R_contrib], writes=[R_moe], inc=1)
            p.barrier()
        if debug and stop_after == 7:
            tap(dbg["moe"], moe_d, [R_moe])

        with ExitStack() as ph:
            mo = [mk(ph, f"mo{i}", [128, D], F32) for i in range(2)]
            x1r = [mk(ph, f"x1r{i}", [128, D], F32) for i in range(2)]
            sq8 = mk(ph, "sq8", [128, D], BF16)
            s8 = [mk(ph, f"s8{i}", [128, 2], F32) for i in range(2)]
            for n in range(16):
                ts = slice(128 * n, 128 * (n + 1))
                m_, x_, s_ = mo[n % 2], x1r[n % 2], s8[n % 2]
                ld("sync", m_, m_.t[:], moe_d[ts, :], reads=[R_moe])
                ld("sync", x_, x_.t[:], x1_d[ts, :], reads=[R_x1])
                A(lambda e, m_=m_, s_=s_: e.activation(out=sq8.t[:], in_=m_.t[:], func=AF.Square, accum_out=s_.t[:, 0:1]), r=[m_], w=[sq8, s_])
                A(lambda e, s_=s_: e.activation(out=s_.t[:, 1:2], in_=s_.t[:, 0:1], func=AF.Sqrt, scale=1.0 / D, bias=cst.t[:, 1:2]), r=[s_, cst], w=[s_])
                V(lambda e, s_=s_: e.reciprocal(out=s_.t[:, 1:2], in_=s_.t[:, 1:2]), r=[s_], w=[s_])
                V(lambda e, m_=m_, s_=s_: e.scalar_tensor_tensor(out=m_.t[:], in0=m_.t[:], scalar=s_.t[:, 1:2], in1=rows["gf"].t[:],
                                                               op0=ALU.mult, op1=ALU.mult), r=[m_, s_, rows["gf"]], w=[m_])
                PL(lambda e, m_=m_, x_=x_: e.tensor_tensor(out=m_.t[:], in0=m_.t[:], in1=x_.t[:], op=ALU.add), r=[m_, x_], w=[m_])
                p.dma("sync", lambda e, ts=ts, m_=m_: e.dma_start(out=out[ts, :], in_=m_.t[:]), "out", reads=[m_], writes=[R_out])
            p.barrier()
    p.finish()
    return nc, p, dbg


def _consts(q):
    j = np.arange(128, dtype=np.float32)[:, None]
    i = np.arange(128, dtype=np.float32)[None, :]
    rc = np.zeros((128, 514), np.float32)
    rc[:, 0:128] = np.maximum(i - j, 0)
    rc[:, 128:256] = np.maximum(j - i, 0)
    rc[:, 256:384] = i + 1
    rc[:, 384:512] = 128 - i
    rc[:, 512] = 127 - j[:, 0]
    rc[:, 513] = j[:, 0]
    masks = np.zeros((128, 2, 3, 3, 2, 128), np.float32)
    kk = np.arange(128)[:, None, None]
    jj = np.arange(2)[None, :, None]
    ii = np.arange(128)[None, None, :]
    delta = np.abs(kk + 128 * jj - 64 - ii).astype(np.float32)
    band = (delta <= 64).astype(np.float32)
    for h in range(2):
        slope = SLOPES[2 * q + h]
        for c, d in enumerate(CONFIGS):
            m = band * np.exp(-slope * d * delta)
            masks[:, h, c, 0] = m
            m1 = m.copy()
            m1[0:64, 0, :] = 0
            masks[:, h, c, 1] = m1
            m2 = m.copy()
            m2[64:128, 1, :] = 0
            masks[:, h, c, 2] = m2
    slotid = (128 * np.arange(8)[None, :] + np.arange(128)[:, None]).astype(np.float32)
    tri = (np.arange(128)[:, None] < np.arange(128)[None, :]).astype(np.float32)
    return rc, masks.reshape(128, 18 * 256), slotid, tri


def prep(inputs):
    f = lambda a: np.ascontiguousarray(np.asarray(a, dtype=np.float32))
    x, c = f(inputs["x"]), f(inputs["c"])
    w_in = f(inputs["w_in"])[0]
    w_out = f(inputs["w_out"])[0]
    col = lambda v: np.ascontiguousarray(v.reshape(-1, 128).T)
    vcols = np.concatenate([col(f(inputs["b_ada"])[0]), col(f(inputs["g_pre_mix"])[0]), col(f(inputs["g_post_mix"])[0]),
                            col(f(inputs["g_pre_ffn"])[0]), col(f(inputs["g_post_ffn"])[0])], axis=1)
    wada = f(inputs["w_ada"])[0]
    wr = f(inputs["w_router"])[0]
    wge, wue, wde = f(inputs["w_gate_e"])[0], f(inputs["w_up_e"])[0], f(inputs["w_down_e"])[0]
    df, db = f(inputs["ret_decay_fwd"])[0], f(inputs["ret_decay_bwd"])[0]
    ident = np.eye(128, dtype=np.float32)
    maps = []
    for i in range(8):
        b, q = i // 4, i % 4
        rq = np.arange(64 * q, 64 * q + 64)
        rk = 256 + rq
        rvc = 512 + np.arange(128 * q, 128 * q + 128)
        rgc = 1024 + np.arange(128 * q, 128 * q + 128)
        aqc = 1536 + np.arange(128 * q, 128 * q + 128)
        akc = 2048 + np.arange(128 * q, 128 * q + 128)
        avc = 2560 + np.arange(128 * q, 128 * q + 128)
        cols = np.concatenate([rq, rk, aqc, akc, avc, rk, rvc, rgc])
        rows = np.concatenate([np.concatenate([np.arange(128 * r_, 128 * r_ + 128), 512 + np.arange(128 * r_, 128 * r_ + 128)]) for r_ in range(4)])
        rc, masks, slotid, tri = _consts(q)
        oh = np.zeros((128, 4), np.float32)
        oh[:, q] = 1.0
        dec = np.zeros((128, 2), np.float32)
        dec[:, 0] = df[q]
        dec[:, 1] = db[q]
        maps.append({
            "xb": x[b], "xo": np.ascontiguousarray(x[b, OWN * q:OWN * (q + 1)]), "ccol": col(c[b]),
            "wada": wada, "vcols": vcols, "win": np.ascontiguousarray(w_in[:, cols]), "dec": dec,
            "wout": np.ascontiguousarray(w_out[rows]), "wr": wr, "oh": oh,
            "wg": np.ascontiguousarray(wge[4 * q:4 * q + 4]), "wu": np.ascontiguousarray(wue[4 * q:4 * q + 4]),
            "wd": np.ascontiguousarray(wde[4 * q:4 * q + 4]),
            "ident": ident, "masks": masks, "rc": rc, "slotid": slotid, "tri": tri,
        })
    return maps


_NC_CACHE = {}


def kernel(**inputs):
    maps = prep(inputs)
    if "nc" not in _NC_CACHE:
        _NC_CACHE["nc"] = build()[0]
    res = run_bass_kernel_spmd(_NC_CACHE["nc"], maps, core_ids=list(range(8)))
    out = np.zeros((2, S, D), np.float32)
    for i in range(8):
        b, q = i // 4, i % 4
        out[b, OWN * q:OWN * (q + 1)] = res.results[i]["out"]
    return out
```

```python
import numpy as np
from contextlib import ExitStack
import concourse.bass as bass
import concourse.mybir as mybir
from concourse.bass_utils import run_bass_kernel_spmd

F32 = mybir.dt.float32
BF16 = mybir.dt.bfloat16
I32 = mybir.dt.int32
ALU = mybir.AluOpType
AF = mybir.ActivationFunctionType
AX = mybir.AxisListType
ENGS = ["sync", "scalar", "vector", "gpsimd", "tensor"]

S = 8192
D = 1024
NT = S // 128
OWN = 2048
CAP = 1024
LN8 = -2.0794415416798357
SLOPES = [2.0 ** (-(h + 1)) for h in range(8)]
CONFIGS = (1, 4, 16)


class Reg:
    __slots__ = ("w", "r", "name", "psum", "cw")

    def __init__(self, name="", psum=False):
        self.w = None
        self.cw = {}
        self.r = {}
        self.name = name
        self.psum = psum


class Buf:
    __slots__ = ("t", "r")

    def __init__(self, t, name):
        self.t = t
        self.r = Reg(name)


class Prog:
    def __init__(self, nc):
        self.nc = nc
        self.stack = ExitStack()
        self.ops = {e: [] for e in ENGS}
        self.cnt = {e: 0 for e in ENGS}
        self.sem = {e: self.stack.enter_context(nc.semaphore(f"c_{e}")) for e in ENGS}
        self.known = {e: {} for e in ENGS}
        self.dsem = {}
        self.dcnt = {}

    def _need(self, eng, tok, waits):
        if tok is None:
            return
        sem, val, src = tok
        if src == eng and eng == "tensor":
            return
        if src == eng and val <= self.cnt[eng] - 3:
            return
        k = self.known[eng]
        if k.get(id(sem), 0) >= val:
            return
        k[id(sem)] = val
        waits.append((sem, val))

    def _deps(self, eng, reads, writes, cwrites=()):
        waits = []
        for c in cwrites:
            self._need(eng, c.w, waits)
            for t in c.r.values():
                self._need(eng, t, waits)
        for r in reads:
            self._need(eng, r.w, waits)
            for t in r.cw.values():
                self._need(eng, t, waits)
            if r.psum:
                for t in r.r.values():
                    if t[2] != eng:
                        self._need(eng, t, waits)
        for w in writes:
            self._need(eng, w.w, waits)
            for t in w.cw.values():
                self._need(eng, t, waits)
            for t in w.r.values():
                self._need(eng, t, waits)
        best = {}
        for sem, val in waits:
            if id(sem) not in best or best[id(sem)][1] < val:
                best[id(sem)] = (sem, val)
        return list(best.values())

    def _commit(self, tok, reads, writes, cwrites=()):
        for r in reads:
            r.r[id(tok[0])] = tok
        for w in writes:
            w.w = tok
            w.cw = {}
            w.r = {}
        for c in cwrites:
            c.cw[id(tok[0])] = tok

    def op(self, eng, fn, reads=(), writes=(), cwrites=()):
        reads = [x.r if isinstance(x, Buf) else x for x in reads]
        writes = [x.r if isinstance(x, Buf) else x for x in writes]
        cwrites = [x.r if isinstance(x, Buf) else x for x in cwrites]
        waits = self._deps(eng, reads, writes, cwrites)
        self.cnt[eng] += 1
        tok = (self.sem[eng], self.cnt[eng], eng)
        self.ops[eng].append((waits, fn, (self.sem[eng], 1)))
        self._commit(tok, reads, writes, cwrites)
        return tok

    def dma(self, q, fn, key, reads=(), writes=(), inc=16):
        reads = [x.r if isinstance(x, Buf) else x for x in reads]
        writes = [x.r if isinstance(x, Buf) else x for x in writes]
        if key not in self.dsem:
            self.dsem[key] = self.stack.enter_context(self.nc.semaphore(f"d_{key}"))
            self.dcnt[key] = 0
        waits = self._deps(q, reads, writes)
        self.dcnt[key] += inc
        tok = (self.dsem[key], self.dcnt[key], "dma")
        self.ops[q].append((waits, fn, (self.dsem[key], inc)))
        self._commit(tok, reads, writes)
        return tok

    def barrier(self):
        for eng in ENGS:
            waits = []
            for e in ENGS:
                if self.cnt[e] > 0 and e != eng:
                    self._need(eng, (self.sem[e], self.cnt[e], e), waits)
            for key, sem in self.dsem.items():
                self._need(eng, (sem, self.dcnt[key], "dma"), waits)
            self.ops[eng].append((waits, None, None))

    def emit(self):
        return

    def finish(self):
        nc = self.nc
        ops = self.ops
        self.ops = {e: [] for e in ENGS}

        def replay(name, e):
            for waits, fn, inc in ops[name]:
                for sem, val in waits:
                    e.wait_ge(sem, val)
                if fn is not None:
                    fn(e).then_inc(inc[0], inc[1])

        with nc.Block() as block:
            @block.sync
            def _(e):
                replay("sync", e)

            @block.scalar
            def _(e):
                replay("scalar", e)

            @block.vector
            def _(e):
                replay("vector", e)

            @block.gpsimd
            def _(e):
                replay("gpsimd", e)

            @block.tensor
            def _(e):
                replay("tensor", e)


def build(stop_after=99, debug=False):
    import os
    NGRP = int(os.environ.get('NGRP', '16'))
    FLAGS = os.environ.get('KFLAGS', '').split(',')
    nc = bass.Bass("TRN2", target_bir_lowering=False)

    def din(name, shape, dt=F32):
        return nc.dram_tensor(name, list(shape), dt, kind="ExternalInput").ap()

    def dsc(name, shape, dt=F32):
        return nc.dram_tensor(name, list(shape), dt).ap()

    xb = din("xb", [S, D])
    xo = din("xo", [OWN, D])
    ccol = din("ccol", [128, 8])
    wada = din("wada", [D, 6 * D])
    vcols = din("vcols", [128, 80])
    win = din("win", [D, 832])
    decin = din("dec", [128, 2])
    wout = din("wout", [D, D])
    wrin = din("wr", [D, 16])
    ohin = din("oh", [128, 4])
    if stop_after >= 7:
        wg = din("wg", [4, D, 2048])
        wu = din("wu", [4, D, 2048])
        wd = din("wd", [4, 2048, D])
    identin = din("ident", [128, 128])
    masksin = din("masks", [128, 18 * 256])
    rcin = din("rc", [128, 514])
    slotin = din("slotid", [128, 8])
    triin = din("tri", [128, 128])
    out = nc.dram_tensor("out", [OWN, D], F32, kind="ExternalOutput").ap()
    dbg = {}
    if debug:
        if stop_after in (2, 3):
            dbg["yT"] = nc.dram_tensor("dbg_yT", [256, S], F32, kind="ExternalOutput").ap()
        if stop_after in (5, 6):
            dbg["x1"] = nc.dram_tensor("dbg_x1", [OWN, D], F32, kind="ExternalOutput").ap()
            dbg["aff"] = nc.dram_tensor("dbg_aff", [OWN, 16], F32, kind="ExternalOutput").ap()
            dbg["tok"] = nc.dram_tensor("dbg_tok", [128, 32], F32, kind="ExternalOutput").ap()
        if stop_after == 7:
            dbg["moe"] = nc.dram_tensor("dbg_moe", [OWN, D], F32, kind="ExternalOutput").ap()

    aq_d = dsc("aq_d", [128, S], BF16)
    ak_d = dsc("ak_d", [128, S], BF16)
    av_d = dsc("av_d", [128, S], BF16)

    h2_in = dsc("h2_in", [OWN, D], BF16)
    h2_all = [dsc(f"h2_all{c4}", [2048, D], BF16) for c4 in range(4)]
    h2_tab = dsc("h2_tab", [S, D], BF16)
    aff_in = dsc("aff_in", [OWN, 16])
    aff_all = dsc("aff_all", [S, 16])
    aff_tab = dsc("aff_tab", [S, 16])
    cdr = dsc("cdr", [4, S])
    contrib = dsc("contrib", [S, D])
    moe_d = dsc("moe_d", [OWN, D])
    R_aq, R_ak, R_av = Reg("aq_d"), Reg("ak_d"), Reg("av_d")

    R_h2in, R_h2tab = Reg("h2in"), Reg("h2tab")
    R_h2all = [Reg(f"h2all{c}") for c in range(4)]
    R_affin, R_affall, R_afftab = Reg("affin"), Reg("affall"), Reg("afftab")
    R_cdr, R_contrib, R_moe, R_out = Reg("cdr"), Reg("contrib"), Reg("moe"), Reg("out")
    RG = [[0, 1, 2, 3], [4, 5, 6, 7]] if 'half' not in FLAGS else [[0, 1, 2, 3]]

    p = Prog(nc)
    GS = p.stack

    def mk(stack, name, shape, dt, psum=False):
        if psum:
            t = stack.enter_context(nc.psum_tensor(name, [128, 512 if dt == F32 else 1024], dt))
        else:
            t = stack.enter_context(nc.sbuf_tensor(name, list(shape), dt))
        b = Buf(t, name)
        b.r.psum = psum
        return b

    V = lambda fn, r=(), w=(), cw=(): p.op("vector", fn, r, w, cw)
    A = lambda fn, r=(), w=(), cw=(): p.op("scalar", fn, r, w, cw)
    PL = lambda fn, r=(), w=(), cw=(): p.op("gpsimd", fn, r, w, cw)
    T = lambda fn, r=(), w=(), cw=(): p.op("tensor", fn, r, w, cw)

    def ld(q, dst, dst_ap, src_ap, reads=()):
        return p.dma(q, lambda e: e.dma_start(out=dst_ap, in_=src_ap), dst.r.name, reads=reads, writes=[dst])

    identf = mk(GS, "identf", [128, 128], F32)
    identb = mk(GS, "identb", [128, 128], BF16)
    onesf = mk(GS, "onesf", [128, 128], F32)
    vc = mk(GS, "vc", [128, 80], F32)
    mod = mk(GS, "mod", [128, 48], F32)
    der = mk(GS, "der", [128, 32], F32)
    ohs = mk(GS, "ohs", [128, 4], F32)
    ld("sync", identf, identf.t[:], identin)
    ld("gpsimd", identb, identb.t[:], identin)
    ld("sync", vc, vc.t[:], vcols)
    ld("sync", ohs, ohs.t[:], ohin)
    V(lambda e: e.memset(onesf.t[:], 1.0), w=[onesf])
    cst = mk(GS, "cst", [128, 4], F32)
    V(lambda e: e.memset(cst.t[:, 0:1], LN8), w=[cst])
    V(lambda e: e.memset(cst.t[:, 1:2], 1e-6), w=[cst])
    V(lambda e: e.memset(cst.t[:, 2:3], 1e-5), w=[cst])
    V(lambda e: e.memset(cst.t[:, 3:4], 0.0), w=[cst])
    ymix = ExitStack()
    yT = mk(ymix, "yT", [128, S], BF16)
    yA = mk(ymix, "yA", [64, S], BF16)
    yB = mk(ymix, "yB", [64, S], BF16)

    with ExitStack() as ph:
        cc = mk(ph, "cc", [128, 8], F32)
        scb = mk(ph, "scb", [128, 8], BF16)
        wa = [mk(ph, f"wa{i}", [128, 8, D], BF16) for i in range(2)]
        psm = mk(ph, "psm", [128, 48], F32, psum=True)
        ld("sync", cc, cc.t[:], ccol)
        A(lambda e: e.activation(out=scb.t[:], in_=cc.t[:], func=AF.Silu), r=[cc], w=[scb])
        wada_v = wada.rearrange("(k p) n -> p k n", p=128)
        for g in range(6):
            w_ = wa[g % 2]
            ld("gpsimd", w_, w_.t[:], wada_v[:, :, g * D:(g + 1) * D])
            for j in range(8):
                for k in range(8):
                    T(lambda e, w_=w_, g=g, j=j, k=k: e.matmul(
                        psm.t[:, g * 8 + j:g * 8 + j + 1], lhsT=w_.t[:, k, j * 128:(j + 1) * 128],
                        rhs=scb.t[:, k:k + 1], start=(k == 0), stop=(k == 7)), r=[w_, scb], w=[psm])
        V(lambda e: e.tensor_tensor(out=mod.t[:], in0=psm.t[:, 0:48], in1=vc.t[:, 0:48], op=ALU.add), r=[psm, vc], w=[mod])
        V(lambda e: e.scalar_tensor_tensor(out=der.t[:, 0:8], in0=mod.t[:, 8:16], scalar=1.0, in1=vc.t[:, 48:56],
                                           op0=ALU.add, op1=ALU.mult), r=[mod, vc], w=[der])
        V(lambda e: e.tensor_tensor(out=der.t[:, 8:16], in0=mod.t[:, 16:24], in1=vc.t[:, 56:64], op=ALU.mult), r=[mod, vc], w=[der])
        V(lambda e: e.scalar_tensor_tensor(out=der.t[:, 16:24], in0=mod.t[:, 32:40], scalar=1.0, in1=vc.t[:, 64:72],
                                           op0=ALU.add, op1=ALU.mult), r=[mod, vc], w=[der])
        V(lambda e: e.tensor_tensor(out=der.t[:, 24:32], in0=mod.t[:, 40:48], in1=vc.t[:, 72:80], op=ALU.mult), r=[mod, vc], w=[der])
        p.barrier()
        p.emit()

    if stop_after <= 0:
        p.finish()
        return nc, p, dbg
    with ExitStack() as ph:
        rqT = mk(ph, "rqT", [64, S], BF16)
        rkT = mk(ph, "rkT", [64, S], BF16)
        ktf = mk(ph, "ktf", [128, NT, 64], BF16)
        ktb = mk(ph, "ktb", [128, NT, 64], BF16)
        rv = mk(ph, "rv", [128, NT, 128], BF16)
        sg = mk(ph, "sg", [128, NT, 128], BF16)
        rcs = mk(ph, "rcs", [128, 514], F32)
        dcs = mk(ph, "dcs", [128, 2], F32)
        lg = mk(ph, "lg", [128, 2], F32)
        tfb = mk(ph, "tfb", [128, 4], F32)
        DT = mk(ph, "DT", [128, 128], F32)
        QF = mk(ph, "QF", [128, 128], BF16)
        QB = mk(ph, "QB", [128, 128], BF16)
        ld("sync", rcs, rcs.t[:], rcin)
        ld("sync", dcs, dcs.t[:], decin)
        A(lambda e: e.activation(out=lg.t[:], in_=dcs.t[:], func=AF.Exp, scale=-1.0), r=[dcs], w=[lg])
        V(lambda e: e.tensor_scalar(out=lg.t[:], in0=lg.t[:], scalar1=1.0, scalar2=None, op0=ALU.add), r=[lg], w=[lg])
        A(lambda e: e.activation(out=lg.t[:], in_=lg.t[:], func=AF.Ln), r=[lg], w=[lg])
        V(lambda e: e.tensor_scalar(out=lg.t[:], in0=lg.t[:], scalar1=-1.0, scalar2=None, op0=ALU.mult), r=[lg], w=[lg])
        A(lambda e: e.activation(out=tfb.t[:, 0:1], in_=rcs.t[:, 512:513], func=AF.Exp, scale=lg.t[:, 0:1], bias=cst.t[:, 0:1]), r=[rcs, lg, cst], w=[tfb])
        A(lambda e: e.activation(out=tfb.t[:, 1:2], in_=rcs.t[:, 513:514], func=AF.Exp, scale=lg.t[:, 1:2], bias=cst.t[:, 0:1]), r=[rcs, lg, cst], w=[tfb])
        A(lambda e: e.activation(out=tfb.t[:, 2:4], in_=lg.t[:, 0:2], func=AF.Exp, scale=128.0), r=[lg], w=[tfb])
        A(lambda e: e.activation(out=QF.t[:], in_=rcs.t[:, 256:384], func=AF.Exp, scale=lg.t[:, 0:1]), r=[rcs, lg], w=[QF])
        A(lambda e: e.activation(out=QB.t[:], in_=rcs.t[:, 384:512], func=AF.Exp, scale=lg.t[:, 1:2]), r=[rcs, lg], w=[QB])
        V(lambda e: e.tensor_scalar(out=DT.t[:], in0=rcs.t[:, 0:128], scalar1=lg.t[:, 0:1], scalar2=None, op0=ALU.mult), r=[rcs, lg], w=[DT])
        V(lambda e: e.scalar_tensor_tensor(out=DT.t[:], in0=rcs.t[:, 128:256], scalar=lg.t[:, 1:2], in1=DT.t[:],
                                           op0=ALU.mult, op1=ALU.add), r=[rcs, lg, DT], w=[DT])
        A(lambda e: e.activation(out=DT.t[:], in_=DT.t[:], func=AF.Exp, bias=cst.t[:, 0:1]), r=[DT, cst], w=[DT])

        if 'pre_only' in FLAGS:
            p.barrier()
            p.finish()
            return nc, p, dbg
        with ExitStack() as ph1:
            winb = mk(ph1, "winb", [128, 8, 832], BF16)
            xs = [mk(ph1, f"xs{i}", [128, D], F32) for i in range(2)]
            sqj = mk(ph1, "sqj", [128, D], BF16)
            ssq = [mk(ph1, f"ssq{i}", [128, 2], F32) for i in range(2)]
            xn = [mk(ph1, f"xn{i}", [128, D], BF16) for i in range(2)]
            h1T = [mk(ph1, f"h1T{i}", [128, 8, 512], BF16) for i in range(2)]
            stg = [[mk(ph1, f"stg{a}{i}", [128, 512], BF16) for i in range(2)] for a in range(3)]
            psXa = [mk(ph1, f"psXa{i}", None, BF16, psum=True) for i in range(2)]
            psXb = [mk(ph1, f"psXb{i}", None, BF16, psum=True) for i in range(2)]
            psF = [mk(ph1, f"psF{i}", [128, 512], F32, psum=True) for i in range(2)]
            psT = [mk(ph1, f"psT{i}", [128, 320], F32, psum=True) for i in range(2)]
            ld("gpsimd", winb, winb.t[:], win.rearrange("(k p) n -> p k n", p=128))
            zt = mk(ph1, "zt", [128, D], BF16)
            PL(lambda e: e.memset(zt.t[:], 0.0), w=[zt])
            for zi in range(64):
                p.dma("gpsimd", lambda e, zi=zi: e.dma_start(out=contrib[128 * zi:128 * (zi + 1), :], in_=zt.t[:]),
                      "contrib0", reads=[zt], writes=[R_contrib])
            xb_v = xb.rearrange("(n p) d -> p n d", p=128)
            fstate = {"f": 0}

            def prep_tile(Gi, tt, part):
                h1 = h1T[Gi % 2]
                n = 4 * Gi + tt
                x_ = xs[n % 2]
                s_ = ssq[n % 2]
                xn_ = xn[n % 2]
                pxa, pxb = psXa[n % 2], psXb[n % 2]
                if part == "a":
                  ld("sync", x_, x_.t[:], xb_v[:, n, :])
                  A(lambda e, x_=x_, s_=s_: e.activation(out=sqj.t[:], in_=x_.t[:], func=AF.Square, accum_out=s_.t[:, 0:1]),
                    r=[x_], w=[sqj, s_])
                  A(lambda e, s_=s_: e.activation(out=s_.t[:, 1:2], in_=s_.t[:, 0:1], func=AF.Sqrt, scale=1.0 / D, bias=cst.t[:, 1:2]),
                    r=[s_, cst], w=[s_])
                  V(lambda e, s_=s_: e.reciprocal(out=s_.t[:, 1:2], in_=s_.t[:, 1:2]), r=[s_], w=[s_])
                  V(lambda e, x_=x_, s_=s_, xn_=xn_: e.tensor_scalar(out=xn_.t[:], in0=x_.t[:], scalar1=s_.t[:, 1:2], scalar2=None,
                                                                   op0=ALU.mult), r=[x_, s_], w=[xn_])
                  return
                for k in range(8 if part == "t" else 0):
                    px = pxa if k % 2 == 0 else pxb
                    T(lambda e, k=k, px=px, xn_=xn_: e.transpose(out=px.t[:, (k // 2) * 128:(k // 2 + 1) * 128], in_=xn_.t[:, k * 128:(k + 1) * 128],
                                                               identity=identb.t[:]), r=[xn_, identb], w=[px])
                for k in range(8 if part == "e" else 0):
                    px = pxa if k % 2 == 0 else pxb
                    o_ = h1.t[:, k, tt * 128:(tt + 1) * 128]
                    i_ = px.t[:, (k // 2) * 128:(k // 2 + 1) * 128]
                    if k % 2 == 0:
                        A(lambda e, o_=o_, i_=i_, k=k: e.activation(out=o_, in_=i_, func=AF.Identity, scale=der.t[:, k:k + 1],
                                                                   bias=mod.t[:, k:k + 1]), r=[px, der, mod], cw=[h1])
                    else:
                        V(lambda e, o_=o_, i_=i_, k=k: e.tensor_scalar(out=o_, in0=i_, scalar1=der.t[:, k:k + 1], scalar2=mod.t[:, k:k + 1],
                                                                     op0=ALU.mult, op1=ALU.add), r=[px, der, mod], cw=[h1])

            FG = [(0, 64), (64, 64), (128, 128), (256, 128), (384, 128)]

            def mm_fgroup(Gi, fi, part):
                h1 = h1T[Gi % 2]
                c0, wdt = FG[fi]
                if part == "m":
                    fstate[(Gi, fi)] = psF[fstate["f"] % 2]
                    fstate["f"] += 1
                pf = fstate[(Gi, fi)]
                for k in range(8 if part == "m" else 0):
                    T(lambda e, pf=pf, k=k, c0=c0, wdt=wdt, h1=h1: e.matmul(pf.t[0:wdt, :], lhsT=winb.t[:, k, c0:c0 + wdt], rhs=h1.t[:, k, :],
                                                                         start=(k == 0), stop=(k == 7)), r=[winb, h1], w=[pf])
                if part == "m":
                    return
                sl = slice(Gi * 512, (Gi + 1) * 512)
                if fi == 0:
                    A(lambda e, pf=pf, sl=sl: e.copy(out=rqT.t[:, sl], in_=pf.t[0:64, :]), r=[pf], cw=[rqT])
                elif fi == 1:
                    V(lambda e, pf=pf, sl=sl: e.tensor_copy(out=rkT.t[:, sl], in_=pf.t[0:64, :]), r=[pf], cw=[rkT])
                else:
                    sb_ = stg[fi - 2][Gi % 2]
                    dr, dreg = [(aq_d, R_aq), (ak_d, R_ak), (av_d, R_av)][fi - 2]
                    if fi == 3:
                        V(lambda e, pf=pf, sb_=sb_: e.tensor_copy(out=sb_.t[:], in_=pf.t[:]), r=[pf], w=[sb_])
                    else:
                        A(lambda e, pf=pf, sb_=sb_: e.copy(out=sb_.t[:], in_=pf.t[:]), r=[pf], w=[sb_])
                    p.dma("sync", lambda e, dr=dr, sl=sl, sb_=sb_: e.dma_start(out=dr[:, sl], in_=sb_.t[:]), dreg.name,
                          reads=[sb_], writes=[dreg])

            def mm_ttile(Gi, tt, part):
                h1 = h1T[Gi % 2]
                n = 4 * Gi + tt
                pt = psT[n % 2]
                for k in range(8 if part == "m" else 0):
                    T(lambda e, pt=pt, k=k, tt=tt, h1=h1: e.matmul(pt.t[:, 0:320], lhsT=h1.t[:, k, tt * 128:(tt + 1) * 128], rhs=winb.t[:, k, 512:832],
                                                                 start=(k == 0), stop=(k == 7)), r=[winb, h1], w=[pt])
                if part == "m":
                    return
                A(lambda e, pt=pt, n=n: e.activation(out=ktf.t[:, n, :], in_=pt.t[:, 0:64], func=AF.Identity, scale=tfb.t[:, 0:1]),
                  r=[pt, tfb], cw=[ktf])
                A(lambda e, pt=pt, n=n: e.copy(out=sg.t[:, n, :], in_=pt.t[:, 192:320]), r=[pt], cw=[sg])
                V(lambda e, pt=pt, n=n: e.tensor_scalar(out=ktb.t[:, n, :], in0=pt.t[:, 0:64], scalar1=tfb.t[:, 1:2], scalar2=None,
                                                      op0=ALU.mult), r=[pt, tfb], cw=[ktb])
                V(lambda e, pt=pt, n=n: e.tensor_copy(out=rv.t[:, n, :], in_=pt.t[:, 64:192]), r=[pt], cw=[rv])

            for tt in range(4):
                prep_tile(0, tt, "a")
                prep_tile(0, tt, "t")
                prep_tile(0, tt, "e")
            for Gi in range(NGRP):
                for tt in range(4):
                    nxt = Gi + 1 < NGRP
                    if nxt:
                        prep_tile(Gi + 1, tt, "a")
                    fis = ([0, 1], [2], [3], [4])[tt]
                    for fi in fis:
                        mm_fgroup(Gi, fi, "m")
                    mm_ttile(Gi, tt, "m")
                    if nxt:
                        prep_tile(Gi + 1, tt, "t")
                    for fi in fis:
                        mm_fgroup(Gi, fi, "e")
                    mm_ttile(Gi, tt, "e")
                    if nxt:
                        prep_tile(Gi + 1, tt, "e")
            p.barrier()
            p.emit()
        if stop_after <= 1:
            p.finish()
            return nc, p, dbg

        with ExitStack() as ph2:
            Nbf = mk(ph2, "Nbf", [64, NT, 128], BF16)
            Nrun = [mk(ph2, f"Nrun{i}", [64, 128], F32) for i in range(2)]
            Prun = [mk(ph2, f"Prun{i}", [64, 128], F32) for i in range(2)]
            Pbf = [mk(ph2, f"Pbf{i}", [64, 128], BF16) for i in range(2)]
            SM = [mk(ph2, f"SM{i}", [128, 128], BF16) for i in range(2)]
            qf = [mk(ph2, f"qf{i}", [64, 128], BF16) for i in range(2)]
            qb = [mk(ph2, f"qb{i}", [64, 128], BF16) for i in range(2)]
            osq = mk(ph2, "osq", [128, 4, 128], F32)
            st4 = [mk(ph2, f"st4{i}", [128, 16], F32) for i in range(2)]
            yr = [mk(ph2, f"yr{i}", [128, 128], BF16) for i in range(2)]
            psK = [mk(ph2, f"psK{i}", [64, 128], F32, psum=True) for i in range(2)]
            psS = [mk(ph2, f"psS{i}", [128, 128], F32, psum=True) for i in range(2)]
            psO = [mk(ph2, f"psO{i}", [128, 4, 128], F32, psum=True) for i in range(2)]
            psY = [mk(ph2, f"psY{i}", [128, 128], BF16, psum=True) for i in range(2)]
            for g4 in range(4):
                A(lambda e, g4=g4: e.activation(out=sg.t[:, 16 * g4:16 * (g4 + 1), :], in_=sg.t[:, 16 * g4:16 * (g4 + 1), :], func=AF.Silu), r=[sg], w=[sg])
            V(lambda e: e.memset(Nrun[1].t[:], 0.0), w=[Nrun[1]])
            V(lambda e: e.memset(Nbf.t[:, NT - 1, :], 0.0), w=[Nbf])
            for n in range(NT - 1, 0, -1):
                pk = psK[n % 2]
                cur, nxt = Nrun[n % 2], Nrun[(n + 1) % 2]
                T(lambda e, pk=pk, n=n: e.matmul(pk.t[0:64, 0:128], lhsT=ktb.t[:, n, :], rhs=rv.t[:, n, :], start=True, stop=True), r=[ktb, rv], w=[pk])
                V(lambda e, pk=pk, cur=cur, nxt=nxt: e.scalar_tensor_tensor(out=nxt.t[:], in0=cur.t[:], scalar=tfb.t[0:64, 3:4], in1=pk.t[0:64, 0:128],
                                                                          op0=ALU.mult, op1=ALU.add), r=[cur, pk, tfb], w=[nxt])
                V(lambda e, pk=pk, cur=cur, n=n: e.scalar_tensor_tensor(out=Nbf.t[:, n - 1, :], in0=cur.t[:], scalar=tfb.t[0:64, 3:4], in1=pk.t[0:64, 0:128],
                                                                     op0=ALU.mult, op1=ALU.add), r=[cur, pk, tfb], cw=[Nbf])
            V(lambda e: e.memset(Prun[0].t[:], 0.0), w=[Prun[0]])
            V(lambda e: e.memset(Pbf[0].t[:], 0.0), w=[Pbf[0]])
            def ret_stage1(n):
                cs = slice(n * 128, (n + 1) * 128)
                ps_ = psS[n % 2]
                sm_ = SM[n % 2]
                qf_, qb_ = qf[n % 2], qb[n % 2]
                T(lambda e, ps_=ps_, cs=cs: e.matmul(ps_.t[:, 0:128], lhsT=rkT.t[:, cs], rhs=rqT.t[:, cs], start=True, stop=True), r=[rkT, rqT], w=[ps_])
                V(lambda e, ps_=ps_, sm_=sm_: e.tensor_tensor(out=sm_.t[:], in0=ps_.t[:, 0:128], in1=DT.t[:], op=ALU.mult), r=[ps_, DT], w=[sm_])
                PL(lambda e, qf_=qf_, cs=cs: e.tensor_tensor(out=qf_.t[:], in0=rqT.t[:, cs], in1=QF.t[0:64, :], op=ALU.mult), r=[rqT, QF], w=[qf_])
                PL(lambda e, qb_=qb_, cs=cs: e.tensor_tensor(out=qb_.t[:], in0=rqT.t[:, cs], in1=QB.t[0:64, :], op=ALU.mult), r=[rqT, QB], w=[qb_])

            ret_stage1(0)
            for n in range(NT):
                if n + 1 < NT:
                    ret_stage1(n + 1)
                sm_ = SM[n % 2]
                po = psO[(n // 4) % 2]
                j4 = n % 4
                qf_, qb_ = qf[n % 2], qb[n % 2]
                pb_cur, pb_nxt = Pbf[n % 2], Pbf[(n + 1) % 2]
                pr_cur, pr_nxt = Prun[n % 2], Prun[(n + 1) % 2]
                if n < NT - 1:
                    pk = psK[n % 2]
                    T(lambda e, pk=pk, n=n: e.matmul(pk.t[0:64, 0:128], lhsT=ktf.t[:, n, :], rhs=rv.t[:, n, :], start=True, stop=True), r=[ktf, rv], w=[pk])
                    V(lambda e, pk=pk, pr_cur=pr_cur, pr_nxt=pr_nxt: e.scalar_tensor_tensor(out=pr_nxt.t[:], in0=pr_cur.t[:], scalar=tfb.t[0:64, 2:3],
                                                                                          in1=pk.t[0:64, 0:128], op0=ALU.mult, op1=ALU.add),
                      r=[pr_cur, pk, tfb], w=[pr_nxt])
                    V(lambda e, pk=pk, pr_cur=pr_cur, pb_nxt=pb_nxt: e.scalar_tensor_tensor(out=pb_nxt.t[:], in0=pr_cur.t[:], scalar=tfb.t[0:64, 2:3],
                                                                                          in1=pk.t[0:64, 0:128], op0=ALU.mult, op1=ALU.add),
                      r=[pr_cur, pk, tfb], w=[pb_nxt])
                T(lambda e, po=po, j4=j4, sm_=sm_, n=n: e.matmul(po.t[:, j4 * 128:(j4 + 1) * 128], lhsT=sm_.t[:], rhs=rv.t[:, n, :], start=True, stop=False), r=[sm_, rv], w=[po])
                T(lambda e, po=po, j4=j4, qf_=qf_, pb_cur=pb_cur: e.matmul(po.t[:, j4 * 128:(j4 + 1) * 128], lhsT=qf_.t[:], rhs=pb_cur.t[:], start=False, stop=False),
                  r=[qf_, pb_cur], w=[po])
                T(lambda e, po=po, j4=j4, qb_=qb_, n=n: e.matmul(po.t[:, j4 * 128:(j4 + 1) * 128], lhsT=qb_.t[:], rhs=Nbf.t[:, n, :], start=False, stop=True),
                  r=[qb_, Nbf], w=[po])
                if j4 == 3:
                    s4 = st4[(n // 4) % 2]
                    V(lambda e, po=po, s4=s4: e.tensor_reduce(out=s4.t[:, 0:4], in_=po.t[:].rearrange("p (a b) -> p a b", a=4), axis=AX.X, op=ALU.add), r=[po], w=[s4])
                    A(lambda e, po=po: e.activation(out=osq.t[:].rearrange("p a b -> p (a b)"), in_=po.t[:], func=AF.Square), r=[po], w=[osq])
                    V(lambda e, s4=s4: e.tensor_reduce(out=s4.t[:, 4:8], in_=osq.t[:], axis=AX.X, op=ALU.add), r=[osq], w=[s4])
                    V(lambda e, s4=s4: e.tensor_scalar(out=s4.t[:, 8:12], in0=s4.t[:, 0:4], scalar1=1.0 / 128, scalar2=None, op0=ALU.mult), r=[s4], w=[s4])
                    V(lambda e, s4=s4: e.tensor_tensor(out=s4.t[:, 0:4], in0=s4.t[:, 8:12], in1=s4.t[:, 8:12], op=ALU.mult), r=[s4], w=[s4])
                    V(lambda e, s4=s4: e.scalar_tensor_tensor(out=s4.t[:, 12:16], in0=s4.t[:, 4:8], scalar=1.0 / 128, in1=s4.t[:, 0:4],
                                                              op0=ALU.mult, op1=ALU.subtract), r=[s4], w=[s4])
                    A(lambda e, s4=s4: e.activation(out=s4.t[:, 12:16], in_=s4.t[:, 12:16], func=AF.Sqrt, bias=cst.t[:, 2:3]), r=[s4, cst], w=[s4])
                    V(lambda e, s4=s4: e.reciprocal(out=s4.t[:, 12:16], in_=s4.t[:, 12:16]), r=[s4], w=[s4])
                    for jj in range(4):
                        m = n - 3 + jj
                        y_ = yr[m % 2]
                        py = psY[m % 2]
                        V(lambda e, po=po, jj=jj, s4=s4, y_=y_: e.tensor_scalar(out=y_.t[:], in0=po.t[:, jj * 128:(jj + 1) * 128], scalar1=s4.t[:, 8 + jj:9 + jj],
                                                                              scalar2=s4.t[:, 12 + jj:13 + jj], op0=ALU.subtract, op1=ALU.mult),
                          r=[po, s4], w=[y_])
                        PL(lambda e, y_=y_, m=m: e.tensor_tensor(out=y_.t[:], in0=y_.t[:], in1=sg.t[:, m, :], op=ALU.mult), r=[y_, sg], w=[y_])
                        T(lambda e, py=py, y_=y_: e.transpose(out=py.t[:, 0:128], in_=y_.t[:], identity=identb.t[:]), r=[y_, identb], w=[py])
                        A(lambda e, py=py, m=m: e.copy(out=yT.t[:, m * 128:(m + 1) * 128], in_=py.t[:, 0:128]), r=[py], cw=[yT])
            p.barrier()
            p.emit()
    def tap(dst, src_ap, reads):
        p.dma("gpsimd", lambda e: e.dma_start(out=dst, in_=src_ap), "tap", reads=reads)
        p.barrier()

    if stop_after <= 2:
        if debug:
            tap(dbg["yT"][0:128, :], yT.t[:], [yT])
        p.finish()
        return nc, p, dbg
    PAD = 1024
    with ExitStack() as ph:
        aqT = mk(ph, "aqT", [128, S], BF16)
        akT = mk(ph, "akT", [128, S + 2 * PAD], BF16)
        avT = mk(ph, "avT", [128, S + 2 * PAD], BF16)
        mkb = mk(ph, "mkb", [128, 18 * 256], BF16)
        acc = [mk(ph, f"acc{h}", [65, S], F32) for h in range(2)]
        Vaug = [mk(ph, f"Vaug{i}", [128, 2, 65], BF16) for i in range(8)]
        Et = [mk(ph, f"Et{i}", [128, 256], BF16) for i in range(4)]
        NPT = 6
        PTt = [mk(ph, f"PTt{i}", [128, 256], BF16) for i in range(NPT)]
        rd = mk(ph, "rd", [65, 512], F32)
        psA = [mk(ph, f"psA{i}", None, F32, psum=True) for i in range(3)]
        psV = [mk(ph, f"psV{i}", None, BF16, psum=True) for i in range(1)]
        psB = [mk(ph, f"psB{i}", None, F32, psum=True) for i in range(3)]
        psR = mk(ph, "psR", None, F32, psum=True)
        ld("sync", aqT, aqT.t[:], aq_d, reads=[R_aq])
        ld("sync", akT, akT.t[:, PAD:PAD + S], ak_d, reads=[R_ak])
        ld("sync", avT, avT.t[:, PAD:PAD + S], av_d, reads=[R_av])
        ld("gpsimd", mkb, mkb.t[:], masksin)
        for tns in (akT, avT):
            V(lambda e, tns=tns: e.memset(tns.t[:, 0:PAD], 0.0), w=[tns])
            V(lambda e, tns=tns: e.memset(tns.t[:, PAD + S:PAD + S + PAD], 0.0), w=[tns])
        for vb in Vaug:
            V(lambda e, vb=vb: e.memset(vb.t[:, :, 64:65], 1.0), w=[vb])
        items = []
        for c, d in enumerate(CONFIGS):
            nb = (S // d) // 128
            for r in range(d):
                for m in range(nb):
                    for h in range(2):
                        items.append((c, d, r, m, h, nb))
        LAG = 3
        NV = 8
        vt = {}
        vstate = {"vi": 0}

        def ksl_(d, r, u):
            st0 = PAD + d * (128 * u - 64) + r
            return slice(st0, st0 + 127 * d + 1, d)

        def stage1(idx):
            c, d, r, m, h, nb = items[idx]
            for u in (m, m + 1):
                if (c, r, u) in vt:
                    continue
                vi = vstate["vi"]
                vstate["vi"] += 1
                vb = Vaug[vi % NV]
                pv = psV[0]
                ks = ksl_(d, r, u)
                T(lambda e, pv=pv, ks=ks: e.transpose(out=pv.t[:, 0:128], in_=avT.t[:, ks], identity=identb.t[:]), r=[avT, identb], w=[pv])
                if vi % 2 == 0:
                    A(lambda e, pv=pv, vb=vb: e.copy(out=vb.t[:, :, 0:64], in_=pv.t[:, 0:128].rearrange("p (h x) -> p h x", h=2)), r=[pv], w=[vb])
                else:
                    V(lambda e, pv=pv, vb=vb: e.tensor_copy(out=vb.t[:, :, 0:64], in_=pv.t[:, 0:128].rearrange("p (h x) -> p h x", h=2)), r=[pv], w=[vb])
                vt[(c, r, u)] = vb
            q0 = d * 128 * m + r
            qs = slice(q0, q0 + 127 * d + 1, d)
            var = 1 if m == 0 else (2 if m == nb - 1 else 0)
            rows_ = slice(64 * h, 64 * h + 64)
            pa = psA[idx % 3]
            e_ = Et[idx % 4]
            pt_ = PTt[idx % NPT]
            for j in range(2):
                ks = ksl_(d, r, m + j)
                T(lambda e, pa=pa, j=j, rows_=rows_, qs=qs, ks=ks: e.matmul(pa.t[:, j * 128:(j + 1) * 128], lhsT=akT.t[rows_, ks], rhs=aqT.t[rows_, qs],
                                                                      start=True, stop=True), r=[akT, aqT], w=[pa])
            A(lambda e, pa=pa, e_=e_: e.activation(out=e_.t[:], in_=pa.t[:, 0:256], func=AF.Exp, scale=0.125), r=[pa], w=[e_])
            mo = ((h * 3 + c) * 3 + var) * 256
            V(lambda e, e_=e_, pt_=pt_, mo=mo: e.tensor_tensor(out=pt_.t[:], in0=e_.t[:], in1=mkb.t[:, mo:mo + 256], op=ALU.mult), r=[e_, mkb], w=[pt_])

        def stage2(idx):
            c, d, r, m, h, nb = items[idx]
            q0 = d * 128 * m + r
            qs = slice(q0, q0 + 127 * d + 1, d)
            pt_ = PTt[idx % NPT]
            po = psB[idx % 3]
            for j in range(2):
                vb = vt[(c, r, m + j)]
                T(lambda e, po=po, j=j, vb=vb, pt_=pt_, h=h: e.matmul(po.t[0:65, 0:128], lhsT=vb.t[:, h, :], rhs=pt_.t[:, j * 128:(j + 1) * 128],
                                                                   start=(j == 0), stop=(j == 1)), r=[vb, pt_], w=[po])
            ac = acc[h]
            if c == 0:
                A(lambda e, ac=ac, qs=qs, po=po: e.copy(out=ac.t[:, qs], in_=po.t[0:65, 0:128]), r=[po], w=[ac])
            else:
                V(lambda e, ac=ac, qs=qs, po=po: e.tensor_tensor(out=ac.t[:, qs], in0=ac.t[:, qs], in1=po.t[0:65, 0:128], op=ALU.add), r=[po, ac], w=[ac])

        for idx in range(len(items) + LAG):
            if idx < len(items):
                stage1(idx)
            if idx - LAG >= 0:
                stage2(idx - LAG)
        for h in range(2):
            yh = (yA, yB)[h]
            ac = acc[h]
            for t in range(16):
                sl = slice(512 * t, 512 * (t + 1))
                V(lambda e, ac=ac, sl=sl: e.reciprocal(out=rd.t[64:65, :], in_=ac.t[64:65, sl]), r=[ac], w=[rd])
                T(lambda e: e.matmul(psR.t[0:64, 0:512], lhsT=onesf.t[64:65, 0:64], rhs=rd.t[64:65, :], start=True, stop=True), r=[onesf, rd], w=[psR])
                V(lambda e, yh=yh, ac=ac, sl=sl: e.tensor_tensor(out=yh.t[:, sl], in0=ac.t[0:64, sl], in1=psR.t[0:64, 0:512], op=ALU.mult),
                  r=[ac, psR], w=[yh])
        p.barrier()
    if stop_after <= 3:
        if debug:
            tap(dbg["yT"][0:128, :], yT.t[:], [yT])
            tap(dbg["yT"][128:192, :], yA.t[:], [yA])
            tap(dbg["yT"][192:256, :], yB.t[:], [yB])
        p.finish()
        return nc, p, dbg

    y_in = [dsc(f"y_in{c}", [256, OWN], BF16) for c in range(4)]
    y_all = [dsc(f"y_all{c}", [1024, OWN], BF16) for c in range(4)]
    R_yin = [Reg(f"y_in{c}") for c in range(4)]
    R_yall = [Reg(f"y_all{c}") for c in range(4)]
    for c4 in range(4):
        sl = slice(OWN * c4, OWN * (c4 + 1))
        p.dma("sync", lambda e, c4=c4, sl=sl: e.dma_start(out=y_in[c4][0:128, :], in_=yT.t[:, sl]), f"y_in{c4}", reads=[yT], writes=[R_yin[c4]])
        p.dma("sync", lambda e, c4=c4, sl=sl: e.dma_start(out=y_in[c4][128:192, :], in_=yA.t[:, sl]), f"y_in{c4}", reads=[yA], writes=[R_yin[c4]])
        p.dma("sync", lambda e, c4=c4, sl=sl: e.dma_start(out=y_in[c4][192:256, :], in_=yB.t[:, sl]), f"y_in{c4}", reads=[yB], writes=[R_yin[c4]])
        p.dma("gpsimd", lambda e, c4=c4: e.collective_compute("AllGather", ALU.bypass, replica_groups=RG, ins=[y_in[c4]], outs=[y_all[c4]]),
              f"agy_{c4}", reads=[R_yin[c4]], writes=[R_yall[c4]], inc=1)
    p.barrier()
    ymix.close()
    if stop_after <= 4:
        p.finish()
        return nc, p, dbg
    x1_d = dsc("x1_d", [OWN, D])
    R_x1 = Reg("x1_d")
    with ExitStack() as ph58:
        rows = {k: mk(ph58, f"row_{k}", [128, D], F32) for k in ("gm", "gsf", "shf", "gf")}
        toki = mk(ph58, "toki", [128, 32], I32)
        with ExitStack() as ph:
            diag = [mk(ph, f"diag{i}", [128, 128], F32) for i in range(2)]
            psD = [mk(ph, f"psD{i}", None, F32, psum=True) for i in range(2)]
            srcs = {"gm": der.t[:, 8:16], "gsf": der.t[:, 16:24], "shf": mod.t[:, 24:32], "gf": der.t[:, 24:32]}
            di = 0
            for key in ("gm", "gsf", "shf", "gf"):
                for half in range(2):
                    pd = psD[half]
                    for kk in range(4):
                        k = half * 4 + kk
                        dg = diag[di % 2]
                        di += 1
                        V(lambda e, dg=dg, key=key, k=k: e.tensor_scalar(out=dg.t[:], in0=identf.t[:], scalar1=srcs[key][:, k:k + 1], scalar2=None,
                                                                       op0=ALU.mult), r=[identf, der, mod], w=[dg])
                        T(lambda e, pd=pd, kk=kk, dg=dg: e.matmul(pd.t[:, kk * 128:(kk + 1) * 128], lhsT=onesf.t[:], rhs=dg.t[:], start=True, stop=True),
                          r=[onesf, dg], w=[pd])
                    A(lambda e, pd=pd, key=key, half=half: e.copy(out=rows[key].t[:, half * 512:(half + 1) * 512], in_=pd.t[:, 0:512]),
                      r=[pd], w=[rows[key]])
            wrs = mk(ph, "wrs", [128, 8, 16], F32)
            ld("sync", wrs, wrs.t[:], wrin.rearrange("(k p) e -> p k e", p=128))
            wob = mk(ph, "wob", [128, 8, D], BF16)
            ld("gpsimd", wob, wob.t[:], wout.rearrange("(k p) n -> p k n", p=128))
            yown = mk(ph, "yown", [128, 8, OWN], BF16)
            ytmp = [mk(ph, f"ytmp{i}", [128, 8, OWN], BF16) for i in range(1)]
            ohb = mk(ph, "ohb", [128, 4], BF16)
            V(lambda e: e.tensor_copy(out=ohb.t[:], in_=ohs.t[:]), r=[ohs], w=[ohb])
            for c4 in range(4):
                yt_ = ytmp[0]
                ld("sync", yt_, yt_.t[:], y_all[c4].rearrange("(k p) t -> p k t", p=128), reads=[R_yall[c4]])
                for kq in range(4):
                    ks_ = slice(2 * kq, 2 * kq + 2)
                    eng = V
                    if c4 == 0:
                        eng(lambda e, yt_=yt_, ks_=ks_: e.tensor_scalar(out=yown.t[:, ks_, :], in0=yt_.t[:, ks_, :], scalar1=ohs.t[:, 0:1], scalar2=None,
                                                                        op0=ALU.mult), r=[yt_, ohs], cw=[yown])
                    else:
                        eng(lambda e, yt_=yt_, ks_=ks_, c4=c4: e.scalar_tensor_tensor(out=yown.t[:, ks_, :], in0=yt_.t[:, ks_, :], scalar=ohs.t[:, c4:c4 + 1],
                                                                                      in1=yown.t[:, ks_, :], op0=ALU.mult, op1=ALU.add),
                            r=[yt_, ohs], cw=[yown])
            psM = [mk(ph, f"psM{i}", None, F32, psum=True) for i in range(2)]
            RD5 = 3
            mx = [mk(ph, f"mx{i}", [128, D], F32) for i in range(RD5)]
            xo_t = [mk(ph, f"xo{i}", [128, D], F32) for i in range(RD5)]
            x1t = [mk(ph, f"x1t{i}", [128, D], F32) for i in range(RD5)]
            h2t = [mk(ph, f"h2t{i}", [128, D], F32) for i in range(RD5)]
            h2T = [mk(ph, f"h2T{i}", [128, 8, 128], F32) for i in range(RD5)]
            sq2 = mk(ph, "sq2", [128, D], BF16)
            ss5 = [mk(ph, f"ss5{i}", [128, 8], F32) for i in range(RD5)]
            afft = [mk(ph, f"afft{i}", [128, 16], F32) for i in range(2)]
            ext = [mk(ph, f"ext{i}", [128, 16], F32) for i in range(2)]
            psH = [mk(ph, f"psH{i}", None, F32, psum=True) for i in range(2)]
            psL = [mk(ph, f"psL{i}", None, F32, psum=True) for i in range(2)]
            lgall = mk(ph, "lgall", [128, 16, 16], F32)
            smx = mk(ph, "smx", [128, 32], F32)
            def p5_a(n):
                ts = slice(128 * n, 128 * (n + 1))
                m_, xo_, x1_, h2_, hT_, s5 = mx[n % RD5], xo_t[n % RD5], x1t[n % RD5], h2t[n % RD5], h2T[n % RD5], ss5[n % RD5]
                for half in range(2):
                    pm = psM[half]
                    for kc in range(8):
                        T(lambda e, pm=pm, kc=kc, ts=ts, half=half: e.matmul(pm.t[:, 0:512], lhsT=yown.t[:, kc, ts], rhs=wob.t[:, kc, half * 512:(half + 1) * 512],
                                                                           start=(kc == 0), stop=(kc == 7)), r=[yown, wob], w=[pm])
                    if half == 0:
                        A(lambda e, pm=pm, m_=m_: e.copy(out=m_.t[:, 0:512], in_=pm.t[:, 0:512]), r=[pm], cw=[m_])
                    else:
                        V(lambda e, pm=pm, m_=m_: e.tensor_copy(out=m_.t[:, 512:1024], in_=pm.t[:, 0:512]), r=[pm], cw=[m_])
                ld("sync", xo_, xo_.t[:], xo[ts, :])
                A(lambda e, m_=m_, s5=s5: e.activation(out=sq2.t[:], in_=m_.t[:], func=AF.Square, accum_out=s5.t[:, 0:1]), r=[m_], w=[sq2, s5])
                A(lambda e, s5=s5: e.activation(out=s5.t[:, 1:2], in_=s5.t[:, 0:1], func=AF.Sqrt, scale=1.0 / D, bias=cst.t[:, 1:2]), r=[s5, cst], w=[s5])
                V(lambda e, s5=s5: e.reciprocal(out=s5.t[:, 1:2], in_=s5.t[:, 1:2]), r=[s5], w=[s5])
                V(lambda e, m_=m_, s5=s5: e.scalar_tensor_tensor(out=m_.t[:], in0=m_.t[:], scalar=s5.t[:, 1:2], in1=rows["gm"].t[:],
                                                               op0=ALU.mult, op1=ALU.mult), r=[m_, s5, rows["gm"]], w=[m_])
                PL(lambda e, m_=m_, xo_=xo_, x1_=x1_: e.tensor_tensor(out=x1_.t[:], in0=xo_.t[:], in1=m_.t[:], op=ALU.add), r=[m_, xo_], w=[x1_])
                p.dma("sync", lambda e, ts=ts, x1_=x1_: e.dma_start(out=x1_d[ts, :], in_=x1_.t[:]), "x1_d", reads=[x1_], writes=[R_x1])

            def p5_b(n):
                ts = slice(128 * n, 128 * (n + 1))
                m_, xo_, x1_, h2_, hT_, s5 = mx[n % RD5], xo_t[n % RD5], x1t[n % RD5], h2t[n % RD5], h2T[n % RD5], ss5[n % RD5]
                A(lambda e, x1_=x1_, s5=s5: e.activation(out=sq2.t[:], in_=x1_.t[:], func=AF.Square, accum_out=s5.t[:, 2:3]), r=[x1_], w=[sq2, s5])
                A(lambda e, s5=s5: e.activation(out=s5.t[:, 3:4], in_=s5.t[:, 2:3], func=AF.Sqrt, scale=1.0 / D, bias=cst.t[:, 1:2]), r=[s5, cst], w=[s5])
                V(lambda e, s5=s5: e.reciprocal(out=s5.t[:, 3:4], in_=s5.t[:, 3:4]), r=[s5], w=[s5])
                V(lambda e, x1_=x1_, s5=s5, h2_=h2_: e.scalar_tensor_tensor(out=h2_.t[:], in0=x1_.t[:], scalar=s5.t[:, 3:4], in1=rows["gsf"].t[:],
                                                                          op0=ALU.mult, op1=ALU.mult), r=[x1_, s5, rows["gsf"]], w=[h2_])
                PL(lambda e, h2_=h2_: e.tensor_tensor(out=h2_.t[:], in0=h2_.t[:], in1=rows["shf"].t[:], op=ALU.add), r=[h2_, rows["shf"]], w=[h2_])
                p.dma("gpsimd", lambda e, ts=ts, h2_=h2_: e.dma_start(out=h2_in[ts, :], in_=h2_.t[:]), "h2_in", reads=[h2_], writes=[R_h2in])
                if n % 4 == 3:
                    c4 = n // 4
                    p.dma("gpsimd", lambda e, c4=c4: e.collective_compute("AllGather", ALU.bypass, replica_groups=RG,
                                                                          ins=[h2_in[512 * c4:512 * (c4 + 1), :]], outs=[h2_all[c4]]),
                          f"ag1_{c4}", reads=[R_h2in], writes=[R_h2all[c4]], inc=1)

            def p5_c(n):
                ts = slice(128 * n, 128 * (n + 1))
                m_, xo_, x1_, h2_, hT_, s5 = mx[n % RD5], xo_t[n % RD5], x1t[n % RD5], h2t[n % RD5], h2T[n % RD5], ss5[n % RD5]
                for k in range(8):
                    ph_ = psH[k // 4]
                    T(lambda e, ph_=ph_, k=k, h2_=h2_: e.transpose(out=ph_.t[:, (k % 4) * 128:(k % 4 + 1) * 128], in_=h2_.t[:, k * 128:(k + 1) * 128],
                                                                 identity=identf.t[:]), r=[h2_, identf], w=[ph_])
                A(lambda e, hT_=hT_: e.copy(out=hT_.t[:, 0:4, :], in_=psH[0].t[:, 0:512].rearrange("p (k s) -> p k s", k=4)), r=[psH[0]], w=[hT_])
                V(lambda e, hT_=hT_: e.tensor_copy(out=hT_.t[:, 4:8, :], in_=psH[1].t[:, 0:512].rearrange("p (k s) -> p k s", k=4)), r=[psH[1]], w=[hT_])
                pl = psL[n % 2]
                for k in range(8):
                    T(lambda e, pl=pl, k=k, hT_=hT_: e.matmul(pl.t[:, 0:16], lhsT=hT_.t[:, k, :], rhs=wrs.t[:, k, :], start=(k == 0), stop=(k == 7)),
                      r=[hT_, wrs], w=[pl])
                V(lambda e, pl=pl, n=n: e.tensor_copy(out=lgall.t[:, n, :], in_=pl.t[:, 0:16]), r=[pl], cw=[lgall])

            for step in range(16 + 2):
                if step < 16:
                    p5_a(step)
                if 0 <= step - 1 < 16:
                    p5_b(step - 1)
                if 0 <= step - 2 < 16:
                    p5_c(step - 2)
            V(lambda e: e.tensor_reduce(out=smx.t[:, 0:16], in_=lgall.t[:], axis=AX.X, op=ALU.max), r=[lgall], w=[smx])
            for n in range(16):
                V(lambda e, n=n: e.tensor_scalar(out=lgall.t[:, n, :], in0=lgall.t[:, n, :], scalar1=smx.t[:, n:n + 1], scalar2=None, op0=ALU.subtract),
                  r=[lgall, smx], w=[lgall])
            A(lambda e: e.activation(out=lgall.t[:].rearrange("p a b -> p (a b)"), in_=lgall.t[:].rearrange("p a b -> p (a b)"), func=AF.Exp),
              r=[lgall], w=[lgall])
            V(lambda e: e.tensor_reduce(out=smx.t[:, 16:32], in_=lgall.t[:], axis=AX.X, op=ALU.add), r=[lgall], w=[smx])
            V(lambda e: e.reciprocal(out=smx.t[:, 16:32], in_=smx.t[:, 16:32]), r=[smx], w=[smx])
            for n in range(16):
                V(lambda e, n=n: e.tensor_scalar(out=lgall.t[:, n, :], in0=lgall.t[:, n, :], scalar1=smx.t[:, 16 + n:17 + n], scalar2=None, op0=ALU.mult),
                  r=[lgall, smx], w=[lgall])
            p.dma("sync", lambda e: e.dma_start(out=aff_in.rearrange("(n p) e -> p n e", p=128), in_=lgall.t[:]), "aff_in", reads=[lgall], writes=[R_affin])
            p.dma("gpsimd", lambda e: e.collective_compute("AllGather", ALU.bypass, replica_groups=RG, ins=[aff_in], outs=[aff_all]),
                  "ag2", reads=[R_affin], writes=[R_affall], inc=1)
            for c4 in range(4):
                for r4 in range(4):
                    p.dma("sync", lambda e, c4=c4, r4=r4: e.dma_start(out=h2_tab[2048 * r4 + 512 * c4:2048 * r4 + 512 * (c4 + 1), :],
                                                                      in_=h2_all[c4][512 * r4:512 * (r4 + 1), :]),
                          "h2tab", reads=[R_h2all[c4]], writes=[R_h2tab])
            p.dma("sync", lambda e: e.dma_start(out=aff_tab, in_=aff_all), "afftab", reads=[R_affall], writes=[R_afftab])
            p.barrier()
        if stop_after <= 5:
            if debug:
                tap(dbg["x1"], x1_d, [R_x1])
                tap(dbg["aff"], aff_in, [R_affin])
            p.finish()
            return nc, p, dbg

        with ExitStack() as ph:
            Aall = mk(ph, "Aall", [128, 64, 16], F32)
            A4 = mk(ph, "A4", [128, 4, 64], F32)
            cmp_ = mk(ph, "cmp", [128, 4, 64], F32)
            cc0 = mk(ph, "cc0", [128, 4, 64], F32)
            cc1 = mk(ph, "cc1", [128, 4, 64], F32)
            lo = mk(ph, "lo", [128, 4], F32)
            mid = mk(ph, "mid", [128, 4], F32)
            cnt = mk(ph, "cnt", [128, 4], F32)
            ge = mk(ph, "ge", [128, 4], F32)
            offs = mk(ph, "offs", [128, 4], F32)
            tris = mk(ph, "tris", [128, 128], F32)
            slot = mk(ph, "slot", [128, 8], F32)
            tokf = mk(ph, "tokf", [128, 32], F32)
            cb = [mk(ph, f"cb{i}", [128, S], F32) for i in range(2)]
            junk = mk(ph, "junk", [128, S], BF16)
            psC = mk(ph, "psC", None, F32, psum=True)
            ld("sync", Aall, Aall.t[:], aff_tab.rearrange("(p j) e -> p j e", j=64), reads=[R_afftab])
            ld("sync", tris, tris.t[:], triin)
            ld("sync", slot, slot.t[:], slotin)
            for i in range(4):
                V(lambda e, i=i: e.tensor_scalar(out=A4.t[:, i, :], in0=Aall.t[:, :, i], scalar1=ohs.t[:, 0:1], scalar2=None, op0=ALU.mult),
                  r=[Aall, ohs], w=[A4])
                for r in range(1, 4):
                    V(lambda e, i=i, r=r: e.scalar_tensor_tensor(out=A4.t[:, i, :], in0=Aall.t[:, :, 4 * r + i], scalar=ohs.t[:, r:r + 1], in1=A4.t[:, i, :],
                                                                 op0=ALU.mult, op1=ALU.add), r=[Aall, ohs, A4], w=[A4])
            V(lambda e: e.memset(lo.t[:], 0.0), w=[lo])
            for it in range(26):
                wv = 2.0 ** (-(it + 1))
                V(lambda e, wv=wv: e.tensor_scalar(out=mid.t[:], in0=lo.t[:], scalar1=wv, scalar2=None, op0=ALU.add), r=[lo], w=[mid])
                V(lambda e: e.memset(cnt.t[:], 0.0), w=[cnt])
                for i in range(4):
                    V(lambda e, i=i: e.tensor_scalar(out=cmp_.t[:, i, :], in0=A4.t[:, i, :], scalar1=mid.t[:, i:i + 1], scalar2=0.0, op0=ALU.is_gt,
                                                    op1=ALU.add, accum_out=cnt.t[:, i:i + 1]), r=[A4, mid, cnt], w=[cmp_, cnt])
                T(lambda e: e.matmul(psC.t[:, 0:4], lhsT=onesf.t[:], rhs=cnt.t[:], start=True, stop=True), r=[onesf, cnt], w=[psC])
                V(lambda e: e.tensor_scalar(out=ge.t[:], in0=psC.t[:, 0:4], scalar1=CAP - 0.5, scalar2=None, op0=ALU.is_ge), r=[psC], w=[ge])
                V(lambda e, wv=wv: e.scalar_tensor_tensor(out=lo.t[:], in0=ge.t[:], scalar=wv, in1=lo.t[:], op0=ALU.mult, op1=ALU.add), r=[ge, lo], w=[lo])
            for i in range(4):
                V(lambda e, i=i: e.tensor_scalar(out=cc0.t[:, i, :], in0=A4.t[:, i, :], scalar1=lo.t[:, i:i + 1], scalar2=None, op0=ALU.is_gt),
                  r=[A4, lo], w=[cc0])
            ca, cbuf = cc0, cc1
            for sh in (1, 2, 4, 8, 16, 32):
                V(lambda e, ca=ca, cbuf=cbuf, sh=sh: e.tensor_tensor(out=cbuf.t[:, :, sh:64], in0=ca.t[:, :, sh:64], in1=ca.t[:, :, 0:64 - sh], op=ALU.add),
                  r=[ca], w=[cbuf])
                V(lambda e, ca=ca, cbuf=cbuf, sh=sh: e.tensor_copy(out=cbuf.t[:, :, 0:sh], in_=ca.t[:, :, 0:sh]), r=[ca], w=[cbuf])
                ca, cbuf = cbuf, ca
            V(lambda e, ca=ca: e.tensor_copy(out=cnt.t[:], in_=ca.t[:, :, 63]), r=[ca], w=[cnt])
            T(lambda e: e.matmul(psC.t[:, 0:4], lhsT=tris.t[:], rhs=cnt.t[:], start=True, stop=True), r=[tris, cnt], w=[psC])
            V(lambda e: e.tensor_copy(out=offs.t[:], in_=psC.t[:, 0:4]), r=[psC], w=[offs])
            cdr_v = cdr.rearrange("e (p j) -> e p j", j=64)
            for i in range(4):
                V(lambda e, i=i, ca=ca: e.tensor_scalar(out=ca.t[:, i, :], in0=ca.t[:, i, :], scalar1=offs.t[:, i:i + 1], scalar2=None, op0=ALU.add),
                  r=[ca, offs], w=[ca])
                p.dma("sync", lambda e, i=i, ca=ca: e.dma_start(out=cdr_v[i], in_=ca.t[:, i, :]), "cdr", reads=[ca], writes=[R_cdr])
            V(lambda e: e.memset(tokf.t[:], 0.0), w=[tokf])
            for i in range(4):
                cb_ = cb[i % 2]
                ld("sync", cb_, cb_.t[:], cdr[i:i + 1, :].partition_broadcast(128), reads=[R_cdr])
                for st in range(8):
                    col = i * 8 + st
                    V(lambda e, cb_=cb_, st=st, col=col: e.tensor_scalar(out=junk.t[:], in0=cb_.t[:], scalar1=slot.t[:, st:st + 1], scalar2=0.0,
                                                                       op0=ALU.is_le, op1=ALU.add, accum_out=tokf.t[:, col:col + 1]),
                      r=[cb_, slot, tokf], w=[junk, tokf])
            V(lambda e: e.tensor_copy(out=toki.t[:], in_=tokf.t[:]), r=[tokf], w=[toki])
            if debug and stop_after == 6:
                tap(dbg["tok"], tokf.t[:], [tokf])
            p.barrier()
        if stop_after <= 6:
            if debug:
                tap(dbg["x1"], x1_d, [R_x1])
                tap(dbg["aff"], aff_in, [R_affin])
            p.finish()
            return nc, p, dbg

        with ExitStack() as ph:
            xeT = [mk(ph, f"xeT{i}", [128, 8, CAP], BF16) for i in range(2)]
            hT = mk(ph, "hT", [128, 16, CAP], BF16)
            wdb = [mk(ph, f"wdb{i}", [128, 16, D], BF16) for i in range(2)]
            NPC = 8
            wgb = [mk(ph, f"wgb{i}", [128, 8, 256], BF16) for i in range(3)]
            wub = [mk(ph, f"wub{i}", [128, 8, 256], BF16) for i in range(3)]
            xet = [mk(ph, f"xet{i}", [128, D], BF16) for i in range(2)]
            gat = [mk(ph, f"gat{i}", [128, 16], F32) for i in range(2)]
            gate = [mk(ph, f"gate{i}", [128, 8], F32) for i in range(2)]
            yet = [mk(ph, f"yet{i}", [128, D], F32) for i in range(2)]
            sgt = [mk(ph, f"sgt{i}", [128, 512], BF16) for i in range(2)]
            psXT = mk(ph, "psXT", None, BF16, psum=True)
            psG = [mk(ph, f"psG{i}", None, F32, psum=True) for i in range(2)]
            psU = [mk(ph, f"psU{i}", None, F32, psum=True) for i in range(2)]
            psY = [mk(ph, f"psYd{i}", None, F32, psum=True) for i in range(2)]
            cnts = {"g": 0, "pc": 0, "m": 0, "y": 0}

            def load_piece(i, pc):
                ws = (i * NPC + pc) % 3
                wg_v = wg[i].rearrange("(k p) f -> p k f", p=128)
                wu_v = wu[i].rearrange("(k p) f -> p k f", p=128)
                ld("gpsimd", wgb[ws], wgb[ws].t[:], wg_v[:, :, pc * 256:(pc + 1) * 256])
                ld("gpsimd", wub[ws], wub[ws].t[:], wu_v[:, :, pc * 256:(pc + 1) * 256])

            def gather_expert(i):
                xT = xeT[i % 2]
                gt_ = gate[i % 2]
                for st in range(8):
                    col = i * 8 + st
                    xe_ = xet[cnts["g"] % 2]
                    ga_ = gat[cnts["g"] % 2]
                    cnts["g"] += 1
                    p.dma("gpsimd", lambda e, xe_=xe_, col=col: e.indirect_dma_start(
                        out=xe_.t[:], out_offset=None, in_=h2_tab, in_offset=bass.IndirectOffsetOnAxis(ap=toki.t[:, col:col + 1], axis=0)),
                        xe_.r.name, reads=[R_h2tab, toki], writes=[xe_])
                    p.dma("gpsimd", lambda e, ga_=ga_, col=col: e.indirect_dma_start(
                        out=ga_.t[:], out_offset=None, in_=aff_tab, in_offset=bass.IndirectOffsetOnAxis(ap=toki.t[:, col:col + 1], axis=0)),
                        ga_.r.name, reads=[R_afftab, toki], writes=[ga_])
                    V(lambda e, ga_=ga_, gt_=gt_, st=st, i=i: e.tensor_scalar(out=gt_.t[:, st:st + 1], in0=ga_.t[:, i:i + 1], scalar1=ohs.t[:, 0:1], scalar2=None,
                                                                            op0=ALU.mult), r=[ga_, ohs], w=[gt_])
                    for r in range(1, 4):
                        V(lambda e, ga_=ga_, gt_=gt_, st=st, i=i, r=r: e.scalar_tensor_tensor(
                            out=gt_.t[:, st:st + 1], in0=ga_.t[:, 4 * r + i:4 * r + i + 1], scalar=ohs.t[:, r:r + 1], in1=gt_.t[:, st:st + 1],
                            op0=ALU.mult, op1=ALU.add), r=[ga_, ohs, gt_], w=[gt_])
                    for k in range(8):
                        T(lambda e, k=k, xe_=xe_: e.transpose(out=psXT.t[:, k * 128:(k + 1) * 128], in_=xe_.t[:, k * 128:(k + 1) * 128], identity=identb.t[:]),
                          r=[xe_, identb], w=[psXT])
                    if st % 2 == 0:
                        A(lambda e, st=st, xT=xT: e.copy(out=xT.t[:, :, st * 128:(st + 1) * 128], in_=psXT.t[:].rearrange("p (k s) -> p k s", k=8)),
                          r=[psXT], cw=[xT])
                    else:
                        V(lambda e, st=st, xT=xT: e.tensor_copy(out=xT.t[:, :, st * 128:(st + 1) * 128], in_=psXT.t[:].rearrange("p (k s) -> p k s", k=8)),
                          r=[psXT], cw=[xT])

            ld("gpsimd", wdb[0], wdb[0].t[:], wd[0].rearrange("(k p) d -> p k d", p=128))
            gather_expert(0)
            load_piece(0, 0)
            load_piece(0, 1)
            for i in range(4):
                xT = xeT[i % 2]
                gt_ = gate[i % 2]
                wd_ = wdb[i % 2]
                for pc in range(NPC):
                    if pc + 2 < NPC:
                        load_piece(i, pc + 2)
                    ws = (i * NPC + pc) % 3
                    wg_, wu_ = wgb[ws], wub[ws]
                    for fc in range(2):
                        f = pc * 2 + fc
                        for half in range(2):
                            pg, pu, sg_ = psG[cnts["m"] % 2], psU[cnts["m"] % 2], sgt[cnts["m"] % 2]
                            cnts["m"] += 1
                            hs = slice(512 * half, 512 * (half + 1))
                            for k in range(8):
                                T(lambda e, pg=pg, k=k, wg_=wg_, fc=fc, hs=hs, xT=xT: e.matmul(pg.t[:, 0:512], lhsT=wg_.t[:, k, fc * 128:(fc + 1) * 128], rhs=xT.t[:, k, hs],
                                                                                           start=(k == 0), stop=(k == 7)), r=[wg_, xT], w=[pg])
                            for k in range(8):
                                T(lambda e, pu=pu, k=k, wu_=wu_, fc=fc, hs=hs, xT=xT: e.matmul(pu.t[:, 0:512], lhsT=wu_.t[:, k, fc * 128:(fc + 1) * 128], rhs=xT.t[:, k, hs],
                                                                                           start=(k == 0), stop=(k == 7)), r=[wu_, xT], w=[pu])
                            A(lambda e, pg=pg, sg_=sg_: e.activation(out=sg_.t[:], in_=pg.t[:, 0:512], func=AF.Silu), r=[pg], w=[sg_])
                            V(lambda e, pu=pu, sg_=sg_, f=f, hs=hs: e.tensor_tensor(out=hT.t[:, f, hs], in0=sg_.t[:], in1=pu.t[:, 0:512], op=ALU.mult),
                              r=[sg_, pu], cw=[hT])
                if i + 1 < 4:
                    ld("gpsimd", wdb[(i + 1) % 2], wdb[(i + 1) % 2].t[:], wd[i + 1].rearrange("(k p) d -> p k d", p=128))
                    gather_expert(i + 1)
                    load_piece(i + 1, 0)
                    load_piece(i + 1, 1)
                for st in range(8):
                    col = i * 8 + st
                    ye_ = yet[st % 2]
                    for dh in range(2):
                        py = psY[cnts["y"] % 2]
                        cnts["y"] += 1
                        ds_ = slice(512 * dh, 512 * (dh + 1))
                        for f in range(16):
                            T(lambda e, py=py, f=f, st=st, ds_=ds_, wd_=wd_: e.matmul(py.t[:, 0:512], lhsT=hT.t[:, f, st * 128:(st + 1) * 128], rhs=wd_.t[:, f, ds_],
                                                                                   start=(f == 0), stop=(f == 15)), r=[hT, wd_], w=[py])
                        if dh == 0:
                            A(lambda e, py=py, ye_=ye_, ds_=ds_, gt_=gt_, st=st: e.activation(out=ye_.t[:, ds_], in_=py.t[:, 0:512], func=AF.Identity,
                                                                                           scale=gt_.t[:, st:st + 1]), r=[py, gt_], cw=[ye_])
                        else:
                            V(lambda e, py=py, ye_=ye_, ds_=ds_, gt_=gt_, st=st: e.tensor_scalar(out=ye_.t[:, ds_], in0=py.t[:, 0:512], scalar1=gt_.t[:, st:st + 1],
                                                                                              scalar2=None, op0=ALU.mult), r=[py, gt_], cw=[ye_])
                    p.dma("gpsimd", lambda e, ye_=ye_, col=col: e.indirect_dma_start(
                        out=contrib, out_offset=bass.IndirectOffsetOnAxis(ap=toki.t[:, col:col + 1], axis=0), in_=ye_.t[:], in_offset=None,
                        compute_op=ALU.add), "scat", reads=[ye_, toki, R_contrib], writes=[R_contrib])
            p.dma("gpsimd", lambda e: e.collective_compute("ReduceScatter", ALU.add, replica_groups=RG, ins=[contrib], outs=[moe_d]),
                  "rs2", reads=[R_contrib], writes=[R_moe], inc=1)
            p.barrier()
        if debug and stop_after == 7:
            tap(dbg["moe"], moe_d, [R_moe])

        with ExitStack() as ph:
            mo = [mk(ph, f"mo{i}", [128, D], F32) for i in range(2)]
            x1r = [mk(ph, f"x1r{i}", [128, D], F32) for i in range(2)]
            sq8 = mk(ph, "sq8", [128, D], BF16)
            s8 = [mk(ph, f"s8{i}", [128, 2], F32) for i in range(2)]
            for n in range(16):
                ts = slice(128 * n, 128 * (n + 1))
                m_, x_, s_ = mo[n % 2], x1r[n % 2], s8[n % 2]
                ld("sync", m_, m_.t[:], moe_d[ts, :], reads=[R_moe])
                ld("sync", x_, x_.t[:], x1_d[ts, :], reads=[R_x1])
                A(lambda e, m_=m_, s_=s_: e.activation(out=sq8.t[:], in_=m_.t[:], func=AF.Square, accum_out=s_.t[:, 0:1]), r=[m_], w=[sq8, s_])
                A(lambda e, s_=s_: e.activation(out=s_.t[:, 1:2], in_=s_.t[:, 0:1], func=AF.Sqrt, scale=1.0 / D, bias=cst.t[:, 1:2]), r=[s_, cst], w=[s_])
                V(lambda e, s_=s_: e.reciprocal(out=s_.t[:, 1:2], in_=s_.t[:, 1:2]), r=[s_], w=[s_])
                V(lambda e, m_=m_, s_=s_: e.scalar_tensor_tensor(out=m_.t[:], in0=m_.t[:], scalar=s_.t[:, 1:2], in1=rows["gf"].t[:],
                                                               op0=ALU.mult, op1=ALU.mult), r=[m_, s_, rows["gf"]], w=[m_])
                PL(lambda e, m_=m_, x_=x_: e.tensor_tensor(out=m_.t[:], in0=m_.t[:], in1=x_.t[:], op=ALU.add), r=[m_, x_], w=[m_])
                p.dma("sync", lambda e, ts=ts, m_=m_: e.dma_start(out=out[ts, :], in_=m_.t[:]), "out", reads=[m_], writes=[R_out])
            p.barrier()
    p.finish()
    return nc, p, dbg


def _consts(q):
    j = np.arange(128, dtype=np.float32)[:, None]
    i = np.arange(128, dtype=np.float32)[None, :]
    rc = np.zeros((128, 514), np.float32)
    rc[:, 0:128] = np.maximum(i - j, 0)
    rc[:, 128:256] = np.maximum(j - i, 0)
    rc[:, 256:384] = i + 1
    rc[:, 384:512] = 128 - i
    rc[:, 512] = 127 - j[:, 0]
    rc[:, 513] = j[:, 0]
    masks = np.zeros((128, 2, 3, 3, 2, 128), np.float32)
    kk = np.arange(128)[:, None, None]
    jj = np.arange(2)[None, :, None]
    ii = np.arange(128)[None, None, :]
    delta = np.abs(kk + 128 * jj - 64 - ii).astype(np.float32)
    band = (delta <= 64).astype(np.float32)
    for h in range(2):
        slope = SLOPES[2 * q + h]
        for c, d in enumerate(CONFIGS):
            m = band * np.exp(-slope * d * delta)
            masks[:, h, c, 0] = m
            m1 = m.copy()
            m1[0:64, 0, :] = 0
            masks[:, h, c, 1] = m1
            m2 = m.copy()
            m2[64:128, 1, :] = 0
            masks[:, h, c, 2] = m2
    slotid = (128 * np.arange(8)[None, :] + np.arange(128)[:, None]).astype(np.float32)
    tri = (np.arange(128)[:, None] < np.arange(128)[None, :]).astype(np.float32)
    return rc, masks.reshape(128, 18 * 256), slotid, tri


def prep(inputs):
    f = lambda a: np.ascontiguousarray(np.asarray(a, dtype=np.float32))
    x, c = f(inputs["x"]), f(inputs["c"])
    w_in = f(inputs["w_in"])[0]
    w_out = f(inputs["w_out"])[0]
    col = lambda v: np.ascontiguousarray(v.reshape(-1, 128).T)
    vcols = np.concatenate([col(f(inputs["b_ada"])[0]), col(f(inputs["g_pre_mix"])[0]), col(f(inputs["g_post_mix"])[0]),
                            col(f(inputs["g_pre_ffn"])[0]), col(f(inputs["g_post_ffn"])[0])], axis=1)
    wada = f(inputs["w_ada"])[0]
    wr = f(inputs["w_router"])[0]
    wge, wue, wde = f(inputs["w_gate_e"])[0], f(inputs["w_up_e"])[0], f(inputs["w_down_e"])[0]
    df, db = f(inputs["ret_decay_fwd"])[0], f(inputs["ret_decay_bwd"])[0]
    ident = np.eye(128, dtype=np.float32)
    maps = []
    for i in range(8):
        b, q = i // 4, i % 4
        rq = np.arange(64 * q, 64 * q + 64)
        rk = 256 + rq
        rvc = 512 + np.arange(128 * q, 128 * q + 128)
        rgc = 1024 + np.arange(128 * q, 128 * q + 128)
        aqc = 1536 + np.arange(128 * q, 128 * q + 128)
        akc = 2048 + np.arange(128 * q, 128 * q + 128)
        avc = 2560 + np.arange(128 * q, 128 * q + 128)
        cols = np.concatenate([rq, rk, aqc, akc, avc, rk, rvc, rgc])
        rows = np.concatenate([np.concatenate([np.arange(128 * r_, 128 * r_ + 128), 512 + np.arange(128 * r_, 128 * r_ + 128)]) for r_ in range(4)])
        rc, masks, slotid, tri = _consts(q)
        oh = np.zeros((128, 4), np.float32)
        oh[:, q] = 1.0
        dec = np.zeros((128, 2), np.float32)
        dec[:, 0] = df[q]
        dec[:, 1] = db[q]
        maps.append({
            "xb": x[b], "xo": np.ascontiguousarray(x[b, OWN * q:OWN * (q + 1)]), "ccol": col(c[b]),
            "wada": wada, "vcols": vcols, "win": np.ascontiguousarray(w_in[:, cols]), "dec": dec,
            "wout": np.ascontiguousarray(w_out[rows]), "wr": wr, "oh": oh,
            "wg": np.ascontiguousarray(wge[4 * q:4 * q + 4]), "wu": np.ascontiguousarray(wue[4 * q:4 * q + 4]),
            "wd": np.ascontiguousarray(wde[4 * q:4 * q + 4]),
            "ident": ident, "masks": masks, "rc": rc, "slotid": slotid, "tri": tri,
        })
    return maps


_NC_CACHE = {}


def kernel(**inputs):
    maps = prep(inputs)
    if "nc" not in _NC_CACHE:
        _NC_CACHE["nc"] = build()[0]
    res = run_bass_kernel_spmd(_NC_CACHE["nc"], maps, core_ids=list(range(8)))
    out = np.zeros((2, S, D), np.float32)
    for i in range(8):
        b, q = i // 4, i % 4
        out[b, OWN * q:OWN * (q + 1)] = res.results[i]["out"]
    return out
```

```python
import numpy as np
from contextlib import ExitStack
import concourse.bass as bass
import concourse.mybir as mybir
from concourse.bass_utils import run_bass_kernel_spmd

F32 = mybir.dt.float32
BF16 = mybir.dt.bfloat16
I32 = mybir.dt.int32
ALU = mybir.AluOpType
AF = mybir.ActivationFunctionType
AX = mybir.AxisListType
ENGS = ["sync", "scalar", "vector", "gpsimd", "tensor"]

S = 8192
D = 1024
NT = S // 128
OWN = 2048
CAP = 1024
LN8 = -2.0794415416798357
SLOPES = [2.0 ** (-(h + 1)) for h in range(8)]
CONFIGS = (1, 4, 16)


class Reg:
    __slots__ = ("w", "r", "name", "psum", "cw")

    def __init__(self, name="", psum=False):
        self.w = None
        self.cw = {}
        self.r = {}
        self.name = name
        self.psum = psum


class Buf:
    __slots__ = ("t", "r")

    def __init__(self, t, name):
        self.t = t
        self.r = Reg(name)


class Prog:
    def __init__(self, nc):
        self.nc = nc
        self.stack = ExitStack()
        self.ops = {e: [] for e in ENGS}
        self.cnt = {e: 0 for e in ENGS}
        self.sem = {e: self.stack.enter_context(nc.semaphore(f"c_{e}")) for e in ENGS}
        self.known = {e: {} for e in ENGS}
        self.dsem = {}
        self.dcnt = {}

    def _need(self, eng, tok, waits):
        if tok is None:
            return
        sem, val, src = tok
        if src == eng and eng == "tensor":
            return
        if src == eng and val <= self.cnt[eng] - 3:
            return
        k = self.known[eng]
        if k.get(id(sem), 0) >= val:
            return
        k[id(sem)] = val
        waits.append((sem, val))

    def _deps(self, eng, reads, writes, cwrites=()):
        waits = []
        for c in cwrites:
            self._need(eng, c.w, waits)
            for t in c.r.values():
                self._need(eng, t, waits)
        for r in reads:
            self._need(eng, r.w, waits)
            for t in r.cw.values():
                self._need(eng, t, waits)
            if r.psum:
                for t in r.r.values():
                    if t[2] != eng:
                        self._need(eng, t, waits)
        for w in writes:
            self._need(eng, w.w, waits)
            for t in w.cw.values():
                self._need(eng, t, waits)
            for t in w.r.values():
                self._need(eng, t, waits)
        best = {}
        for sem, val in waits:
            if id(sem) not in best or best[id(sem)][1] < val:
                best[id(sem)] = (sem, val)
        return list(best.values())

    def _commit(self, tok, reads, writes, cwrites=()):
        for r in reads:
            r.r[id(tok[0])] = tok
        for w in writes:
            w.w = tok
            w.cw = {}
            w.r = {}
        for c in cwrites:
            c.cw[id(tok[0])] = tok

    def op(self, eng, fn, reads=(), writes=(), cwrites=()):
        reads = [x.r if isinstance(x, Buf) else x for x in reads]
        writes = [x.r if isinstance(x, Buf) else x for x in writes]
        cwrites = [x.r if isinstance(x, Buf) else x for x in cwrites]
        waits = self._deps(eng, reads, writes, cwrites)
        self.cnt[eng] += 1
        tok = (self.sem[eng], self.cnt[eng], eng)
        self.ops[eng].append((waits, fn, (self.sem[eng], 1)))
        self._commit(tok, reads, writes, cwrites)
        return tok

    def dma(self, q, fn, key, reads=(), writes=(), inc=16):
        reads = [x.r if isinstance(x, Buf) else x for x in reads]
        writes = [x.r if isinstance(x, Buf) else x for x in writes]
        if key not in self.dsem:
            self.dsem[key] = self.stack.enter_context(self.nc.semaphore(f"d_{key}"))
            self.dcnt[key] = 0
        waits = self._deps(q, reads, writes)
        self.dcnt[key] += inc
        tok = (self.dsem[key], self.dcnt[key], "dma")
        self.ops[q].append((waits, fn, (self.dsem[key], inc)))
        self._commit(tok, reads, writes)
        return tok

    def barrier(self):
        for eng in ENGS:
            waits = []
            for e in ENGS:
                if self.cnt[e] > 0 and e != eng:
                    self._need(eng, (self.sem[e], self.cnt[e], e), waits)
            for key, sem in self.dsem.items():
                self._need(eng, (sem, self.dcnt[key], "dma"), waits)
            self.ops[eng].append((waits, None, None))

    def emit(self):
        return

    def finish(self):
        nc = self.nc
        ops = self.ops
        self.ops = {e: [] for e in ENGS}

        def replay(name, e):
            for waits, fn, inc in ops[name]:
                for sem, val in waits:
                    e.wait_ge(sem, val)
                if fn is not None:
                    fn(e).then_inc(inc[0], inc[1])

        with nc.Block() as block:
            @block.sync
            def _(e):
                replay("sync", e)

            @block.scalar
            def _(e):
                replay("scalar", e)

            @block.vector
            def _(e):
                replay("vector", e)

            @block.gpsimd
            def _(e):
                replay("gpsimd", e)

            @block.tensor
            def _(e):
                replay("tensor", e)


def build(stop_after=99, debug=False):
    import os
    NGRP = int(os.environ.get('NGRP', '16'))
    FLAGS = os.environ.get('KFLAGS', '').split(',')
    nc = bass.Bass("TRN2", target_bir_lowering=False)

    def din(name, shape, dt=F32):
        return nc.dram_tensor(name, list(shape), dt, kind="ExternalInput").ap()

    def dsc(name, shape, dt=F32):
        return nc.dram_tensor(name, list(shape), dt).ap()

    xb = din("xb", [S, D])
    xo = din("xo", [OWN, D])
    ccol = din("ccol", [128, 8])
    wada = din("wada", [D, 6 * D])
    vcols = din("vcols", [128, 80])
    win = din("win", [D, 832])
    decin = din("dec", [128, 2])
    wout = din("wout", [D, D])
    wrin = din("wr", [D, 16])
    ohin = din("oh", [128, 4])
    if stop_after >= 7:
        wg = din("wg", [4, D, 2048])
        wu = din("wu", [4, D, 2048])
        wd = din("wd", [4, 2048, D])
    identin = din("ident", [128, 128])
    masksin = din("masks", [128, 18 * 256])
    rcin = din("rc", [128, 514])
    slotin = din("slotid", [128, 8])
    triin = din("tri", [128, 128])
    out = nc.dram_tensor("out", [OWN, D], F32, kind="ExternalOutput").ap()
    dbg = {}
    if debug:
        if stop_after in (2, 3):
            dbg["yT"] = nc.dram_tensor("dbg_yT", [256, S], F32, kind="ExternalOutput").ap()
        if stop_after in (5, 6):
            dbg["x1"] = nc.dram_tensor("dbg_x1", [OWN, D], F32, kind="ExternalOutput").ap()
            dbg["aff"] = nc.dram_tensor("dbg_aff", [OWN, 16], F32, kind="ExternalOutput").ap()
            dbg["tok"] = nc.dram_tensor("dbg_tok", [128, 32], F32, kind="ExternalOutput").ap()
        if stop_after == 7:
            dbg["moe"] = nc.dram_tensor("dbg_moe", [OWN, D], F32, kind="ExternalOutput").ap()

    aq_d = dsc("aq_d", [128, S], BF16)
    ak_d = dsc("ak_d", [128, S], BF16)
    av_d = dsc("av_d", [128, S], BF16)

    h2_in = dsc("h2_in", [OWN, D], BF16)
    h2_all = [dsc(f"h2_all{c4}", [2048, D], BF16) for c4 in range(4)]
    h2_tab = dsc("h2_tab", [S, D], BF16)
    aff_in = dsc("aff_in", [OWN, 16])
    aff_all = dsc("aff_all", [S, 16])
    aff_tab = dsc("aff_tab", [S, 16])
    cdr = dsc("cdr", [4, S])
    contrib = dsc("contrib", [S, D])
    moe_d = dsc("moe_d", [OWN, D])
    R_aq, R_ak, R_av = Reg("aq_d"), Reg("ak_d"), Reg("av_d")

    R_h2in, R_h2tab = Reg("h2in"), Reg("h2tab")
    R_h2all = [Reg(f"h2all{c}") for c in range(4)]
    R_affin, R_affall, R_afftab = Reg("affin"), Reg("affall"), Reg("afftab")
    R_cdr, R_contrib, R_moe, R_out = Reg("cdr"), Reg("contrib"), Reg("moe"), Reg("out")
    RG = [[0, 1, 2, 3], [4, 5, 6, 7]] if 'half' not in FLAGS else [[0, 1, 2, 3]]

    p = Prog(nc)
    GS = p.stack

    def mk(stack, name, shape, dt, psum=False):
        if psum:
            t = stack.enter_context(nc.psum_tensor(name, [128, 512 if dt == F32 else 1024], dt))
        else:
            t = stack.enter_context(nc.sbuf_tensor(name, list(shape), dt))
        b = Buf(t, name)
        b.r.psum = psum
        return b

    V = lambda fn, r=(), w=(), cw=(): p.op("vector", fn, r, w, cw)
    A = lambda fn, r=(), w=(), cw=(): p.op("scalar", fn, r, w, cw)
    PL = lambda fn, r=(), w=(), cw=(): p.op("gpsimd", fn, r, w, cw)
    T = lambda fn, r=(), w=(), cw=(): p.op("tensor", fn, r, w, cw)

    def ld(q, dst, dst_ap, src_ap, reads=()):
        return p.dma(q, lambda e: e.dma_start(out=dst_ap, in_=src_ap), dst.r.name, reads=reads, writes=[dst])

    identf = mk(GS, "identf", [128, 128], F32)
    identb = mk(GS, "identb", [128, 128], BF16)
    onesf = mk(GS, "onesf", [128, 128], F32)
    vc = mk(GS, "vc", [128, 80], F32)
    mod = mk(GS, "mod", [128, 48], F32)
    der = mk(GS, "der", [128, 32], F32)
    ohs = mk(GS, "ohs", [128, 4], F32)
    ld("sync", identf, identf.t[:], identin)
    ld("gpsimd", identb, identb.t[:], identin)
    ld("sync", vc, vc.t[:], vcols)
    ld("sync", ohs, ohs.t[:], ohin)
    V(lambda e: e.memset(onesf.t[:], 1.0), w=[onesf])
    cst = mk(GS, "cst", [128, 4], F32)
    V(lambda e: e.memset(cst.t[:, 0:1], LN8), w=[cst])
    V(lambda e: e.memset(cst.t[:, 1:2], 1e-6), w=[cst])
    V(lambda e: e.memset(cst.t[:, 2:3], 1e-5), w=[cst])
    V(lambda e: e.memset(cst.t[:, 3:4], 0.0), w=[cst])
    ymix = ExitStack()
    yT = mk(ymix, "yT", [128, S], BF16)
    yA = mk(ymix, "yA", [64, S], BF16)
    yB = mk(ymix, "yB", [64, S], BF16)

    with ExitStack() as ph:
        cc = mk(ph, "cc", [128, 8], F32)
        scb = mk(ph, "scb", [128, 8], BF16)
        wa = [mk(ph, f"wa{i}", [128, 8, D], BF16) for i in range(2)]
        psm = mk(ph, "psm", [128, 48], F32, psum=True)
        ld("sync", cc, cc.t[:], ccol)
        A(lambda e: e.activation(out=scb.t[:], in_=cc.t[:], func=AF.Silu), r=[cc], w=[scb])
        wada_v = wada.rearrange("(k p) n -> p k n", p=128)
        for g in range(6):
            w_ = wa[g % 2]
            ld("gpsimd", w_, w_.t[:], wada_v[:, :, g * D:(g + 1) * D])
            for j in range(8):
                for k in range(8):
                    T(lambda e, w_=w_, g=g, j=j, k=k: e.matmul(
                        psm.t[:, g * 8 + j:g * 8 + j + 1], lhsT=w_.t[:, k, j * 128:(j + 1) * 128],
                        rhs=scb.t[:, k:k + 1], start=(k == 0), stop=(k == 7)), r=[w_, scb], w=[psm])
        V(lambda e: e.tensor_tensor(out=mod.t[:], in0=psm.t[:, 0:48], in1=vc.t[:, 0:48], op=ALU.add), r=[psm, vc], w=[mod])
        V(lambda e: e.scalar_tensor_tensor(out=der.t[:, 0:8], in0=mod.t[:, 8:16], scalar=1.0, in1=vc.t[:, 48:56],
                                           op0=ALU.add, op1=ALU.mult), r=[mod, vc], w=[der])
        V(lambda e: e.tensor_tensor(out=der.t[:, 8:16], in0=mod.t[:, 16:24], in1=vc.t[:, 56:64], op=ALU.mult), r=[mod, vc], w=[der])
        V(lambda e: e.scalar_tensor_tensor(out=der.t[:, 16:24], in0=mod.t[:, 32:40], scalar=1.0, in1=vc.t[:, 64:72],
                                           op0=ALU.add, op1=ALU.mult), r=[mod, vc], w=[der])
        V(lambda e: e.tensor_tensor(out=der.t[:, 24:32], in0=mod.t[:, 40:48], in1=vc.t[:, 72:80], op=ALU.mult), r=[mod, vc], w=[der])
        p.barrier()
        p.emit()

    if stop_after <= 0:
        p.finish()
        return nc, p, dbg
    with ExitStack() as ph:
        rqT = mk(ph, "rqT", [64, S], BF16)
        rkT = mk(ph, "rkT", [64, S], BF16)
        ktf = mk(ph, "ktf", [128, NT, 64], BF16)
        ktb = mk(ph, "ktb", [128, NT, 64], BF16)
        rv = mk(ph, "rv", [128, NT, 128], BF16)
        sg = mk(ph, "sg", [128, NT, 128], BF16)
        rcs = mk(ph, "rcs", [128, 514], F32)
        dcs = mk(ph, "dcs", [128, 2], F32)
        lg = mk(ph, "lg", [128, 2], F32)
        tfb = mk(ph, "tfb", [128, 4], F32)
        DT = mk(ph, "DT", [128, 128], F32)
        QF = mk(ph, "QF", [128, 128], BF16)
        QB = mk(ph, "QB", [128, 128], BF16)
        ld("sync", rcs, rcs.t[:], rcin)
        ld("sync", dcs, dcs.t[:], decin)
        A(lambda e: e.activation(out=lg.t[:], in_=dcs.t[:], func=AF.Exp, scale=-1.0), r=[dcs], w=[lg])
        V(lambda e: e.tensor_scalar(out=lg.t[:], in0=lg.t[:], scalar1=1.0, scalar2=None, op0=ALU.add), r=[lg], w=[lg])
        A(lambda e: e.activation(out=lg.t[:], in_=lg.t[:], func=AF.Ln), r=[lg], w=[lg])
        V(lambda e: e.tensor_scalar(out=lg.t[:], in0=lg.t[:], scalar1=-1.0, scalar2=None, op0=ALU.mult), r=[lg], w=[lg])
        A(lambda e: e.activation(out=tfb.t[:, 0:1], in_=rcs.t[:, 512:513], func=AF.Exp, scale=lg.t[:, 0:1], bias=cst.t[:, 0:1]), r=[rcs, lg, cst], w=[tfb])
        A(lambda e: e.activation(out=tfb.t[:, 1:2], in_=rcs.t[:, 513:514], func=AF.Exp, scale=lg.t[:, 1:2], bias=cst.t[:, 0:1]), r=[rcs, lg, cst], w=[tfb])
        A(lambda e: e.activation(out=tfb.t[:, 2:4], in_=lg.t[:, 0:2], func=AF.Exp, scale=128.0), r=[lg], w=[tfb])
        A(lambda e: e.activation(out=QF.t[:], in_=rcs.t[:, 256:384], func=AF.Exp, scale=lg.t[:, 0:1]), r=[rcs, lg], w=[QF])
        A(lambda e: e.activation(out=QB.t[:], in_=rcs.t[:, 384:512], func=AF.Exp, scale=lg.t[:, 1:2]), r=[rcs, lg], w=[QB])
        V(lambda e: e.tensor_scalar(out=DT.t[:], in0=rcs.t[:, 0:128], scalar1=lg.t[:, 0:1], scalar2=None, op0=ALU.mult), r=[rcs, lg], w=[DT])
        V(lambda e: e.scalar_tensor_tensor(out=DT.t[:], in0=rcs.t[:, 128:256], scalar=lg.t[:, 1:2], in1=DT.t[:],
                                           op0=ALU.mult, op1=ALU.add), r=[rcs, lg, DT], w=[DT])
        A(lambda e: e.activation(out=DT.t[:], in_=DT.t[:], func=AF.Exp, bias=cst.t[:, 0:1]), r=[DT, cst], w=[DT])

        if 'pre_only' in FLAGS:
            p.barrier()
            p.finish()
            return nc, p, dbg
        with ExitStack() as ph1:
            winb = mk(ph1, "winb", [128, 8, 832], BF16)
            xs = [mk(ph1, f"xs{i}", [128, D], F32) for i in range(2)]
            sqj = mk(ph1, "sqj", [128, D], BF16)
            ssq = [mk(ph1, f"ssq{i}", [128, 2], F32) for i in range(2)]
            xn = [mk(ph1, f"xn{i}", [128, D], BF16) for i in range(2)]
            h1T = [mk(ph1, f"h1T{i}", [128, 8, 512], BF16) for i in range(2)]
            stg = [[mk(ph1, f"stg{a}{i}", [128, 512], BF16) for i in range(2)] for a in range(3)]
            psXa = [mk(ph1, f"psXa{i}", None, BF16, psum=True) for i in range(2)]
            psXb = [mk(ph1, f"psXb{i}", None, BF16, psum=True) for i in range(2)]
            psF = [mk(ph1, f"psF{i}", [128, 512], F32, psum=True) for i in range(2)]
            psT = [mk(ph1, f"psT{i}", [128, 320], F32, psum=True) for i in range(2)]
            ld("gpsimd", winb, winb.t[:], win.rearrange("(k p) n -> p k n", p=128))
            zt = mk(ph1, "zt", [128, D], BF16)
            PL(lambda e: e.memset(zt.t[:], 0.0), w=[zt])
            for zi in range(64):
                p.dma("gpsimd", lambda e, zi=zi: e.dma_start(out=contrib[128 * zi:128 * (zi + 1), :], in_=zt.t[:]),
                      "contrib0", reads=[zt], writes=[R_contrib])
            xb_v = xb.rearrange("(n p) d -> p n d", p=128)
            fstate = {"f": 0}

            def prep_tile(Gi, tt, part):
                h1 = h1T[Gi % 2]
                n = 4 * Gi + tt
                x_ = xs[n % 2]
                s_ = ssq[n % 2]
                xn_ = xn[n % 2]
                pxa, pxb = psXa[n % 2], psXb[n % 2]
                if part == "a":
                  ld("sync", x_, x_.t[:], xb_v[:, n, :])
                  A(lambda e, x_=x_, s_=s_: e.activation(out=sqj.t[:], in_=x_.t[:], func=AF.Square, accum_out=s_.t[:, 0:1]),
                    r=[x_], w=[sqj, s_])
                  A(lambda e, s_=s_: e.activation(out=s_.t[:, 1:2], in_=s_.t[:, 0:1], func=AF.Sqrt, scale=1.0 / D, bias=cst.t[:, 1:2]),
                    r=[s_, cst], w=[s_])
                  V(lambda e, s_=s_: e.reciprocal(out=s_.t[:, 1:2], in_=s_.t[:, 1:2]), r=[s_], w=[s_])
                  V(lambda e, x_=x_, s_=s_, xn_=xn_: e.tensor_scalar(out=xn_.t[:], in0=x_.t[:], scalar1=s_.t[:, 1:2], scalar2=None,
                                                                   op0=ALU.mult), r=[x_, s_], w=[xn_])
                  return
                for k in range(8 if part == "t" else 0):
                    px = pxa if k % 2 == 0 else pxb
                    T(lambda e, k=k, px=px, xn_=xn_: e.transpose(out=px.t[:, (k // 2) * 128:(k // 2 + 1) * 128], in_=xn_.t[:, k * 128:(k + 1) * 128],
                                                               identity=identb.t[:]), r=[xn_, identb], w=[px])
                for k in range(8 if part == "e" else 0):
                    px = pxa if k % 2 == 0 else pxb
                    o_ = h1.t[:, k, tt * 128:(tt + 1) * 128]
                    i_ = px.t[:, (k // 2) * 128:(k // 2 + 1) * 128]
                    if k % 2 == 0:
                        A(lambda e, o_=o_, i_=i_, k=k: e.activation(out=o_, in_=i_, func=AF.Identity, scale=der.t[:, k:k + 1],
                                                                   bias=mod.t[:, k:k + 1]), r=[px, der, mod], cw=[h1])
                    else:
                        V(lambda e, o_=o_, i_=i_, k=k: e.tensor_scalar(out=o_, in0=i_, scalar1=der.t[:, k:k + 1], scalar2=mod.t[:, k:k + 1],
                                                                     op0=ALU.mult, op1=ALU.add), r=[px, der, mod], cw=[h1])

            FG = [(0, 64), (64, 64), (128, 128), (256, 128), (384, 128)]

            def mm_fgroup(Gi, fi, part):
                h1 = h1T[Gi % 2]
                c0, wdt = FG[fi]
                if part == "m":
                    fstate[(Gi, fi)] = psF[fstate["f"] % 2]
                    fstate["f"] += 1
                pf = fstate[(Gi, fi)]
                for k in range(8 if part == "m" else 0):
                    T(lambda e, pf=pf, k=k, c0=c0, wdt=wdt, h1=h1: e.matmul(pf.t[0:wdt, :], lhsT=winb.t[:, k, c0:c0 + wdt], rhs=h1.t[:, k, :],
                                                                         start=(k == 0), stop=(k == 7)), r=[winb, h1], w=[pf])
                if part == "m":
                    return
                sl = slice(Gi * 512, (Gi + 1) * 512)
                if fi == 0:
                    A(lambda e, pf=pf, sl=sl: e.copy(out=rqT.t[:, sl], in_=pf.t[0:64, :]), r=[pf], cw=[rqT])
                elif fi == 1:
                    V(lambda e, pf=pf, sl=sl: e.tensor_copy(out=rkT.t[:, sl], in_=pf.t[0:64, :]), r=[pf], cw=[rkT])
                else:
                    sb_ = stg[fi - 2][Gi % 2]
                    dr, dreg = [(aq_d, R_aq), (ak_d, R_ak), (av_d, R_av)][fi - 2]
                    if fi == 3:
                        V(lambda e, pf=pf, sb_=sb_: e.tensor_copy(out=sb_.t[:], in_=pf.t[:]), r=[pf], w=[sb_])
                    else:
                        A(lambda e, pf=pf, sb_=sb_: e.copy(out=sb_.t[:], in_=pf.t[:]), r=[pf], w=[sb_])
                    p.dma("sync", lambda e, dr=dr, sl=sl, sb_=sb_: e.dma_start(out=dr[:, sl], in_=sb_.t[:]), dreg.name,
                          reads=[sb_], writes=[dreg])

            def mm_ttile(Gi, tt, part):
                h1 = h1T[Gi % 2]
                n = 4 * Gi + tt
                pt = psT[n % 2]
                for k in range(8 if part == "m" else 0):
                    T(lambda e, pt=pt, k=k, tt=tt, h1=h1: e.matmul(pt.t[:, 0:320], lhsT=h1.t[:, k, tt * 128:(tt + 1) * 128], rhs=winb.t[:, k, 512:832],
                                                                 start=(k == 0), stop=(k == 7)), r=[winb, h1], w=[pt])
                if part == "m":
                    return
                A(lambda e, pt=pt, n=n: e.activation(out=ktf.t[:, n, :], in_=pt.t[:, 0:64], func=AF.Identity, scale=tfb.t[:, 0:1]),
                  r=[pt, tfb], cw=[ktf])
                A(lambda e, pt=pt, n=n: e.copy(out=sg.t[:, n, :], in_=pt.t[:, 192:320]), r=[pt], cw=[sg])
                V(lambda e, pt=pt, n=n: e.tensor_scalar(out=ktb.t[:, n, :], in0=pt.t[:, 0:64], scalar1=tfb.t[:, 1:2], scalar2=None,
                                                      op0=ALU.mult), r=[pt, tfb], cw=[ktb])
                V(lambda e, pt=pt, n=n: e.tensor_copy(out=rv.t[:, n, :], in_=pt.t[:, 64:192]), r=[pt], cw=[rv])

            for tt in range(4):
                prep_tile(0, tt, "a")
                prep_tile(0, tt, "t")
                prep_tile(0, tt, "e")
            for Gi in range(NGRP):
                for tt in range(4):
                    nxt = Gi + 1 < NGRP
                    if nxt:
                        prep_tile(Gi + 1, tt, "a")
                    fis = ([0, 1], [2], [3], [4])[tt]
                    for fi in fis:
                        mm_fgroup(Gi, fi, "m")
                    mm_ttile(Gi, tt, "m")
                    if nxt:
                        prep_tile(Gi + 1, tt, "t")
                    for fi in fis:
                        mm_fgroup(Gi, fi, "e")
                    mm_ttile(Gi, tt, "e")
                    if nxt:
                        prep_tile(Gi + 1, tt, "e")
            p.barrier()
            p.emit()
        if stop_after <= 1:
            p.finish()
            return nc, p, dbg

        with ExitStack() as ph2:
            Nbf = mk(ph2, "Nbf", [64, NT, 128], BF16)
            Nrun = [mk(ph2, f"Nrun{i}", [64, 128], F32) for i in range(2)]
            Prun = [mk(ph2, f"Prun{i}", [64, 128], F32) for i in range(2)]
            Pbf = [mk(ph2, f"Pbf{i}", [64, 128], BF16) for i in range(2)]
            SM = [mk(ph2, f"SM{i}", [128, 128], BF16) for i in range(2)]
            qf = [mk(ph2, f"qf{i}", [64, 128], BF16) for i in range(2)]
            qb = [mk(ph2, f"qb{i}", [64, 128], BF16) for i in range(2)]
            osq = mk(ph2, "osq", [128, 4, 128], F32)
            st4 = [mk(ph2, f"st4{i}", [128, 16], F32) for i in range(2)]
            yr = [mk(ph2, f"yr{i}", [128, 128], BF16) for i in range(2)]
            psK = [mk(ph2, f"psK{i}", [64, 128], F32, psum=True) for i in range(2)]
            psS = [mk(ph2, f"psS{i}", [128, 128], F32, psum=True) for i in range(2)]
            psO = [mk(ph2, f"psO{i}", [128, 4, 128], F32, psum=True) for i in range(2)]
            psY = [mk(ph2, f"psY{i}", [128, 128], BF16, psum=True) for i in range(2)]
            for g4 in range(4):
                A(lambda e, g4=g4: e.activation(out=sg.t[:, 16 * g4:16 * (g4 + 1), :], in_=sg.t[:, 16 * g4:16 * (g4 + 1), :], func=AF.Silu), r=[sg], w=[sg])
            V(lambda e: e.memset(Nrun[1].t[:], 0.0), w=[Nrun[1]])
            V(lambda e: e.memset(Nbf.t[:, NT - 1, :], 0.0), w=[Nbf])
            for n in range(NT - 1, 0, -1):
                pk = psK[n % 2]
                cur, nxt = Nrun[n % 2], Nrun[(n + 1) % 2]
                T(lambda e, pk=pk, n=n: e.matmul(pk.t[0:64, 0:128], lhsT=ktb.t[:, n, :], rhs=rv.t[:, n, :], start=True, stop=True), r=[ktb, rv], w=[pk])
                V(lambda e, pk=pk, cur=cur, nxt=nxt: e.scalar_tensor_tensor(out=nxt.t[:], in0=cur.t[:], scalar=tfb.t[0:64, 3:4], in1=pk.t[0:64, 0:128],
                                                                          op0=ALU.mult, op1=ALU.add), r=[cur, pk, tfb], w=[nxt])
                V(lambda e, pk=pk, cur=cur, n=n: e.scalar_tensor_tensor(out=Nbf.t[:, n - 1, :], in0=cur.t[:], scalar=tfb.t[0:64, 3:4], in1=pk.t[0:64, 0:128],
                                                                     op0=ALU.mult, op1=ALU.add), r=[cur, pk, tfb], cw=[Nbf])
            V(lambda e: e.memset(Prun[0].t[:], 0.0), w=[Prun[0]])
            V(lambda e: e.memset(Pbf[0].t[:], 0.0), w=[Pbf[0]])
            def ret_stage1(n):
                cs = slice(n * 128, (n + 1) * 128)
                ps_ = psS[n % 2]
                sm_ = SM[n % 2]
                qf_, qb_ = qf[n % 2], qb[n % 2]
                T(lambda e, ps_=ps_, cs=cs: e.matmul(ps_.t[:, 0:128], lhsT=rkT.t[:, cs], rhs=rqT.t[:, cs], start=True, stop=True), r=[rkT, rqT], w=[ps_])
                V(lambda e, ps_=ps_, sm_=sm_: e.tensor_tensor(out=sm_.t[:], in0=ps_.t[:, 0:128], in1=DT.t[:], op=ALU.mult), r=[ps_, DT], w=[sm_])
                PL(lambda e, qf_=qf_, cs=cs: e.tensor_tensor(out=qf_.t[:], in0=rqT.t[:, cs], in1=QF.t[0:64, :], op=ALU.mult), r=[rqT, QF], w=[qf_])
                PL(lambda e, qb_=qb_, cs=cs: e.tensor_tensor(out=qb_.t[:], in0=rqT.t[:, cs], in1=QB.t[0:64, :], op=ALU.mult), r=[rqT, QB], w=[qb_])

            ret_stage1(0)
            for n in range(NT):
                if n + 1 < NT:
                    ret_stage1(n + 1)
                sm_ = SM[n % 2]
                po = psO[(n // 4) % 2]
                j4 = n % 4
                qf_, qb_ = qf[n % 2], qb[n % 2]
                pb_cur, pb_nxt = Pbf[n % 2], Pbf[(n + 1) % 2]
                pr_cur, pr_nxt = Prun[n % 2], Prun[(n + 1) % 2]
                if n < NT - 1:
                    pk = psK[n % 2]
                    T(lambda e, pk=pk, n=n: e.matmul(pk.t[0:64, 0:128], lhsT=ktf.t[:, n, :], rhs=rv.t[:, n, :], start=True, stop=True), r=[ktf, rv], w=[pk])
                    V(lambda e, pk=pk, pr_cur=pr_cur, pr_nxt=pr_nxt: e.scalar_tensor_tensor(out=pr_nxt.t[:], in0=pr_cur.t[:], scalar=tfb.t[0:64, 2:3],
                                                                                          in1=pk.t[0:64, 0:128], op0=ALU.mult, op1=ALU.add),
                      r=[pr_cur, pk, tfb], w=[pr_nxt])
                    V(lambda e, pk=pk, pr_cur=pr_cur, pb_nxt=pb_nxt: e.scalar_tensor_tensor(out=pb_nxt.t[:], in0=pr_cur.t[:], scalar=tfb.t[0:64, 2:3],
                                                                                          in1=pk.t[0:64, 0:128], op0=ALU.mult, op1=ALU.add),
                      r=[pr_cur, pk, tfb], w=[pb_nxt])
                T(lambda e, po=po, j4=j4, sm_=sm_, n=n: e.matmul(po.t[:, j4 * 128:(j4 + 1) * 128], lhsT=sm_.t[:], rhs=rv.t[:, n, :], start=True, stop=False), r=[sm_, rv], w=[po])
                T(lambda e, po=po, j4=j4, qf_=qf_, pb_cur=pb_cur: e.matmul(po.t[:, j4 * 128:(j4 + 1) * 128], lhsT=qf_.t[:], rhs=pb_cur.t[:], start=False, stop=False),
                  r=[qf_, pb_cur], w=[po])
                T(lambda e, po=po, j4=j4, qb_=qb_, n=n: e.matmul(po.t[:, j4 * 128:(j4 + 1) * 128], lhsT=qb_.t[:], rhs=Nbf.t[:, n, :], start=False, stop=True),
                  r=[qb_, Nbf], w=[po])
                if j4 == 3:
                    s4 = st4[(n // 4) % 2]
                    V(lambda e, po=po, s4=s4: e.tensor_reduce(out=s4.t[:, 0:4], in_=po.t[:].rearrange("p (a b) -> p a b", a=4), axis=AX.X, op=ALU.add), r=[po], w=[s4])
                    A(lambda e, po=po: e.activation(out=osq.t[:].rearrange("p a b -> p (a b)"), in_=po.t[:], func=AF.Square), r=[po], w=[osq])
                    V(lambda e, s4=s4: e.tensor_reduce(out=s4.t[:, 4:8], in_=osq.t[:], axis=AX.X, op=ALU.add), r=[osq], w=[s4])
                    V(lambda e, s4=s4: e.tensor_scalar(out=s4.t[:, 8:12], in0=s4.t[:, 0:4], scalar1=1.0 / 128, scalar2=None, op0=ALU.mult), r=[s4], w=[s4])
                    V(lambda e, s4=s4: e.tensor_tensor(out=s4.t[:, 0:4], in0=s4.t[:, 8:12], in1=s4.t[:, 8:12], op=ALU.mult), r=[s4], w=[s4])
                    V(lambda e, s4=s4: e.scalar_tensor_tensor(out=s4.t[:, 12:16], in0=s4.t[:, 4:8], scalar=1.0 / 128, in1=s4.t[:, 0:4],
                                                              op0=ALU.mult, op1=ALU.subtract), r=[s4], w=[s4])
                    A(lambda e, s4=s4: e.activation(out=s4.t[:, 12:16], in_=s4.t[:, 12:16], func=AF.Sqrt, bias=cst.t[:, 2:3]), r=[s4, cst], w=[s4])
                    V(lambda e, s4=s4: e.reciprocal(out=s4.t[:, 12:16], in_=s4.t[:, 12:16]), r=[s4], w=[s4])
                    for jj in range(4):
                        m = n - 3 + jj
                        y_ = yr[m % 2]
                        py = psY[m % 2]
                        V(lambda e, po=po, jj=jj, s4=s4, y_=y_: e.tensor_scalar(out=y_.t[:], in0=po.t[:, jj * 128:(jj + 1) * 128], scalar1=s4.t[:, 8 + jj:9 + jj],
                                                                              scalar2=s4.t[:, 12 + jj:13 + jj], op0=ALU.subtract, op1=ALU.mult),
                          r=[po, s4], w=[y_])
                        PL(lambda e, y_=y_, m=m: e.tensor_tensor(out=y_.t[:], in0=y_.t[:], in1=sg.t[:, m, :], op=ALU.mult), r=[y_, sg], w=[y_])
                        T(lambda e, py=py, y_=y_: e.transpose(out=py.t[:, 0:128], in_=y_.t[:], identity=identb.t[:]), r=[y_, identb], w=[py])
                        A(lambda e, py=py, m=m: e.copy(out=yT.t[:, m * 128:(m + 1) * 128], in_=py.t[:, 0:128]), r=[py], cw=[yT])
            p.barrier()
            p.emit()
    def tap(dst, src_ap, reads):
        p.dma("gpsimd", lambda e: e.dma_start(out=dst, in_=src_ap), "tap", reads=reads)
        p.barrier()

    if stop_after <= 2:
        if debug:
            tap(dbg["yT"][0:128, :], yT.t[:], [yT])
        p.finish()
        return nc, p, dbg
    PAD = 1024
    with ExitStack() as ph:
        aqT = mk(ph, "aqT", [128, S], BF16)
        akT = mk(ph, "akT", [128, S + 2 * PAD], BF16)
        avT = mk(ph, "avT", [128, S + 2 * PAD], BF16)
        mkb = mk(ph, "mkb", [128, 18 * 256], BF16)
        acc = [mk(ph, f"acc{h}", [65, S], F32) for h in range(2)]
        Vaug = [mk(ph, f"Vaug{i}", [128, 2, 65], BF16) for i in range(8)]
        Et = [mk(ph, f"Et{i}", [128, 256], BF16) for i in range(4)]
        NPT = 6
        PTt = [mk(ph, f"PTt{i}", [128, 256], BF16) for i in range(NPT)]
        rd = mk(ph, "rd", [65, 512], F32)
        psA = [mk(ph, f"psA{i}", None, F32, psum=True) for i in range(3)]
        psV = [mk(ph, f"psV{i}", None, BF16, psum=True) for i in range(1)]
        psB = [mk(ph, f"psB{i}", None, F32, psum=True) for i in range(3)]
        psR = mk(ph, "psR", None, F32, psum=True)
        ld("sync", aqT, aqT.t[:], aq_d, reads=[R_aq])
        ld("sync", akT, akT.t[:, PAD:PAD + S], ak_d, reads=[R_ak])
        ld("sync", avT, avT.t[:, PAD:PAD + S], av_d, reads=[R_av])
        ld("gpsimd", mkb, mkb.t[:], masksin)
        for tns in (akT, avT):
            V(lambda e, tns=tns: e.memset(tns.t[:, 0:PAD], 0.0), w=[tns])
            V(lambda e, tns=tns: e.memset(tns.t[:, PAD + S:PAD + S + PAD], 0.0), w=[tns])
        for vb in Vaug:
            V(lambda e, vb=vb: e.memset(vb.t[:, :, 64:65], 1.0), w=[vb])
        items = []
        for c, d in enumerate(CONFIGS):
            nb = (S // d) // 128
            for r in range(d):
                for m in range(nb):
                    for h in range(2):
                        items.append((c, d, r, m, h, nb))
        LAG = 3
        NV = 8
        vt = {}
        vstate = {"vi": 0}

        def ksl_(d, r, u):
            st0 = PAD + d * (128 * u - 64) + r
            return slice(st0, st0 + 127 * d + 1, d)

        def stage1(idx):
            c, d, r, m, h, nb = items[idx]
            for u in (m, m + 1):
                if (c, r, u) in vt:
                    continue
                vi = vstate["vi"]
                vstate["vi"] += 1
                vb = Vaug[vi % NV]
                pv = psV[0]
                ks = ksl_(d, r, u)
                T(lambda e, pv=pv, ks=ks: e.transpose(out=pv.t[:, 0:128], in_=avT.t[:, ks], identity=identb.t[:]), r=[avT, identb], w=[pv])
                if vi % 2 == 0:
                    A(lambda e, pv=pv, vb=vb: e.copy(out=vb.t[:, :, 0:64], in_=pv.t[:, 0:128].rearrange("p (h x) -> p h x", h=2)), r=[pv], w=[vb])
                else:
                    V(lambda e, pv=pv, vb=vb: e.tensor_copy(out=vb.t[:, :, 0:64], in_=pv.t[:, 0:128].rearrange("p (h x) -> p h x", h=2)), r=[pv], w=[vb])
                vt[(c, r, u)] = vb
            q0 = d * 128 * m + r
            qs = slice(q0, q0 + 127 * d + 1, d)
            var = 1 if m == 0 else (2 if m == nb - 1 else 0)
            rows_ = slice(64 * h, 64 * h + 64)
            pa = psA[idx % 3]
            e_ = Et[idx % 4]
            pt_ = PTt[idx % NPT]
            for j in range(2):
                ks = ksl_(d, r, m + j)
                T(lambda e, pa=pa, j=j, rows_=rows_, qs=qs, ks=ks: e.matmul(pa.t[:, j * 128:(j + 1) * 128], lhsT=akT.t[rows_, ks], rhs=aqT.t[rows_, qs],
                                                                      start=True, stop=True), r=[akT, aqT], w=[pa])
            A(lambda e, pa=pa, e_=e_: e.activation(out=e_.t[:], in_=pa.t[:, 0:256], func=AF.Exp, scale=0.125), r=[pa], w=[e_])
            mo = ((h * 3 + c) * 3 + var) * 256
            V(lambda e, e_=e_, pt_=pt_, mo=mo: e.tensor_tensor(out=pt_.t[:], in0=e_.t[:], in1=mkb.t[:, mo:mo + 256], op=ALU.mult), r=[e_, mkb], w=[pt_])

        def stage2(idx):
            c, d, r, m, h, nb = items[idx]
            q0 = d * 128 * m + r
            qs = slice(q0, q0 + 127 * d + 1, d)
            pt_ = PTt[idx % NPT]
            po = psB[idx % 3]
            for j in range(2):
                vb = vt[(c, r, m + j)]
                T(lambda e, po=po, j=j, vb=vb, pt_=pt_, h=h: e.matmul(po.t[0:65, 0:128], lhsT=vb.t[:, h, :], rhs=pt_.t[:, j * 128:(j + 1) * 128],
                                                                   start=(j == 0), stop=(j == 1)), r=[vb, pt_], w=[po])
            ac = acc[h]
            if c == 0:
                A(lambda e, ac=ac, qs=qs, po=po: e.copy(out=ac.t[:, qs], in_=po.t[0:65, 0:128]), r=[po], w=[ac])
            else:
                V(lambda e, ac=ac, qs=qs, po=po: e.tensor_tensor(out=ac.t[:, qs], in0=ac.t[:, qs], in1=po.t[0:65, 0:128], op=ALU.add), r=[po, ac], w=[ac])

        for idx in range(len(items) + LAG):
            if idx < len(items):
                stage1(idx)
            if idx - LAG >= 0:
                stage2(idx - LAG)
        for h in range(2):
            yh = (yA, yB)[h]
            ac = acc[h]
            V(lambda e, ac=ac: e.reciprocal(out=ac.t[64:65, :], in_=ac.t[64:65, :]), r=[ac], w=[ac])
            for t in range(16):
                sl = slice(512 * t, 512 * (t + 1))
                T(lambda e, ac=ac, sl=sl: e.matmul(psR.t[0:64, 0:512], lhsT=onesf.t[64:65, 0:64], rhs=ac.t[64:65, sl], start=True, stop=True),
                  r=[onesf, ac], w=[psR])
                V(lambda e, yh=yh, ac=ac, sl=sl: e.tensor_tensor(out=yh.t[:, sl], in0=ac.t[0:64, sl], in1=psR.t[0:64, 0:512], op=ALU.mult),
                  r=[ac, psR], cw=[yh])
        p.barrier()
    if stop_after <= 3:
        if debug:
            tap(dbg["yT"][0:128, :], yT.t[:], [yT])
            tap(dbg["yT"][128:192, :], yA.t[:], [yA])
            tap(dbg["yT"][192:256, :], yB.t[:], [yB])
        p.finish()
        return nc, p, dbg

    y_in = [dsc(f"y_in{c}", [256, OWN], BF16) for c in range(4)]
    y_all = [dsc(f"y_all{c}", [1024, OWN], BF16) for c in range(4)]
    R_yin = [Reg(f"y_in{c}") for c in range(4)]
    R_yall = [Reg(f"y_all{c}") for c in range(4)]
    for c4 in range(4):
        sl = slice(OWN * c4, OWN * (c4 + 1))
        p.dma("sync", lambda e, c4=c4, sl=sl: e.dma_start(out=y_in[c4][0:128, :], in_=yT.t[:, sl]), f"y_in{c4}", reads=[yT], writes=[R_yin[c4]])
        p.dma("sync", lambda e, c4=c4, sl=sl: e.dma_start(out=y_in[c4][128:192, :], in_=yA.t[:, sl]), f"y_in{c4}", reads=[yA], writes=[R_yin[c4]])
        p.dma("sync", lambda e, c4=c4, sl=sl: e.dma_start(out=y_in[c4][192:256, :], in_=yB.t[:, sl]), f"y_in{c4}", reads=[yB], writes=[R_yin[c4]])
        p.dma("gpsimd", lambda e, c4=c4: e.collective_compute("AllGather", ALU.bypass, replica_groups=RG, ins=[y_in[c4]], outs=[y_all[c4]]),
              f"agy_{c4}", reads=[R_yin[c4]], writes=[R_yall[c4]], inc=1)
    p.barrier()
    ymix.close()
    if stop_after <= 4:
        p.finish()
        return nc, p, dbg
    x1_d = dsc("x1_d", [OWN, D])
    R_x1 = Reg("x1_d")
    with ExitStack() as ph58:
        rows = {k: mk(ph58, f"row_{k}", [128, D], F32) for k in ("gm", "gsf", "shf", "gf")}
        toki = mk(ph58, "toki", [128, 32], I32)
        with ExitStack() as ph:
            diag = [mk(ph, f"diag{i}", [128, 128], F32) for i in range(2)]
            psD = [mk(ph, f"psD{i}", None, F32, psum=True) for i in range(2)]
            srcs = {"gm": der.t[:, 8:16], "gsf": der.t[:, 16:24], "shf": mod.t[:, 24:32], "gf": der.t[:, 24:32]}
            di = 0
            for key in ("gm", "gsf", "shf", "gf"):
                for half in range(2):
                    pd = psD[half]
                    for kk in range(4):
                        k = half * 4 + kk
                        dg = diag[di % 2]
                        di += 1
                        V(lambda e, dg=dg, key=key, k=k: e.tensor_scalar(out=dg.t[:], in0=identf.t[:], scalar1=srcs[key][:, k:k + 1], scalar2=None,
                                                                       op0=ALU.mult), r=[identf, der, mod], w=[dg])
                        T(lambda e, pd=pd, kk=kk, dg=dg: e.matmul(pd.t[:, kk * 128:(kk + 1) * 128], lhsT=onesf.t[:], rhs=dg.t[:], start=True, stop=True),
                          r=[onesf, dg], w=[pd])
                    A(lambda e, pd=pd, key=key, half=half: e.copy(out=rows[key].t[:, half * 512:(half + 1) * 512], in_=pd.t[:, 0:512]),
                      r=[pd], w=[rows[key]])
            wrs = mk(ph, "wrs", [128, 8, 16], F32)
            ld("sync", wrs, wrs.t[:], wrin.rearrange("(k p) e -> p k e", p=128))
            wob = mk(ph, "wob", [128, 8, D], BF16)
            ld("gpsimd", wob, wob.t[:], wout.rearrange("(k p) n -> p k n", p=128))
            yown = mk(ph, "yown", [128, 8, OWN], BF16)
            ytmp = [mk(ph, f"ytmp{i}", [128, 8, OWN], BF16) for i in range(1)]
            ohb = mk(ph, "ohb", [128, 4], BF16)
            V(lambda e: e.tensor_copy(out=ohb.t[:], in_=ohs.t[:]), r=[ohs], w=[ohb])
            for c4 in range(4):
                yt_ = ytmp[0]
                ld("sync", yt_, yt_.t[:], y_all[c4].rearrange("(k p) t -> p k t", p=128), reads=[R_yall[c4]])
                for kq in range(4):
                    ks_ = slice(2 * kq, 2 * kq + 2)
                    eng = V
                    if c4 == 0:
                        eng(lambda e, yt_=yt_, ks_=ks_: e.tensor_scalar(out=yown.t[:, ks_, :], in0=yt_.t[:, ks_, :], scalar1=ohs.t[:, 0:1], scalar2=None,
                                                                        op0=ALU.mult), r=[yt_, ohs], cw=[yown])
                    else:
                        eng(lambda e, yt_=yt_, ks_=ks_, c4=c4: e.scalar_tensor_tensor(out=yown.t[:, ks_, :], in0=yt_.t[:, ks_, :], scalar=ohs.t[:, c4:c4 + 1],
                                                                                      in1=yown.t[:, ks_, :], op0=ALU.mult, op1=ALU.add),
                            r=[yt_, ohs], cw=[yown])
            psM = [mk(ph, f"psM{i}", None, F32, psum=True) for i in range(2)]
            RD5 = 3
            mx = [mk(ph, f"mx{i}", [128, D], F32) for i in range(RD5)]
            xo_t = [mk(ph, f"xo{i}", [128, D], F32) for i in range(RD5)]
            x1t = [mk(ph, f"x1t{i}", [128, D], F32) for i in range(RD5)]
            h2t = [mk(ph, f"h2t{i}", [128, D], F32) for i in range(RD5)]
            h2T = [mk(ph, f"h2T{i}", [128, 8, 128], F32) for i in range(RD5)]
            sq2 = mk(ph, "sq2", [128, D], BF16)
            ss5 = [mk(ph, f"ss5{i}", [128, 8], F32) for i in range(RD5)]
            afft = [mk(ph, f"afft{i}", [128, 16], F32) for i in range(2)]
            ext = [mk(ph, f"ext{i}", [128, 16], F32) for i in range(2)]
            psH = [mk(ph, f"psH{i}", None, F32, psum=True) for i in range(2)]
            psL = [mk(ph, f"psL{i}", None, F32, psum=True) for i in range(2)]
            lgall = mk(ph, "lgall", [128, 16, 16], F32)
            smx = mk(ph, "smx", [128, 32], F32)
            def p5_a(n):
                ts = slice(128 * n, 128 * (n + 1))
                m_, xo_, x1_, h2_, hT_, s5 = mx[n % RD5], xo_t[n % RD5], x1t[n % RD5], h2t[n % RD5], h2T[n % RD5], ss5[n % RD5]
                for half in range(2):
                    pm = psM[half]
                    for kc in range(8):
                        T(lambda e, pm=pm, kc=kc, ts=ts, half=half: e.matmul(pm.t[:, 0:512], lhsT=yown.t[:, kc, ts], rhs=wob.t[:, kc, half * 512:(half + 1) * 512],
                                                                           start=(kc == 0), stop=(kc == 7)), r=[yown, wob], w=[pm])
                    if half == 0:
                        A(lambda e, pm=pm, m_=m_: e.copy(out=m_.t[:, 0:512], in_=pm.t[:, 0:512]), r=[pm], cw=[m_])
                    else:
                        V(lambda e, pm=pm, m_=m_: e.tensor_copy(out=m_.t[:, 512:1024], in_=pm.t[:, 0:512]), r=[pm], cw=[m_])
                ld("sync", xo_, xo_.t[:], xo[ts, :])
                A(lambda e, m_=m_, s5=s5: e.activation(out=sq2.t[:], in_=m_.t[:], func=AF.Square, accum_out=s5.t[:, 0:1]), r=[m_], w=[sq2, s5])
                A(lambda e, s5=s5: e.activation(out=s5.t[:, 1:2], in_=s5.t[:, 0:1], func=AF.Sqrt, scale=1.0 / D, bias=cst.t[:, 1:2]), r=[s5, cst], w=[s5])
                V(lambda e, s5=s5: e.reciprocal(out=s5.t[:, 1:2], in_=s5.t[:, 1:2]), r=[s5], w=[s5])
                V(lambda e, m_=m_, s5=s5: e.scalar_tensor_tensor(out=m_.t[:], in0=m_.t[:], scalar=s5.t[:, 1:2], in1=rows["gm"].t[:],
                                                               op0=ALU.mult, op1=ALU.mult), r=[m_, s5, rows["gm"]], w=[m_])
                PL(lambda e, m_=m_, xo_=xo_, x1_=x1_: e.tensor_tensor(out=x1_.t[:], in0=xo_.t[:], in1=m_.t[:], op=ALU.add), r=[m_, xo_], w=[x1_])
                p.dma("sync", lambda e, ts=ts, x1_=x1_: e.dma_start(out=x1_d[ts, :], in_=x1_.t[:]), "x1_d", reads=[x1_], writes=[R_x1])

            def p5_b(n):
                ts = slice(128 * n, 128 * (n + 1))
                m_, xo_, x1_, h2_, hT_, s5 = mx[n % RD5], xo_t[n % RD5], x1t[n % RD5], h2t[n % RD5], h2T[n % RD5], ss5[n % RD5]
                A(lambda e, x1_=x1_, s5=s5: e.activation(out=sq2.t[:], in_=x1_.t[:], func=AF.Square, accum_out=s5.t[:, 2:3]), r=[x1_], w=[sq2, s5])
                A(lambda e, s5=s5: e.activation(out=s5.t[:, 3:4], in_=s5.t[:, 2:3], func=AF.Sqrt, scale=1.0 / D, bias=cst.t[:, 1:2]), r=[s5, cst], w=[s5])
                V(lambda e, s5=s5: e.reciprocal(out=s5.t[:, 3:4], in_=s5.t[:, 3:4]), r=[s5], w=[s5])
                V(lambda e, x1_=x1_, s5=s5, h2_=h2_: e.scalar_tensor_tensor(out=h2_.t[:], in0=x1_.t[:], scalar=s5.t[:, 3:4], in1=rows["gsf"].t[:],
                                                                          op0=ALU.mult, op1=ALU.mult), r=[x1_, s5, rows["gsf"]], w=[h2_])
                PL(lambda e, h2_=h2_: e.tensor_tensor(out=h2_.t[:], in0=h2_.t[:], in1=rows["shf"].t[:], op=ALU.add), r=[h2_, rows["shf"]], w=[h2_])
                p.dma("gpsimd", lambda e, ts=ts, h2_=h2_: e.dma_start(out=h2_in[ts, :], in_=h2_.t[:]), "h2_in", reads=[h2_], writes=[R_h2in])
                if n % 4 == 3:
                    c4 = n // 4
                    p.dma("gpsimd", lambda e, c4=c4: e.collective_compute("AllGather", ALU.bypass, replica_groups=RG,
                                                                          ins=[h2_in[512 * c4:512 * (c4 + 1), :]], outs=[h2_all[c4]]),
                          f"ag1_{c4}", reads=[R_h2in], writes=[R_h2all[c4]], inc=1)

            def p5_c(n):
                ts = slice(128 * n, 128 * (n + 1))
                m_, xo_, x1_, h2_, hT_, s5 = mx[n % RD5], xo_t[n % RD5], x1t[n % RD5], h2t[n % RD5], h2T[n % RD5], ss5[n % RD5]
                for k in range(8):
                    ph_ = psH[k // 4]
                    T(lambda e, ph_=ph_, k=k, h2_=h2_: e.transpose(out=ph_.t[:, (k % 4) * 128:(k % 4 + 1) * 128], in_=h2_.t[:, k * 128:(k + 1) * 128],
                                                                 identity=identf.t[:]), r=[h2_, identf], w=[ph_])
                A(lambda e, hT_=hT_: e.copy(out=hT_.t[:, 0:4, :], in_=psH[0].t[:, 0:512].rearrange("p (k s) -> p k s", k=4)), r=[psH[0]], w=[hT_])
                V(lambda e, hT_=hT_: e.tensor_copy(out=hT_.t[:, 4:8, :], in_=psH[1].t[:, 0:512].rearrange("p (k s) -> p k s", k=4)), r=[psH[1]], w=[hT_])
                pl = psL[n % 2]
                for k in range(8):
                    T(lambda e, pl=pl, k=k, hT_=hT_: e.matmul(pl.t[:, 0:16], lhsT=hT_.t[:, k, :], rhs=wrs.t[:, k, :], start=(k == 0), stop=(k == 7)),
                      r=[hT_, wrs], w=[pl])
                V(lambda e, pl=pl, n=n: e.tensor_copy(out=lgall.t[:, n, :], in_=pl.t[:, 0:16]), r=[pl], cw=[lgall])

            for step in range(16 + 2):
                if step < 16:
                    p5_a(step)
                if 0 <= step - 1 < 16:
                    p5_b(step - 1)
                if 0 <= step - 2 < 16:
                    p5_c(step - 2)
            V(lambda e: e.tensor_reduce(out=smx.t[:, 0:16], in_=lgall.t[:], axis=AX.X, op=ALU.max), r=[lgall], w=[smx])
            for n in range(16):
                V(lambda e, n=n: e.tensor_scalar(out=lgall.t[:, n, :], in0=lgall.t[:, n, :], scalar1=smx.t[:, n:n + 1], scalar2=None, op0=ALU.subtract),
                  r=[lgall, smx], w=[lgall])
            A(lambda e: e.activation(out=lgall.t[:].rearrange("p a b -> p (a b)"), in_=lgall.t[:].rearrange("p a b -> p (a b)"), func=AF.Exp),
              r=[lgall], w=[lgall])
            V(lambda e: e.tensor_reduce(out=smx.t[:, 16:32], in_=lgall.t[:], axis=AX.X, op=ALU.add), r=[lgall], w=[smx])
            V(lambda e: e.reciprocal(out=smx.t[:, 16:32], in_=smx.t[:, 16:32]), r=[smx], w=[smx])
            for n in range(16):
                V(lambda e, n=n: e.tensor_scalar(out=lgall.t[:, n, :], in0=lgall.t[:, n, :], scalar1=smx.t[:, 16 + n:17 + n], scalar2=None, op0=ALU.mult),
                  r=[lgall, smx], w=[lgall])
            p.dma("sync", lambda e: e.dma_start(out=aff_in.rearrange("(n p) e -> p n e", p=128), in_=lgall.t[:]), "aff_in", reads=[lgall], writes=[R_affin])
            p.dma("gpsimd", lambda e: e.collective_compute("AllGather", ALU.bypass, replica_groups=RG, ins=[aff_in], outs=[aff_all]),
                  "ag2", reads=[R_affin], writes=[R_affall], inc=1)
            for c4 in range(4):
                for r4 in range(4):
                    p.dma("sync", lambda e, c4=c4, r4=r4: e.dma_start(out=h2_tab[2048 * r4 + 512 * c4:2048 * r4 + 512 * (c4 + 1), :],
                                                                      in_=h2_all[c4][512 * r4:512 * (r4 + 1), :]),
                          "h2tab", reads=[R_h2all[c4]], writes=[R_h2tab])
            p.dma("sync", lambda e: e.dma_start(out=aff_tab, in_=aff_all), "afftab", reads=[R_affall], writes=[R_afftab])
            p.barrier()
        if stop_after <= 5:
            if debug:
                tap(dbg["x1"], x1_d, [R_x1])
                tap(dbg["aff"], aff_in, [R_affin])
            p.finish()
            return nc, p, dbg

        with ExitStack() as ph:
            Aall = mk(ph, "Aall", [128, 64, 16], F32)
            A4 = mk(ph, "A4", [128, 4, 64], F32)
            cmp_ = mk(ph, "cmp", [128, 4, 64], F32)
            cc0 = mk(ph, "cc0", [128, 4, 64], F32)
            cc1 = mk(ph, "cc1", [128, 4, 64], F32)
            lo = mk(ph, "lo", [128, 4], F32)
            mid = mk(ph, "mid", [128, 4], F32)
            cnt = mk(ph, "cnt", [128, 4], F32)
            ge = mk(ph, "ge", [128, 4], F32)
            offs = mk(ph, "offs", [128, 4], F32)
            tris = mk(ph, "tris", [128, 128], F32)
            slot = mk(ph, "slot", [128, 8], F32)
            tokf = mk(ph, "tokf", [128, 32], F32)
            cb = [mk(ph, f"cb{i}", [128, S], F32) for i in range(2)]
            junk = mk(ph, "junk", [128, S], BF16)
            psC = mk(ph, "psC", None, F32, psum=True)
            ld("sync", Aall, Aall.t[:], aff_tab.rearrange("(p j) e -> p j e", j=64), reads=[R_afftab])
            ld("sync", tris, tris.t[:], triin)
            ld("sync", slot, slot.t[:], slotin)
            for i in range(4):
                V(lambda e, i=i: e.tensor_scalar(out=A4.t[:, i, :], in0=Aall.t[:, :, i], scalar1=ohs.t[:, 0:1], scalar2=None, op0=ALU.mult),
                  r=[Aall, ohs], w=[A4])
                for r in range(1, 4):
                    V(lambda e, i=i, r=r: e.scalar_tensor_tensor(out=A4.t[:, i, :], in0=Aall.t[:, :, 4 * r + i], scalar=ohs.t[:, r:r + 1], in1=A4.t[:, i, :],
                                                                 op0=ALU.mult, op1=ALU.add), r=[Aall, ohs, A4], w=[A4])
            V(lambda e: e.memset(lo.t[:], 0.0), w=[lo])
            for it in range(26):
                wv = 2.0 ** (-(it + 1))
                V(lambda e, wv=wv: e.tensor_scalar(out=mid.t[:], in0=lo.t[:], scalar1=wv, scalar2=None, op0=ALU.add), r=[lo], w=[mid])
                V(lambda e: e.memset(cnt.t[:], 0.0), w=[cnt])
                for i in range(4):
                    V(lambda e, i=i: e.tensor_scalar(out=cmp_.t[:, i, :], in0=A4.t[:, i, :], scalar1=mid.t[:, i:i + 1], scalar2=0.0, op0=ALU.is_gt,
                                                    op1=ALU.add, accum_out=cnt.t[:, i:i + 1]), r=[A4, mid, cnt], w=[cmp_, cnt])
                T(lambda e: e.matmul(psC.t[:, 0:4], lhsT=onesf.t[:], rhs=cnt.t[:], start=True, stop=True), r=[onesf, cnt], w=[psC])
                V(lambda e: e.tensor_scalar(out=ge.t[:], in0=psC.t[:, 0:4], scalar1=CAP - 0.5, scalar2=None, op0=ALU.is_ge), r=[psC], w=[ge])
                V(lambda e, wv=wv: e.scalar_tensor_tensor(out=lo.t[:], in0=ge.t[:], scalar=wv, in1=lo.t[:], op0=ALU.mult, op1=ALU.add), r=[ge, lo], w=[lo])
            for i in range(4):
                V(lambda e, i=i: e.tensor_scalar(out=cc0.t[:, i, :], in0=A4.t[:, i, :], scalar1=lo.t[:, i:i + 1], scalar2=None, op0=ALU.is_gt),
                  r=[A4, lo], w=[cc0])
            ca, cbuf = cc0, cc1
            for sh in (1, 2, 4, 8, 16, 32):
                V(lambda e, ca=ca, cbuf=cbuf, sh=sh: e.tensor_tensor(out=cbuf.t[:, :, sh:64], in0=ca.t[:, :, sh:64], in1=ca.t[:, :, 0:64 - sh], op=ALU.add),
                  r=[ca], w=[cbuf])
                V(lambda e, ca=ca, cbuf=cbuf, sh=sh: e.tensor_copy(out=cbuf.t[:, :, 0:sh], in_=ca.t[:, :, 0:sh]), r=[ca], w=[cbuf])
                ca, cbuf = cbuf, ca
            V(lambda e, ca=ca: e.tensor_copy(out=cnt.t[:], in_=ca.t[:, :, 63]), r=[ca], w=[cnt])
            T(lambda e: e.matmul(psC.t[:, 0:4], lhsT=tris.t[:], rhs=cnt.t[:], start=True, stop=True), r=[tris, cnt], w=[psC])
            V(lambda e: e.tensor_copy(out=offs.t[:], in_=psC.t[:, 0:4]), r=[psC], w=[offs])
            cdr_v = cdr.rearrange("e (p j) -> e p j", j=64)
            for i in range(4):
                V(lambda e, i=i, ca=ca: e.tensor_scalar(out=ca.t[:, i, :], in0=ca.t[:, i, :], scalar1=offs.t[:, i:i + 1], scalar2=None, op0=ALU.add),
                  r=[ca, offs], w=[ca])
                p.dma("sync", lambda e, i=i, ca=ca: e.dma_start(out=cdr_v[i], in_=ca.t[:, i, :]), "cdr", reads=[ca], writes=[R_cdr])
            V(lambda e: e.memset(tokf.t[:], 0.0), w=[tokf])
            for i in range(4):
                cb_ = cb[i % 2]
                ld("sync", cb_, cb_.t[:], cdr[i:i + 1, :].partition_broadcast(128), reads=[R_cdr])
                for st in range(8):
                    col = i * 8 + st
                    V(lambda e, cb_=cb_, st=st, col=col: e.tensor_scalar(out=junk.t[:], in0=cb_.t[:], scalar1=slot.t[:, st:st + 1], scalar2=0.0,
                                                                       op0=ALU.is_le, op1=ALU.add, accum_out=tokf.t[:, col:col + 1]),
                      r=[cb_, slot, tokf], w=[junk, tokf])
            V(lambda e: e.tensor_copy(out=toki.t[:], in_=tokf.t[:]), r=[tokf], w=[toki])
            if debug and stop_after == 6:
                tap(dbg["tok"], tokf.t[:], [tokf])
            p.barrier()
        if stop_after <= 6:
            if debug:
                tap(dbg["x1"], x1_d, [R_x1])
                tap(dbg["aff"], aff_in, [R_affin])
            p.finish()
            return nc, p, dbg

        with ExitStack() as ph:
            xeT = [mk(ph, f"xeT{i}", [128, 8, CAP], BF16) for i in range(2)]
            hT = mk(ph, "hT", [128, 16, CAP], BF16)
            wdb = [mk(ph, f"wdb{i}", [128, 16, D], BF16) for i in range(2)]
            NPC = 8
            wgb = [mk(ph, f"wgb{i}", [128, 8, 256], BF16) for i in range(3)]
            wub = [mk(ph, f"wub{i}", [128, 8, 256], BF16) for i in range(3)]
            xet = [mk(ph, f"xet{i}", [128, D], BF16) for i in range(2)]
            gat = [mk(ph, f"gat{i}", [128, 16], F32) for i in range(2)]
            gate = [mk(ph, f"gate{i}", [128, 8], F32) for i in range(2)]
            yet = [mk(ph, f"yet{i}", [128, D], F32) for i in range(2)]
            sgt = [mk(ph, f"sgt{i}", [128, 512], BF16) for i in range(2)]
            psXT = mk(ph, "psXT", None, BF16, psum=True)
            psG = [mk(ph, f"psG{i}", None, F32, psum=True) for i in range(2)]
            psU = [mk(ph, f"psU{i}", None, F32, psum=True) for i in range(2)]
            psY = [mk(ph, f"psYd{i}", None, F32, psum=True) for i in range(2)]
            cnts = {"g": 0, "pc": 0, "m": 0, "y": 0}

            def load_piece(i, pc):
                ws = (i * NPC + pc) % 3
                wg_v = wg[i].rearrange("(k p) f -> p k f", p=128)
                wu_v = wu[i].rearrange("(k p) f -> p k f", p=128)
                ld("gpsimd", wgb[ws], wgb[ws].t[:], wg_v[:, :, pc * 256:(pc + 1) * 256])
                ld("gpsimd", wub[ws], wub[ws].t[:], wu_v[:, :, pc * 256:(pc + 1) * 256])

            def gather_expert(i):
                xT = xeT[i % 2]
                gt_ = gate[i % 2]
                for st in range(8):
                    col = i * 8 + st
                    xe_ = xet[cnts["g"] % 2]
                    ga_ = gat[cnts["g"] % 2]
                    cnts["g"] += 1
                    p.dma("gpsimd", lambda e, xe_=xe_, col=col: e.indirect_dma_start(
                        out=xe_.t[:], out_offset=None, in_=h2_tab, in_offset=bass.IndirectOffsetOnAxis(ap=toki.t[:, col:col + 1], axis=0)),
                        xe_.r.name, reads=[R_h2tab, toki], writes=[xe_])
                    p.dma("gpsimd", lambda e, ga_=ga_, col=col: e.indirect_dma_start(
                        out=ga_.t[:], out_offset=None, in_=aff_tab, in_offset=bass.IndirectOffsetOnAxis(ap=toki.t[:, col:col + 1], axis=0)),
                        ga_.r.name, reads=[R_afftab, toki], writes=[ga_])
                    V(lambda e, ga_=ga_, gt_=gt_, st=st, i=i: e.tensor_scalar(out=gt_.t[:, st:st + 1], in0=ga_.t[:, i:i + 1], scalar1=ohs.t[:, 0:1], scalar2=None,
                                                                            op0=ALU.mult), r=[ga_, ohs], w=[gt_])
                    for r in range(1, 4):
                        V(lambda e, ga_=ga_, gt_=gt_, st=st, i=i, r=r: e.scalar_tensor_tensor(
                            out=gt_.t[:, st:st + 1], in0=ga_.t[:, 4 * r + i:4 * r + i + 1], scalar=ohs.t[:, r:r + 1], in1=gt_.t[:, st:st + 1],
                            op0=ALU.mult, op1=ALU.add), r=[ga_, ohs, gt_], w=[gt_])
                    for k in range(8):
                        T(lambda e, k=k, xe_=xe_: e.transpose(out=psXT.t[:, k * 128:(k + 1) * 128], in_=xe_.t[:, k * 128:(k + 1) * 128], identity=identb.t[:]),
                          r=[xe_, identb], w=[psXT])
                    if st % 2 == 0:
                        A(lambda e, st=st, xT=xT: e.copy(out=xT.t[:, :, st * 128:(st + 1) * 128], in_=psXT.t[:].rearrange("p (k s) -> p k s", k=8)),
                          r=[psXT], cw=[xT])
                    else:
                        V(lambda e, st=st, xT=xT: e.tensor_copy(out=xT.t[:, :, st * 128:(st + 1) * 128], in_=psXT.t[:].rearrange("p (k s) -> p k s", k=8)),
                          r=[psXT], cw=[xT])

            ld("gpsimd", wdb[0], wdb[0].t[:], wd[0].rearrange("(k p) d -> p k d", p=128))
            gather_expert(0)
            load_piece(0, 0)
            load_piece(0, 1)
            for i in range(4):
                xT = xeT[i % 2]
                gt_ = gate[i % 2]
                wd_ = wdb[i % 2]
                for pc in range(NPC):
                    if pc + 2 < NPC:
                        load_piece(i, pc + 2)
                    ws = (i * NPC + pc) % 3
                    wg_, wu_ = wgb[ws], wub[ws]
                    for fc in range(2):
                        f = pc * 2 + fc
                        for half in range(2):
                            pg, pu, sg_ = psG[cnts["m"] % 2], psU[cnts["m"] % 2], sgt[cnts["m"] % 2]
                            cnts["m"] += 1
                            hs = slice(512 * half, 512 * (half + 1))
                            for k in range(8):
                                T(lambda e, pg=pg, k=k, wg_=wg_, fc=fc, hs=hs, xT=xT: e.matmul(pg.t[:, 0:512], lhsT=wg_.t[:, k, fc * 128:(fc + 1) * 128], rhs=xT.t[:, k, hs],
                                                                                           start=(k == 0), stop=(k == 7)), r=[wg_, xT], w=[pg])
                            for k in range(8):
                                T(lambda e, pu=pu, k=k, wu_=wu_, fc=fc, hs=hs, xT=xT: e.matmul(pu.t[:, 0:512], lhsT=wu_.t[:, k, fc * 128:(fc + 1) * 128], rhs=xT.t[:, k, hs],
                                                                                           start=(k == 0), stop=(k == 7)), r=[wu_, xT], w=[pu])
                            A(lambda e, pg=pg, sg_=sg_: e.activation(out=sg_.t[:], in_=pg.t[:, 0:512], func=AF.Silu), r=[pg], w=[sg_])
                            V(lambda e, pu=pu, sg_=sg_, f=f, hs=hs: e.tensor_tensor(out=hT.t[:, f, hs], in0=sg_.t[:], in1=pu.t[:, 0:512], op=ALU.mult),
                              r=[sg_, pu], cw=[hT])
                if i + 1 < 4:
                    ld("gpsimd", wdb[(i + 1) % 2], wdb[(i + 1) % 2].t[:], wd[i + 1].rearrange("(k p) d -> p k d", p=128))
                    gather_expert(i + 1)
                    load_piece(i + 1, 0)
                    load_piece(i + 1, 1)
                for st in range(8):
                    col = i * 8 + st
                    ye_ = yet[st % 2]
                    for dh in range(2):
                        py = psY[cnts["y"] % 2]
                        cnts["y"] += 1
                        ds_ = slice(512 * dh, 512 * (dh + 1))
                        for f in range(16):
                            T(lambda e, py=py, f=f, st=st, ds_=ds_, wd_=wd_: e.matmul(py.t[:, 0:512], lhsT=hT.t[:, f, st * 128:(st + 1) * 128], rhs=wd_.t[:, f, ds_],
                                                                                   start=(f == 0), stop=(f == 15)), r=[hT, wd_], w=[py])
                        if dh == 0:
                            A(lambda e, py=py, ye_=ye_, ds_=ds_, gt_=gt_, st=st: e.activation(out=ye_.t[:, ds_], in_=py.t[:, 0:512], func=AF.Identity,
                                                                                           scale=gt_.t[:, st:st + 1]), r=[py, gt_], cw=[ye_])
                        else:
                            V(lambda e, py=py, ye_=ye_, ds_=ds_, gt_=gt_, st=st: e.tensor_scalar(out=ye_.t[:, ds_], in0=py.t[:, 0:512], scalar1=gt_.t[:, st:st + 1],
                                                                                              scalar2=None, op0=ALU.mult), r=[py, gt_], cw=[ye_])
                    p.dma("gpsimd", lambda e, ye_=ye_, col=col: e.indirect_dma_start(
                        out=contrib, out_offset=bass.IndirectOffsetOnAxis(ap=toki.t[:, col:col + 1], axis=0), in_=ye_.t[:], in_offset=None,
                        compute_op=ALU.add), "scat", reads=[ye_, toki, R_contrib], writes=[R_contrib])
            p.dma("gpsimd", lambda e: e.collective_compute("ReduceScatter", ALU.add, replica_groups=RG, ins=[contrib], outs=[moe_d]),
                  "rs2", reads=[R_contrib], writes=[R_moe], inc=1)
            p.barrier()
        if debug and stop_after == 7:
            tap(dbg["moe"], moe_d, [R_moe])

        with ExitStack() as ph:
            mo = [mk(ph, f"mo{i}", [128, D], F32) for i in range(2)]
            x1r = [mk(ph, f"x1r{i}", [128, D], F32) for i in range(2)]
            sq8 = mk(ph, "sq8", [128, D], BF16)
            s8 = [mk(ph, f"s8{i}", [128, 2], F32) for i in range(2)]
            for n in range(16):
                ts = slice(128 * n, 128 * (n + 1))
                m_, x_, s_ = mo[n % 2], x1r[n % 2], s8[n % 2]
                ld("sync", m_, m_.t[:], moe_d[ts, :], reads=[R_moe])
                ld("sync", x_, x_.t[:], x1_d[ts, :], reads=[R_x1])
                A(lambda e, m_=m_, s_=s_: e.activation(out=sq8.t[:], in_=m_.t[:], func=AF.Square, accum_out=s_.t[:, 0:1]), r=[m_], w=[sq8, s_])
                A(lambda e, s_=s_: e.activation(out=s_.t[:, 1:2], in_=s_.t[:, 0:1], func=AF.Sqrt, scale=1.0 / D, bias=cst.t[:, 1:2]), r=[s_, cst], w=[s_])
                V(lambda e, s_=s_: e.reciprocal(out=s_.t[:, 1:2], in_=s_.t[:, 1:2]), r=[s_], w=[s_])
                V(lambda e, m_=m_, s_=s_: e.scalar_tensor_tensor(out=m_.t[:], in0=m_.t[:], scalar=s_.t[:, 1:2], in1=rows["gf"].t[:],
                                                               op0=ALU.mult, op1=ALU.mult), r=[m_, s_, rows["gf"]], w=[m_])
                PL(lambda e, m_=m_, x_=x_: e.tensor_tensor(out=m_.t[:], in0=m_.t[:], in1=x_.t[:], op=ALU.add), r=[m_, x_], w=[m_])
                p.dma("sync", lambda e, ts=ts, m_=m_: e.dma_start(out=out[ts, :], in_=m_.t[:]), "out", reads=[m_], writes=[R_out])
            p.barrier()
    p.finish()
    return nc, p, dbg


def _consts(q):
    j = np.arange(128, dtype=np.float32)[:, None]
    i = np.arange(128, dtype=np.float32)[None, :]
    rc = np.zeros((128, 514), np.float32)
    rc[:, 0:128] = np.maximum(i - j, 0)
    rc[:, 128:256] = np.maximum(j - i, 0)
    rc[:, 256:384] = i + 1
    rc[:, 384:512] = 128 - i
    rc[:, 512] = 127 - j[:, 0]
    rc[:, 513] = j[:, 0]
    masks = np.zeros((128, 2, 3, 3, 2, 128), np.float32)
    kk = np.arange(128)[:, None, None]
    jj = np.arange(2)[None, :, None]
    ii = np.arange(128)[None, None, :]
    delta = np.abs(kk + 128 * jj - 64 - ii).astype(np.float32)
    band = (delta <= 64).astype(np.float32)
    for h in range(2):
        slope = SLOPES[2 * q + h]
        for c, d in enumerate(CONFIGS):
            m = band * np.exp(-slope * d * delta)
            masks[:, h, c, 0] = m
            m1 = m.copy()
            m1[0:64, 0, :] = 0
            masks[:, h, c, 1] = m1
            m2 = m.copy()
            m2[64:128, 1, :] = 0
            masks[:, h, c, 2] = m2
    slotid = (128 * np.arange(8)[None, :] + np.arange(128)[:, None]).astype(np.float32)
    tri = (np.arange(128)[:, None] < np.arange(128)[None, :]).astype(np.float32)
    return rc, masks.reshape(128, 18 * 256), slotid, tri


def prep(inputs):
    f = lambda a: np.ascontiguousarray(np.asarray(a, dtype=np.float32))
    x, c = f(inputs["x"]), f(inputs["c"])
    w_in = f(inputs["w_in"])[0]
    w_out = f(inputs["w_out"])[0]
    col = lambda v: np.ascontiguousarray(v.reshape(-1, 128).T)
    vcols = np.concatenate([col(f(inputs["b_ada"])[0]), col(f(inputs["g_pre_mix"])[0]), col(f(inputs["g_post_mix"])[0]),
                            col(f(inputs["g_pre_ffn"])[0]), col(f(inputs["g_post_ffn"])[0])], axis=1)
    wada = f(inputs["w_ada"])[0]
    wr = f(inputs["w_router"])[0]
    wge, wue, wde = f(inputs["w_gate_e"])[0], f(inputs["w_up_e"])[0], f(inputs["w_down_e"])[0]
    df, db = f(inputs["ret_decay_fwd"])[0], f(inputs["ret_decay_bwd"])[0]
    ident = np.eye(128, dtype=np.float32)
    maps = []
    for i in range(8):
        b, q = i // 4, i % 4
        rq = np.arange(64 * q, 64 * q + 64)
        rk = 256 + rq
        rvc = 512 + np.arange(128 * q, 128 * q + 128)
        rgc = 1024 + np.arange(128 * q, 128 * q + 128)
        aqc = 1536 + np.arange(128 * q, 128 * q + 128)
        akc = 2048 + np.arange(128 * q, 128 * q + 128)
        avc = 2560 + np.arange(128 * q, 128 * q + 128)
        cols = np.concatenate([rq, rk, aqc, akc, avc, rk, rvc, rgc])
        rows = np.concatenate([np.concatenate([np.arange(128 * r_, 128 * r_ + 128), 512 + np.arange(128 * r_, 128 * r_ + 128)]) for r_ in range(4)])
        rc, masks, slotid, tri = _consts(q)
        oh = np.zeros((128, 4), np.float32)
        oh[:, q] = 1.0
        dec = np.zeros((128, 2), np.float32)
        dec[:, 0] = df[q]
        dec[:, 1] = db[q]
        maps.append({
            "xb": x[b], "xo": np.ascontiguousarray(x[b, OWN * q:OWN * (q + 1)]), "ccol": col(c[b]),
            "wada": wada, "vcols": vcols, "win": np.ascontiguousarray(w_in[:, cols]), "dec": dec,
            "wout": np.ascontiguousarray(w_out[rows]), "wr": wr, "oh": oh,
            "wg": np.ascontiguousarray(wge[4 * q:4 * q + 4]), "wu": np.ascontiguousarray(wue[4 * q:4 * q + 4]),
            "wd": np.ascontiguousarray(wde[4 * q:4 * q + 4]),
            "ident": ident, "masks": masks, "rc": rc, "slotid": slotid, "tri": tri,
        })
    return maps


_NC_CACHE = {}


def kernel(**inputs):
    maps = prep(inputs)
    if "nc" not in _NC_CACHE:
        _NC_CACHE["nc"] = build()[0]
    res = run_bass_kernel_spmd(_NC_CACHE["nc"], maps, core_ids=list(range(8)))
    out = np.zeros((2, S, D), np.float32)
    for i in range(8):
        b, q = i // 4, i % 4
        out[b, OWN * q:OWN * (q + 1)] = res.results[i]["out"]
    return out
```
